# Optimizing a Trainium2 kernel written in Bass

```python
import jax, jax.numpy as jnp
from jax import lax
import numpy as np

D_MODEL = 2048
BATCH = 4
SEQ = 4096
DEPTH = 2

CTX_LEN = 256
GRID_W = 64
D_MIX = D_MODEL
N_GROUPS = 4
GROUP_W = D_MIX // N_GROUPS
CONV_W = GROUP_W
CONV_K = 3
SWA_HEAD_DIM = 64
SWA_HEADS = GROUP_W // SWA_HEAD_DIM
SWA_KV_HEADS = 2
SWA_WINDOW = 128
SWA_BLOCK = 128
FNET_HEAD_DIM = 64
FNET_HEADS = GROUP_W // FNET_HEAD_DIM
MLA_HEADS = 8
MLA_NOPE = 64
MLA_ROPE = 32
MLA_V = GROUP_W // MLA_HEADS
MLA_Q_LORA = 512
MLA_KV_LORA = 256
MLA_BLOCK = 128
N_EXPERTS = 16
EXPERT_FF = 2048
EC_CAPACITY = 2
ROPE_THETA = 10000.0
EPS = 1e-6

IN_SIZES = (CONV_W, CONV_W, CONV_W,
            SWA_HEADS * SWA_HEAD_DIM, SWA_KV_HEADS * SWA_HEAD_DIM, SWA_KV_HEADS * SWA_HEAD_DIM,
            GROUP_W,
            MLA_Q_LORA, MLA_KV_LORA, MLA_ROPE)
IN_COLS = 3 * CONV_W + (SWA_HEADS + 2 * SWA_KV_HEADS) * SWA_HEAD_DIM + GROUP_W + MLA_Q_LORA + MLA_KV_LORA + MLA_ROPE

kernel_name = "hybrid_parallel_groups_ec_moe_diffusion"


def rmsnorm(x, g):
    xf = x.astype(jnp.float32)
    y = xf * lax.rsqrt(jnp.mean(xf * xf, axis=-1, keepdims=True) + EPS)
    return (y * g.astype(jnp.float32)).astype(x.dtype)


def group_rmsnorm(y, g):
    shp = y.shape
    out = rmsnorm(y.reshape(shp[:-1] + (N_GROUPS, GROUP_W)), g.reshape(N_GROUPS, GROUP_W))
    return out.reshape(shp)


def modulate(h, shift, scale):
    return h * (1.0 + scale) + shift


def axial_rope_tables(rows_n, rot_dim):
    rows = jnp.broadcast_to(jnp.arange(rows_n, dtype=jnp.float32)[:, None], (rows_n, GRID_W)).reshape(-1)
    cols = jnp.broadcast_to(jnp.arange(GRID_W, dtype=jnp.float32)[None, :], (rows_n, GRID_W)).reshape(-1)
    n_freq = rot_dim // 4
    inv_freq = ROPE_THETA ** (-jnp.arange(n_freq, dtype=jnp.float32) / n_freq)
    ang = jnp.concatenate([rows[:, None] * inv_freq, cols[:, None] * inv_freq], axis=-1)
    return jnp.cos(ang), jnp.sin(ang)


def apply_rope(x, cos, sin):
    half = x.shape[-1] // 2
    cs = cos[None, :, None, :].astype(x.dtype)
    sn = sin[None, :, None, :].astype(x.dtype)
    x1, x2 = x[..., :half], x[..., half:]
    return jnp.concatenate([x1 * cs - x2 * sn, x2 * cs + x1 * sn], axis=-1)


def mixer_inputs(h, w_in, q_norm_g, w_uq, kv_norm_g, w_ukv, rope):
    B, N, _ = h.shape
    p = h @ w_in
    parts, start = [], 0
    for width in IN_SIZES:
        parts.append(p[..., start:start + width])
        start += width
    cb, cc, ch, sq, sk, sv, fu, mcq, mckv, mkpe = parts
    sq = sq.reshape(B, N, SWA_HEADS, SWA_HEAD_DIM)
    sk = sk.reshape(B, N, SWA_KV_HEADS, SWA_HEAD_DIM)
    sv = sv.reshape(B, N, SWA_KV_HEADS, SWA_HEAD_DIM)
    q = (rmsnorm(mcq, q_norm_g) @ w_uq).reshape(B, N, MLA_HEADS, MLA_NOPE + MLA_ROPE)
    kv = (rmsnorm(mckv, kv_norm_g) @ w_ukv).reshape(B, N, MLA_HEADS, MLA_NOPE + MLA_V)
    q_nope, q_pe = q[..., :MLA_NOPE], q[..., MLA_NOPE:]
    k_nope, mv = kv[..., :MLA_NOPE], kv[..., MLA_NOPE:]
    k_pe = mkpe[:, :, None, :]
    if rope is not None:
        cos_s, sin_s, cos_m, sin_m = rope
        sq = apply_rope(sq, cos_s, sin_s)
        sk = apply_rope(sk, cos_s, sin_s)
        q_pe = apply_rope(q_pe, cos_m, sin_m)
        k_pe = apply_rope(k_pe, cos_m, sin_m)
    mq = jnp.concatenate([q_nope, q_pe], axis=-1)
    mk = jnp.concatenate([k_nope, jnp.broadcast_to(k_pe, (B, N, MLA_HEADS, MLA_ROPE))], axis=-1)
    return cb, cc, ch, sq, sk, sv, fu, mq, mk, mv


def short_conv_mixer(gate_b, gate_c, h, w):
    u = gate_c * h
    up = jnp.pad(u, ((0, 0), (1, 1), (0, 0)))
    y = up[:, :-2] * w[0] + up[:, 1:-1] * w[1] + up[:, 2:] * w[2]
    return gate_b * y


def fourier_mixer(u):
    B, N, _ = u.shape
    uh = u.reshape(B, N, FNET_HEADS, FNET_HEAD_DIM).astype(jnp.float32)
    y = jnp.fft.fft2(uh, axes=(1, 3), norm="ortho").real
    return y.reshape(B, N, GROUP_W).astype(u.dtype)


def swa_latent(q, k, v, kc, vc, sink):
    B, N, Hq, d = q.shape
    Hkv = k.shape[2]
    G = Hq // Hkv
    T = SWA_BLOCK
    nb = N // T
    scale = d ** -0.5
    qb = q.reshape(B, nb, T, Hkv, G, d)

    def band(t):
        tp = jnp.pad(t, ((0, 0), (T, T), (0, 0), (0, 0))).reshape(B, nb + 2, T, Hkv, d)
        return jnp.concatenate([tp[:, :-2], tp[:, 1:-1], tp[:, 2:]], axis=2)

    kb, vb = band(k), band(v)
    s_loc = jnp.einsum('bnqhgd,bnkhd->bnhgqk', qb, kb, preferred_element_type=jnp.float32) * scale
    qpos = jnp.arange(nb)[:, None] * T + jnp.arange(T)[None, :]
    kpos = jnp.arange(nb)[:, None] * T - T + jnp.arange(3 * T)[None, :]
    rel = kpos[:, None, :] - qpos[:, :, None]
    valid = (jnp.abs(rel) <= SWA_WINDOW) & (kpos[:, None, :] >= 0) & (kpos[:, None, :] < N)
    s_loc = jnp.where(valid[None, :, None, None], s_loc, -1e30)
    s_ctx = jnp.einsum('bnqhgd,blhd->bnhgql', qb, kc, preferred_element_type=jnp.float32) * scale
    s_sink = jnp.broadcast_to(sink.astype(jnp.float32).reshape(1, 1, Hkv, G, 1, 1), s_loc.shape[:-1] + (1,))
    p = jax.nn.softmax(jnp.concatenate([s_loc, s_ctx, s_sink], axis=-1), axis=-1)
    p_loc = p[..., :3 * T].astype(v.dtype)
    p_ctx = p[..., 3 * T:3 * T + kc.shape[1]].astype(v.dtype)
    o = (jnp.einsum('bnhgqk,bnkhd->bnqhgd', p_loc, vb)
         + jnp.einsum('bnhgql,blhd->bnqhgd', p_ctx, vc))
    return o.reshape(B, N, Hq * d)


def swa_context(q, k, v, sink):
    B, L, Hq, d = q.shape
    Hkv = k.shape[2]
    G = Hq // Hkv
    qg = q.reshape(B, L, Hkv, G, d)
    s = jnp.einsum('blhgd,bmhd->bhglm', qg, k, preferred_element_type=jnp.float32) * (d ** -0.5)
    s_sink = jnp.broadcast_to(sink.astype(jnp.float32).reshape(1, Hkv, G, 1, 1), (B, Hkv, G, L, 1))
    p = jax.nn.softmax(jnp.concatenate([s, s_sink], axis=-1), axis=-1)[..., :L].astype(v.dtype)
    o = jnp.einsum('bhglm,bmhd->blhgd', p, v)
    return o.reshape(B, L, Hq * d)


def mla_latent(q, k, v, kc, vc):
    B, N, H, dq = q.shape
    nb = N // MLA_BLOCK
    scale = dq ** -0.5
    qb = q.reshape(B, nb, MLA_BLOCK, H, dq).transpose(1, 0, 2, 3, 4)

    def block(qi):
        s_lat = jnp.einsum('bqhd,bkhd->bhqk', qi, k, preferred_element_type=jnp.float32) * scale
        s_ctx = jnp.einsum('bqhd,blhd->bhql', qi, kc, preferred_element_type=jnp.float32) * scale
        p = jax.nn.softmax(jnp.concatenate([s_lat, s_ctx], axis=-1), axis=-1).astype(v.dtype)
        return (jnp.einsum('bhqk,bkhd->bqhd', p[..., :N], v)
                + jnp.einsum('bhql,blhd->bqhd', p[..., N:], vc))

    o = lax.map(block, qb)
    return o.transpose(1, 0, 2, 3, 4).reshape(B, N, H * v.shape[-1])


def mla_context(q, k, v):
    B, L, H, dq = q.shape
    s = jnp.einsum('blhd,bmhd->bhlm', q, k, preferred_element_type=jnp.float32) * (dq ** -0.5)
    p = jax.nn.softmax(s, axis=-1).astype(v.dtype)
    return jnp.einsum('bhlm,bmhd->blhd', p, v).reshape(B, L, H * v.shape[-1])


def mix_latent(px, pc, conv_w, sink):
    cb, cc, ch, sq, sk, sv, fu, mq, mk, mv = px
    skc, svc, mkc, mvc = pc[4], pc[5], pc[8], pc[9]
    return jnp.concatenate([
        short_conv_mixer(cb, cc, ch, conv_w),
        swa_latent(sq, sk, sv, skc, svc, sink),
        fourier_mixer(fu),
        mla_latent(mq, mk, mv, mkc, mvc),
    ], axis=-1)


def mix_context(pc, conv_w, sink):
    cb, cc, ch, sq, sk, sv, fu, mq, mk, mv = pc
    return jnp.concatenate([
        short_conv_mixer(cb, cc, ch, conv_w),
        swa_context(sq, sk, sv, sink),
        fourier_mixer(fu),
        mla_context(mq, mk, mv),
    ], axis=-1)


def expert_choice_ffn(h, w_router, w_gate, w_up, w_down):
    B, N, D = h.shape
    cap = EC_CAPACITY * N // N_EXPERTS
    aff = jax.nn.softmax(jnp.einsum('bnd,de->bne', h, w_router, preferred_element_type=jnp.float32), axis=-1)
    g, idx = lax.top_k(jnp.swapaxes(aff, 1, 2), cap)
    xs = jax.vmap(lambda hb, ib: hb[ib])(h, idx)
    a = jnp.einsum('becd,edf->becf', xs, w_gate)
    u = jnp.einsum('becd,edf->becf', xs, w_up)
    y = jnp.einsum('becf,efd->becd', jax.nn.silu(a) * u, w_down)
    y = y * g[..., None].astype(y.dtype)
    return jax.vmap(lambda ib, yb: jnp.zeros((N, D), yb.dtype).at[ib.reshape(-1)].add(yb.reshape(-1, D)))(idx, y)


def setup_inputs(seed: int = 0) -> dict:
    key = jax.random.key(seed)
    ks = jax.random.split(key, 24)
    f32 = jnp.float32

    def nrm(k, shape, fan_in):
        return jax.random.normal(k, shape, f32) * (fan_in ** -0.5)

    def gain(k, shape):
        return 1.0 + 0.02 * jax.random.normal(k, shape, f32)

    return {
        "x": jax.random.normal(ks[0], (BATCH, SEQ, D_MODEL), f32),
        "c": jax.random.normal(ks[1], (BATCH, D_MODEL), f32),
        "ctx": jax.random.normal(ks[2], (BATCH, CTX_LEN, D_MODEL), f32),
        "c_ctx": jax.random.normal(ks[3], (D_MODEL,), f32),
        "ada_w": nrm(ks[4], (DEPTH, D_MODEL, 6 * D_MODEL), D_MODEL),
        "ada_b": 0.01 * jax.random.normal(ks[5], (DEPTH, 6 * D_MODEL), f32),
        "norm1_g": gain(ks[6], (DEPTH, D_MODEL)),
        "norm2_g": gain(ks[7], (DEPTH, D_MODEL)),
        "w_in": nrm(ks[8], (DEPTH, D_MODEL, IN_COLS), D_MODEL),
        "conv_w": nrm(ks[9], (DEPTH, CONV_K, CONV_W), CONV_K),
        "swa_sink": 0.5 * jax.random.normal(ks[10], (DEPTH, SWA_HEADS), f32),
        "mla_q_norm_g": gain(ks[11], (DEPTH, MLA_Q_LORA)),
        "mla_w_uq": nrm(ks[12], (DEPTH, MLA_Q_LORA, MLA_HEADS * (MLA_NOPE + MLA_ROPE)), MLA_Q_LORA),
        "mla_kv_norm_g": gain(ks[13], (DEPTH, MLA_KV_LORA)),
        "mla_w_ukv": nrm(ks[14], (DEPTH, MLA_KV_LORA, MLA_HEADS * (MLA_NOPE + MLA_V)), MLA_KV_LORA),
        "out_norm_g": gain(ks[15], (DEPTH, D_MIX)),
        "w_out": nrm(ks[16], (DEPTH, D_MIX, D_MODEL), D_MIX),
        "w_router": nrm(ks[17], (DEPTH, D_MODEL, N_EXPERTS), D_MODEL),
        "w_gate": nrm(ks[18], (DEPTH, N_EXPERTS, D_MODEL, EXPERT_FF), D_MODEL),
        "w_up": nrm(ks[19], (DEPTH, N_EXPERTS, D_MODEL, EXPERT_FF), D_MODEL),
        "w_down": nrm(ks[20], (DEPTH, N_EXPERTS, EXPERT_FF, D_MODEL), EXPERT_FF),
        "final_norm_g": gain(ks[21], (D_MODEL,)),
    }


def reference(x, c, ctx, c_ctx, ada_w, ada_b, norm1_g, norm2_g, w_in, conv_w, swa_sink,
              mla_q_norm_g, mla_w_uq, mla_kv_norm_g, mla_w_ukv, out_norm_g, w_out,
              w_router, w_gate, w_up, w_down, final_norm_g):
    n_tok = x.shape[1]
    ROWS = n_tok // GRID_W
    cos_s, sin_s = axial_rope_tables(ROWS, SWA_HEAD_DIM)
    cos_m, sin_m = axial_rope_tables(ROWS, MLA_ROPE)
    rope = (cos_s, sin_s, cos_m, sin_m)
    for layer in range(DEPTH):
        last = layer == DEPTH - 1
        mx = jnp.split(jax.nn.silu(c) @ ada_w[layer] + ada_b[layer], 6, axis=-1)
        sh1, sc1, g1, sh2, sc2, g2 = [m[:, None, :] for m in mx]
        csh1, csc1, cg1, csh2, csc2, cg2 = jnp.split(
            jax.nn.silu(c_ctx) @ ada_w[layer] + ada_b[layer], 6, axis=-1)

        hx = modulate(rmsnorm(x, norm1_g[layer]), sh1, sc1)
        hc = modulate(rmsnorm(ctx, norm1_g[layer]), csh1, csc1)
        px = mixer_inputs(hx, w_in[layer], mla_q_norm_g[layer], mla_w_uq[layer],
                          mla_kv_norm_g[layer], mla_w_ukv[layer], rope)
        pc = mixer_inputs(hc, w_in[layer], mla_q_norm_g[layer], mla_w_uq[layer],
                          mla_kv_norm_g[layer], mla_w_ukv[layer], None)
        yx = mix_latent(px, pc, conv_w[layer], swa_sink[layer])
        x = x + g1 * (group_rmsnorm(yx, out_norm_g[layer]) @ w_out[layer])
        if not last:
            yc = mix_context(pc, conv_w[layer], swa_sink[layer])
            ctx = ctx + cg1 * (group_rmsnorm(yc, out_norm_g[layer]) @ w_out[layer])

        fx = modulate(rmsnorm(x, norm2_g[layer]), sh2, sc2)
        x = x + g2 * expert_choice_ffn(fx, w_router[layer], w_gate[layer], w_up[layer], w_down[layer])
        if not last:
            fc = modulate(rmsnorm(ctx, norm2_g[layer]), csh2, csc2)
            ctx = ctx + cg2 * expert_choice_ffn(fc, w_router[layer], w_gate[layer], w_up[layer], w_down[layer])
    return rmsnorm(x, final_norm_g)
```

```python
import numpy as np
import ml_dtypes
from contextlib import ExitStack
import concourse.bass as bass
import concourse.mybir as mybir
from concourse.bass_utils import run_bass_kernel_spmd

F32 = mybir.dt.float32
BF16 = mybir.dt.bfloat16
I32 = mybir.dt.int32
AF = mybir.ActivationFunctionType
ALU = mybir.AluOpType
NPBF = ml_dtypes.bfloat16

D = 2048
NL = 4096
NCX = 256
NT = NL + NCX
NPAD = NT + 128
DEPTH = 2
EPS = 1e-6
NE = 16
CAP_L = 512
CAP_C = 32

WA_COLS = 3456
OFF_CB, OFF_CC, OFF_CH = 0, 512, 1024
OFF_SQ, OFF_SQW = 1536, 2048
OFF_SK, OFF_SKW = 2560, 2688
OFF_SV = 2816
OFF_FU = 2944
WB_COLS = 960
OFF_CQ, OFF_CKV, OFF_KPA, OFF_KPB = 0, 512, 768, 864

SEM_ROLL = 30000
NDMA_SEMS = 24


def I(name, *args, **kwargs):
    return (name, args, kwargs)


class Tok:
    __slots__ = ("w", "r", "base")

    def __init__(self):
        self.w = {}
        self.r = {}
        self.base = {}


class Sched:
    ENGS = ("pe", "act", "dve", "pool", "sp")

    def __init__(self, nc, stack):
        self.nc = nc
        self.stack = stack
        self.sems = []
        self.prog = {e: [] for e in self.ENGS}
        self.cur_sem = {}
        self.cnt = {}
        for e in ("pe", "act", "dve", "pool"):
            self.cur_sem[e] = self._new_sem(e)
            self.cnt[e] = 0
        self.dma_sems = {q: [self._new_sem("dma" + q) for _ in range(NDMA_SEMS)] for q in ("sp", "pool", "act")}
        self.dma_cnt = {q: [0] * NDMA_SEMS for q in ("sp", "pool", "act")}
        self.dma_rr = {"sp": 0, "pool": 0, "act": 0}
        self.waited = {e: {} for e in self.ENGS}
        self.n_inst = 0
        self.regcache = {}
        self.marks = []
        self.tot_pe = 0

    def _new_sem(self, name):
        h = self.stack.enter_context(self.nc.semaphore(f"s_{name}_{len(self.sems)}"))
        self.sems.append(h)
        return len(self.sems) - 1

    def _collect(self, eng, reads, writes, merge):
        need = {}
        for t in reads:
            for s, v in t.w.items():
                if need.get(s, 0) < v:
                    need[s] = v
        for t in writes:
            src = (t.base, t.r) if merge else (t.w, t.r)
            for dct in src:
                for s, v in dct.items():
                    if need.get(s, 0) < v:
                        need[s] = v
        out = []
        wd = self.waited[eng]
        for s, v in need.items():
            if eng == "pe" and s == self.cur_sem["pe"]:
                continue
            if wd.get(s, 0) >= v:
                continue
            wd[s] = v
            out.append((s, v))
        return out

    def _post(self, ev, reads, writes, merge):
        s, v = ev
        for t in reads:
            if t.r.get(s, 0) < v:
                t.r[s] = v
        for t in writes:
            if merge:
                if t.r:
                    nb = dict(t.base)
                    for rs, rv in t.r.items():
                        if nb.get(rs, 0) < rv:
                            nb[rs] = rv
                    t.base = nb
                if t.w.get(s, 0) < v:
                    t.w[s] = v
            else:
                nb = dict(t.w)
                for rs, rv in t.r.items():
                    if nb.get(rs, 0) < rv:
                        nb[rs] = rv
                t.base = nb
                t.w = {s: v}
            t.r = {}

    def op(self, eng, fn, reads=(), writes=(), merge=False):
        waits = self._collect(eng, reads, writes, merge)
        if self.cnt[eng] >= SEM_ROLL:
            self.cur_sem[eng] = self._new_sem(eng)
            self.cnt[eng] = 0
        self.cnt[eng] += 1
        if eng == "pe":
            self.tot_pe += 1
        s = self.cur_sem[eng]
        v = self.cnt[eng]
        sems = self.sems

        def emit(h, waits=waits, s=s, fn=fn):
            for ws, wv in waits:
                h.wait_ge(sems[ws], wv)
            getattr(h, fn[0])(*fn[1], **fn[2]).then_inc(sems[s], 1)

        self.prog[eng].append(emit)
        self._post((s, v), reads, writes, merge)
        self.n_inst += 1

    def dma(self, eng, fn, reads=(), writes=(), merge=False):
        i = self.dma_rr[eng]
        self.dma_rr[eng] = (i + 1) % NDMA_SEMS
        s = self.dma_sems[eng][i]
        waits = self._collect(eng, reads, writes, merge)
        prev = self.dma_cnt[eng][i]
        if prev > 0 and self.waited[eng].get(s, 0) < prev:
            self.waited[eng][s] = prev
            waits.append((s, prev))
        self.dma_cnt[eng][i] += 16
        v = self.dma_cnt[eng][i]
        sems = self.sems

        def emit(h, waits=waits, s=s, fn=fn):
            for ws, wv in waits:
                h.wait_ge(sems[ws], wv)
            try:
                kw = fn[2]
                if isinstance(kw.get("bounds_check"), int):
                    kw = dict(kw)
                    key = (id(h), kw["bounds_check"])
                    if key not in self.regcache:
                        self.regcache[key] = h.to_reg(kw["bounds_check"])
                    kw["bounds_check"] = self.regcache[key]
                ins = getattr(h, fn[0])(*fn[1], **kw)
            except Exception:
                print("DMA FAIL", fn[0], {k: (getattr(v, "shape", v), getattr(v, "ap", None)) for k, v in fn[2].items()})
                raise
            ins.then_inc(sems[s], 16)

        self.prog[eng].append(emit)
        self._post((s, v), reads, writes, merge)
        self.n_inst += 1

    def wait_all(self, eng, toks):
        waits = self._collect(eng, toks, (), False)
        sems = self.sems

        def emit(h, waits=waits):
            for ws, wv in waits:
                h.wait_ge(sems[ws], wv)

        self.prog[eng].append(emit)

    def barrier(self):
        cur = {}
        for e in ("pe", "act", "dve", "pool"):
            if self.cnt[e] > 0:
                cur[self.cur_sem[e]] = self.cnt[e]
        for q in self.dma_sems:
            for i, s in enumerate(self.dma_sems[q]):
                if self.dma_cnt[q][i] > 0:
                    cur[s] = self.dma_cnt[q][i]
        sems = self.sems
        for eng in self.ENGS:
            wd = self.waited[eng]
            waits = []
            for s, v in cur.items():
                if wd.get(s, 0) < v:
                    wd[s] = v
                    waits.append((s, v))

            def emit(h, waits=waits):
                for ws, wv in waits:
                    h.wait_ge(sems[ws], wv)

            self.prog[eng].append(emit)

    def flush(self, name=None):
        self.barrier()
        self.regcache = {}
        self.marks.append((name, self.tot_pe))
        nc = self.nc
        prog = self.prog
        with nc.Block(name) as block:
            if prog["sp"]:
                @block.sync
                def _(h):
                    for f in prog["sp"]:
                        f(h)
            if prog["pe"]:
                @block.tensor
                def _(h):
                    for f in prog["pe"]:
                        f(h)
            if prog["act"]:
                @block.scalar
                def _(h):
                    for f in prog["act"]:
                        f(h)
            if prog["dve"]:
                @block.vector
                def _(h):
                    for f in prog["dve"]:
                        f(h)
            if prog["pool"]:
                @block.gpsimd
                def _(h):
                    for f in prog["pool"]:
                        f(h)
        self.prog = {e: [] for e in self.ENGS}


def _rope_tables(rot_dim):
    rows = np.repeat(np.arange(64, dtype=np.float32), 64)
    cols = np.tile(np.arange(64, dtype=np.float32), 64)
    n_freq = rot_dim // 4
    inv = (np.float32(10000.0) ** (-np.arange(n_freq, dtype=np.float32) / np.float32(n_freq))).astype(np.float32)
    ang = np.concatenate([rows[:, None] * inv, cols[:, None] * inv], axis=-1).astype(np.float32)
    return np.cos(ang).astype(np.float32), np.sin(ang).astype(np.float32)


def host_consts():
    c = {}
    cs, sn = _rope_tables(64)
    cos2 = np.ones((128, NT), np.float32)
    sin2 = np.zeros((128, NT), np.float32)
    for hh in range(2):
        cos2[hh * 64:hh * 64 + 32, :NL] = cs.T
        cos2[hh * 64 + 32:hh * 64 + 64, :NL] = cs.T
        sin2[hh * 64:hh * 64 + 32, :NL] = -sn.T
        sin2[hh * 64 + 32:hh * 64 + 64, :NL] = sn.T
    c["ropeS"] = np.stack([cos2, sin2], 0)
    cm, sm = _rope_tables(32)
    cosm = np.ones((96, NT), np.float32)
    sinm = np.zeros((96, NT), np.float32)
    cosm[64:80, :NL] = cm.T
    cosm[80:96, :NL] = cm.T
    sinm[64:80, :NL] = -sm.T
    sinm[80:96, :NL] = sm.T
    c["ropeM"] = np.stack([cosm, sinm], 0)
    k = np.arange(64)
    a = 2 * np.pi * np.outer(k, k) / 64.0
    c64 = np.zeros((2, 128, 128), np.float64)
    for g in range(2):
        c64[0, g * 64:(g + 1) * 64, g * 64:(g + 1) * 64] = np.cos(a) / 8.0
        c64[1, g * 64:(g + 1) * 64, g * 64:(g + 1) * 64] = np.sin(a) / 8.0
    c["dft64"] = c64.astype(NPBF)
    def pos_tables(n):
        nt = n // 128
        idx = np.arange(n, dtype=np.int64)
        m = (np.outer(idx, idx) % n).astype(np.float64)
        ang = 2 * np.pi * m / n
        cn = np.cos(ang) / np.sqrt(n)
        sn_ = -np.sin(ang) / np.sqrt(n)
        out = np.zeros((nt, 128, 2, nt, 128), np.float32)
        for kt in range(nt):
            blkc = cn[:, kt * 128:(kt + 1) * 128].reshape(nt, 128, 128)
            blks = sn_[:, kt * 128:(kt + 1) * 128].reshape(nt, 128, 128)
            out[kt, :, 0] = blkc.transpose(1, 0, 2)
            out[kt, :, 1] = blks.transpose(1, 0, 2)
        return out.reshape(nt, 128, 2 * nt * 128).astype(NPBF)
    c["dftL"] = pos_tables(NL)
    c["dftC"] = pos_tables(NCX)
    ident = np.eye(128, dtype=np.float32)
    c["identf"] = ident
    kk = np.arange(128)[:, None]
    qq = np.arange(128)[None, :]
    mprev = (kk >= qq).astype(np.float32)
    mnext = (kk <= qq).astype(np.float32)
    c["masks"] = np.stack([np.tile(mprev, (1, 4)), np.tile(mnext, (1, 4))], 0).astype(NPBF)
    c["triu"] = (kk < qq).astype(NPBF)
    c["iota512"] = np.tile(np.arange(512, dtype=np.float32)[None, :], (128, 1))
    pp_ = np.arange(128, dtype=np.float32)
    c["pidx"] = np.stack([pp_, NT + pp_, -(NT + pp_), np.zeros(128, np.float32)], 1).astype(np.float32)
    c["tgrid"] = np.tile(np.repeat(np.arange(34, dtype=np.float32), NE)[None, :], (128, 1))
    return c


def host_weights(inp):
    w = {}
    w_in = inp["w_in"]
    L = w_in.shape[0]
    wa = np.zeros((L, D, WA_COLS), np.float32)
    wa[:, :, 0:1536] = w_in[:, :, 0:1536]
    sq = w_in[:, :, 1536:2048]
    wa[:, :, OFF_SQ:OFF_SQ + 512] = sq
    sq4 = sq.reshape(L, D, 8, 2, 32)
    wa[:, :, OFF_SQW:OFF_SQW + 512] = sq4[:, :, :, ::-1, :].reshape(L, D, 512)
    sk = w_in[:, :, 2048:2176]
    wa[:, :, OFF_SK:OFF_SK + 128] = sk
    wa[:, :, OFF_SKW:OFF_SKW + 128] = sk.reshape(L, D, 2, 2, 32)[:, :, :, ::-1, :].reshape(L, D, 128)
    wa[:, :, OFF_SV:OFF_SV + 128] = w_in[:, :, 2176:2304]
    wa[:, :, OFF_FU:OFF_FU + 512] = w_in[:, :, 2304:2816]
    w["w_a"] = wa
    wb = np.zeros((L, D, WB_COLS), np.float32)
    wb[:, :, 0:768] = w_in[:, :, 2816:3584]
    kpe = w_in[:, :, 3584:3616]
    wb[:, :, OFF_KPA + 64:OFF_KPA + 96] = kpe
    wb[:, :, OFF_KPB + 64:OFF_KPB + 96] = kpe.reshape(L, D, 2, 16)[:, :, ::-1, :].reshape(L, D, 32)
    w["w_b"] = wb
    uq = inp["mla_w_uq"]
    uq4 = uq.reshape(L, 512, 8, 96)
    uqb = uq4.copy()
    uqb[:, :, :, 64:80] = uq4[:, :, :, 80:96]
    uqb[:, :, :, 80:96] = uq4[:, :, :, 64:80]
    w["w_uq"] = np.stack([uq, uqb.reshape(L, 512, 768)], 1)
    ukv = inp["mla_w_ukv"].reshape(L, 256, 8, 2, 64)
    w["w_uk"] = np.ascontiguousarray(ukv[:, :, :, 0, :]).reshape(L, 256, 512)
    w["w_uv"] = np.ascontiguousarray(ukv[:, :, :, 1, :]).reshape(L, 256, 512)
    for k_ in ("ada_w", "ada_b", "norm1_g", "norm2_g", "conv_w", "swa_sink", "mla_q_norm_g",
               "mla_kv_norm_g", "out_norm_g", "w_out", "w_router", "w_gate", "w_up", "w_down",
               "final_norm_g"):
        w[k_] = inp[k_]
    return w


class Ctx:
    pass


def build_program(debug=(), n_layers=DEPTH, stop_after=None):
    nc = bass.Bass("TRN2", target_bir_lowering=False)
    G = Ctx()
    G.nc = nc
    G.debug = set(debug)
    din = {}

    def inp(name, shape, dt=F32):
        din[name] = nc.dram_tensor(name, list(shape), dt, kind="ExternalInput").ap()
        return din[name]

    G.xin = inp("xin", [NPAD, D])
    G.cvec = inp("cvec", [D, 2])
    G.ada_w = inp("ada_w", [DEPTH, D, 6 * D])
    G.ada_b = inp("ada_b", [DEPTH, 6 * D])
    G.norm1_g = inp("norm1_g", [DEPTH, D])
    G.norm2_g = inp("norm2_g", [DEPTH, D])
    G.w_a = inp("w_a", [DEPTH, D, WA_COLS])
    G.w_b = inp("w_b", [DEPTH, D, WB_COLS])
    G.conv_w = inp("conv_w", [DEPTH, 3, 512])
    G.swa_sink = inp("swa_sink", [DEPTH, 8])
    G.q_norm_g = inp("mla_q_norm_g", [DEPTH, 512])
    G.kv_norm_g = inp("mla_kv_norm_g", [DEPTH, 256])
    G.w_uq = inp("w_uq", [DEPTH, 2, 512, 768])
    G.w_uk = inp("w_uk", [DEPTH, 256, 512])
    G.w_uv = inp("w_uv", [DEPTH, 256, 512])
    G.out_norm_g = inp("out_norm_g", [DEPTH, D])
    G.w_out = inp("w_out", [DEPTH, D, D])
    G.w_router = inp("w_router", [DEPTH, D, NE])
    if stop_after is None or stop_after in ("moe", "moetest"):
        G.w_gate = inp("w_gate", [DEPTH, NE, D, D])
        G.w_up = inp("w_up", [DEPTH, NE, D, D])
        G.w_down = inp("w_down", [DEPTH, NE, D, D])
    G.final_g = inp("final_norm_g", [D])
    G.ropeS = inp("ropeS", [2, 128, NT])
    G.ropeM = inp("ropeM", [2, 96, NT])
    G.dft64 = inp("dft64", [2, 128, 128], BF16)
    G.dftL = inp("dftL", [32, 128, 2 * 32 * 128], BF16)
    G.dftC = inp("dftC", [2, 128, 2 * 2 * 128], BF16)
    G.identf = inp("identf", [128, 128])
    G.masks = inp("masks", [2, 128, 512], BF16)
    G.triu = inp("triu", [128, 128], BF16)
    G.iota512 = inp("iota512", [128, 512])
    G.pidx = inp("pidx", [128, 4])
    G.tgrid = inp("tgrid", [128, 34 * NE])

    G.out = nc.dram_tensor("out", [NL, D], F32, kind="ExternalOutput").ap()

    def scratch(name, shape, dt):
        kind = "ExternalOutput" if name in G.debug else "Internal"
        return nc.dram_tensor(name, list(shape), dt, kind=kind).ap()

    G.xres = scratch("xres", [NPAD, D], F32)
    G.hT = scratch("hT", [D, NT], BF16)
    G.cbT = scratch("cbT", [512, NT], F32)
    G.uT = scratch("uT", [512, NT], F32)
    G.sqT = scratch("sqT", [512, NT], BF16)
    G.skT = scratch("skT", [128, NT], BF16)
    G.svd = scratch("svd", [NT, 128], BF16)
    G.ucd = scratch("ucd", [NT, 512], BF16)
    G.usd = scratch("usd", [NT, 512], BF16)
    G.mqT = scratch("mqT", [8, 96, NT], BF16)
    G.mkT = scratch("mkT", [8, 96, NT], BF16)
    G.mvd = scratch("mvd", [NT, 512], BF16)
    G.ynT = scratch("ynT", [D, NT], BF16)
    G.fxd = scratch("fxd", [NPAD, D], BF16)
    G.modrow = scratch("modrow", [DEPTH, 2, 6 * D], F32)
    G.affd = scratch("affd", [NT, NE], F32)
    if "idxd" in G.debug:
        G.idxd = nc.dram_tensor("idxd", [128, NE * 5], I32, kind="ExternalOutput").ap()
        G.gd = nc.dram_tensor("gd", [128, NE * 5], F32, kind="ExternalOutput").ap()
    if stop_after == "moetest":
        G.aff_in = inp("aff_in", [NT, NE])
        G.fx_in = inp("fx_in", [NPAD, D], BF16)

    with ExitStack() as st:
        S = Sched(nc, st)
        G.S = S
        G.st = st
        G.t = {k: Tok() for k in ("xres", "hT", "cbT", "uT", "sqT", "skT", "svd", "ucd", "usd", "mqT",
                                  "mkT", "mvd", "ynT", "fxd", "out", "modrow", "affd", "idxd", "gd")}
        G.ident_f = st.enter_context(nc.sbuf_tensor("ident_f", [128, 128], F32))
        G.ident_b = st.enter_context(nc.sbuf_tensor("ident_b", [128, 128], BF16))
        G.ones_b = st.enter_context(nc.sbuf_tensor("ones_b", [128, 128], BF16))
        G.ones_f = st.enter_context(nc.sbuf_tensor("ones_f", [128, 128], F32))
        G.t_const = Tok()
        G.eps_t = st.enter_context(nc.sbuf_tensor("eps_t", [128, 1], F32))
        S.op("pool", I("memset", G.eps_t[:], EPS), writes=[G.t_const], merge=True)
        S.dma("sp", I("dma_start", out=G.ident_f[:], in_=G.identf), writes=[G.t_const])
        S.op("dve", I("tensor_copy", out=G.ident_b[:], in_=G.ident_f[:]), reads=[G.t_const], writes=[G.t_const])
        S.op("pool", I("memset", G.ones_b[:], 1.0), writes=[G.t_const], merge=True)
        S.op("pool", I("memset", G.ones_f[:], 1.0), writes=[G.t_const], merge=True)
        G.aff_all = st.enter_context(nc.sbuf_tensor("aff_all", [128, 34, NE], F32))
        G.t_aff = Tok()
        S.op("pool", I("memset", G.aff_all[:], 0.0), writes=[G.t_aff])
        G.idx_all = st.enter_context(nc.sbuf_tensor("idx_all", [128, NE, 5], I32))
        G.g_all = st.enter_context(nc.sbuf_tensor("g_all", [128, NE, 5], F32))
        G.t_idx = Tok()
        S.op("pool", I("memset", G.idx_all[:], 0), writes=[G.t_idx])
        S.op("pool", I("memset", G.g_all[:], 0.0), writes=[G.t_idx], merge=True)
        S.dma("sp", I("dma_start", out=G.xres.rearrange("(a p) d -> p a d", p=128),
                                          in_=G.xin.rearrange("(a p) d -> p a d", p=128)),
              writes=[G.t["xres"]])
        S.dma("pool", I("dma_start", out=G.fxd[NT:NPAD, :], in_=G.xin[NT:NPAD, :]), writes=[G.t["fxd"]], merge=True)
        S.flush("init")
        if stop_after == "moetest":
            phase_adaln(G, 0)
            S.dma("sp", I("dma_start", out=G.aff_all[:], in_=G.aff_in.rearrange("(t p) e -> p t e", p=128)), writes=[G.t_aff])
            S.dma("sp", I("dma_start", out=G.fxd.rearrange("(a p) d -> p a d", p=128), in_=G.fx_in.rearrange("(a p) d -> p a d", p=128)),
                  writes=[G.t["fxd"]])
            phase_route(G, 0, True)
            if "moe" in G.debug:
                phase_moe(G, 0, True)
            n_layers = 0
        for l in range(n_layers):
            last = (l == DEPTH - 1)
            phase_adaln(G, l)
            if stop_after == "adaln":
                break
            phase_proj_a(G, l)
            if stop_after == "proj_a":
                break
            phase_proj_b(G, l)
            if stop_after == "proj_b":
                break
            phase_conv(G, l, not last)
            if stop_after == "conv":
                break
            phase_swa(G, l, not last)
            if stop_after == "swa":
                break
            phase_fnet(G, l, not last)
            if stop_after == "fnet":
                break
            phase_mla(G, l, not last)
            if stop_after == "mla":
                break
            phase_outproj(G, l, not last)
            if stop_after == "outproj":
                break
            phase_route(G, l, not last)
            if stop_after == "route":
                break
            phase_moe(G, l, not last)
            if stop_after == "moe":
                break
        if stop_after is None and n_layers == DEPTH:
            phase_final(G)
        S.wait_all("sp", list(G.t.values()))
        S.flush("fin")
        G.n_inst = S.n_inst
    G.in_names = list(din.keys())
    return nc, G


def _alloc(G, ps):
    nc = G.nc
    G.uid = getattr(G, "uid", 0) + 1
    u = G.uid
    sb = lambda name, shape, dt: ps.enter_context(nc.sbuf_tensor(f"{name}_{u}", list(shape), dt))
    pp = lambda name, shape, dt=F32: ps.enter_context(nc.psum_tensor(f"{name}_{u}", list(shape), dt))
    return sb, pp


BLOCKS = [(i * 512, 512, 0) for i in range(8)] + [(NL, NCX, 1)]


def phase_adaln(G, l):
    nc, S = G.nc, G.S
    with ExitStack() as ps:
        sb, pp = _alloc(G, ps)
        cT = sb("ad_cT", [128, 16, 2], F32)
        sl = sb("ad_sl", [128, 16, 64], BF16)
        sel = sb("ad_sel", [1, 64], BF16)
        brow = sb("ad_brow", [1, 6 * D], BF16)
        rows = sb("ad_rows", [64, 6 * D], F32)
        wt = [sb(f"ad_wt{i}", [128, 16, 512], BF16) for i in range(2)]
        pacc = [pp(f"ad_ps{i}", [64, 512]) for i in range(2)]
        t_c, t_sl, t_sel, t_b, t_rows = Tok(), Tok(), Tok(), Tok(), Tok()
        t_wt = [Tok(), Tok()]
        t_ps = [Tok(), Tok()]
        S.dma("sp", I("dma_start", out=cT[:], in_=G.cvec.rearrange("(j p) t -> p j t", p=128)), writes=[t_c])
        S.op("pool", I("memset", sl[:], 0.0), writes=[t_sl])
        S.op("pool", I("memset", sel[:], 0.0), writes=[t_sel])
        S.op("pool", I("memset", sel[0:1, 0:1], 1.0), writes=[t_sel])
        S.op("pool", I("memset", sel[0:1, 32:33], 1.0), writes=[t_sel])
        S.op("act", I("activation", out=sl[:, :, 0], in_=cT[:, :, 0], func=AF.Silu), reads=[t_c], writes=[t_sl])
        S.op("act", I("activation", out=sl[:, :, 32], in_=cT[:, :, 1], func=AF.Silu), reads=[t_c], writes=[t_sl])
        for q in range(6):
            S.dma("pool", I("dma_start", out=brow[0:1, q * D:(q + 1) * D], in_=G.ada_b[l:l + 1, q * D:(q + 1) * D]),
                  writes=[t_b], merge=True)
        for nb in range(24):
            b = nb % 2
            S.dma("pool", I("dma_start",
                out=wt[b][:], in_=G.ada_w[l][:, nb * 512:(nb + 1) * 512].rearrange("(j p) n -> p j n", p=128)),
                writes=[t_wt[b]])
            for j in range(16):
                S.op("pe", I("matmul", pacc[b][:], lhsT=sl[:, j, :], rhs=wt[b][:, j, :], start=(j == 0), stop=False),
                     reads=[t_sl, t_wt[b]], writes=[t_ps[b]], merge=(j > 0))
            S.op("pe", I("matmul", pacc[b][:], lhsT=sel[:], rhs=brow[0:1, nb * 512:(nb + 1) * 512], start=False, stop=True),
                 reads=[t_sel, t_b], writes=[t_ps[b]], merge=True)
            S.op("act", I("copy", out=rows[0:1, nb * 512:(nb + 1) * 512], in_=pacc[b][0:1, :]),
                 reads=[t_ps[b]], writes=[t_rows], merge=True)
            S.op("dve", I("tensor_copy", out=rows[32:33, nb * 512:(nb + 1) * 512], in_=pacc[b][32:33, :]),
                 reads=[t_ps[b]], writes=[t_rows], merge=True)
        S.dma("sp", I("dma_start", out=G.modrow[l, 0:1, :], in_=rows[0:1, :]), reads=[t_rows], writes=[G.t["modrow"]], merge=True)
        S.dma("sp", I("dma_start", out=G.modrow[l, 1:2, :], in_=rows[32:33, :]), reads=[t_rows], writes=[G.t["modrow"]], merge=True)
        S.flush(f"adaln{l}")


def load_T(G, row_aps, sb, pp, name):
    S = G.S
    nv = len(row_aps)
    t_stg0 = Tok()
    n = max(ap.shape[-1] for ap in row_aps) // 128
    stg = sb(name + "_stg", [16, nv, 128], F32)
    out = sb(name, [128, nv, n], F32)
    S.op("pool", I("memset", stg[:], 0.0), writes=[t_stg0])
    pst_full = pp(name + "_ps", [128, 512])
    pst = pst_full[:, 0:nv * 16].rearrange("p (v n) -> p v n", n=16)
    t_stg, t_ps, t_out = t_stg0, Tok(), Tok()
    for v, ap in enumerate(row_aps):
        S.dma("sp", I("dma_start", out=stg[0:ap.shape[-1] // 128, v, :], in_=ap.rearrange("(j p) -> j p", p=128)),
              reads=[G.t["modrow"]], writes=[t_stg], merge=(v > 0))
    for v in range(nv):
        S.op("pe", I("transpose", out=pst[:, v, 0:n], in_=stg[0:n, v, :], identity=G.ident_f[0:n, 0:n]),
             reads=[t_stg, G.t_const], writes=[t_ps], merge=(v > 0))
    S.op("dve", I("tensor_copy", out=out[:], in_=pst[:, :, 0:n]), reads=[t_ps], writes=[t_out])
    return out, t_out


def rstd_from_ss(G, out_ap, ss_ap, n, reads, writes, merge=False):
    S = G.S
    S.op("act", I("activation", out=out_ap, in_=ss_ap, func=AF.Ln, bias=G.eps_t[0:out_ap.shape[0], 0:1], scale=1.0 / n),
         reads=list(reads) + [G.t_const], writes=writes, merge=merge)
    S.op("act", I("activation", out=out_ap, in_=out_ap, func=AF.Exp, scale=-0.5),
         reads=writes, writes=writes)


def phase_proj_a(G, l):
    nc, S = G.nc, G.S
    with ExitStack() as ps:
        sb, pp = _alloc(G, ps)
        NCOL = 2560
        wA = sb("pa_w", [128, 16, NCOL], BF16)
        t_w = Tok()
        for c0 in range(0, NCOL, 512):
            S.dma("pool", I("dma_start",
                out=wA[:, :, c0:c0 + 512], in_=G.w_a[l][:, c0:c0 + 512].rearrange("(j p) n -> p j n", p=128)),
                writes=[t_w], merge=True)
        mv, t_mv = load_T(G, [G.modrow[l, 0, 0:D], G.modrow[l, 1, 0:D], G.modrow[l, 0, D:2 * D],
                              G.modrow[l, 1, D:2 * D], G.norm1_g[l]], sb, pp, "pa_mv")
        sh1T = mv
        t_sh = t_mv
        gs1T = sb("pa_gs", [128, 2, 16], F32)
        t_gs = Tok()
        for s in range(2):
            S.op("dve", I("scalar_tensor_tensor", out=gs1T[:, s, :], in0=mv[:, 2 + s, :], scalar=1.0, in1=mv[:, 4, :],
                                                           op0=ALU.add, op1=ALU.mult),
                 reads=[t_mv], writes=[t_gs], merge=True)
        xt = [sb(f"pa_xt{i}", [128, D], F32) for i in range(2)]
        t_xt = [Tok(), Tok()]
        junk = sb("pa_junk", [128, D], BF16)
        t_junk = Tok()
        ssq = sb("pa_ss", [128, 8], F32)
        t_ss = [Tok() for _ in range(8)]
        xh = sb("pa_xh", [128, 4, D], BF16)
        t_xh = [Tok() for _ in range(4)]
        hTbs = [sb(f"pa_hT{i}", [128, 16, 512], BF16) for i in range(2)]
        t_hTs = [Tok(), Tok()]
        rS = [sb(f"pa_rS{i}", [128, 2, 512], F32) for i in range(2)]
        t_rS = [Tok(), Tok()]
        cc_s = sb("pa_cc", [128, 4, 512], F32)
        t_cc = [Tok() for _ in range(4)]
        t1 = sb("pa_t1", [128, 4, 512], F32)
        t_t1 = [Tok() for _ in range(4)]
        NST = 4
        stf = [sb(f"pa_stf{i}", [128, 512], F32) for i in range(NST)]
        t_stf = [Tok() for _ in range(NST)]
        stb = [sb(f"pa_stb{i}", [128, 512], BF16) for i in range(NST)]
        t_stb = [Tok() for _ in range(NST)]
        pT = [pp(f"pa_pT{i}", [128, 1024], BF16) for i in range(2)]
        t_pT = [Tok(), Tok()]
        NACC = 4
        acc = [pp(f"pa_acc{i}", [128, 512]) for i in range(NACC)]
        t_acc = [Tok() for _ in range(NACC)]
        cnt = {"f": 0, "b": 0, "a": 0, "x": 0}

        import os
        LV = int(os.environ.get("PA_STOP", "9"))
        for bi, (tok0, nb, s) in enumerate(BLOCKS):
            if LV <= 1 or (LV <= 3 and bi > 0):
                break
            ntile = nb // 128
            rb = bi % 2
            hTb = hTbs[bi % 2]
            t_hT = t_hTs[bi % 2]
            S.dma("sp", I("dma_start",
                out=rS[rb][:, :, 0:nb], in_=G.ropeS[:, :, tok0:tok0 + nb].rearrange("t p n -> p t n")),
                writes=[t_rS[rb]])
            for t in range(ntile):
                xi = cnt["x"] % 2
                cnt["x"] += 1
                si = (bi * 4 + t) % 8
                r0 = tok0 + t * 128
                S.dma("sp", I("dma_start", out=xt[xi][:], in_=G.xres[r0:r0 + 128, :]),
                      reads=[G.t["xres"]], writes=[t_xt[xi]])
                S.op("act", I("activation", out=junk[:], in_=xt[xi][:], func=AF.Square,
                                                                 accum_out=ssq[:, si:si + 1]),
                     reads=[t_xt[xi]], writes=[t_junk, t_ss[si]])
                rstd_from_ss(G, ssq[:, si:si + 1], ssq[:, si:si + 1], D, [t_ss[si]], [t_ss[si]])
                S.op("act", I("activation", out=xh[:, t, :], in_=xt[xi][:], func=AF.Copy,
                                                                      scale=ssq[:, si:si + 1]),
                     reads=[t_xt[xi], t_ss[si]], writes=[t_xh[t]])
            for j in range(16):
                pb = j % 2
                for t in range(ntile):
                    S.op("pe", I("transpose", out=pT[pb][:, t * 128:(t + 1) * 128],
                                                                     in_=xh[:, t, j * 128:(j + 1) * 128], identity=G.ident_b[:]),
                         reads=[t_xh[t], G.t_const], writes=[t_pT[pb]], merge=(t > 0))
                S.op("act", I("activation",
                    out=hTb[:, j, 0:nb], in_=pT[pb][:, 0:nb], func=AF.Identity,
                    bias=sh1T[:, s, j:j + 1], scale=gs1T[:, s, j:j + 1]),
                    reads=[t_pT[pb], t_sh, t_gs], writes=[t_hT], merge=(j > 0))
            S.dma("sp", I("dma_start",
                out=G.hT[:, tok0:tok0 + nb].rearrange("(j p) n -> p j n", p=128), in_=hTb[:, :, 0:nb]),
                reads=[t_hT], writes=[G.t["hT"]], merge=True)

            if LV <= 2:
                break
            def proj(c0, width):
                ai = cnt["a"] % NACC
                cnt["a"] += 1
                for j in range(16):
                    S.op("pe", I("matmul",
                        acc[ai][0:width, 0:nb], lhsT=wA[:, j, c0:c0 + width], rhs=hTb[:, j, 0:nb],
                        start=(j == 0), stop=(j == 15)),
                        reads=[t_w, t_hT], writes=[t_acc[ai]], merge=(j > 0))
                return ai

            def stage_f():
                i = cnt["f"] % NST
                cnt["f"] += 1
                return i

            def stage_b():
                i = cnt["b"] % NST
                cnt["b"] += 1
                return i

            for c in range(4):
                ai = proj(OFF_CB + c * 128, 128)
                fi = stage_f()
                S.op("act", I("copy", out=stf[fi][:, 0:nb], in_=acc[ai][:, 0:nb]),
                     reads=[t_acc[ai]], writes=[t_stf[fi]])
                S.dma("sp", I("dma_start", out=G.cbT[c * 128:(c + 1) * 128, tok0:tok0 + nb], in_=stf[fi][:, 0:nb]),
                      reads=[t_stf[fi]], writes=[G.t["cbT"]], merge=True)
                ai = proj(OFF_CC + c * 128, 128)
                S.op("act", I("copy", out=cc_s[:, c, 0:nb], in_=acc[ai][:, 0:nb]),
                     reads=[t_acc[ai]], writes=[t_cc[c]])
                ai = proj(OFF_CH + c * 128, 128)
                fi = stage_f()
                S.op("dve", I("tensor_tensor", out=stf[fi][:, 0:nb], in0=acc[ai][:, 0:nb],
                                                                        in1=cc_s[:, c, 0:nb], op=ALU.mult),
                     reads=[t_acc[ai], t_cc[c]], writes=[t_stf[fi]])
                S.dma("sp", I("dma_start", out=G.uT[c * 128:(c + 1) * 128, tok0:tok0 + nb], in_=stf[fi][:, 0:nb]),
                      reads=[t_stf[fi]], writes=[G.t["uT"]], merge=True)
            for c in range(4):
                ai = proj(OFF_SQ + c * 128, 128)
                S.op("dve", I("tensor_tensor", out=t1[:, c, 0:nb], in0=acc[ai][:, 0:nb],
                                                                        in1=rS[rb][:, 0, 0:nb], op=ALU.mult),
                     reads=[t_acc[ai], t_rS[rb]], writes=[t_t1[c]])
                ai = proj(OFF_SQW + c * 128, 128)
                fi = stage_f()
                S.op("dve", I("tensor_tensor", out=stf[fi][:, 0:nb], in0=acc[ai][:, 0:nb],
                                                                          in1=rS[rb][:, 1, 0:nb], op=ALU.mult),
                     reads=[t_acc[ai], t_rS[rb]], writes=[t_stf[fi]])
                bi_ = stage_b()
                S.op("pool", I("tensor_tensor", out=stb[bi_][:, 0:nb], in0=t1[:, c, 0:nb],
                                                                           in1=stf[fi][:, 0:nb], op=ALU.add),
                     reads=[t_t1[c], t_stf[fi]], writes=[t_stb[bi_]])
                S.dma("sp", I("dma_start", out=G.sqT[c * 128:(c + 1) * 128, tok0:tok0 + nb], in_=stb[bi_][:, 0:nb]),
                      reads=[t_stb[bi_]], writes=[G.t["sqT"]], merge=True)
        S.flush(f"proja{l}")


def phase_proj_b(G, l):
    nc, S = G.nc, G.S
    with ExitStack() as ps:
        sb, pp = _alloc(G, ps)
        w1 = sb("pb_w1", [128, 16, 896], BF16)
        w2 = sb("pb_w2", [128, 16, 960], BF16)
        wq = sb("pb_wq", [128, 4, 2, 768], BF16)
        wk = sb("pb_wk", [128, 2, 512], BF16)
        wv = sb("pb_wv", [128, 2, 512], BF16)
        d64 = sb("pb_d64", [128, 2, 128], BF16)
        t_w = Tok()
        for c0 in range(0, 896, 448):
            S.dma("pool", I("dma_start", out=w1[:, :, c0:c0 + 448],
                            in_=G.w_a[l][:, 2560 + c0:2560 + c0 + 448].rearrange("(j p) n -> p j n", p=128)),
                  writes=[t_w], merge=True)
        for c0 in range(0, 960, 480):
            S.dma("pool", I("dma_start", out=w2[:, :, c0:c0 + 480],
                            in_=G.w_b[l][:, c0:c0 + 480].rearrange("(j p) n -> p j n", p=128)),
                  writes=[t_w], merge=True)
        for ab in range(2):
            S.dma("pool", I("dma_start", out=wq[:, :, ab, :], in_=G.w_uq[l, ab].rearrange("(c p) n -> p c n", p=128)),
                  writes=[t_w], merge=True)
        S.dma("pool", I("dma_start", out=wk[:], in_=G.w_uk[l].rearrange("(c p) n -> p c n", p=128)), writes=[t_w], merge=True)
        S.dma("pool", I("dma_start", out=wv[:], in_=G.w_uv[l].rearrange("(c p) n -> p c n", p=128)), writes=[t_w], merge=True)
        S.dma("sp", I("dma_start", out=d64[:], in_=G.dft64.rearrange("t p n -> p t n")), writes=[t_w], merge=True)
        gT, t_gT = load_T(G, [G.q_norm_g[l], G.kv_norm_g[l]], sb, pp, "pb_g")
        hTb = [sb(f"pb_hT{i}", [128, 16, 512], BF16) for i in range(2)]
        t_hT = [Tok(), Tok()]
        rS = sb("pb_rS", [128, 2, 512], F32)
        rM = sb("pb_rM", [96, 2, 512], F32)
        t_r = Tok()
        cq_f = sb("pb_cqf", [128, 4, 512], F32)
        t_cqf = [Tok() for _ in range(4)]
        sqb = [sb(f"pb_sqb{i}", [128, 512], BF16) for i in range(2)]
        t_sqb = [Tok(), Tok()]
        rstd = sb("pb_rstd", [128, 2, 512], F32)
        t_rstd = [Tok(), Tok()]
        cqn = sb("pb_cqn", [128, 4, 512], BF16)
        t_cqn = Tok()
        ckf = sb("pb_ckf", [128, 2, 512], F32)
        t_ckf = [Tok(), Tok()]
        ckn = sb("pb_ckn", [128, 2, 512], BF16)
        t_ckn = Tok()
        fuT = sb("pb_fuT", [128, 4, 512], BF16)
        t_fuT = Tok()
        mq_st = sb("pb_mq", [96, 8, 512], BF16)
        t_mq = Tok()
        mk_st = sb("pb_mk", [96, 8, 512], BF16)
        t_mk = Tok()
        kpe_r = sb("pb_kpe", [96, 512], BF16)
        t_kpe = Tok()
        tA = [sb(f"pb_tA{i}", [128, 512], F32) for i in range(2)]
        t_tA = [Tok(), Tok()]
        tB = [sb(f"pb_tB{i}", [128, 512], F32) for i in range(2)]
        t_tB = [Tok(), Tok()]
        NST = 3
        stb = [sb(f"pb_stb{i}", [128, 512], BF16) for i in range(NST)]
        t_stb = [Tok() for _ in range(NST)]
        NACC = 3
        acc = [pp(f"pb_acc{i}", [128, 512]) for i in range(NACC)]
        t_acc = [Tok() for _ in range(NACC)]
        ssp = pp("pb_ss", [128, 512])
        t_ssp = Tok()
        tkp = [pp(f"pb_tk{i}", [128, 512]) for i in range(2)]
        t_tkp = [Tok(), Tok()]
        cnt = {"a": 0, "b": 0, "t": 0, "k": 0}
        import os
        LV = int(os.environ.get("PB_STOP", "9"))

        for bi, (tok0, nb, s) in enumerate(BLOCKS):
            if LV <= 3 and bi > 0:
                break
            ntile = nb // 128
            hb = bi % 2
            hT_ = hTb[hb]
            S.dma("sp", I("dma_start", out=hT_[:, :, 0:nb], in_=G.hT[:, tok0:tok0 + nb].rearrange("(j p) n -> p j n", p=128)),
                  reads=[G.t["hT"]], writes=[t_hT[hb]])
            S.dma("sp", I("dma_start", out=rS[:, :, 0:nb], in_=G.ropeS[:, :, tok0:tok0 + nb].rearrange("t p n -> p t n")),
                  writes=[t_r])
            S.dma("sp", I("dma_start", out=rM[:, :, 0:nb], in_=G.ropeM[:, :, tok0:tok0 + nb].rearrange("t p n -> p t n")),
                  writes=[t_r], merge=True)

            def proj(wt, c0, width, rhs_tile=None):
                ai = cnt["a"] % NACC
                cnt["a"] += 1
                for j in range(16):
                    S.op("pe", I("matmul", acc[ai][0:width, 0:nb], lhsT=wt[:, j, c0:c0 + width], rhs=hT_[:, j, 0:nb],
                                 start=(j == 0), stop=(j == 15)),
                         reads=[t_w, t_hT[hb]], writes=[t_acc[ai]], merge=(j > 0))
                return ai

            def rope_pair(aiA, aiB, p0, p1, table, out_ap, t_out, merge_out=False):
                ti = cnt["t"] % 2
                cnt["t"] += 1
                S.op("dve", I("tensor_tensor", out=tA[ti][p0:p1, 0:nb], in0=acc[aiA][p0:p1, 0:nb], in1=table[p0:p1, 0, 0:nb], op=ALU.mult),
                     reads=[t_acc[aiA], t_r], writes=[t_tA[ti]])
                S.op("dve", I("tensor_tensor", out=tB[ti][p0:p1, 0:nb], in0=acc[aiB][p0:p1, 0:nb], in1=table[p0:p1, 1, 0:nb], op=ALU.mult),
                     reads=[t_acc[aiB], t_r], writes=[t_tB[ti]])
                S.op("pool", I("tensor_tensor", out=out_ap, in0=tA[ti][p0:p1, 0:nb], in1=tB[ti][p0:p1, 0:nb], op=ALU.add),
                     reads=[t_tA[ti], t_tB[ti]], writes=[t_out], merge=merge_out)

            def stage_b():
                i = cnt["b"] % NST
                cnt["b"] += 1
                return i

            aiA = proj(w1, 0, 128)
            aiB = proj(w1, 128, 128)
            bi_ = stage_b()
            rope_pair(aiA, aiB, 0, 128, rS, stb[bi_][:, 0:nb], t_stb[bi_])
            S.dma("sp", I("dma_start", out=G.skT[:, tok0:tok0 + nb], in_=stb[bi_][:, 0:nb]), reads=[t_stb[bi_]],
                  writes=[G.t["skT"]], merge=True)
            ki = cnt["k"] % 2
            cnt["k"] += 1
            for t in range(ntile):
                for j in range(16):
                    S.op("pe", I("matmul", tkp[ki][:, t * 128:(t + 1) * 128], lhsT=hT_[:, j, t * 128:(t + 1) * 128],
                                 rhs=w1[:, j, 256:384], start=(j == 0), stop=(j == 15)),
                         reads=[t_w, t_hT[hb]], writes=[t_tkp[ki]], merge=(j > 0 or t > 0))
            bi_ = stage_b()
            S.op("act", I("copy", out=stb[bi_][:, 0:nb], in_=tkp[ki][:, 0:nb]), reads=[t_tkp[ki]], writes=[t_stb[bi_]])
            S.dma("sp", I("dma_start", out=G.svd[tok0:tok0 + nb, :].rearrange("(t p) c -> p t c", p=128),
                          in_=stb[bi_][:, 0:nb].rearrange("p (t c) -> p t c", c=128)),
                  reads=[t_stb[bi_]], writes=[G.t["svd"]], merge=True)
            for c in range(4):
                ai = proj(w1, 384 + c * 128, 128)
                S.op("act", I("copy", out=fuT[:, c, 0:nb], in_=acc[ai][:, 0:nb]), reads=[t_acc[ai]], writes=[t_fuT], merge=(c > 0))
            for t in range(ntile):
                for cs_, dst, key in ((0, G.ucd, "ucd"), (1, G.usd, "usd")):
                    ki = cnt["k"] % 2
                    cnt["k"] += 1
                    for c in range(4):
                        S.op("pe", I("matmul", tkp[ki][:, c * 128:(c + 1) * 128], lhsT=fuT[:, c, t * 128:(t + 1) * 128],
                                     rhs=d64[:, cs_, :], start=True, stop=True),
                             reads=[t_w, t_fuT], writes=[t_tkp[ki]], merge=(c > 0))
                    bi_ = stage_b()
                    S.op("act" if cs_ == 0 else "dve",
                         I("copy", out=stb[bi_][:], in_=tkp[ki][:]) if cs_ == 0 else I("tensor_copy", out=stb[bi_][:], in_=tkp[ki][:]),
                         reads=[t_tkp[ki]], writes=[t_stb[bi_]])
                    S.dma("sp", I("dma_start", out=dst[tok0 + t * 128:tok0 + (t + 1) * 128, :], in_=stb[bi_][:]),
                          reads=[t_stb[bi_]], writes=[G.t[key]], merge=True)
            def latent_norm(c0, nch, f_tile, t_f, n_tile, t_n, gi, ri):
                for c in range(nch):
                    ai = proj(w2, c0 + c * 128, 128)
                    S.op("act", I("copy", out=f_tile[:, c, 0:nb], in_=acc[ai][:, 0:nb]), reads=[t_acc[ai]], writes=[t_f[c]])
                    qi = c % 2
                    S.op("act", I("activation", out=sqb[qi][:, 0:nb], in_=acc[ai][:, 0:nb], func=AF.Square),
                         reads=[t_acc[ai]], writes=[t_sqb[qi]])
                    S.op("pe", I("matmul", ssp[:, 0:nb], lhsT=G.ones_b[:], rhs=sqb[qi][:, 0:nb], start=(c == 0), stop=(c == nch - 1)),
                         reads=[t_sqb[qi], G.t_const], writes=[t_ssp], merge=(c > 0))
                rstd_from_ss(G, rstd[:, ri, 0:nb], ssp[:, 0:nb], nch * 128, [t_ssp], [t_rstd[ri]])
                for c in range(nch):
                    S.op("dve", I("scalar_tensor_tensor", out=n_tile[:, c, 0:nb], in0=f_tile[:, c, 0:nb], scalar=gT[:, gi, c:c + 1],
                                  in1=rstd[:, ri, 0:nb], op0=ALU.mult, op1=ALU.mult),
                         reads=[t_f[c], t_gT, t_rstd[ri]], writes=[t_n], merge=(c > 0))

            latent_norm(0, 4, cq_f, t_cqf, cqn, t_cqn, 0, 0)
            latent_norm(512, 2, ckf, t_ckf, ckn, t_ckn, 1, 1)
            for h in range(8):
                ais = []
                for ab in range(2):
                    ai = cnt["a"] % NACC
                    cnt["a"] += 1
                    for c in range(4):
                        S.op("pe", I("matmul", acc[ai][0:96, 0:nb], lhsT=wq[:, c, ab, h * 96:(h + 1) * 96], rhs=cqn[:, c, 0:nb],
                                     start=(c == 0), stop=(c == 3)),
                             reads=[t_w, t_cqn], writes=[t_acc[ai]], merge=(c > 0))
                    ais.append(ai)
                S.op("act", I("copy", out=mq_st[0:64, h, 0:nb], in_=acc[ais[0]][0:64, 0:nb]), reads=[t_acc[ais[0]]],
                     writes=[t_mq], merge=(h > 0))
                rope_pair(ais[0], ais[1], 64, 96, rM, mq_st[64:96, h, 0:nb], t_mq, merge_out=True)
            S.dma("sp", I("dma_start", out=G.mqT[:, :, tok0:tok0 + nb].rearrange("h p n -> p h n"), in_=mq_st[:, :, 0:nb]),
                  reads=[t_mq], writes=[G.t["mqT"]], merge=True)
            aiA = proj(w2, OFF_KPA, 96)
            aiB = proj(w2, OFF_KPB, 96)
            rope_pair(aiA, aiB, 64, 96, rM, kpe_r[64:96, 0:nb], t_kpe)
            for h in range(8):
                ai = cnt["a"] % NACC
                cnt["a"] += 1
                for c in range(2):
                    S.op("pe", I("matmul", acc[ai][0:64, 0:nb], lhsT=wk[:, c, h * 64:(h + 1) * 64], rhs=ckn[:, c, 0:nb],
                                 start=(c == 0), stop=(c == 1)),
                         reads=[t_w, t_ckn], writes=[t_acc[ai]], merge=(c > 0))
                S.op("act", I("copy", out=mk_st[0:64, h, 0:nb], in_=acc[ai][0:64, 0:nb]), reads=[t_acc[ai]],
                     writes=[t_mk], merge=(h > 0))
                S.op("pool" if h % 2 else "dve", I("tensor_copy", out=mk_st[64:96, h, 0:nb], in_=kpe_r[64:96, 0:nb]),
                     reads=[t_kpe], writes=[t_mk], merge=True)
            S.dma("sp", I("dma_start", out=G.mkT[:, :, tok0:tok0 + nb].rearrange("h p n -> p h n"), in_=mk_st[:, :, 0:nb]),
                  reads=[t_mk], writes=[G.t["mkT"]], merge=True)
            for t in range(ntile):
                ki = cnt["k"] % 2
                cnt["k"] += 1
                for c in range(2):
                    S.op("pe", I("matmul", tkp[ki][:], lhsT=ckn[:, c, t * 128:(t + 1) * 128], rhs=wv[:, c, :],
                                 start=(c == 0), stop=(c == 1)),
                         reads=[t_w, t_ckn], writes=[t_tkp[ki]], merge=(c > 0))
                bi_ = stage_b()
                S.op("act", I("copy", out=stb[bi_][:], in_=tkp[ki][:]), reads=[t_tkp[ki]], writes=[t_stb[bi_]])
                S.dma("sp", I("dma_start", out=G.mvd[tok0 + t * 128:tok0 + (t + 1) * 128, :], in_=stb[bi_][:]),
                      reads=[t_stb[bi_]], writes=[G.t["mvd"]], merge=True)
        S.flush(f"projb{l}")


class GroupTail:
    def __init__(self, G, l, gi, sb, pp, name):
        self.G, self.gi = G, gi
        self.gT, self.t_gT = load_T(G, [G.out_norm_g[l, gi * 512:(gi + 1) * 512]], sb, pp, name + "_g")
        self.junk = sb(name + "_junk", [128, 512], BF16)
        self.t_junk = Tok()
        self.ss = [sb(name + f"_ss{i}", [128, 1], F32) for i in range(2)]
        self.t_ss = [Tok(), Tok()]
        self.ynb = [sb(name + f"_ynb{i}", [128, 512], BF16) for i in range(2)]
        self.t_ynb = [Tok(), Tok()]
        self.tp = pp(name + "_tp", [128, 1024], BF16)
        self.t_tp = Tok()
        self.st = [sb(name + f"_st{i}", [128, 4, 128], BF16) for i in range(2)]
        self.t_st = [Tok(), Tok()]
        self.k = 0

    def emit(self, y_ap, t_y, tok0):
        G, S = self.G, self.G.S
        i = self.k % 2
        self.k += 1
        S.op("act", I("activation", out=self.junk[:], in_=y_ap, func=AF.Square, accum_out=self.ss[i][:, 0:1]),
             reads=[t_y], writes=[self.t_junk, self.t_ss[i]])
        rstd_from_ss(G, self.ss[i][:, 0:1], self.ss[i][:, 0:1], 512, [self.t_ss[i]], [self.t_ss[i]])
        S.op("act", I("activation", out=self.ynb[i][:], in_=y_ap, func=AF.Copy, scale=self.ss[i][:, 0:1]),
             reads=[t_y, self.t_ss[i]], writes=[self.t_ynb[i]])
        for c in range(4):
            S.op("pe", I("transpose", out=self.tp[:, c * 128:(c + 1) * 128], in_=self.ynb[i][:, c * 128:(c + 1) * 128],
                         identity=G.ident_b[:]),
                 reads=[self.t_ynb[i], G.t_const], writes=[self.t_tp], merge=(c > 0))
        for c in range(4):
            S.op("dve", I("tensor_scalar", out=self.st[i][:, c, :], in0=self.tp[:, c * 128:(c + 1) * 128],
                          scalar1=self.gT[:, 0, c:c + 1], scalar2=None, op0=ALU.mult),
                 reads=[self.t_tp, self.t_gT], writes=[self.t_st[i]], merge=(c > 0))
        r0 = self.gi * 512
        S.dma("sp", I("dma_start", out=G.ynT[r0:r0 + 512, tok0:tok0 + 128].rearrange("(c p) n -> p c n", p=128), in_=self.st[i][:]),
              reads=[self.t_st[i]], writes=[G.t["ynT"]], merge=True)


def phase_conv(G, l, do_ctx):
    nc, S = G.nc, G.S
    with ExitStack() as ps:
        sb, pp = _alloc(G, ps)
        cw, t_cw = load_T(G, [G.conv_w[l, 0], G.conv_w[l, 1], G.conv_w[l, 2], G.out_norm_g[l, 0:512]], sb, pp, "cv_w")
        ut = [sb(f"cv_u{i}", [128, 514], F32) for i in range(2)]
        t_ut = [Tok(), Tok()]
        cbt = [sb(f"cv_cb{i}", [128, 512], F32) for i in range(2)]
        t_cbt = [Tok(), Tok()]
        acc_t = [sb(f"cv_a{i}", [128, 512], F32) for i in range(2)]
        t_at = [Tok(), Tok()]
        yc = sb("cv_y", [128, 4, 512], F32)
        t_yc = [Tok() for _ in range(4)]
        sqb = [sb(f"cv_sq{i}", [128, 512], BF16) for i in range(2)]
        t_sqb = [Tok(), Tok()]
        rstd = sb("cv_rstd", [128, 512], F32)
        t_rstd = Tok()
        stb = [sb(f"cv_st{i}", [128, 512], BF16) for i in range(2)]
        t_stb = [Tok(), Tok()]
        ssp = pp("cv_ss", [128, 512])
        t_ssp = Tok()
        segs = [(0, NL)] + ([(NL, NT)] if do_ctx else [])
        k = 0
        for (s0, s1) in segs:
            for b0 in range(s0, s1, 512):
                nb = min(512, s1 - b0)
                for c in range(4):
                    i = k % 2
                    k += 1
                    lo = b0 - 1 if b0 > s0 else b0
                    hi = b0 + nb + 1 if b0 + nb < s1 else b0 + nb
                    first = True
                    if lo == b0:
                        S.op("pool", I("memset", ut[i][:, 0:1], 0.0), writes=[t_ut[i]])
                        first = False
                    if hi == b0 + nb:
                        S.op("pool", I("memset", ut[i][:, nb + 1:nb + 2], 0.0), writes=[t_ut[i]], merge=not first)
                        first = False
                    S.dma("sp", I("dma_start", out=ut[i][:, lo - (b0 - 1):hi - (b0 - 1)], in_=G.uT[c * 128:(c + 1) * 128, lo:hi]),
                          reads=[G.t["uT"]], writes=[t_ut[i]], merge=not first)
                    S.dma("sp", I("dma_start", out=cbt[i][:, 0:nb], in_=G.cbT[c * 128:(c + 1) * 128, b0:b0 + nb]),
                          reads=[G.t["cbT"]], writes=[t_cbt[i]])
                    S.op("dve", I("tensor_scalar", out=acc_t[i][:, 0:nb], in0=ut[i][:, 0:nb], scalar1=cw[:, 0, c:c + 1], scalar2=None,
                                  op0=ALU.mult), reads=[t_ut[i], t_cw], writes=[t_at[i]])
                    S.op("dve", I("scalar_tensor_tensor", out=acc_t[i][:, 0:nb], in0=ut[i][:, 1:nb + 1], scalar=cw[:, 1, c:c + 1],
                                  in1=acc_t[i][:, 0:nb], op0=ALU.mult, op1=ALU.add), reads=[t_ut[i], t_cw, t_at[i]], writes=[t_at[i]])
                    S.op("dve", I("scalar_tensor_tensor", out=acc_t[i][:, 0:nb], in0=ut[i][:, 2:nb + 2], scalar=cw[:, 2, c:c + 1],
                                  in1=acc_t[i][:, 0:nb], op0=ALU.mult, op1=ALU.add), reads=[t_ut[i], t_cw, t_at[i]], writes=[t_at[i]])
                    S.op("pool", I("tensor_tensor", out=yc[:, c, 0:nb], in0=acc_t[i][:, 0:nb], in1=cbt[i][:, 0:nb], op=ALU.mult),
                         reads=[t_at[i], t_cbt[i]], writes=[t_yc[c]])
                    S.op("act", I("activation", out=sqb[i][:, 0:nb], in_=yc[:, c, 0:nb], func=AF.Square), reads=[t_yc[c]], writes=[t_sqb[i]])
                    S.op("pe", I("matmul", ssp[:, 0:nb], lhsT=G.ones_b[:], rhs=sqb[i][:, 0:nb], start=(c == 0), stop=(c == 3)),
                         reads=[t_sqb[i], G.t_const], writes=[t_ssp], merge=(c > 0))
                rstd_from_ss(G, rstd[:, 0:nb], ssp[:, 0:nb], 512, [t_ssp], [t_rstd])
                for c in range(4):
                    i = k % 2
                    k += 1
                    S.op("dve", I("scalar_tensor_tensor", out=stb[i][:, 0:nb], in0=yc[:, c, 0:nb], scalar=cw[:, 3, c:c + 1],
                                  in1=rstd[:, 0:nb], op0=ALU.mult, op1=ALU.mult), reads=[t_yc[c], t_cw, t_rstd], writes=[t_stb[i]])
                    S.dma("sp", I("dma_start", out=G.ynT[c * 128:(c + 1) * 128, b0:b0 + nb], in_=stb[i][:, 0:nb]),
                          reads=[t_stb[i]], writes=[G.t["ynT"]], merge=True)
        S.flush(f"conv{l}")


def phase_swa(G, l, do_ctx):
    nc, S = G.nc, G.S
    SCALE = 64 ** -0.5
    with ExitStack() as ps:
        sb, pp = _alloc(G, ps)
        Qs = sb("sw_Q", [64, 8, NT], BF16)
        Ks = sb("sw_K", [64, 2, NT], BF16)
        Vs = sb("sw_V", [128, 34, 2, 65], BF16)
        mk = sb("sw_mask", [128, 2, 512], BF16)
        snk = sb("sw_sink", [128, 8], F32)
        t_in = Tok()
        t_V = Tok()
        for h in range(8):
            S.dma("sp", I("dma_start", out=Qs[:, h, :], in_=G.sqT[h * 64:(h + 1) * 64, :]), reads=[G.t["sqT"]], writes=[t_in], merge=True)
        for h in range(2):
            S.dma("sp", I("dma_start", out=Ks[:, h, :], in_=G.skT[h * 64:(h + 1) * 64, :]), reads=[G.t["skT"]], writes=[t_in], merge=True)
        S.op("pool", I("memset", Vs[:, :, :, 64:65], 1.0), writes=[t_V])
        for h in range(2):
            S.dma("sp", I("dma_start", out=Vs[:, :, h, 0:64], in_=G.svd[:, h * 64:(h + 1) * 64].rearrange("(t p) d -> p t d", p=128)),
                  reads=[G.t["svd"]], writes=[t_V], merge=True)
        S.dma("sp", I("dma_start", out=mk[:], in_=G.masks.rearrange("t p n -> p t n")), writes=[t_in], merge=True)
        S.dma("sp", I("dma_start", out=snk[:], in_=G.swa_sink[l:l + 1, :].to_broadcast([128, 8])), writes=[t_in], merge=True)
        S.op("act", I("activation", out=snk[:], in_=snk[:], func=AF.Exp), reads=[t_in], writes=[t_in])
        tail = GroupTail(G, l, 1, sb, pp, "sw_t")
        pT = [sb(f"sw_pT{i}", [128, 5, 512], BF16) for i in range(2)]
        t_pT = [[Tok() for _ in range(5)] for _ in range(2)]
        sps = [pp(f"sw_s{i}", [128, 512]) for i in range(2)]
        t_sps = [Tok(), Tok()]
        ops_ = [pp(f"sw_o{i}", [128, 512]) for i in range(2)]
        t_ops = [Tok(), Tok()]
        den = [sb(f"sw_den{i}", [128, 8], F32) for i in range(2)]
        t_den = [Tok(), Tok()]
        ysw = [sb(f"sw_y{i}", [128, 512], F32) for i in range(2)]
        t_ysw = [Tok(), Tok()]
        qblocks = [(i, "lat") for i in range(32)] + ([(32, "ctx"), (33, "ctx")] if do_ctx else [])
        import os
        LV = int(os.environ.get("SW_STOP", "99"))
        kq = 0
        ks = 0
        for (i, kind) in qblocks[:LV]:
            if kind == "lat":
                kts = ([(i - 1, 0)] if i > 0 else []) + [(i, None)] + ([(i + 1, 1)] if i < 31 else []) + [(32, None), (33, None)]
            else:
                kts = [(32, None), (33, None)]
            yi = kq % 2
            for kvh in range(2):
                pi = kq % 2
                oi = kq % 2
                kq += 1
                for n, (kt, msk) in enumerate(kts):
                    si = ks % 2
                    ks += 1
                    S.op("pe", I("matmul", sps[si][:], lhsT=Ks[:, kvh, kt * 128:(kt + 1) * 128],
                                 rhs=Qs[:, kvh * 4:(kvh + 1) * 4, i * 128:(i + 1) * 128], start=True, stop=True),
                         reads=[t_in], writes=[t_sps[si]])
                    S.op("act", I("activation", out=pT[pi][:, n, :], in_=sps[si][:], func=AF.Exp, scale=SCALE),
                         reads=[t_sps[si]], writes=[t_pT[pi][n]])
                    if msk is not None:
                        S.op("dve", I("tensor_tensor", out=pT[pi][:, n, :], in0=pT[pi][:, n, :], in1=mk[:, msk, :], op=ALU.mult),
                             reads=[t_pT[pi][n], t_in], writes=[t_pT[pi][n]])
                for g in range(4):
                    for n, (kt, msk) in enumerate(kts):
                        S.op("pe", I("matmul", ops_[oi][:, g * 65:(g + 1) * 65], lhsT=pT[pi][:, n, g * 128:(g + 1) * 128],
                                     rhs=Vs[:, kt, kvh, :], start=(n == 0), stop=(n == len(kts) - 1)),
                             reads=[t_pT[pi][n], t_V], writes=[t_ops[oi]], merge=(n > 0 or g > 0))
                ov = ops_[oi][:, 0:260].rearrange("p (g e) -> p g e", e=65)
                S.op("dve", I("tensor_tensor", out=den[yi][:, kvh * 4:(kvh + 1) * 4], in0=ov[:, :, 64], in1=snk[:, kvh * 4:(kvh + 1) * 4], op=ALU.add),
                     reads=[t_ops[oi], t_in], writes=[t_den[yi]], merge=(kvh > 0))
                S.op("dve", I("reciprocal", out=den[yi][:, kvh * 4:(kvh + 1) * 4], in_=den[yi][:, kvh * 4:(kvh + 1) * 4]),
                     reads=[t_den[yi]], writes=[t_den[yi]])
                for g in range(4):
                    h = kvh * 4 + g
                    S.op("dve", I("tensor_scalar", out=ysw[yi][:, h * 64:(h + 1) * 64], in0=ov[:, g, 0:64], scalar1=den[yi][:, h:h + 1],
                                  scalar2=None, op0=ALU.mult),
                         reads=[t_ops[oi], t_den[yi]], writes=[t_ysw[yi]], merge=(h > 0))
            tail.emit(ysw[yi][:], t_ysw[yi], i * 128)
        S.flush(f"swa{l}")


def phase_fnet(G, l, do_ctx):
    nc, S = G.nc, G.S
    with ExitStack() as ps:
        sb, pp = _alloc(G, ps)
        uc = sb("fn_uc", [128, 34, 512], BF16)
        us = sb("fn_us", [128, 34, 512], BF16)
        t_u = Tok()
        for (t0, t1) in ((0, 16), (16, 34)):
            S.dma("sp", I("dma_start", out=uc[:, t0:t1, :], in_=G.ucd[t0 * 128:t1 * 128, :].rearrange("(t p) c -> p t c", p=128)),
                  reads=[G.t["ucd"]], writes=[t_u], merge=True)
            S.dma("sp", I("dma_start", out=us[:, t0:t1, :], in_=G.usd[t0 * 128:t1 * 128, :].rearrange("(t p) c -> p t c", p=128)),
                  reads=[G.t["usd"]], writes=[t_u], merge=True)
        dt_ = [sb(f"fn_d{i}", [128, 2 * 32 * 128], BF16) for i in range(2)]
        t_dt = [Tok(), Tok()]
        acc = [pp(f"fn_acc{i}", [128, 512]) for i in range(2)]
        t_acc = [Tok(), Tok()]
        yf = [sb(f"fn_y{i}", [128, 512], F32) for i in range(2)]
        t_yf = [Tok(), Tok()]
        tail = GroupTail(G, l, 2, sb, pp, "fn_t")
        import os
        LV = int(os.environ.get("FN_STOP", "99"))
        jobs = [(kt, 32, 0, G.dftL) for kt in range(32)][:LV] + ([(kt, 2, 32, G.dftC) for kt in range(2)] if do_ctx else [])
        for k, (kt, nt, tb, tab) in enumerate(jobs):
            i = k % 2
            dv = dt_[i][:, 0:2 * nt * 128]
            S.dma("sp", I("dma_start", out=dv, in_=tab[kt]), writes=[t_dt[i]])
            d4 = dv.rearrange("p (a n k) -> p a n k", a=2, k=128)
            for n in range(nt):
                S.op("pe", I("matmul", acc[i][:], lhsT=d4[:, 0, n, :], rhs=uc[:, tb + n, :], start=(n == 0), stop=False),
                     reads=[t_dt[i], t_u], writes=[t_acc[i]], merge=(n > 0))
            for n in range(nt):
                S.op("pe", I("matmul", acc[i][:], lhsT=d4[:, 1, n, :], rhs=us[:, tb + n, :], start=False, stop=(n == nt - 1)),
                     reads=[t_dt[i], t_u], writes=[t_acc[i]], merge=True)
            S.op("act", I("copy", out=yf[i][:], in_=acc[i][:]), reads=[t_acc[i]], writes=[t_yf[i]])
            tail.emit(yf[i][:], t_yf[i], (tb + kt) * 128)
        S.flush(f"fnet{l}")


def phase_mla(G, l, do_ctx):
    nc, S = G.nc, G.S
    SCALE = 96 ** -0.5
    with ExitStack() as ps:
        sb, pp = _alloc(G, ps)
        Kh = [sb(f"ml_K{i}", [96, NT], BF16) for i in range(2)]
        Qh = [sb(f"ml_Q{i}", [96, NT], BF16) for i in range(2)]
        Vh = [sb(f"ml_V{i}", [128, 34, 65], BF16) for i in range(2)]
        t_K = [Tok(), Tok()]
        t_Q = [Tok(), Tok()]
        t_V = [Tok(), Tok()]
        PT = [sb(f"ml_PT{i}", [128, 34, 512], BF16) for i in range(2)]
        t_PT = [[Tok() for _ in range(34)] for _ in range(2)]
        yall = sb("ml_y", [128, 34, 512], F32)
        t_y = [Tok() for _ in range(34)]
        rc = [sb(f"ml_rc{i}", [128, 1], F32) for i in range(4)]
        t_rc = [Tok() for _ in range(4)]
        sps = [pp(f"ml_s{i}", [128, 512]) for i in range(2)]
        t_sps = [Tok(), Tok()]
        ops_ = [pp(f"ml_o{i}", [128, 512]) for i in range(4)]
        t_ops = [Tok() for _ in range(4)]
        import os
        LVH = int(os.environ.get("ML_HEADS", "8"))
        LVQ = int(os.environ.get("ML_QB", "99"))
        qblocks = [(i * 512, 512, list(range(34))) for i in range(8)][:LVQ] + ([(NL, NCX, [32, 33])] if do_ctx else [])
        ks = 0
        kp = 0
        ko = 0
        for h in range(LVH):
            hb = h % 2
            S.dma("sp", I("dma_start", out=Kh[hb][:], in_=G.mkT[h]), reads=[G.t["mkT"]], writes=[t_K[hb]])
            S.dma("sp", I("dma_start", out=Qh[hb][:], in_=G.mqT[h]), reads=[G.t["mqT"]], writes=[t_Q[hb]])
            S.op("pool", I("memset", Vh[hb][:, :, 64:65], 1.0), writes=[t_V[hb]])
            S.dma("sp", I("dma_start", out=Vh[hb][:, :, 0:64], in_=G.mvd[:, h * 64:(h + 1) * 64].rearrange("(t p) d -> p t d", p=128)),
                  reads=[G.t["mvd"]], writes=[t_V[hb]], merge=True)
            for (q0, nq, kts) in qblocks:
                pi = kp % 2
                kp += 1
                for kt in kts:
                    si = ks % 2
                    ks += 1
                    S.op("pe", I("matmul", sps[si][:, 0:nq], lhsT=Kh[hb][:, kt * 128:(kt + 1) * 128], rhs=Qh[hb][:, q0:q0 + nq],
                                 start=True, stop=True), reads=[t_K[hb], t_Q[hb]], writes=[t_sps[si]])
                    S.op("act", I("activation", out=PT[pi][:, kt, 0:nq], in_=sps[si][:, 0:nq], func=AF.Exp, scale=SCALE),
                         reads=[t_sps[si]], writes=[t_PT[pi][kt]])
                for j in range(nq // 128):
                    oi = ko % 4
                    ko += 1
                    for n, kt in enumerate(kts):
                        S.op("pe", I("matmul", ops_[oi][:, 0:65], lhsT=PT[pi][:, kt, j * 128:(j + 1) * 128], rhs=Vh[hb][:, kt, :],
                                     start=(n == 0), stop=(n == len(kts) - 1)),
                             reads=[t_PT[pi][kt], t_V[hb]], writes=[t_ops[oi]], merge=(n > 0))
                    S.op("dve", I("reciprocal", out=rc[oi][:], in_=ops_[oi][:, 64:65]), reads=[t_ops[oi]], writes=[t_rc[oi]])
                    tile_i = q0 // 128 + j
                    S.op("dve", I("tensor_scalar", out=yall[:, tile_i, h * 64:(h + 1) * 64], in0=ops_[oi][:, 0:64], scalar1=rc[oi][:, 0:1],
                                  scalar2=None, op0=ALU.mult),
                         reads=[t_ops[oi], t_rc[oi]], writes=[t_y[tile_i]], merge=(h > 0))
        if LVH == 8:
            tail = GroupTail(G, l, 3, sb, pp, "ml_t")
            ntile = (qblocks[-1][0] + qblocks[-1][1]) // 128 if LVQ >= 8 else LVQ * 4
            tiles = list(range(min(32, ntile))) + ([32, 33] if do_ctx else [])
            for ti in tiles:
                tail.emit(yall[:, ti, :], t_y[ti], ti * 128)
        else:
            G.dbg_yall = (yall, t_y)
        S.flush(f"mla{l}")


def phase_outproj(G, l, do_ctx):
    nc, S = G.nc, G.S
    with ExitStack() as ps:
        sb, pp = _alloc(G, ps)
        wo = sb("op_wo", [128, 16, D], BF16)
        t_w = Tok()
        for c0 in range(0, D, 512):
            S.dma("pool", I("dma_start", out=wo[:, :, c0:c0 + 512], in_=G.w_out[l][:, c0:c0 + 512].rearrange("(j p) n -> p j n", p=128)),
                  writes=[t_w], merge=True)
        wr = sb("op_wr", [128, 16, NE], F32)
        S.dma("sp", I("dma_start", out=wr[:], in_=G.w_router[l].rearrange("(j p) e -> p j e", p=128)), writes=[t_w], merge=True)
        g1b = sb("op_g1b", [128, D], F32)
        sh2b = sb("op_sh2b", [128, D], F32)
        gs2b = sb("op_gs2b", [128, D], F32)
        t_bc = Tok()
        NB = 2
        xt = [sb(f"op_x{i}", [128, D], F32) for i in range(NB)]
        t_xt = [Tok() for _ in range(NB)]
        xn = [sb(f"op_xn{i}", [128, D], F32) for i in range(NB)]
        t_xn = [Tok() for _ in range(NB)]
        fx = [sb(f"op_fx{i}", [128, D], F32) for i in range(NB)]
        t_fx = [Tok() for _ in range(NB)]
        fxb = [sb(f"op_fxb{i}", [128, D], BF16) for i in range(NB)]
        t_fxb = [Tok() for _ in range(NB)]
        junk = sb("op_junk", [128, D], BF16)
        t_junk = Tok()
        yn = [sb(f"op_yn{i}", [128, 16, 128], BF16) for i in range(NB)]
        t_yn = [Tok() for _ in range(NB)]
        fxT = [sb(f"op_fxT{i}", [128, 16, 128], F32) for i in range(NB)]
        t_fxT = [Tok() for _ in range(NB)]
        ss = [sb(f"op_ss{i}", [128, 1], F32) for i in range(NB)]
        t_ss = [Tok() for _ in range(NB)]
        sm = [sb(f"op_sm{i}", [128, 4], F32) for i in range(NB)]
        t_sm = [Tok() for _ in range(NB)]
        ex = [sb(f"op_ex{i}", [128, NE], F32) for i in range(NB)]
        t_ex = [Tok() for _ in range(NB)]
        acc = [pp(f"op_acc{i}", [128, 512]) for i in range(4)]
        t_acc = [Tok() for _ in range(4)]
        trp = [pp(f"op_tr{i}", [128, 512]) for i in range(2)]
        t_trp = [Tok(), Tok()]
        lgp = [pp(f"op_lg{i}", [128, 512]) for i in range(2)]
        t_lgp = [Tok(), Tok()]
        import os
        LV = int(os.environ.get("OP_STOP", "99"))
        tiles = (list(range(32)) + ([32, 33] if do_ctx else []))[:LV]
        cur_s = None
        kt = [0]
        def part1(k, ti):
            nonlocal cur_s
            s_ = 1 if ti >= 32 else 0
            if s_ != cur_s:
                cur_s = s_
                S.dma("sp", I("dma_start", out=g1b[:], in_=G.modrow[l, s_:s_ + 1, 2 * D:3 * D].to_broadcast([128, D])),
                      reads=[G.t["modrow"]], writes=[t_bc])
                S.dma("sp", I("dma_start", out=sh2b[:], in_=G.modrow[l, s_:s_ + 1, 3 * D:4 * D].to_broadcast([128, D])),
                      reads=[G.t["modrow"]], writes=[t_bc], merge=True)
                S.dma("sp", I("dma_start", out=gs2b[:], in_=G.modrow[l, s_:s_ + 1, 4 * D:5 * D].to_broadcast([128, D])),
                      reads=[G.t["modrow"]], writes=[t_bc], merge=True)
                S.dma("sp", I("dma_start", out=fx[0][:], in_=G.norm2_g[l:l + 1, :].to_broadcast([128, D])), writes=[t_fx[0]])
                S.op("dve", I("scalar_tensor_tensor", out=gs2b[:], in0=gs2b[:], scalar=1.0, in1=fx[0][:], op0=ALU.add, op1=ALU.mult),
                     reads=[t_bc, t_fx[0]], writes=[t_bc])
            tok0 = ti * 128
            i = k % NB
            S.dma("sp", I("dma_start", out=yn[i][:], in_=G.ynT[:, tok0:tok0 + 128].rearrange("(j p) n -> p j n", p=128)),
                  reads=[G.t["ynT"]], writes=[t_yn[i]])
            S.dma("sp", I("dma_start", out=xt[i][:], in_=G.xres[tok0:tok0 + 128, :]), reads=[G.t["xres"]], writes=[t_xt[i]])
            for nb in range(4):
                for j in range(16):
                    S.op("pe", I("matmul", acc[nb][:], lhsT=yn[i][:, j, :], rhs=wo[:, j, nb * 512:(nb + 1) * 512], start=(j == 0), stop=(j == 15)),
                         reads=[t_yn[i], t_w], writes=[t_acc[nb]], merge=(j > 0))
                S.op("dve", I("tensor_tensor", out=xn[i][:, nb * 512:(nb + 1) * 512], in0=acc[nb][:], in1=g1b[:, nb * 512:(nb + 1) * 512], op=ALU.mult),
                     reads=[t_acc[nb], t_bc], writes=[t_xn[i]], merge=(nb > 0))
            S.op("pool", I("tensor_tensor", out=xn[i][:], in0=xn[i][:], in1=xt[i][:], op=ALU.add), reads=[t_xn[i], t_xt[i]], writes=[t_xn[i]])
            S.dma("sp", I("dma_start", out=G.xres[tok0:tok0 + 128, :], in_=xn[i][:]), reads=[t_xn[i]], writes=[G.t["xres"]], merge=True)
            S.op("act", I("activation", out=junk[:], in_=xn[i][:], func=AF.Square, accum_out=ss[i][:, 0:1]), reads=[t_xn[i]], writes=[t_junk, t_ss[i]])
            rstd_from_ss(G, ss[i][:, 0:1], ss[i][:, 0:1], D, [t_ss[i]], [t_ss[i]])
            S.op("dve", I("scalar_tensor_tensor", out=fx[i][:], in0=xn[i][:], scalar=ss[i][:, 0:1], in1=gs2b[:], op0=ALU.mult, op1=ALU.mult),
                 reads=[t_xn[i], t_ss[i], t_bc], writes=[t_fx[i]])
            S.op("pool", I("tensor_tensor", out=fx[i][:], in0=fx[i][:], in1=sh2b[:], op=ALU.add), reads=[t_fx[i], t_bc], writes=[t_fx[i]])
            S.op("act", I("copy", out=fxb[i][:], in_=fx[i][:]), reads=[t_fx[i]], writes=[t_fxb[i]])
            S.dma("sp", I("dma_start", out=G.fxd[tok0:tok0 + 128, :], in_=fxb[i][:]), reads=[t_fxb[i]], writes=[G.t["fxd"]], merge=True)

        def part2(k, ti):
            i = k % NB
            for q in range(4):
                ti_ = kt[0] % 2
                kt[0] += 1
                for jj in range(4):
                    j = q * 4 + jj
                    S.op("pe", I("transpose", out=trp[ti_][:, jj * 128:(jj + 1) * 128], in_=fx[i][:, j * 128:(j + 1) * 128], identity=G.ident_f[:]),
                         reads=[t_fx[i], G.t_const], writes=[t_trp[ti_]], merge=(jj > 0))
                if q % 2 == 0:
                    S.op("act", I("copy", out=fxT[i][:, q * 4:(q + 1) * 4, :], in_=trp[ti_][:].rearrange("p (a n) -> p a n", n=128)),
                         reads=[t_trp[ti_]], writes=[t_fxT[i]], merge=(q > 0))
                else:
                    S.op("dve", I("tensor_copy", out=fxT[i][:, q * 4:(q + 1) * 4, :], in_=trp[ti_][:].rearrange("p (a n) -> p a n", n=128)),
                         reads=[t_trp[ti_]], writes=[t_fxT[i]], merge=True)
            for j in range(16):
                S.op("pe", I("matmul", lgp[i][:, 0:NE], lhsT=fxT[i][:, j, :], rhs=wr[:, j, :], start=(j == 0), stop=(j == 15)),
                     reads=[t_fxT[i], t_w], writes=[t_lgp[i]], merge=(j > 0))
            S.op("dve", I("reduce_max", out=sm[i][:, 0:1], in_=lgp[i][:, 0:NE], axis=mybir.AxisListType.X), reads=[t_lgp[i]], writes=[t_sm[i]])
            S.op("dve", I("tensor_scalar", out=sm[i][:, 1:2], in0=sm[i][:, 0:1], scalar1=-1.0, scalar2=None, op0=ALU.mult), reads=[t_sm[i]], writes=[t_sm[i]])
            S.op("act", I("activation", out=ex[i][:], in_=lgp[i][:, 0:NE], func=AF.Exp, bias=sm[i][:, 1:2], accum_out=sm[i][:, 2:3]),
                 reads=[t_lgp[i], t_sm[i]], writes=[t_ex[i], t_sm[i]])
            S.op("dve", I("reciprocal", out=sm[i][:, 3:4], in_=sm[i][:, 2:3]), reads=[t_sm[i]], writes=[t_sm[i]])
            S.op("dve", I("tensor_scalar", out=G.aff_all[:, ti, :], in0=ex[i][:], scalar1=sm[i][:, 3:4], scalar2=None, op0=ALU.mult),
                 reads=[t_ex[i], t_sm[i]], writes=[G.t_aff], merge=True)

        for k in range(len(tiles) + 1):
            if k < len(tiles):
                part1(k, tiles[k])
            if k >= 1:
                part2(k - 1, tiles[k - 1])
        if "affd" in G.debug:
            S.dma("sp", I("dma_start", out=G.affd.rearrange("(t p) e -> p t e", p=128), in_=G.aff_all[:]), reads=[G.t_aff], writes=[G.t["affd"]])
        S.flush(f"outproj{l}")


def phase_route(G, l, do_ctx):
    nc, S = G.nc, G.S
    NI = 24
    with ExitStack() as ps:
        sb, pp = _alloc(G, ps)
        U = sb("rt_U", [128, 128], BF16)
        iot = sb("rt_iota", [128, 512], F32)
        pidx = sb("rt_pidx", [128, 4], F32)
        tgrid = sb("rt_tgrid", [128, 34, NE], F32)
        t_c = Tok()
        S.dma("sp", I("dma_start", out=U[:], in_=G.triu), writes=[t_c], merge=True)
        S.dma("sp", I("dma_start", out=iot[:], in_=G.iota512), writes=[t_c], merge=True)
        S.dma("sp", I("dma_start", out=pidx[:], in_=G.pidx), writes=[t_c], merge=True)
        S.dma("sp", I("dma_start", out=tgrid[:].rearrange("p t e -> p (t e)"), in_=G.tgrid), writes=[t_c], merge=True)
        aff = G.aff_all
        sets = [(0, 32, CAP_L)] + ([(32, 34, CAP_C)] if do_ctx else [])
        R = sb("rt_R", [128, 34, NE, 6], BF16)
        t_R = Tok()
        r1 = sb("rt_r1", [128, 34, NE], F32)
        t_r1 = Tok()
        S.op("pool", I("memset", R[:], 1.0), writes=[t_R])
        S.op("dve", I("tensor_scalar", out=R[:, :, :, 0], in0=tgrid[:], scalar1=0.0, scalar2=pidx[:, 0:1], op0=ALU.mult, op1=ALU.add),
             reads=[t_c], writes=[t_R])
        S.op("dve", I("tensor_copy", out=R[:, :, :, 1], in_=tgrid[:]), reads=[t_c], writes=[t_R])
        S.op("dve", I("tensor_copy", out=R[:, :, :, 2], in_=aff[:]), reads=[G.t_aff], writes=[t_R])
        S.op("dve", I("tensor_tensor", out=r1[:], in0=aff[:], in1=R[:, :, :, 2], op=ALU.subtract), reads=[G.t_aff, t_R], writes=[t_r1])
        S.op("dve", I("tensor_copy", out=R[:, :, :, 3], in_=r1[:]), reads=[t_r1], writes=[t_R])
        S.op("dve", I("tensor_tensor", out=r1[:], in0=r1[:], in1=R[:, :, :, 3], op=ALU.subtract), reads=[t_r1, t_R], writes=[t_r1])
        S.op("dve", I("tensor_copy", out=R[:, :, :, 4], in_=r1[:]), reads=[t_r1], writes=[t_R])
        mids, t_mid, cmps, t_cmp, parts, t_part, cps, t_cps, tmps, t_tmp = [], [], [], [], [], [], [], [], [], []
        for si, (t0, t1, cap) in enumerate(sets):
            mids.append(sb(f"rt_mid{si}", [128, NE], F32)); t_mid.append(Tok())
            cmps.append(sb(f"rt_cmp{si}", [128, t1 - t0, NE], BF16)); t_cmp.append(Tok())
            parts.append(sb(f"rt_part{si}", [128, NE], F32)); t_part.append(Tok())
            cps.append(pp(f"rt_cps{si}", [128, 512])); t_cps.append(Tok())
            tmps.append(sb(f"rt_tmp{si}", [128, NE], F32)); t_tmp.append(Tok())
            S.op("pool", I("memset", mids[si][:], 0.5), writes=[t_mid[si]])
        for k in range(NI):
            wk = 0.5 ** (k + 1)
            wn = 0.5 ** (k + 2) if k < NI - 1 else 0.0
            for si, (t0, t1, cap) in enumerate(sets):
                nt = t1 - t0
                S.op("dve", I("tensor_tensor", out=cmps[si][:], in0=aff[:, t0:t1, :],
                              in1=mids[si][:].unsqueeze(1).to_broadcast([128, nt, NE]), op=ALU.is_gt),
                     reads=[G.t_aff, t_mid[si]], writes=[t_cmp[si]])
                S.op("dve", I("tensor_reduce", out=parts[si][:], in_=cmps[si][:].rearrange("p t e -> p e t"),
                              axis=mybir.AxisListType.X, op=ALU.add),
                     reads=[t_cmp[si]], writes=[t_part[si]])
                S.op("pe", I("matmul", cps[si][:, 0:NE], lhsT=G.ones_f[:], rhs=parts[si][:], start=True, stop=True),
                     reads=[t_part[si], G.t_const], writes=[t_cps[si]])
                S.op("dve", I("tensor_scalar", out=tmps[si][:], in0=cps[si][:, 0:NE], scalar1=cap + 0.5, scalar2=wk, op0=ALU.is_gt, op1=ALU.mult),
                     reads=[t_cps[si]], writes=[t_tmp[si]])
                S.op("dve", I("scalar_tensor_tensor", out=mids[si][:], in0=tmps[si][:], scalar=-wn, in1=mids[si][:], op0=ALU.add, op1=ALU.add),
                     reads=[t_tmp[si], t_mid[si]], writes=[t_mid[si]])
        Mb = sb("rt_Mb", [128, 34, NE], BF16)
        Mf = sb("rt_Mf", [128, 34, NE], F32)
        t_M = Tok()
        if not do_ctx:
            S.op("pool", I("memset", Mb[:, 32:34, :], 0.0), writes=[t_M])
            S.op("pool", I("memset", Mf[:, 32:34, :], 0.0), writes=[t_M], merge=True)
        for si, (t0, t1, cap) in enumerate(sets):
            nt = t1 - t0
            S.op("dve", I("tensor_tensor", out=Mf[:, t0:t1, :], in0=aff[:, t0:t1, :],
                          in1=mids[si][:].unsqueeze(1).to_broadcast([128, nt, NE]), op=ALU.is_gt),
                 reads=[G.t_aff, t_mid[si]], writes=[t_M], merge=True)
        S.op("dve", I("tensor_copy", out=Mb[:, 0:34 if do_ctx else 32, :], in_=Mf[:, 0:34 if do_ctx else 32, :]), reads=[t_M], writes=[t_M])
        posp = [pp(f"rt_pos{i}", [128, 512]) for i in range(2)]
        totp = [pp(f"rt_tot{i}", [128, 512]) for i in range(2)]
        t_pp = Tok()
        spans = [(0, 32)] + ([(32, 34)] if do_ctx else [])
        for i, (t0, t1) in enumerate(spans):
            n = (t1 - t0) * NE
            rhs = Mb[:, t0:t1, :].rearrange("p t e -> p (t e)")
            S.op("pe", I("matmul", posp[i][:, 0:n], lhsT=U[:], rhs=rhs, start=True, stop=True), reads=[t_M, t_c], writes=[t_pp], merge=(i > 0))
            S.op("pe", I("matmul", totp[i][:, 0:n], lhsT=G.ones_b[:], rhs=rhs, start=True, stop=True), reads=[t_M, G.t_const], writes=[t_pp], merge=True)
        incl = sb("rt_incl", [128, 34, NE], F32)
        t_incl = Tok()
        onesf = sb("rt_onesf", [128, 32], F32)
        S.op("pool", I("memset", onesf[:], 1.0), writes=[t_c], merge=True)
        for i, (t0, t1) in enumerate(spans):
            nt = t1 - t0
            tv = totp[i][:, 0:nt * NE].rearrange("p (t e) -> p t e", e=NE)
            for e in range(NE):
                S.op("dve", I("tensor_tensor_scan", out=incl[:, t0:t1, e], data0=onesf[:, 0:nt], data1=tv[:, :, e], initial=0.0,
                              op0=ALU.mult, op1=ALU.add),
                     reads=[t_pp, t_c], writes=[t_incl], merge=(e > 0 or i > 0))
        posf = sb("rt_posf", [128, 34, NE], F32)
        t_posf = Tok()
        for i, (t0, t1) in enumerate(spans):
            nt = t1 - t0
            tv = totp[i][:, 0:nt * NE].rearrange("p (t e) -> p t e", e=NE)
            pv = posp[i][:, 0:nt * NE].rearrange("p (t e) -> p t e", e=NE)
            S.op("dve", I("tensor_tensor", out=incl[:, t0:t1, :], in0=incl[:, t0:t1, :], in1=tv, op=ALU.subtract),
                 reads=[t_incl, t_pp], writes=[t_incl])
            S.op("dve", I("tensor_tensor", out=posf[:, t0:t1, :], in0=incl[:, t0:t1, :], in1=pv, op=ALU.add),
                 reads=[t_incl, t_pp], writes=[t_posf], merge=(i > 0))
        Oall = [sb(f"rt_O{i}", [128, 32, 512], BF16) for i in range(2)]
        t_O = [[Tok() for _ in range(32)] for _ in range(2)]
        Oc = [sb(f"rt_Oc{i}", [128, 2, 128], BF16) for i in range(2)]
        t_Oc = [Tok(), Tok()]
        ips = [pp(f"rt_ips{i}", [128, 512]) for i in range(2)]
        t_ips = [Tok(), Tok()]
        isb = [sb(f"rt_isb{i}", [128, 5, 8], F32) for i in range(2)]
        t_isb = [Tok(), Tok()]
        ia = [sb(f"rt_ia{i}", [128, 5], F32) for i in range(2)]
        t_ia = [Tok(), Tok()]
        nch = 5 if do_ctx else 4
        import os
        LVE = int(os.environ.get("RT_EXP", "16"))
        for e in range(LVE):
            b = e % 2
            for t in range(32):
                S.op("dve", I("tensor_scalar", out=Oall[b][:, t, :], in0=iot[:], scalar1=posf[:, t, e:e + 1], scalar2=Mf[:, t, e:e + 1],
                              op0=ALU.is_equal, op1=ALU.mult),
                     reads=[t_c, t_posf, t_M], writes=[t_O[b][t]])
            first = True
            for sc in range(4):
                for t in range(32):
                    S.op("pe", I("matmul", ips[b][:, sc * 8:sc * 8 + 6], lhsT=Oall[b][:, t, sc * 128:(sc + 1) * 128], rhs=R[:, t, e, :],
                                 start=(t == 0), stop=(t == 31)),
                         reads=[t_O[b][t], t_R], writes=[t_ips[b]], merge=not first)
                    first = False
            if do_ctx:
                for t in range(2):
                    S.op("dve", I("tensor_scalar", out=Oc[b][:, t, :], in0=iot[:, 0:128], scalar1=posf[:, 32 + t, e:e + 1],
                                  scalar2=Mf[:, 32 + t, e:e + 1], op0=ALU.is_equal, op1=ALU.mult),
                         reads=[t_c, t_posf, t_M], writes=[t_Oc[b]], merge=(t > 0))
                for t in range(2):
                    S.op("pe", I("matmul", ips[b][:, 32:38], lhsT=Oc[b][:, t, :], rhs=R[:, 32 + t, e, :], start=(t == 0), stop=(t == 1)),
                         reads=[t_Oc[b], t_R], writes=[t_ips[b]], merge=True)
            S.op("act", I("copy", out=isb[b][:, 0:nch, 0:6], in_=ips[b][:, 0:nch * 8].rearrange("p (c k) -> p c k", k=8)[:, :, 0:6]),
                 reads=[t_ips[b]], writes=[t_isb[b]])
            v = isb[b]
            S.op("dve", I("scalar_tensor_tensor", out=ia[b][:, 0:nch], in0=v[:, 0:nch, 1], scalar=128.0, in1=v[:, 0:nch, 0], op0=ALU.mult, op1=ALU.add),
                 reads=[t_isb[b]], writes=[t_ia[b]])
            S.op("dve", I("scalar_tensor_tensor", out=ia[b][:, 0:nch], in0=v[:, 0:nch, 5], scalar=pidx[:, 2:3], in1=ia[b][:, 0:nch], op0=ALU.mult, op1=ALU.add),
                 reads=[t_isb[b], t_ia[b], t_c], writes=[t_ia[b]])
            S.op("dve", I("tensor_scalar", out=ia[b][:, 0:nch], in0=ia[b][:, 0:nch], scalar1=pidx[:, 1:2], scalar2=None, op0=ALU.add),
                 reads=[t_ia[b], t_c], writes=[t_ia[b]])
            S.op("dve", I("tensor_copy", out=G.idx_all[:, e, 0:nch], in_=ia[b][:, 0:nch]), reads=[t_ia[b]], writes=[G.t_idx], merge=(e > 0))
            S.op("pool", I("tensor_tensor", out=G.g_all[:, e, 0:nch], in0=v[:, 0:nch, 2], in1=v[:, 0:nch, 3], op=ALU.add),
                 reads=[t_isb[b]], writes=[G.t_idx], merge=True)
            S.op("pool", I("tensor_tensor", out=G.g_all[:, e, 0:nch], in0=G.g_all[:, e, 0:nch], in1=v[:, 0:nch, 4], op=ALU.add),
                 reads=[t_isb[b], G.t_idx], writes=[G.t_idx], merge=True)
        if "idxd" in G.debug:
            S.dma("sp", I("dma_start", out=G.idxd, in_=G.idx_all[:].rearrange("p e c -> p (e c)")), reads=[G.t_idx], writes=[G.t["idxd"]])
            S.dma("sp", I("dma_start", out=G.gd, in_=G.g_all[:].rearrange("p e c -> p (e c)")), reads=[G.t_idx], writes=[G.t["gd"]])
        S.flush(f"route{l}")


def phase_moe(G, l, do_ctx):
    nc, S = G.nc, G.S
    with ExitStack() as ps:
        sb, pp = _alloc(G, ps)
        nch = 5 if do_ctx else 4
        NS = 544 if do_ctx else 512
        FB = 256
        ns = 2 if do_ctx else 1
        g2b = []
        t_bc = Tok()
        for s_ in range(ns):
            a = sb(f"mo_g2b{s_}", [128, D], F32)
            S.dma("sp", I("dma_start", out=a[:], in_=G.modrow[l, s_:s_ + 1, 5 * D:6 * D].to_broadcast([128, D])),
                  reads=[G.t["modrow"]], writes=[t_bc], merge=True)
            g2b.append(a)
        xs = [sb(f"mo_xs{i}", [128, D], BF16) for i in range(2)]
        t_xs = [Tok(), Tok()]
        xsT = sb("mo_xsT", [128, 16, NS], BF16)
        t_xsT = Tok()
        wg = [sb(f"mo_wg{i}", [128, 16, FB], BF16) for i in range(2)]
        wu = [sb(f"mo_wu{i}", [128, 16, FB], BF16) for i in range(2)]
        t_wgu = [Tok(), Tok()]
        wd = [sb(f"mo_wd{i}", [128, 16, 512], BF16) for i in range(2)]
        t_wd = [Tok(), Tok()]
        wdf = sb("mo_wdf", [128, 16, 512], F32)
        t_wdf = Tok()
        hm = sb("mo_hm", [128, 16, NS], BF16)
        t_hm = [Tok() for _ in range(16)]
        st = [sb(f"mo_st{i}", [128, 544], F32) for i in range(2)]
        t_st = [Tok(), Tok()]
        yst = [sb(f"mo_y{i}", [128, D], F32) for i in range(nch)]
        t_yst = [Tok() for _ in range(nch)]
        if do_ctx:
            S.op("pool", I("memset", yst[4][:], 0.0), writes=[t_yst[4]])
        aps = [pp(f"mo_a{i}", [128, 512]) for i in range(2)]
        ups = [pp(f"mo_u{i}", [128, 512]) for i in range(2)]
        t_aps = [Tok(), Tok()]
        t_ups = [Tok(), Tok()]
        cps = pp("mo_c", [128, 512])
        t_cps = Tok()
        yps = [pp(f"mo_yp{i}", [128, 512]) for i in range(2)]
        t_yps = [Tok(), Tok()]
        trp = pp("mo_tr", [128, 1024], BF16)
        t_trp = Tok()
        import os
        LVE = int(os.environ.get("MOE_EXP", "16"))
        jobs = []
        for e in range(LVE):
            for fb in range(D // FB):
                jobs.append(("gu", e, fb))
            for db in range(4):
                jobs.append(("d", e, db))
        cnt = {"gu": 0, "d": 0}
        slot_of = {}

        def issue(jb):
            kind, e, b = jb
            i = cnt[kind] % 2
            cnt[kind] += 1
            slot_of[jb] = i
            if kind == "gu":
                S.dma("pool", I("dma_start", out=wg[i][:], in_=G.w_gate[l, e][:, b * FB:(b + 1) * FB].rearrange("(j p) n -> p j n", p=128)),
                      writes=[t_wgu[i]])
                S.dma("pool", I("dma_start", out=wu[i][:], in_=G.w_up[l, e][:, b * FB:(b + 1) * FB].rearrange("(j p) n -> p j n", p=128)),
                      writes=[t_wgu[i]], merge=True)
            else:
                for hh in range(2):
                    S.dma("sp", I("dma_start", out=wdf[:, hh * 8:(hh + 1) * 8, :],
                                  in_=G.w_down[l, e][hh * 1024:(hh + 1) * 1024, b * 512:(b + 1) * 512].rearrange("(j p) n -> p j n", p=128)),
                          writes=[t_wdf], merge=(hh > 0))
                S.op("act", I("copy", out=wd[i][:], in_=wdf[:]), reads=[t_wdf], writes=[t_wd[i]])

        def gather(e):
            for sc in range(nch):
                i = sc % 2
                m = 128 if sc < 4 else 32
                S.dma("pool", I("indirect_dma_start", out=xs[i][:], out_offset=None, in_=G.fxd[:, :],
                                in_offset=bass.IndirectOffsetOnAxis(ap=G.idx_all[:, e, sc:sc + 1], axis=0)),
                      reads=[G.t_idx, G.t["fxd"]], writes=[t_xs[i]])
                for q in range(2):
                    for jj in range(8):
                        j = q * 8 + jj
                        S.op("pe", I("transpose", out=trp[:, jj * 128:jj * 128 + m], in_=xs[i][0:m, j * 128:(j + 1) * 128],
                                     identity=G.ident_b[0:m, 0:m]),
                             reads=[t_xs[i], G.t_const], writes=[t_trp], merge=(jj > 0))
                    src = trp[:].rearrange("p (a n) -> p a n", n=128)[:, :, 0:m]
                    dst = xsT[:, q * 8:(q + 1) * 8, sc * 128:sc * 128 + m]
                    if q == 0:
                        S.op("act", I("copy", out=dst, in_=src), reads=[t_trp], writes=[t_xsT], merge=not (sc == 0))
                    else:
                        S.op("dve", I("tensor_copy", out=dst, in_=src), reads=[t_trp], writes=[t_xsT], merge=True)

        ji = 0
        issue(jobs[0])
        if LVE > 0:
            gather(0)
        kk = 0
        for e in range(LVE):
            for fb in range(D // FB):
                jb = jobs[ji]
                ji += 1
                if ji < len(jobs):
                    issue(jobs[ji])
                wi = slot_of[jb]
                for fl in range(FB // 128):
                    fo = fb * (FB // 128) + fl
                    ab = kk % 2
                    kk += 1
                    for j in range(16):
                        S.op("pe", I("matmul", aps[ab][:], lhsT=wg[wi][:, j, fl * 128:(fl + 1) * 128], rhs=xsT[:, j, 0:512],
                                     start=(j == 0), stop=(j == 15)),
                             reads=[t_wgu[wi], t_xsT], writes=[t_aps[ab]], merge=(j > 0))
                    for j in range(16):
                        S.op("pe", I("matmul", ups[ab][:], lhsT=wu[wi][:, j, fl * 128:(fl + 1) * 128], rhs=xsT[:, j, 0:512],
                                     start=(j == 0), stop=(j == 15)),
                             reads=[t_wgu[wi], t_xsT], writes=[t_ups[ab]], merge=(j > 0))
                    if do_ctx:
                        for j in range(16):
                            S.op("pe", I("matmul", cps[:, 0:32], lhsT=wg[wi][:, j, fl * 128:(fl + 1) * 128], rhs=xsT[:, j, 512:544],
                                         start=(j == 0), stop=(j == 15)),
                                 reads=[t_wgu[wi], t_xsT], writes=[t_cps], merge=(j > 0))
                        for j in range(16):
                            S.op("pe", I("matmul", cps[:, 32:64], lhsT=wu[wi][:, j, fl * 128:(fl + 1) * 128], rhs=xsT[:, j, 512:544],
                                         start=(j == 0), stop=(j == 15)),
                                 reads=[t_wgu[wi], t_xsT], writes=[t_cps], merge=True)
                    S.op("act", I("activation", out=st[ab][:, 0:512], in_=aps[ab][:], func=AF.Silu), reads=[t_aps[ab]], writes=[t_st[ab]])
                    S.op("dve", I("tensor_tensor", out=hm[:, fo, 0:512], in0=ups[ab][:], in1=st[ab][:, 0:512], op=ALU.mult),
                         reads=[t_ups[ab], t_st[ab]], writes=[t_hm[fo]])
                    if do_ctx:
                        S.op("act", I("activation", out=st[ab][:, 512:544], in_=cps[:, 0:32], func=AF.Silu), reads=[t_cps], writes=[t_st[ab]], merge=True)
                        S.op("dve", I("tensor_tensor", out=hm[:, fo, 512:544], in0=cps[:, 32:64], in1=st[ab][:, 512:544], op=ALU.mult),
                             reads=[t_cps, t_st[ab]], writes=[t_hm[fo]], merge=True)
            if e + 1 < LVE:
                gather(e + 1)
            for db in range(4):
                jb = jobs[ji]
                ji += 1
                if ji < len(jobs):
                    issue(jobs[ji])
                wi = slot_of[jb]
                for sc in range(nch):
                    m = 128 if sc < 4 else 32
                    s_ = 0 if sc < 4 else 1
                    yi = kk % 2
                    kk += 1
                    for fo in range(16):
                        S.op("pe", I("matmul", yps[yi][0:m, :], lhsT=hm[:, fo, sc * 128:sc * 128 + m], rhs=wd[wi][:, fo, :],
                                     start=(fo == 0), stop=(fo == 15)),
                             reads=[t_hm[fo], t_wd[wi]], writes=[t_yps[yi]], merge=(fo > 0))
                    S.op("dve", I("scalar_tensor_tensor", out=yst[sc][0:m, db * 512:(db + 1) * 512], in0=yps[yi][0:m, :],
                                  scalar=G.g_all[0:m, e, sc:sc + 1], in1=g2b[s_][0:m, db * 512:(db + 1) * 512], op0=ALU.mult, op1=ALU.mult),
                         reads=[t_yps[yi], G.t_idx, t_bc], writes=[t_yst[sc]], merge=(db > 0))
            for sc in range(nch):
                S.dma("pool", I("indirect_dma_start", out=G.xres[:, :],
                                out_offset=bass.IndirectOffsetOnAxis(ap=G.idx_all[:, e, sc:sc + 1], axis=0),
                                in_=yst[sc][:], in_offset=None, bounds_check=NPAD - 1, oob_is_err=True, compute_op=ALU.add),
                      reads=[t_yst[sc], G.t_idx, G.t["xres"]], writes=[G.t["xres"]])
        S.flush(f"moe{l}")


def phase_final(G):
    nc, S = G.nc, G.S
    with ExitStack() as ps:
        sb, pp = _alloc(G, ps)
        fg = sb("fi_g", [128, D], F32)
        t_fg = Tok()
        S.dma("sp", I("dma_start", out=fg[:], in_=G.final_g.rearrange("(o d) -> o d", o=1).to_broadcast([128, D])), writes=[t_fg])
        xt = [sb(f"fi_x{i}", [128, D], F32) for i in range(2)]
        t_xt = [Tok(), Tok()]
        ot = [sb(f"fi_o{i}", [128, D], F32) for i in range(2)]
        t_ot = [Tok(), Tok()]
        junk = sb("fi_junk", [128, D], BF16)
        t_junk = Tok()
        ss = [sb(f"fi_ss{i}", [128, 1], F32) for i in range(2)]
        t_ss = [Tok(), Tok()]
        for ti in range(32):
            i = ti % 2
            S.dma("sp", I("dma_start", out=xt[i][:], in_=G.xres[ti * 128:(ti + 1) * 128, :]), reads=[G.t["xres"]], writes=[t_xt[i]])
            S.op("act", I("activation", out=junk[:], in_=xt[i][:], func=AF.Square, accum_out=ss[i][:, 0:1]), reads=[t_xt[i]], writes=[t_junk, t_ss[i]])
            rstd_from_ss(G, ss[i][:, 0:1], ss[i][:, 0:1], D, [t_ss[i]], [t_ss[i]])
            S.op("dve", I("scalar_tensor_tensor", out=ot[i][:], in0=xt[i][:], scalar=ss[i][:, 0:1], in1=fg[:], op0=ALU.mult, op1=ALU.mult),
                 reads=[t_xt[i], t_ss[i], t_fg], writes=[t_ot[i]])
            S.dma("sp", I("dma_start", out=G.out[ti * 128:(ti + 1) * 128, :], in_=ot[i][:]), reads=[t_ot[i]], writes=[G.t["out"]], merge=True)
        S.flush("final")


_CONSTS = None


def make_in_maps(inputs, batches, names=None):
    global _CONSTS
    if _CONSTS is None:
        _CONSTS = host_consts()
    w = host_weights(inputs)
    maps = []
    for b in batches:
        m = dict(w)
        m.update(_CONSTS)
        xin = np.zeros((NPAD, D), np.float32)
        xin[:NL] = inputs["x"][b]
        xin[NL:NT] = inputs["ctx"][b]
        m["xin"] = xin
        m["cvec"] = np.ascontiguousarray(np.stack([inputs["c"][b], inputs["c_ctx"]], axis=1)).astype(np.float32)
        if names is not None:
            m = {k: m[k] for k in names}
        maps.append(m)
    return maps


_PROG = None


def kernel(**inputs):
    global _PROG
    inputs = {k: np.asarray(v) for k, v in inputs.items()}
    if _PROG is None:
        _PROG = build_program()
    nc, G = _PROG
    B = inputs["x"].shape[0]
    maps = make_in_maps(inputs, list(range(B)), G.in_names)
    res = run_bass_kernel_spmd(nc, maps, core_ids=list(range(B)))
    out = np.stack([np.asarray(r["out"]) for r in res.results], axis=0)
    return out.astype(np.float32)
```

```python
import numpy as np
import ml_dtypes
from contextlib import ExitStack
import concourse.bass as bass
import concourse.mybir as mybir
from concourse.bass_utils import run_bass_kernel_spmd

F32 = mybir.dt.float32
BF16 = mybir.dt.bfloat16
I32 = mybir.dt.int32
AF = mybir.ActivationFunctionType
ALU = mybir.AluOpType
NPBF = ml_dtypes.bfloat16

D = 2048
NL = 4096
NCX = 256
NT = NL + NCX
NPAD = NT + 128
DEPTH = 2
EPS = 1e-6
NE = 16
CAP_L = 512
CAP_C = 32

WA_COLS = 3456
OFF_CB, OFF_CC, OFF_CH = 0, 512, 1024
OFF_SQ, OFF_SQW = 1536, 2048
OFF_SK, OFF_SKW = 2560, 2688
OFF_SV = 2816
OFF_FU = 2944
WB_COLS = 960
OFF_CQ, OFF_CKV, OFF_KPA, OFF_KPB = 0, 512, 768, 864

SEM_ROLL = 30000
NDMA_SEMS = 24


def I(name, *args, **kwargs):
    return (name, args, kwargs)


class Tok:
    __slots__ = ("w", "r", "base")

    def __init__(self):
        self.w = {}
        self.r = {}
        self.base = {}


class Sched:
    ENGS = ("pe", "act", "dve", "pool", "sp")

    def __init__(self, nc, stack):
        self.nc = nc
        self.stack = stack
        self.sems = []
        self.prog = {e: [] for e in self.ENGS}
        self.cur_sem = {}
        self.cnt = {}
        for e in ("pe", "act", "dve", "pool"):
            self.cur_sem[e] = self._new_sem(e)
            self.cnt[e] = 0
        self.dma_sems = {q: [self._new_sem("dma" + q) for _ in range(NDMA_SEMS)] for q in ("sp", "pool", "act")}
        self.dma_cnt = {q: [0] * NDMA_SEMS for q in ("sp", "pool", "act")}
        self.dma_rr = {"sp": 0, "pool": 0, "act": 0}
        self.waited = {e: {} for e in self.ENGS}
        self.n_inst = 0
        self.regcache = {}
        self.marks = []
        self.tot_pe = 0

    def _new_sem(self, name):
        h = self.stack.enter_context(self.nc.semaphore(f"s_{name}_{len(self.sems)}"))
        self.sems.append(h)
        return len(self.sems) - 1

    def _collect(self, eng, reads, writes, merge):
        need = {}
        for t in reads:
            for s, v in t.w.items():
                if need.get(s, 0) < v:
                    need[s] = v
        for t in writes:
            src = (t.base, t.r) if merge else (t.w, t.r)
            for dct in src:
                for s, v in dct.items():
                    if need.get(s, 0) < v:
                        need[s] = v
        out = []
        wd = self.waited[eng]
        for s, v in need.items():
            if eng == "pe" and s == self.cur_sem["pe"]:
                continue
            if wd.get(s, 0) >= v:
                continue
            wd[s] = v
            out.append((s, v))
        return out

    def _post(self, ev, reads, writes, merge):
        s, v = ev
        for t in reads:
            if t.r.get(s, 0) < v:
                t.r[s] = v
        for t in writes:
            if merge:
                if t.r:
                    nb = dict(t.base)
                    for rs, rv in t.r.items():
                        if nb.get(rs, 0) < rv:
                            nb[rs] = rv
                    t.base = nb
                if t.w.get(s, 0) < v:
                    t.w[s] = v
            else:
                nb = dict(t.w)
                for rs, rv in t.r.items():
                    if nb.get(rs, 0) < rv:
                        nb[rs] = rv
                t.base = nb
                t.w = {s: v}
            t.r = {}

    def op(self, eng, fn, reads=(), writes=(), merge=False):
        waits = self._collect(eng, reads, writes, merge)
        if self.cnt[eng] >= SEM_ROLL:
            self.cur_sem[eng] = self._new_sem(eng)
            self.cnt[eng] = 0
        self.cnt[eng] += 1
        if eng == "pe":
            self.tot_pe += 1
        s = self.cur_sem[eng]
        v = self.cnt[eng]
        sems = self.sems

        def emit(h, waits=waits, s=s, fn=fn):
            for ws, wv in waits:
                h.wait_ge(sems[ws], wv)
            getattr(h, fn[0])(*fn[1], **fn[2]).then_inc(sems[s], 1)

        self.prog[eng].append(emit)
        self._post((s, v), reads, writes, merge)
        self.n_inst += 1

    def dma(self, eng, fn, reads=(), writes=(), merge=False):
        i = self.dma_rr[eng]
        self.dma_rr[eng] = (i + 1) % NDMA_SEMS
        s = self.dma_sems[eng][i]
        waits = self._collect(eng, reads, writes, merge)
        prev = self.dma_cnt[eng][i]
        if prev > 0 and self.waited[eng].get(s, 0) < prev:
            self.waited[eng][s] = prev
            waits.append((s, prev))
        self.dma_cnt[eng][i] += 16
        v = self.dma_cnt[eng][i]
        sems = self.sems

        def emit(h, waits=waits, s=s, fn=fn):
            for ws, wv in waits:
                h.wait_ge(sems[ws], wv)
            try:
                kw = fn[2]
                if isinstance(kw.get("bounds_check"), int):
                    kw = dict(kw)
                    key = (id(h), kw["bounds_check"])
                    if key not in self.regcache:
                        self.regcache[key] = h.to_reg(kw["bounds_check"])
                    kw["bounds_check"] = self.regcache[key]
                ins = getattr(h, fn[0])(*fn[1], **kw)
            except Exception:
                print("DMA FAIL", fn[0], {k: (getattr(v, "shape", v), getattr(v, "ap", None)) for k, v in fn[2].items()})
                raise
            ins.then_inc(sems[s], 16)

        self.prog[eng].append(emit)
        self._post((s, v), reads, writes, merge)
        self.n_inst += 1

    def wait_all(self, eng, toks):
        waits = self._collect(eng, toks, (), False)
        sems = self.sems

        def emit(h, waits=waits):
            for ws, wv in waits:
                h.wait_ge(sems[ws], wv)

        self.prog[eng].append(emit)

    def barrier(self):
        cur = {}
        for e in ("pe", "act", "dve", "pool"):
            if self.cnt[e] > 0:
                cur[self.cur_sem[e]] = self.cnt[e]
        for q in self.dma_sems:
            for i, s in enumerate(self.dma_sems[q]):
                if self.dma_cnt[q][i] > 0:
                    cur[s] = self.dma_cnt[q][i]
        sems = self.sems
        for eng in self.ENGS:
            wd = self.waited[eng]
            waits = []
            for s, v in cur.items():
                if wd.get(s, 0) < v:
                    wd[s] = v
                    waits.append((s, v))

            def emit(h, waits=waits):
                for ws, wv in waits:
                    h.wait_ge(sems[ws], wv)

            self.prog[eng].append(emit)

    def flush(self, name=None):
        self.barrier()
        self.regcache = {}
        self.marks.append((name, self.tot_pe))
        nc = self.nc
        prog = self.prog
        with nc.Block(name) as block:
            if prog["sp"]:
                @block.sync
                def _(h):
                    for f in prog["sp"]:
                        f(h)
            if prog["pe"]:
                @block.tensor
                def _(h):
                    for f in prog["pe"]:
                        f(h)
            if prog["act"]:
                @block.scalar
                def _(h):
                    for f in prog["act"]:
                        f(h)
            if prog["dve"]:
                @block.vector
                def _(h):
                    for f in prog["dve"]:
                        f(h)
            if prog["pool"]:
                @block.gpsimd
                def _(h):
                    for f in prog["pool"]:
                        f(h)
        self.prog = {e: [] for e in self.ENGS}


def _rope_tables(rot_dim):
    rows = np.repeat(np.arange(64, dtype=np.float32), 64)
    cols = np.tile(np.arange(64, dtype=np.float32), 64)
    n_freq = rot_dim // 4
    inv = (np.float32(10000.0) ** (-np.arange(n_freq, dtype=np.float32) / np.float32(n_freq))).astype(np.float32)
    ang = np.concatenate([rows[:, None] * inv, cols[:, None] * inv], axis=-1).astype(np.float32)
    return np.cos(ang).astype(np.float32), np.sin(ang).astype(np.float32)


def host_consts():
    c = {}
    cs, sn = _rope_tables(64)
    cos2 = np.ones((128, NT), np.float32)
    sin2 = np.zeros((128, NT), np.float32)
    for hh in range(2):
        cos2[hh * 64:hh * 64 + 32, :NL] = cs.T
        cos2[hh * 64 + 32:hh * 64 + 64, :NL] = cs.T
        sin2[hh * 64:hh * 64 + 32, :NL] = -sn.T
        sin2[hh * 64 + 32:hh * 64 + 64, :NL] = sn.T
    c["ropeS"] = np.stack([cos2, sin2], 0)
    cm, sm = _rope_tables(32)
    cosm = np.ones((96, NT), np.float32)
    sinm = np.zeros((96, NT), np.float32)
    cosm[64:80, :NL] = cm.T
    cosm[80:96, :NL] = cm.T
    sinm[64:80, :NL] = -sm.T
    sinm[80:96, :NL] = sm.T
    c["ropeM"] = np.stack([cosm, sinm], 0)
    k = np.arange(64)
    a = 2 * np.pi * np.outer(k, k) / 64.0
    c64 = np.zeros((2, 128, 128), np.float64)
    for g in range(2):
        c64[0, g * 64:(g + 1) * 64, g * 64:(g + 1) * 64] = np.cos(a) / 8.0
        c64[1, g * 64:(g + 1) * 64, g * 64:(g + 1) * 64] = np.sin(a) / 8.0
    c["dft64"] = c64.astype(NPBF)
    def pos_tables(n):
        nt = n // 128
        idx = np.arange(n, dtype=np.int64)
        m = (np.outer(idx, idx) % n).astype(np.float64)
        ang = 2 * np.pi * m / n
        cn = np.cos(ang) / np.sqrt(n)
        sn_ = -np.sin(ang) / np.sqrt(n)
        out = np.zeros((nt, 128, 2, nt, 128), np.float32)
        for kt in range(nt):
            blkc = cn[:, kt * 128:(kt + 1) * 128].reshape(nt, 128, 128)
            blks = sn_[:, kt * 128:(kt + 1) * 128].reshape(nt, 128, 128)
            out[kt, :, 0] = blkc.transpose(1, 0, 2)
            out[kt, :, 1] = blks.transpose(1, 0, 2)
        return out.reshape(nt, 128, 2 * nt * 128).astype(NPBF)
    c["dftL"] = pos_tables(NL)
    c["dftC"] = pos_tables(NCX)
    ident = np.eye(128, dtype=np.float32)
    c["identf"] = ident
    kk = np.arange(128)[:, None]
    qq = np.arange(128)[None, :]
    mprev = (kk >= qq).astype(np.float32)
    mnext = (kk <= qq).astype(np.float32)
    c["masks"] = np.stack([np.tile(mprev, (1, 4)), np.tile(mnext, (1, 4))], 0).astype(NPBF)
    c["triu"] = (kk < qq).astype(NPBF)
    c["iota512"] = np.tile(np.arange(512, dtype=np.float32)[None, :], (128, 1))
    pp_ = np.arange(128, dtype=np.float32)
    c["pidx"] = np.stack([pp_, NT + pp_, -(NT + pp_), np.zeros(128, np.float32)], 1).astype(np.float32)
    c["tgrid"] = np.tile(np.repeat(np.arange(34, dtype=np.float32), NE)[None, :], (128, 1))
    return c


def host_weights(inp):
    w = {}
    w_in = inp["w_in"]
    L = w_in.shape[0]
    wa = np.zeros((L, D, WA_COLS), np.float32)
    wa[:, :, 0:1536] = w_in[:, :, 0:1536]
    sq = w_in[:, :, 1536:2048]
    wa[:, :, OFF_SQ:OFF_SQ + 512] = sq
    sq4 = sq.reshape(L, D, 8, 2, 32)
    wa[:, :, OFF_SQW:OFF_SQW + 512] = sq4[:, :, :, ::-1, :].reshape(L, D, 512)
    sk = w_in[:, :, 2048:2176]
    wa[:, :, OFF_SK:OFF_SK + 128] = sk
    wa[:, :, OFF_SKW:OFF_SKW + 128] = sk.reshape(L, D, 2, 2, 32)[:, :, :, ::-1, :].reshape(L, D, 128)
    wa[:, :, OFF_SV:OFF_SV + 128] = w_in[:, :, 2176:2304]
    wa[:, :, OFF_FU:OFF_FU + 512] = w_in[:, :, 2304:2816]
    w["w_a"] = wa
    wb = np.zeros((L, D, WB_COLS), np.float32)
    wb[:, :, 0:768] = w_in[:, :, 2816:3584]
    kpe = w_in[:, :, 3584:3616]
    wb[:, :, OFF_KPA + 64:OFF_KPA + 96] = kpe
    wb[:, :, OFF_KPB + 64:OFF_KPB + 96] = kpe.reshape(L, D, 2, 16)[:, :, ::-1, :].reshape(L, D, 32)
    w["w_b"] = wb
    uq = inp["mla_w_uq"]
    uq4 = uq.reshape(L, 512, 8, 96)
    uqb = uq4.copy()
    uqb[:, :, :, 64:80] = uq4[:, :, :, 80:96]
    uqb[:, :, :, 80:96] = uq4[:, :, :, 64:80]
    w["w_uq"] = np.stack([uq, uqb.reshape(L, 512, 768)], 1)
    ukv = inp["mla_w_ukv"].reshape(L, 256, 8, 2, 64)
    w["w_uk"] = np.ascontiguousarray(ukv[:, :, :, 0, :]).reshape(L, 256, 512)
    w["w_uv"] = np.ascontiguousarray(ukv[:, :, :, 1, :]).reshape(L, 256, 512)
    for k_ in ("ada_b", "norm1_g", "norm2_g", "conv_w", "swa_sink", "mla_q_norm_g",
               "mla_kv_norm_g", "out_norm_g", "w_out", "w_router", "final_norm_g"):
        w[k_] = inp[k_]
    def tile_cols(a, W):
        lead = a.shape[:-2]
        K, N = a.shape[-2:]
        a6 = a.reshape(lead + (K // 128, 128, N // W, W))
        nd = len(lead)
        perm = tuple(range(nd)) + (nd + 2, nd + 1, nd + 0, nd + 3)
        return np.ascontiguousarray(a6.transpose(perm)).reshape(lead + (N // W, 128, (K // 128) * W))
    w["ada_w"] = tile_cols(inp["ada_w"], 512)
    if "w_gate" in inp:
        w["w_gate"] = tile_cols(inp["w_gate"], 256)
        w["w_up"] = tile_cols(inp["w_up"], 256)
        w["w_down"] = tile_cols(inp["w_down"], 512)
    return w


class Ctx:
    pass


def build_program(debug=(), n_layers=DEPTH, stop_after=None):
    nc = bass.Bass("TRN2", target_bir_lowering=False)
    G = Ctx()
    G.nc = nc
    G.debug = set(debug)
    din = {}

    def inp(name, shape, dt=F32):
        din[name] = nc.dram_tensor(name, list(shape), dt, kind="ExternalInput").ap()
        return din[name]

    G.xin = inp("xin", [NPAD, D])
    G.cvec = inp("cvec", [D, 2])
    G.ada_w = inp("ada_w", [DEPTH, 24, 128, 16 * 512])
    G.ada_b = inp("ada_b", [DEPTH, 6 * D])
    G.norm1_g = inp("norm1_g", [DEPTH, D])
    G.norm2_g = inp("norm2_g", [DEPTH, D])
    G.w_a = inp("w_a", [DEPTH, D, WA_COLS])
    G.w_b = inp("w_b", [DEPTH, D, WB_COLS])
    G.conv_w = inp("conv_w", [DEPTH, 3, 512])
    G.swa_sink = inp("swa_sink", [DEPTH, 8])
    G.q_norm_g = inp("mla_q_norm_g", [DEPTH, 512])
    G.kv_norm_g = inp("mla_kv_norm_g", [DEPTH, 256])
    G.w_uq = inp("w_uq", [DEPTH, 2, 512, 768])
    G.w_uk = inp("w_uk", [DEPTH, 256, 512])
    G.w_uv = inp("w_uv", [DEPTH, 256, 512])
    G.out_norm_g = inp("out_norm_g", [DEPTH, D])
    G.w_out = inp("w_out", [DEPTH, D, D])
    G.w_router = inp("w_router", [DEPTH, D, NE])
    if stop_after is None or stop_after in ("moe", "moetest"):
        G.w_gate = inp("w_gate", [DEPTH, NE, 8, 128, 16 * 256])
        G.w_up = inp("w_up", [DEPTH, NE, 8, 128, 16 * 256])
        G.w_down = inp("w_down", [DEPTH, NE, 4, 128, 16 * 512])
    G.final_g = inp("final_norm_g", [D])
    G.ropeS = inp("ropeS", [2, 128, NT])
    G.ropeM = inp("ropeM", [2, 96, NT])
    G.dft64 = inp("dft64", [2, 128, 128], BF16)
    G.dftL = inp("dftL", [32, 128, 2 * 32 * 128], BF16)
    G.dftC = inp("dftC", [2, 128, 2 * 2 * 128], BF16)
    G.identf = inp("identf", [128, 128])
    G.masks = inp("masks", [2, 128, 512], BF16)
    G.triu = inp("triu", [128, 128], BF16)
    G.iota512 = inp("iota512", [128, 512])
    G.pidx = inp("pidx", [128, 4])
    G.tgrid = inp("tgrid", [128, 34 * NE])

    G.out = nc.dram_tensor("out", [NL, D], F32, kind="ExternalOutput").ap()

    def scratch(name, shape, dt):
        kind = "ExternalOutput" if name in G.debug else "Internal"
        return nc.dram_tensor(name, list(shape), dt, kind=kind).ap()

    G.xres = scratch("xres", [NPAD, D], F32)
    G.hT = scratch("hT", [D, NT], BF16)
    G.cbT = scratch("cbT", [512, NT], F32)
    G.uT = scratch("uT", [512, NT], F32)
    G.sqT = scratch("sqT", [512, NT], BF16)
    G.skT = scratch("skT", [128, NT], BF16)
    G.svd = scratch("svd", [NT, 128], BF16)
    G.ucd = scratch("ucd", [NT, 512], BF16)
    G.usd = scratch("usd", [NT, 512], BF16)
    G.mqT = scratch("mqT", [8, 96, NT], BF16)
    G.mkT = scratch("mkT", [8, 96, NT], BF16)
    G.mvd = scratch("mvd", [NT, 512], BF16)
    G.ynT = scratch("ynT", [D, NT], BF16)
    G.fxd = scratch("fxd", [NPAD, D], BF16)
    G.modrow = scratch("modrow", [DEPTH, 2, 6 * D], F32)
    G.affd = scratch("affd", [NT, NE], F32)
    if "idxd" in G.debug:
        G.idxd = nc.dram_tensor("idxd", [128, NE * 5], I32, kind="ExternalOutput").ap()
        G.gd = nc.dram_tensor("gd", [128, NE * 5], F32, kind="ExternalOutput").ap()
    if stop_after == "moetest":
        G.aff_in = inp("aff_in", [NT, NE])
        G.fx_in = inp("fx_in", [NPAD, D], BF16)

    with ExitStack() as st:
        S = Sched(nc, st)
        G.S = S
        G.st = st
        G.t = {k: Tok() for k in ("xres", "hT", "cbT", "uT", "sqT", "skT", "svd", "ucd", "usd", "mqT",
                                  "mkT", "mvd", "ynT", "fxd", "out", "modrow", "affd", "idxd", "gd")}
        G.ident_f = st.enter_context(nc.sbuf_tensor("ident_f", [128, 128], F32))
        G.ident_b = st.enter_context(nc.sbuf_tensor("ident_b", [128, 128], BF16))
        G.ones_b = st.enter_context(nc.sbuf_tensor("ones_b", [128, 128], BF16))
        G.ones_f = st.enter_context(nc.sbuf_tensor("ones_f", [128, 128], F32))
        G.t_const = Tok()
        G.eps_t = st.enter_context(nc.sbuf_tensor("eps_t", [128, 1], F32))
        S.op("pool", I("memset", G.eps_t[:], EPS), writes=[G.t_const], merge=True)
        S.dma("sp", I("dma_start", out=G.ident_f[:], in_=G.identf), writes=[G.t_const])
        S.op("dve", I("tensor_copy", out=G.ident_b[:], in_=G.ident_f[:]), reads=[G.t_const], writes=[G.t_const])
        S.op("pool", I("memset", G.ones_b[:], 1.0), writes=[G.t_const], merge=True)
        S.op("pool", I("memset", G.ones_f[:], 1.0), writes=[G.t_const], merge=True)
        G.aff_all = st.enter_context(nc.sbuf_tensor("aff_all", [128, 34, NE], F32))
        G.t_aff = Tok()
        S.op("pool", I("memset", G.aff_all[:], 0.0), writes=[G.t_aff])
        G.idx_all = st.enter_context(nc.sbuf_tensor("idx_all", [128, NE, 5], I32))
        G.g_all = st.enter_context(nc.sbuf_tensor("g_all", [128, NE, 5], F32))
        G.t_idx = Tok()
        S.op("pool", I("memset", G.idx_all[:], 0), writes=[G.t_idx])
        S.op("pool", I("memset", G.g_all[:], 0.0), writes=[G.t_idx], merge=True)
        S.dma("sp", I("dma_start", out=G.xres.rearrange("(a p) d -> p a d", p=128),
                                          in_=G.xin.rearrange("(a p) d -> p a d", p=128)),
              writes=[G.t["xres"]])
        S.dma("pool", I("dma_start", out=G.fxd[NT:NPAD, :], in_=G.xin[NT:NPAD, :]), writes=[G.t["fxd"]], merge=True)
        S.flush("init")
        if stop_after == "moetest":
            phase_adaln(G, 0)
            S.dma("sp", I("dma_start", out=G.aff_all[:], in_=G.aff_in.rearrange("(t p) e -> p t e", p=128)), writes=[G.t_aff])
            S.dma("sp", I("dma_start", out=G.fxd.rearrange("(a p) d -> p a d", p=128), in_=G.fx_in.rearrange("(a p) d -> p a d", p=128)),
                  writes=[G.t["fxd"]])
            phase_route(G, 0, True)
            if "moe" in G.debug:
                phase_moe(G, 0, True)
            n_layers = 0
        for l in range(n_layers):
            last = (l == DEPTH - 1)
            phase_adaln(G, l)
            if stop_after == "adaln":
                break
            phase_proj_a(G, l)
            if stop_after == "proj_a":
                break
            phase_proj_b(G, l)
            if stop_after == "proj_b":
                break
            phase_conv(G, l, not last)
            if stop_after == "conv":
                break
            phase_swa(G, l, not last)
            if stop_after == "swa":
                break
            phase_fnet(G, l, not last)
            if stop_after == "fnet":
                break
            phase_mla(G, l, not last)
            if stop_after == "mla":
                break
            phase_outproj(G, l, not last)
            if stop_after == "outproj":
                break
            phase_route(G, l, not last)
            if stop_after == "route":
                break
            phase_moe(G, l, not last)
            if stop_after == "moe":
                break
        if stop_after is None and n_layers == DEPTH:
            phase_final(G)
        S.wait_all("sp", list(G.t.values()))
        S.flush("fin")
        G.n_inst = S.n_inst
    G.in_names = list(din.keys())
    return nc, G


def _alloc(G, ps):
    nc = G.nc
    G.uid = getattr(G, "uid", 0) + 1
    u = G.uid
    sb = lambda name, shape, dt: ps.enter_context(nc.sbuf_tensor(f"{name}_{u}", list(shape), dt))
    pp = lambda name, shape, dt=F32: ps.enter_context(nc.psum_tensor(f"{name}_{u}", list(shape), dt))
    return sb, pp


BLOCKS = [(i * 512, 512, 0) for i in range(8)] + [(NL, NCX, 1)]


def phase_adaln(G, l):
    nc, S = G.nc, G.S
    with ExitStack() as ps:
        sb, pp = _alloc(G, ps)
        cT = sb("ad_cT", [128, 16, 2], F32)
        sl = sb("ad_sl", [128, 16, 64], BF16)
        sel = sb("ad_sel", [1, 64], BF16)
        brow = sb("ad_brow", [1, 6 * D], BF16)
        rows = sb("ad_rows", [64, 6 * D], F32)
        wt = [sb(f"ad_wt{i}", [128, 16, 512], BF16) for i in range(2)]
        pacc = [pp(f"ad_ps{i}", [64, 512]) for i in range(2)]
        t_c, t_sl, t_sel, t_b, t_rows = Tok(), Tok(), Tok(), Tok(), Tok()
        t_wt = [Tok(), Tok()]
        t_ps = [Tok(), Tok()]
        S.dma("sp", I("dma_start", out=cT[:], in_=G.cvec.rearrange("(j p) t -> p j t", p=128)), writes=[t_c])
        S.op("pool", I("memset", sl[:], 0.0), writes=[t_sl])
        S.op("pool", I("memset", sel[:], 0.0), writes=[t_sel])
        S.op("pool", I("memset", sel[0:1, 0:1], 1.0), writes=[t_sel])
        S.op("pool", I("memset", sel[0:1, 32:33], 1.0), writes=[t_sel])
        S.op("act", I("activation", out=sl[:, :, 0], in_=cT[:, :, 0], func=AF.Silu), reads=[t_c], writes=[t_sl])
        S.op("act", I("activation", out=sl[:, :, 32], in_=cT[:, :, 1], func=AF.Silu), reads=[t_c], writes=[t_sl])
        for q in range(6):
            S.dma("pool", I("dma_start", out=brow[0:1, q * D:(q + 1) * D], in_=G.ada_b[l:l + 1, q * D:(q + 1) * D]),
                  writes=[t_b], merge=True)
        for nb in range(24):
            b = nb % 2
            S.dma("pool", I("dma_start", out=wt[b][:].rearrange("p j n -> p (j n)").rearrange("p (a m) -> p a m", m=2048),
                            in_=G.ada_w[l, nb].rearrange("p (a m) -> p a m", m=2048)),
                  writes=[t_wt[b]])
            for j in range(16):
                S.op("pe", I("matmul", pacc[b][:], lhsT=sl[:, j, :], rhs=wt[b][:, j, :], start=(j == 0), stop=False),
                     reads=[t_sl, t_wt[b]], writes=[t_ps[b]], merge=(j > 0))
            S.op("pe", I("matmul", pacc[b][:], lhsT=sel[:], rhs=brow[0:1, nb * 512:(nb + 1) * 512], start=False, stop=True),
                 reads=[t_sel, t_b], writes=[t_ps[b]], merge=True)
            S.op("act", I("copy", out=rows[0:1, nb * 512:(nb + 1) * 512], in_=pacc[b][0:1, :]),
                 reads=[t_ps[b]], writes=[t_rows], merge=True)
            S.op("dve", I("tensor_copy", out=rows[32:33, nb * 512:(nb + 1) * 512], in_=pacc[b][32:33, :]),
                 reads=[t_ps[b]], writes=[t_rows], merge=True)
        S.dma("sp", I("dma_start", out=G.modrow[l, 0:1, :], in_=rows[0:1, :]), reads=[t_rows], writes=[G.t["modrow"]], merge=True)
        S.dma("sp", I("dma_start", out=G.modrow[l, 1:2, :], in_=rows[32:33, :]), reads=[t_rows], writes=[G.t["modrow"]], merge=True)
        S.flush(f"adaln{l}")


def load_T(G, row_aps, sb, pp, name):
    S = G.S
    nv = len(row_aps)
    t_stg0 = Tok()
    n = max(ap.shape[-1] for ap in row_aps) // 128
    stg = sb(name + "_stg", [16, nv, 128], F32)
    out = sb(name, [128, nv, n], F32)
    S.op("pool", I("memset", stg[:], 0.0), writes=[t_stg0])
    pst_full = pp(name + "_ps", [128, 512])
    pst = pst_full[:, 0:nv * 16].rearrange("p (v n) -> p v n", n=16)
    t_stg, t_ps, t_out = t_stg0, Tok(), Tok()
    for v, ap in enumerate(row_aps):
        S.dma("sp", I("dma_start", out=stg[0:ap.shape[-1] // 128, v, :], in_=ap.rearrange("(j p) -> j p", p=128)),
              reads=[G.t["modrow"]], writes=[t_stg], merge=(v > 0))
    for v in range(nv):
        S.op("pe", I("transpose", out=pst[:, v, 0:n], in_=stg[0:n, v, :], identity=G.ident_f[0:n, 0:n]),
             reads=[t_stg, G.t_const], writes=[t_ps], merge=(v > 0))
    S.op("dve", I("tensor_copy", out=out[:], in_=pst[:, :, 0:n]), reads=[t_ps], writes=[t_out])
    return out, t_out


def rstd_from_ss(G, out_ap, ss_ap, n, reads, writes, merge=False):
    S = G.S
    S.op("act", I("activation", out=out_ap, in_=ss_ap, func=AF.Ln, bias=G.eps_t[0:out_ap.shape[0], 0:1], scale=1.0 / n),
         reads=list(reads) + [G.t_const], writes=writes, merge=merge)
    S.op("act", I("activation", out=out_ap, in_=out_ap, func=AF.Exp, scale=-0.5),
         reads=writes, writes=writes)


def phase_proj_a(G, l):
    nc, S = G.nc, G.S
    with ExitStack() as ps:
        sb, pp = _alloc(G, ps)
        NCOL = 2560
        wA = sb("pa_w", [128, 16, NCOL], BF16)
        t_w = Tok()
        for c0 in range(0, NCOL, 512):
            S.dma("pool", I("dma_start",
                out=wA[:, :, c0:c0 + 512], in_=G.w_a[l][:, c0:c0 + 512].rearrange("(j p) n -> p j n", p=128)),
                writes=[t_w], merge=True)
        mv, t_mv = load_T(G, [G.modrow[l, 0, 0:D], G.modrow[l, 1, 0:D], G.modrow[l, 0, D:2 * D],
                              G.modrow[l, 1, D:2 * D], G.norm1_g[l]], sb, pp, "pa_mv")
        sh1T = mv
        t_sh = t_mv
        gs1T = sb("pa_gs", [128, 2, 16], F32)
        t_gs = Tok()
        for s in range(2):
            S.op("dve", I("scalar_tensor_tensor", out=gs1T[:, s, :], in0=mv[:, 2 + s, :], scalar=1.0, in1=mv[:, 4, :],
                                                           op0=ALU.add, op1=ALU.mult),
                 reads=[t_mv], writes=[t_gs], merge=True)
        xt = [sb(f"pa_xt{i}", [128, D], F32) for i in range(2)]
        t_xt = [Tok(), Tok()]
        junk = sb("pa_junk", [128, D], BF16)
        t_junk = Tok()
        ssq = sb("pa_ss", [128, 8], F32)
        t_ss = [Tok() for _ in range(8)]
        xh = sb("pa_xh", [128, 4, D], BF16)
        t_xh = [Tok() for _ in range(4)]
        hTbs = [sb(f"pa_hT{i}", [128, 16, 512], BF16) for i in range(2)]
        t_hTs = [Tok(), Tok()]
        rS = [sb(f"pa_rS{i}", [128, 2, 512], F32) for i in range(2)]
        t_rS = [Tok(), Tok()]
        cc_s = sb("pa_cc", [128, 4, 512], F32)
        t_cc = [Tok() for _ in range(4)]
        t1 = sb("pa_t1", [128, 4, 512], F32)
        t_t1 = [Tok() for _ in range(4)]
        NST = 4
        stf = [sb(f"pa_stf{i}", [128, 512], F32) for i in range(NST)]
        t_stf = [Tok() for _ in range(NST)]
        stb = [sb(f"pa_stb{i}", [128, 512], BF16) for i in range(NST)]
        t_stb = [Tok() for _ in range(NST)]
        pT = [pp(f"pa_pT{i}", [128, 1024], BF16) for i in range(2)]
        t_pT = [Tok(), Tok()]
        NACC = 4
        acc = [pp(f"pa_acc{i}", [128, 512]) for i in range(NACC)]
        t_acc = [Tok() for _ in range(NACC)]
        cnt = {"f": 0, "b": 0, "a": 0, "x": 0}

        import os
        LV = int(os.environ.get("PA_STOP", "9"))
        for bi, (tok0, nb, s) in enumerate(BLOCKS):
            if LV <= 1 or (LV <= 3 and bi > 0):
                break
            ntile = nb // 128
            rb = bi % 2
            hTb = hTbs[bi % 2]
            t_hT = t_hTs[bi % 2]
            S.dma("sp", I("dma_start",
                out=rS[rb][:, :, 0:nb], in_=G.ropeS[:, :, tok0:tok0 + nb].rearrange("t p n -> p t n")),
                writes=[t_rS[rb]])
            for t in range(ntile):
                xi = cnt["x"] % 2
                cnt["x"] += 1
                si = (bi * 4 + t) % 8
                r0 = tok0 + t * 128
                S.dma("sp", I("dma_start", out=xt[xi][:], in_=G.xres[r0:r0 + 128, :]),
                      reads=[G.t["xres"]], writes=[t_xt[xi]])
                S.op("act", I("activation", out=junk[:], in_=xt[xi][:], func=AF.Square,
                                                                 accum_out=ssq[:, si:si + 1]),
                     reads=[t_xt[xi]], writes=[t_junk, t_ss[si]])
                rstd_from_ss(G, ssq[:, si:si + 1], ssq[:, si:si + 1], D, [t_ss[si]], [t_ss[si]])
                S.op("act", I("activation", out=xh[:, t, :], in_=xt[xi][:], func=AF.Copy,
                                                                      scale=ssq[:, si:si + 1]),
                     reads=[t_xt[xi], t_ss[si]], writes=[t_xh[t]])
            for j in range(16):
                pb = j % 2
                for t in range(ntile):
                    S.op("pe", I("transpose", out=pT[pb][:, t * 128:(t + 1) * 128],
                                                                     in_=xh[:, t, j * 128:(j + 1) * 128], identity=G.ident_b[:]),
                         reads=[t_xh[t], G.t_const], writes=[t_pT[pb]], merge=(t > 0))
                S.op("act", I("activation",
                    out=hTb[:, j, 0:nb], in_=pT[pb][:, 0:nb], func=AF.Identity,
                    bias=sh1T[:, s, j:j + 1], scale=gs1T[:, s, j:j + 1]),
                    reads=[t_pT[pb], t_sh, t_gs], writes=[t_hT], merge=(j > 0))
            S.dma("sp", I("dma_start",
                out=G.hT[:, tok0:tok0 + nb].rearrange("(j p) n -> p j n", p=128), in_=hTb[:, :, 0:nb]),
                reads=[t_hT], writes=[G.t["hT"]], merge=True)

            if LV <= 2:
                break
            def proj(c0, width):
                ai = cnt["a"] % NACC
                cnt["a"] += 1
                for j in range(16):
                    S.op("pe", I("matmul",
                        acc[ai][0:width, 0:nb], lhsT=wA[:, j, c0:c0 + width], rhs=hTb[:, j, 0:nb],
                        start=(j == 0), stop=(j == 15)),
                        reads=[t_w, t_hT], writes=[t_acc[ai]], merge=(j > 0))
                return ai

            def stage_f():
                i = cnt["f"] % NST
                cnt["f"] += 1
                return i

            def stage_b():
                i = cnt["b"] % NST
                cnt["b"] += 1
                return i

            for c in range(4):
                ai = proj(OFF_CB + c * 128, 128)
                fi = stage_f()
                S.op("act", I("copy", out=stf[fi][:, 0:nb], in_=acc[ai][:, 0:nb]),
                     reads=[t_acc[ai]], writes=[t_stf[fi]])
                S.dma("sp", I("dma_start", out=G.cbT[c * 128:(c + 1) * 128, tok0:tok0 + nb], in_=stf[fi][:, 0:nb]),
                      reads=[t_stf[fi]], writes=[G.t["cbT"]], merge=True)
                ai = proj(OFF_CC + c * 128, 128)
                S.op("act", I("copy", out=cc_s[:, c, 0:nb], in_=acc[ai][:, 0:nb]),
                     reads=[t_acc[ai]], writes=[t_cc[c]])
                ai = proj(OFF_CH + c * 128, 128)
                fi = stage_f()
                S.op("dve", I("tensor_tensor", out=stf[fi][:, 0:nb], in0=acc[ai][:, 0:nb],
                                                                        in1=cc_s[:, c, 0:nb], op=ALU.mult),
                     reads=[t_acc[ai], t_cc[c]], writes=[t_stf[fi]])
                S.dma("sp", I("dma_start", out=G.uT[c * 128:(c + 1) * 128, tok0:tok0 + nb], in_=stf[fi][:, 0:nb]),
                      reads=[t_stf[fi]], writes=[G.t["uT"]], merge=True)
            for c in range(4):
                ai = proj(OFF_SQ + c * 128, 128)
                S.op("dve", I("tensor_tensor", out=t1[:, c, 0:nb], in0=acc[ai][:, 0:nb],
                                                                        in1=rS[rb][:, 0, 0:nb], op=ALU.mult),
                     reads=[t_acc[ai], t_rS[rb]], writes=[t_t1[c]])
                ai = proj(OFF_SQW + c * 128, 128)
                fi = stage_f()
                S.op("dve", I("tensor_tensor", out=stf[fi][:, 0:nb], in0=acc[ai][:, 0:nb],
                                                                          in1=rS[rb][:, 1, 0:nb], op=ALU.mult),
                     reads=[t_acc[ai], t_rS[rb]], writes=[t_stf[fi]])
                bi_ = stage_b()
                S.op("pool", I("tensor_tensor", out=stb[bi_][:, 0:nb], in0=t1[:, c, 0:nb],
                                                                           in1=stf[fi][:, 0:nb], op=ALU.add),
                     reads=[t_t1[c], t_stf[fi]], writes=[t_stb[bi_]])
                S.dma("sp", I("dma_start", out=G.sqT[c * 128:(c + 1) * 128, tok0:tok0 + nb], in_=stb[bi_][:, 0:nb]),
                      reads=[t_stb[bi_]], writes=[G.t["sqT"]], merge=True)
        S.flush(f"proja{l}")


def phase_proj_b(G, l):
    nc, S = G.nc, G.S
    with ExitStack() as ps:
        sb, pp = _alloc(G, ps)
        w1 = sb("pb_w1", [128, 16, 896], BF16)
        w2 = sb("pb_w2", [128, 16, 960], BF16)
        wq = sb("pb_wq", [128, 4, 2, 768], BF16)
        wk = sb("pb_wk", [128, 2, 512], BF16)
        wv = sb("pb_wv", [128, 2, 512], BF16)
        d64 = sb("pb_d64", [128, 2, 128], BF16)
        t_w = Tok()
        for c0 in range(0, 896, 448):
            S.dma("pool", I("dma_start", out=w1[:, :, c0:c0 + 448],
                            in_=G.w_a[l][:, 2560 + c0:2560 + c0 + 448].rearrange("(j p) n -> p j n", p=128)),
                  writes=[t_w], merge=True)
        for c0 in range(0, 960, 480):
            S.dma("pool", I("dma_start", out=w2[:, :, c0:c0 + 480],
                            in_=G.w_b[l][:, c0:c0 + 480].rearrange("(j p) n -> p j n", p=128)),
                  writes=[t_w], merge=True)
        for ab in range(2):
            S.dma("pool", I("dma_start", out=wq[:, :, ab, :], in_=G.w_uq[l, ab].rearrange("(c p) n -> p c n", p=128)),
                  writes=[t_w], merge=True)
        S.dma("pool", I("dma_start", out=wk[:], in_=G.w_uk[l].rearrange("(c p) n -> p c n", p=128)), writes=[t_w], merge=True)
        S.dma("pool", I("dma_start", out=wv[:], in_=G.w_uv[l].rearrange("(c p) n -> p c n", p=128)), writes=[t_w], merge=True)
        S.dma("sp", I("dma_start", out=d64[:], in_=G.dft64.rearrange("t p n -> p t n")), writes=[t_w], merge=True)
        gT, t_gT = load_T(G, [G.q_norm_g[l], G.kv_norm_g[l]], sb, pp, "pb_g")
        hTb = [sb(f"pb_hT{i}", [128, 16, 512], BF16) for i in range(2)]
        t_hT = [Tok(), Tok()]
        rS = sb("pb_rS", [128, 2, 512], F32)
        rM = sb("pb_rM", [96, 2, 512], F32)
        t_r = Tok()
        cq_f = sb("pb_cqf", [128, 4, 512], F32)
        t_cqf = [Tok() for _ in range(4)]
        sqb = [sb(f"pb_sqb{i}", [128, 512], BF16) for i in range(2)]
        t_sqb = [Tok(), Tok()]
        rstd = sb("pb_rstd", [128, 2, 512], F32)
        t_rstd = [Tok(), Tok()]
        cqn = sb("pb_cqn", [128, 4, 512], BF16)
        t_cqn = Tok()
        ckf = sb("pb_ckf", [128, 2, 512], F32)
        t_ckf = [Tok(), Tok()]
        ckn = sb("pb_ckn", [128, 2, 512], BF16)
        t_ckn = Tok()
        fuT = sb("pb_fuT", [128, 4, 512], BF16)
        t_fuT = Tok()
        mq_st = sb("pb_mq", [96, 8, 512], BF16)
        t_mq = Tok()
        mk_st = sb("pb_mk", [96, 8, 512], BF16)
        t_mk = Tok()
        kpe_r = sb("pb_kpe", [96, 512], BF16)
        t_kpe = Tok()
        tA = [sb(f"pb_tA{i}", [128, 512], F32) for i in range(2)]
        t_tA = [Tok(), Tok()]
        tB = [sb(f"pb_tB{i}", [128, 512], F32) for i in range(2)]
        t_tB = [Tok(), Tok()]
        NST = 3
        stb = [sb(f"pb_stb{i}", [128, 512], BF16) for i in range(NST)]
        t_stb = [Tok() for _ in range(NST)]
        NACC = 3
        acc = [pp(f"pb_acc{i}", [128, 512]) for i in range(NACC)]
        t_acc = [Tok() for _ in range(NACC)]
        ssp = pp("pb_ss", [128, 512])
        t_ssp = Tok()
        tkp = [pp(f"pb_tk{i}", [128, 512]) for i in range(2)]
        t_tkp = [Tok(), Tok()]
        cnt = {"a": 0, "b": 0, "t": 0, "k": 0}
        import os
        LV = int(os.environ.get("PB_STOP", "9"))

        for bi, (tok0, nb, s) in enumerate(BLOCKS):
            if LV <= 3 and bi > 0:
                break
            ntile = nb // 128
            hb = bi % 2
            hT_ = hTb[hb]
            S.dma("sp", I("dma_start", out=hT_[:, :, 0:nb], in_=G.hT[:, tok0:tok0 + nb].rearrange("(j p) n -> p j n", p=128)),
                  reads=[G.t["hT"]], writes=[t_hT[hb]])
            S.dma("sp", I("dma_start", out=rS[:, :, 0:nb], in_=G.ropeS[:, :, tok0:tok0 + nb].rearrange("t p n -> p t n")),
                  writes=[t_r])
            S.dma("sp", I("dma_start", out=rM[:, :, 0:nb], in_=G.ropeM[:, :, tok0:tok0 + nb].rearrange("t p n -> p t n")),
                  writes=[t_r], merge=True)

            def proj(wt, c0, width, rhs_tile=None):
                ai = cnt["a"] % NACC
                cnt["a"] += 1
                for j in range(16):
                    S.op("pe", I("matmul", acc[ai][0:width, 0:nb], lhsT=wt[:, j, c0:c0 + width], rhs=hT_[:, j, 0:nb],
                                 start=(j == 0), stop=(j == 15)),
                         reads=[t_w, t_hT[hb]], writes=[t_acc[ai]], merge=(j > 0))
                return ai

            def rope_pair(aiA, aiB, p0, p1, table, out_ap, t_out, merge_out=False):
                ti = cnt["t"] % 2
                cnt["t"] += 1
                S.op("dve", I("tensor_tensor", out=tA[ti][p0:p1, 0:nb], in0=acc[aiA][p0:p1, 0:nb], in1=table[p0:p1, 0, 0:nb], op=ALU.mult),
                     reads=[t_acc[aiA], t_r], writes=[t_tA[ti]])
                S.op("dve", I("tensor_tensor", out=tB[ti][p0:p1, 0:nb], in0=acc[aiB][p0:p1, 0:nb], in1=table[p0:p1, 1, 0:nb], op=ALU.mult),
                     reads=[t_acc[aiB], t_r], writes=[t_tB[ti]])
                S.op("pool", I("tensor_tensor", out=out_ap, in0=tA[ti][p0:p1, 0:nb], in1=tB[ti][p0:p1, 0:nb], op=ALU.add),
                     reads=[t_tA[ti], t_tB[ti]], writes=[t_out], merge=merge_out)

            def stage_b():
                i = cnt["b"] % NST
                cnt["b"] += 1
                return i

            aiA = proj(w1, 0, 128)
            aiB = proj(w1, 128, 128)
            bi_ = stage_b()
            rope_pair(aiA, aiB, 0, 128, rS, stb[bi_][:, 0:nb], t_stb[bi_])
            S.dma("sp", I("dma_start", out=G.skT[:, tok0:tok0 + nb], in_=stb[bi_][:, 0:nb]), reads=[t_stb[bi_]],
                  writes=[G.t["skT"]], merge=True)
            ki = cnt["k"] % 2
            cnt["k"] += 1
            for t in range(ntile):
                for j in range(16):
                    S.op("pe", I("matmul", tkp[ki][:, t * 128:(t + 1) * 128], lhsT=hT_[:, j, t * 128:(t + 1) * 128],
                                 rhs=w1[:, j, 256:384], start=(j == 0), stop=(j == 15)),
                         reads=[t_w, t_hT[hb]], writes=[t_tkp[ki]], merge=(j > 0 or t > 0))
            bi_ = stage_b()
            S.op("act", I("copy", out=stb[bi_][:, 0:nb], in_=tkp[ki][:, 0:nb]), reads=[t_tkp[ki]], writes=[t_stb[bi_]])
            S.dma("sp", I("dma_start", out=G.svd[tok0:tok0 + nb, :].rearrange("(t p) c -> p t c", p=128),
                          in_=stb[bi_][:, 0:nb].rearrange("p (t c) -> p t c", c=128)),
                  reads=[t_stb[bi_]], writes=[G.t["svd"]], merge=True)
            for c in range(4):
                ai = proj(w1, 384 + c * 128, 128)
                S.op("act", I("copy", out=fuT[:, c, 0:nb], in_=acc[ai][:, 0:nb]), reads=[t_acc[ai]], writes=[t_fuT], merge=(c > 0))
            for t in range(ntile):
                for cs_, dst, key in ((0, G.ucd, "ucd"), (1, G.usd, "usd")):
                    ki = cnt["k"] % 2
                    cnt["k"] += 1
                    for c in range(4):
                        S.op("pe", I("matmul", tkp[ki][:, c * 128:(c + 1) * 128], lhsT=fuT[:, c, t * 128:(t + 1) * 128],
                                     rhs=d64[:, cs_, :], start=True, stop=True),
                             reads=[t_w, t_fuT], writes=[t_tkp[ki]], merge=(c > 0))
                    bi_ = stage_b()
                    S.op("act" if cs_ == 0 else "dve",
                         I("copy", out=stb[bi_][:], in_=tkp[ki][:]) if cs_ == 0 else I("tensor_copy", out=stb[bi_][:], in_=tkp[ki][:]),
                         reads=[t_tkp[ki]], writes=[t_stb[bi_]])
                    S.dma("sp", I("dma_start", out=dst[tok0 + t * 128:tok0 + (t + 1) * 128, :], in_=stb[bi_][:]),
                          reads=[t_stb[bi_]], writes=[G.t[key]], merge=True)
            def latent_norm(c0, nch, f_tile, t_f, n_tile, t_n, gi, ri):
                for c in range(nch):
                    ai = proj(w2, c0 + c * 128, 128)
                    S.op("act", I("copy", out=f_tile[:, c, 0:nb], in_=acc[ai][:, 0:nb]), reads=[t_acc[ai]], writes=[t_f[c]])
                    qi = c % 2
                    S.op("act", I("activation", out=sqb[qi][:, 0:nb], in_=acc[ai][:, 0:nb], func=AF.Square),
                         reads=[t_acc[ai]], writes=[t_sqb[qi]])
                    S.op("pe", I("matmul", ssp[:, 0:nb], lhsT=G.ones_b[:], rhs=sqb[qi][:, 0:nb], start=(c == 0), stop=(c == nch - 1)),
                         reads=[t_sqb[qi], G.t_const], writes=[t_ssp], merge=(c > 0))
                rstd_from_ss(G, rstd[:, ri, 0:nb], ssp[:, 0:nb], nch * 128, [t_ssp], [t_rstd[ri]])
                for c in range(nch):
                    S.op("dve", I("scalar_tensor_tensor", out=n_tile[:, c, 0:nb], in0=f_tile[:, c, 0:nb], scalar=gT[:, gi, c:c + 1],
                                  in1=rstd[:, ri, 0:nb], op0=ALU.mult, op1=ALU.mult),
                         reads=[t_f[c], t_gT, t_rstd[ri]], writes=[t_n], merge=(c > 0))

            latent_norm(0, 4, cq_f, t_cqf, cqn, t_cqn, 0, 0)
            latent_norm(512, 2, ckf, t_ckf, ckn, t_ckn, 1, 1)
            for h in range(8):
                ais = []
                for ab in range(2):
                    ai = cnt["a"] % NACC
                    cnt["a"] += 1
                    for c in range(4):
                        S.op("pe", I("matmul", acc[ai][0:96, 0:nb], lhsT=wq[:, c, ab, h * 96:(h + 1) * 96], rhs=cqn[:, c, 0:nb],
                                     start=(c == 0), stop=(c == 3)),
                             reads=[t_w, t_cqn], writes=[t_acc[ai]], merge=(c > 0))
                    ais.append(ai)
                S.op("act", I("copy", out=mq_st[0:64, h, 0:nb], in_=acc[ais[0]][0:64, 0:nb]), reads=[t_acc[ais[0]]],
                     writes=[t_mq], merge=(h > 0))
                rope_pair(ais[0], ais[1], 64, 96, rM, mq_st[64:96, h, 0:nb], t_mq, merge_out=True)
            S.dma("sp", I("dma_start", out=G.mqT[:, :, tok0:tok0 + nb].rearrange("h p n -> p h n"), in_=mq_st[:, :, 0:nb]),
                  reads=[t_mq], writes=[G.t["mqT"]], merge=True)
            aiA = proj(w2, OFF_KPA, 96)
            aiB = proj(w2, OFF_KPB, 96)
            rope_pair(aiA, aiB, 64, 96, rM, kpe_r[64:96, 0:nb], t_kpe)
            for h in range(8):
                ai = cnt["a"] % NACC
                cnt["a"] += 1
                for c in range(2):
                    S.op("pe", I("matmul", acc[ai][0:64, 0:nb], lhsT=wk[:, c, h * 64:(h + 1) * 64], rhs=ckn[:, c, 0:nb],
                                 start=(c == 0), stop=(c == 1)),
                         reads=[t_w, t_ckn], writes=[t_acc[ai]], merge=(c > 0))
                S.op("act", I("copy", out=mk_st[0:64, h, 0:nb], in_=acc[ai][0:64, 0:nb]), reads=[t_acc[ai]],
                     writes=[t_mk], merge=(h > 0))
                S.op("pool" if h % 2 else "dve", I("tensor_copy", out=mk_st[64:96, h, 0:nb], in_=kpe_r[64:96, 0:nb]),
                     reads=[t_kpe], writes=[t_mk], merge=True)
            S.dma("sp", I("dma_start", out=G.mkT[:, :, tok0:tok0 + nb].rearrange("h p n -> p h n"), in_=mk_st[:, :, 0:nb]),
                  reads=[t_mk], writes=[G.t["mkT"]], merge=True)
            for t in range(ntile):
                ki = cnt["k"] % 2
                cnt["k"] += 1
                for c in range(2):
                    S.op("pe", I("matmul", tkp[ki][:], lhsT=ckn[:, c, t * 128:(t + 1) * 128], rhs=wv[:, c, :],
                                 start=(c == 0), stop=(c == 1)),
                         reads=[t_w, t_ckn], writes=[t_tkp[ki]], merge=(c > 0))
                bi_ = stage_b()
                S.op("act", I("copy", out=stb[bi_][:], in_=tkp[ki][:]), reads=[t_tkp[ki]], writes=[t_stb[bi_]])
                S.dma("sp", I("dma_start", out=G.mvd[tok0 + t * 128:tok0 + (t + 1) * 128, :], in_=stb[bi_][:]),
                      reads=[t_stb[bi_]], writes=[G.t["mvd"]], merge=True)
        S.flush(f"projb{l}")


class GroupTail:
    def __init__(self, G, l, gi, sb, pp, name):
        self.G, self.gi = G, gi
        self.gT, self.t_gT = load_T(G, [G.out_norm_g[l, gi * 512:(gi + 1) * 512]], sb, pp, name + "_g")
        self.junk = sb(name + "_junk", [128, 512], BF16)
        self.t_junk = Tok()
        self.ss = [sb(name + f"_ss{i}", [128, 1], F32) for i in range(2)]
        self.t_ss = [Tok(), Tok()]
        self.ynb = [sb(name + f"_ynb{i}", [128, 512], BF16) for i in range(2)]
        self.t_ynb = [Tok(), Tok()]
        self.tp = pp(name + "_tp", [128, 1024], BF16)
        self.t_tp = Tok()
        self.st = [sb(name + f"_st{i}", [128, 4, 128], BF16) for i in range(2)]
        self.t_st = [Tok(), Tok()]
        self.k = 0

    def emit(self, y_ap, t_y, tok0):
        G, S = self.G, self.G.S
        i = self.k % 2
        self.k += 1
        S.op("act", I("activation", out=self.junk[:], in_=y_ap, func=AF.Square, accum_out=self.ss[i][:, 0:1]),
             reads=[t_y], writes=[self.t_junk, self.t_ss[i]])
        rstd_from_ss(G, self.ss[i][:, 0:1], self.ss[i][:, 0:1], 512, [self.t_ss[i]], [self.t_ss[i]])
        S.op("act", I("activation", out=self.ynb[i][:], in_=y_ap, func=AF.Copy, scale=self.ss[i][:, 0:1]),
             reads=[t_y, self.t_ss[i]], writes=[self.t_ynb[i]])
        for c in range(4):
            S.op("pe", I("transpose", out=self.tp[:, c * 128:(c + 1) * 128], in_=self.ynb[i][:, c * 128:(c + 1) * 128],
                         identity=G.ident_b[:]),
                 reads=[self.t_ynb[i], G.t_const], writes=[self.t_tp], merge=(c > 0))
        for c in range(4):
            S.op("dve", I("tensor_scalar", out=self.st[i][:, c, :], in0=self.tp[:, c * 128:(c + 1) * 128],
                          scalar1=self.gT[:, 0, c:c + 1], scalar2=None, op0=ALU.mult),
                 reads=[self.t_tp, self.t_gT], writes=[self.t_st[i]], merge=(c > 0))
        r0 = self.gi * 512
        S.dma("sp", I("dma_start", out=G.ynT[r0:r0 + 512, tok0:tok0 + 128].rearrange("(c p) n -> p c n", p=128), in_=self.st[i][:]),
              reads=[self.t_st[i]], writes=[G.t["ynT"]], merge=True)


def phase_conv(G, l, do_ctx):
    nc, S = G.nc, G.S
    with ExitStack() as ps:
        sb, pp = _alloc(G, ps)
        cw, t_cw = load_T(G, [G.conv_w[l, 0], G.conv_w[l, 1], G.conv_w[l, 2], G.out_norm_g[l, 0:512]], sb, pp, "cv_w")
        ut = [sb(f"cv_u{i}", [128, 514], F32) for i in range(2)]
        t_ut = [Tok(), Tok()]
        cbt = [sb(f"cv_cb{i}", [128, 512], F32) for i in range(2)]
        t_cbt = [Tok(), Tok()]
        acc_t = [sb(f"cv_a{i}", [128, 512], F32) for i in range(2)]
        t_at = [Tok(), Tok()]
        yc = sb("cv_y", [128, 4, 512], F32)
        t_yc = [Tok() for _ in range(4)]
        sqb = [sb(f"cv_sq{i}", [128, 512], BF16) for i in range(2)]
        t_sqb = [Tok(), Tok()]
        rstd = sb("cv_rstd", [128, 512], F32)
        t_rstd = Tok()
        stb = [sb(f"cv_st{i}", [128, 512], BF16) for i in range(2)]
        t_stb = [Tok(), Tok()]
        ssp = pp("cv_ss", [128, 512])
        t_ssp = Tok()
        segs = [(0, NL)] + ([(NL, NT)] if do_ctx else [])
        k = 0
        for (s0, s1) in segs:
            for b0 in range(s0, s1, 512):
                nb = min(512, s1 - b0)
                for c in range(4):
                    i = k % 2
                    k += 1
                    lo = b0 - 1 if b0 > s0 else b0
                    hi = b0 + nb + 1 if b0 + nb < s1 else b0 + nb
                    first = True
                    if lo == b0:
                        S.op("pool", I("memset", ut[i][:, 0:1], 0.0), writes=[t_ut[i]])
                        first = False
                    if hi == b0 + nb:
                        S.op("pool", I("memset", ut[i][:, nb + 1:nb + 2], 0.0), writes=[t_ut[i]], merge=not first)
                        first = False
                    S.dma("sp", I("dma_start", out=ut[i][:, lo - (b0 - 1):hi - (b0 - 1)], in_=G.uT[c * 128:(c + 1) * 128, lo:hi]),
                          reads=[G.t["uT"]], writes=[t_ut[i]], merge=not first)
                    S.dma("sp", I("dma_start", out=cbt[i][:, 0:nb], in_=G.cbT[c * 128:(c + 1) * 128, b0:b0 + nb]),
                          reads=[G.t["cbT"]], writes=[t_cbt[i]])
                    S.op("dve", I("tensor_scalar", out=acc_t[i][:, 0:nb], in0=ut[i][:, 0:nb], scalar1=cw[:, 0, c:c + 1], scalar2=None,
                                  op0=ALU.mult), reads=[t_ut[i], t_cw], writes=[t_at[i]])
                    S.op("dve", I("scalar_tensor_tensor", out=acc_t[i][:, 0:nb], in0=ut[i][:, 1:nb + 1], scalar=cw[:, 1, c:c + 1],
                                  in1=acc_t[i][:, 0:nb], op0=ALU.mult, op1=ALU.add), reads=[t_ut[i], t_cw, t_at[i]], writes=[t_at[i]])
                    S.op("dve", I("scalar_tensor_tensor", out=acc_t[i][:, 0:nb], in0=ut[i][:, 2:nb + 2], scalar=cw[:, 2, c:c + 1],
                                  in1=acc_t[i][:, 0:nb], op0=ALU.mult, op1=ALU.add), reads=[t_ut[i], t_cw, t_at[i]], writes=[t_at[i]])
                    S.op("pool", I("tensor_tensor", out=yc[:, c, 0:nb], in0=acc_t[i][:, 0:nb], in1=cbt[i][:, 0:nb], op=ALU.mult),
                         reads=[t_at[i], t_cbt[i]], writes=[t_yc[c]])
                    S.op("act", I("activation", out=sqb[i][:, 0:nb], in_=yc[:, c, 0:nb], func=AF.Square), reads=[t_yc[c]], writes=[t_sqb[i]])
                    S.op("pe", I("matmul", ssp[:, 0:nb], lhsT=G.ones_b[:], rhs=sqb[i][:, 0:nb], start=(c == 0), stop=(c == 3)),
                         reads=[t_sqb[i], G.t_const], writes=[t_ssp], merge=(c > 0))
                rstd_from_ss(G, rstd[:, 0:nb], ssp[:, 0:nb], 512, [t_ssp], [t_rstd])
                for c in range(4):
                    i = k % 2
                    k += 1
                    S.op("dve", I("scalar_tensor_tensor", out=stb[i][:, 0:nb], in0=yc[:, c, 0:nb], scalar=cw[:, 3, c:c + 1],
                                  in1=rstd[:, 0:nb], op0=ALU.mult, op1=ALU.mult), reads=[t_yc[c], t_cw, t_rstd], writes=[t_stb[i]])
                    S.dma("sp", I("dma_start", out=G.ynT[c * 128:(c + 1) * 128, b0:b0 + nb], in_=stb[i][:, 0:nb]),
                          reads=[t_stb[i]], writes=[G.t["ynT"]], merge=True)
        S.flush(f"conv{l}")


def phase_swa(G, l, do_ctx):
    nc, S = G.nc, G.S
    SCALE = 64 ** -0.5
    with ExitStack() as ps:
        sb, pp = _alloc(G, ps)
        Qs = sb("sw_Q", [64, 8, NT], BF16)
        Ks = sb("sw_K", [64, 2, NT], BF16)
        Vs = sb("sw_V", [128, 34, 2, 65], BF16)
        mk = sb("sw_mask", [128, 2, 512], BF16)
        snk = sb("sw_sink", [128, 8], F32)
        t_in = Tok()
        t_V = Tok()
        for h in range(8):
            S.dma("sp", I("dma_start", out=Qs[:, h, :], in_=G.sqT[h * 64:(h + 1) * 64, :]), reads=[G.t["sqT"]], writes=[t_in], merge=True)
        for h in range(2):
            S.dma("sp", I("dma_start", out=Ks[:, h, :], in_=G.skT[h * 64:(h + 1) * 64, :]), reads=[G.t["skT"]], writes=[t_in], merge=True)
        S.op("pool", I("memset", Vs[:, :, :, 64:65], 1.0), writes=[t_V])
        for h in range(2):
            S.dma("sp", I("dma_start", out=Vs[:, :, h, 0:64], in_=G.svd[:, h * 64:(h + 1) * 64].rearrange("(t p) d -> p t d", p=128)),
                  reads=[G.t["svd"]], writes=[t_V], merge=True)
        S.dma("sp", I("dma_start", out=mk[:], in_=G.masks.rearrange("t p n -> p t n")), writes=[t_in], merge=True)
        S.dma("sp", I("dma_start", out=snk[:], in_=G.swa_sink[l:l + 1, :].to_broadcast([128, 8])), writes=[t_in], merge=True)
        S.op("act", I("activation", out=snk[:], in_=snk[:], func=AF.Exp), reads=[t_in], writes=[t_in])
        tail = GroupTail(G, l, 1, sb, pp, "sw_t")
        pT = [sb(f"sw_pT{i}", [128, 5, 512], BF16) for i in range(2)]
        t_pT = [[Tok() for _ in range(5)] for _ in range(2)]
        sps = [pp(f"sw_s{i}", [128, 512]) for i in range(2)]
        t_sps = [Tok(), Tok()]
        ops_ = [pp(f"sw_o{i}", [128, 512]) for i in range(2)]
        t_ops = [Tok(), Tok()]
        den = [sb(f"sw_den{i}", [128, 8], F32) for i in range(2)]
        t_den = [Tok(), Tok()]
        ysw = [sb(f"sw_y{i}", [128, 512], F32) for i in range(2)]
        t_ysw = [Tok(), Tok()]
        qblocks = [(i, "lat") for i in range(32)] + ([(32, "ctx"), (33, "ctx")] if do_ctx else [])
        import os
        LV = int(os.environ.get("SW_STOP", "99"))
        kq = 0
        ks = 0
        for (i, kind) in qblocks[:LV]:
            if kind == "lat":
                kts = ([(i - 1, 0)] if i > 0 else []) + [(i, None)] + ([(i + 1, 1)] if i < 31 else []) + [(32, None), (33, None)]
            else:
                kts = [(32, None), (33, None)]
            yi = kq % 2
            for kvh in range(2):
                pi = kq % 2
                oi = kq % 2
                kq += 1
                for n, (kt, msk) in enumerate(kts):
                    si = ks % 2
                    ks += 1
                    S.op("pe", I("matmul", sps[si][:], lhsT=Ks[:, kvh, kt * 128:(kt + 1) * 128],
                                 rhs=Qs[:, kvh * 4:(kvh + 1) * 4, i * 128:(i + 1) * 128], start=True, stop=True),
                         reads=[t_in], writes=[t_sps[si]])
                    S.op("act", I("activation", out=pT[pi][:, n, :], in_=sps[si][:], func=AF.Exp, scale=SCALE),
                         reads=[t_sps[si]], writes=[t_pT[pi][n]])
                    if msk is not None:
                        S.op("dve", I("tensor_tensor", out=pT[pi][:, n, :], in0=pT[pi][:, n, :], in1=mk[:, msk, :], op=ALU.mult),
                             reads=[t_pT[pi][n], t_in], writes=[t_pT[pi][n]])
                for g in range(4):
                    for n, (kt, msk) in enumerate(kts):
                        S.op("pe", I("matmul", ops_[oi][:, g * 65:(g + 1) * 65], lhsT=pT[pi][:, n, g * 128:(g + 1) * 128],
                                     rhs=Vs[:, kt, kvh, :], start=(n == 0), stop=(n == len(kts) - 1)),
                             reads=[t_pT[pi][n], t_V], writes=[t_ops[oi]], merge=(n > 0 or g > 0))
                ov = ops_[oi][:, 0:260].rearrange("p (g e) -> p g e", e=65)
                S.op("dve", I("tensor_tensor", out=den[yi][:, kvh * 4:(kvh + 1) * 4], in0=ov[:, :, 64], in1=snk[:, kvh * 4:(kvh + 1) * 4], op=ALU.add),
                     reads=[t_ops[oi], t_in], writes=[t_den[yi]], merge=(kvh > 0))
                S.op("dve", I("reciprocal", out=den[yi][:, kvh * 4:(kvh + 1) * 4], in_=den[yi][:, kvh * 4:(kvh + 1) * 4]),
                     reads=[t_den[yi]], writes=[t_den[yi]])
                for g in range(4):
                    h = kvh * 4 + g
                    S.op("dve", I("tensor_scalar", out=ysw[yi][:, h * 64:(h + 1) * 64], in0=ov[:, g, 0:64], scalar1=den[yi][:, h:h + 1],
                                  scalar2=None, op0=ALU.mult),
                         reads=[t_ops[oi], t_den[yi]], writes=[t_ysw[yi]], merge=(h > 0))
            tail.emit(ysw[yi][:], t_ysw[yi], i * 128)
        S.flush(f"swa{l}")


def phase_fnet(G, l, do_ctx):
    nc, S = G.nc, G.S
    with ExitStack() as ps:
        sb, pp = _alloc(G, ps)
        uc = sb("fn_uc", [128, 34, 512], BF16)
        us = sb("fn_us", [128, 34, 512], BF16)
        t_u = Tok()
        for (t0, t1) in ((0, 16), (16, 34)):
            S.dma("sp", I("dma_start", out=uc[:, t0:t1, :], in_=G.ucd[t0 * 128:t1 * 128, :].rearrange("(t p) c -> p t c", p=128)),
                  reads=[G.t["ucd"]], writes=[t_u], merge=True)
            S.dma("sp", I("dma_start", out=us[:, t0:t1, :], in_=G.usd[t0 * 128:t1 * 128, :].rearrange("(t p) c -> p t c", p=128)),
                  reads=[G.t["usd"]], writes=[t_u], merge=True)
        dt_ = [sb(f"fn_d{i}", [128, 2 * 32 * 128], BF16) for i in range(2)]
        t_dt = [Tok(), Tok()]
        acc = [pp(f"fn_acc{i}", [128, 512]) for i in range(2)]
        t_acc = [Tok(), Tok()]
        yf = [sb(f"fn_y{i}", [128, 512], F32) for i in range(2)]
        t_yf = [Tok(), Tok()]
        tail = GroupTail(G, l, 2, sb, pp, "fn_t")
        import os
        LV = int(os.environ.get("FN_STOP", "99"))
        jobs = [(kt, 32, 0, G.dftL) for kt in range(32)][:LV] + ([(kt, 2, 32, G.dftC) for kt in range(2)] if do_ctx else [])
        for k, (kt, nt, tb, tab) in enumerate(jobs):
            i = k % 2
            dv = dt_[i][:, 0:2 * nt * 128]
            S.dma("sp", I("dma_start", out=dv, in_=tab[kt]), writes=[t_dt[i]])
            d4 = dv.rearrange("p (a n k) -> p a n k", a=2, k=128)
            for n in range(nt):
                S.op("pe", I("matmul", acc[i][:], lhsT=d4[:, 0, n, :], rhs=uc[:, tb + n, :], start=(n == 0), stop=False),
                     reads=[t_dt[i], t_u], writes=[t_acc[i]], merge=(n > 0))
            for n in range(nt):
                S.op("pe", I("matmul", acc[i][:], lhsT=d4[:, 1, n, :], rhs=us[:, tb + n, :], start=False, stop=(n == nt - 1)),
                     reads=[t_dt[i], t_u], writes=[t_acc[i]], merge=True)
            S.op("act", I("copy", out=yf[i][:], in_=acc[i][:]), reads=[t_acc[i]], writes=[t_yf[i]])
            tail.emit(yf[i][:], t_yf[i], (tb + kt) * 128)
        S.flush(f"fnet{l}")


def phase_mla(G, l, do_ctx):
    nc, S = G.nc, G.S
    SCALE = 96 ** -0.5
    with ExitStack() as ps:
        sb, pp = _alloc(G, ps)
        Kh = [sb(f"ml_K{i}", [96, NT], BF16) for i in range(2)]
        Qh = [sb(f"ml_Q{i}", [96, NT], BF16) for i in range(2)]
        Vh = [sb(f"ml_V{i}", [128, 34, 65], BF16) for i in range(2)]
        t_K = [Tok(), Tok()]
        t_Q = [Tok(), Tok()]
        t_V = [Tok(), Tok()]
        PT = [sb(f"ml_PT{i}", [128, 34, 512], BF16) for i in range(2)]
        t_PT = [[Tok() for _ in range(34)] for _ in range(2)]
        yall = sb("ml_y", [128, 34, 512], F32)
        t_y = [Tok() for _ in range(34)]
        rc = [sb(f"ml_rc{i}", [128, 1], F32) for i in range(4)]
        t_rc = [Tok() for _ in range(4)]
        sps = [pp(f"ml_s{i}", [128, 512]) for i in range(2)]
        t_sps = [Tok(), Tok()]
        ops_ = [pp(f"ml_o{i}", [128, 512]) for i in range(4)]
        t_ops = [Tok() for _ in range(4)]
        import os
        LVH = int(os.environ.get("ML_HEADS", "8"))
        LVQ = int(os.environ.get("ML_QB", "99"))
        qblocks = [(i * 512, 512, list(range(34))) for i in range(8)][:LVQ] + ([(NL, NCX, [32, 33])] if do_ctx else [])
        ks = 0
        kp = 0
        ko = 0
        for h in range(LVH):
            hb = h % 2
            S.dma("sp", I("dma_start", out=Kh[hb][:], in_=G.mkT[h]), reads=[G.t["mkT"]], writes=[t_K[hb]])
            S.dma("sp", I("dma_start", out=Qh[hb][:], in_=G.mqT[h]), reads=[G.t["mqT"]], writes=[t_Q[hb]])
            S.op("pool", I("memset", Vh[hb][:, :, 64:65], 1.0), writes=[t_V[hb]])
            S.dma("sp", I("dma_start", out=Vh[hb][:, :, 0:64], in_=G.mvd[:, h * 64:(h + 1) * 64].rearrange("(t p) d -> p t d", p=128)),
                  reads=[G.t["mvd"]], writes=[t_V[hb]], merge=True)
            for (q0, nq, kts) in qblocks:
                pi = kp % 2
                kp += 1
                for kt in kts:
                    si = ks % 2
                    ks += 1
                    S.op("pe", I("matmul", sps[si][:, 0:nq], lhsT=Kh[hb][:, kt * 128:(kt + 1) * 128], rhs=Qh[hb][:, q0:q0 + nq],
                                 start=True, stop=True), reads=[t_K[hb], t_Q[hb]], writes=[t_sps[si]])
                    S.op("act", I("activation", out=PT[pi][:, kt, 0:nq], in_=sps[si][:, 0:nq], func=AF.Exp, scale=SCALE),
                         reads=[t_sps[si]], writes=[t_PT[pi][kt]])
                for j in range(nq // 128):
                    oi = ko % 4
                    ko += 1
                    for n, kt in enumerate(kts):
                        S.op("pe", I("matmul", ops_[oi][:, 0:65], lhsT=PT[pi][:, kt, j * 128:(j + 1) * 128], rhs=Vh[hb][:, kt, :],
                                     start=(n == 0), stop=(n == len(kts) - 1)),
                             reads=[t_PT[pi][kt], t_V[hb]], writes=[t_ops[oi]], merge=(n > 0))
                    S.op("dve", I("reciprocal", out=rc[oi][:], in_=ops_[oi][:, 64:65]), reads=[t_ops[oi]], writes=[t_rc[oi]])
                    tile_i = q0 // 128 + j
                    S.op("dve", I("tensor_scalar", out=yall[:, tile_i, h * 64:(h + 1) * 64], in0=ops_[oi][:, 0:64], scalar1=rc[oi][:, 0:1],
                                  scalar2=None, op0=ALU.mult),
                         reads=[t_ops[oi], t_rc[oi]], writes=[t_y[tile_i]], merge=(h > 0))
        if LVH == 8:
            tail = GroupTail(G, l, 3, sb, pp, "ml_t")
            ntile = (qblocks[-1][0] + qblocks[-1][1]) // 128 if LVQ >= 8 else LVQ * 4
            tiles = list(range(min(32, ntile))) + ([32, 33] if do_ctx else [])
            for ti in tiles:
                tail.emit(yall[:, ti, :], t_y[ti], ti * 128)
        else:
            G.dbg_yall = (yall, t_y)
        S.flush(f"mla{l}")


def phase_outproj(G, l, do_ctx):
    nc, S = G.nc, G.S
    with ExitStack() as ps:
        sb, pp = _alloc(G, ps)
        wo = sb("op_wo", [128, 16, D], BF16)
        t_w = Tok()
        for c0 in range(0, D, 512):
            S.dma("pool", I("dma_start", out=wo[:, :, c0:c0 + 512], in_=G.w_out[l][:, c0:c0 + 512].rearrange("(j p) n -> p j n", p=128)),
                  writes=[t_w], merge=True)
        wr = sb("op_wr", [128, 16, NE], F32)
        S.dma("sp", I("dma_start", out=wr[:], in_=G.w_router[l].rearrange("(j p) e -> p j e", p=128)), writes=[t_w], merge=True)
        g1b = sb("op_g1b", [128, D], F32)
        sh2b = sb("op_sh2b", [128, D], F32)
        gs2b = sb("op_gs2b", [128, D], F32)
        t_bc = Tok()
        NB = 2
        xt = [sb(f"op_x{i}", [128, D], F32) for i in range(NB)]
        t_xt = [Tok() for _ in range(NB)]
        xn = [sb(f"op_xn{i}", [128, D], F32) for i in range(NB)]
        t_xn = [Tok() for _ in range(NB)]
        fx = [sb(f"op_fx{i}", [128, D], F32) for i in range(NB)]
        t_fx = [Tok() for _ in range(NB)]
        fxb = [sb(f"op_fxb{i}", [128, D], BF16) for i in range(NB)]
        t_fxb = [Tok() for _ in range(NB)]
        junk = sb("op_junk", [128, D], BF16)
        t_junk = Tok()
        yn = [sb(f"op_yn{i}", [128, 16, 128], BF16) for i in range(NB)]
        t_yn = [Tok() for _ in range(NB)]
        fxT = [sb(f"op_fxT{i}", [128, 16, 128], F32) for i in range(NB)]
        t_fxT = [Tok() for _ in range(NB)]
        ss = [sb(f"op_ss{i}", [128, 1], F32) for i in range(NB)]
        t_ss = [Tok() for _ in range(NB)]
        sm = [sb(f"op_sm{i}", [128, 4], F32) for i in range(NB)]
        t_sm = [Tok() for _ in range(NB)]
        ex = [sb(f"op_ex{i}", [128, NE], F32) for i in range(NB)]
        t_ex = [Tok() for _ in range(NB)]
        acc = [pp(f"op_acc{i}", [128, 512]) for i in range(4)]
        t_acc = [Tok() for _ in range(4)]
        trp = [pp(f"op_tr{i}", [128, 512]) for i in range(2)]
        t_trp = [Tok(), Tok()]
        lgp = [pp(f"op_lg{i}", [128, 512]) for i in range(2)]
        t_lgp = [Tok(), Tok()]
        import os
        LV = int(os.environ.get("OP_STOP", "99"))
        tiles = (list(range(32)) + ([32, 33] if do_ctx else []))[:LV]
        cur_s = None
        kt = [0]
        def part1(k, ti):
            nonlocal cur_s
            s_ = 1 if ti >= 32 else 0
            if s_ != cur_s:
                cur_s = s_
                S.dma("sp", I("dma_start", out=g1b[:], in_=G.modrow[l, s_:s_ + 1, 2 * D:3 * D].to_broadcast([128, D])),
                      reads=[G.t["modrow"]], writes=[t_bc])
                S.dma("sp", I("dma_start", out=sh2b[:], in_=G.modrow[l, s_:s_ + 1, 3 * D:4 * D].to_broadcast([128, D])),
                      reads=[G.t["modrow"]], writes=[t_bc], merge=True)
                S.dma("sp", I("dma_start", out=gs2b[:], in_=G.modrow[l, s_:s_ + 1, 4 * D:5 * D].to_broadcast([128, D])),
                      reads=[G.t["modrow"]], writes=[t_bc], merge=True)
                S.dma("sp", I("dma_start", out=fx[0][:], in_=G.norm2_g[l:l + 1, :].to_broadcast([128, D])), writes=[t_fx[0]])
                S.op("dve", I("scalar_tensor_tensor", out=gs2b[:], in0=gs2b[:], scalar=1.0, in1=fx[0][:], op0=ALU.add, op1=ALU.mult),
                     reads=[t_bc, t_fx[0]], writes=[t_bc])
            tok0 = ti * 128
            i = k % NB
            S.dma("sp", I("dma_start", out=yn[i][:], in_=G.ynT[:, tok0:tok0 + 128].rearrange("(j p) n -> p j n", p=128)),
                  reads=[G.t["ynT"]], writes=[t_yn[i]])
            S.dma("sp", I("dma_start", out=xt[i][:], in_=G.xres[tok0:tok0 + 128, :]), reads=[G.t["xres"]], writes=[t_xt[i]])
            for nb in range(4):
                for j in range(16):
                    S.op("pe", I("matmul", acc[nb][:], lhsT=yn[i][:, j, :], rhs=wo[:, j, nb * 512:(nb + 1) * 512], start=(j == 0), stop=(j == 15)),
                         reads=[t_yn[i], t_w], writes=[t_acc[nb]], merge=(j > 0))
                S.op("dve", I("tensor_tensor", out=xn[i][:, nb * 512:(nb + 1) * 512], in0=acc[nb][:], in1=g1b[:, nb * 512:(nb + 1) * 512], op=ALU.mult),
                     reads=[t_acc[nb], t_bc], writes=[t_xn[i]], merge=(nb > 0))
            S.op("pool", I("tensor_tensor", out=xn[i][:], in0=xn[i][:], in1=xt[i][:], op=ALU.add), reads=[t_xn[i], t_xt[i]], writes=[t_xn[i]])
            S.dma("sp", I("dma_start", out=G.xres[tok0:tok0 + 128, :], in_=xn[i][:]), reads=[t_xn[i]], writes=[G.t["xres"]], merge=True)
            S.op("act", I("activation", out=junk[:], in_=xn[i][:], func=AF.Square, accum_out=ss[i][:, 0:1]), reads=[t_xn[i]], writes=[t_junk, t_ss[i]])
            rstd_from_ss(G, ss[i][:, 0:1], ss[i][:, 0:1], D, [t_ss[i]], [t_ss[i]])
            S.op("dve", I("scalar_tensor_tensor", out=fx[i][:], in0=xn[i][:], scalar=ss[i][:, 0:1], in1=gs2b[:], op0=ALU.mult, op1=ALU.mult),
                 reads=[t_xn[i], t_ss[i], t_bc], writes=[t_fx[i]])
            S.op("pool", I("tensor_tensor", out=fx[i][:], in0=fx[i][:], in1=sh2b[:], op=ALU.add), reads=[t_fx[i], t_bc], writes=[t_fx[i]])
            S.op("act", I("copy", out=fxb[i][:], in_=fx[i][:]), reads=[t_fx[i]], writes=[t_fxb[i]])
            S.dma("sp", I("dma_start", out=G.fxd[tok0:tok0 + 128, :], in_=fxb[i][:]), reads=[t_fxb[i]], writes=[G.t["fxd"]], merge=True)

        def part2(k, ti):
            i = k % NB
            for q in range(4):
                ti_ = kt[0] % 2
                kt[0] += 1
                for jj in range(4):
                    j = q * 4 + jj
                    S.op("pe", I("transpose", out=trp[ti_][:, jj * 128:(jj + 1) * 128], in_=fx[i][:, j * 128:(j + 1) * 128], identity=G.ident_f[:]),
                         reads=[t_fx[i], G.t_const], writes=[t_trp[ti_]], merge=(jj > 0))
                if q % 2 == 0:
                    S.op("act", I("copy", out=fxT[i][:, q * 4:(q + 1) * 4, :], in_=trp[ti_][:].rearrange("p (a n) -> p a n", n=128)),
                         reads=[t_trp[ti_]], writes=[t_fxT[i]], merge=(q > 0))
                else:
                    S.op("dve", I("tensor_copy", out=fxT[i][:, q * 4:(q + 1) * 4, :], in_=trp[ti_][:].rearrange("p (a n) -> p a n", n=128)),
                         reads=[t_trp[ti_]], writes=[t_fxT[i]], merge=True)
            for j in range(16):
                S.op("pe", I("matmul", lgp[i][:, 0:NE], lhsT=fxT[i][:, j, :], rhs=wr[:, j, :], start=(j == 0), stop=(j == 15)),
                     reads=[t_fxT[i], t_w], writes=[t_lgp[i]], merge=(j > 0))
            S.op("dve", I("reduce_max", out=sm[i][:, 0:1], in_=lgp[i][:, 0:NE], axis=mybir.AxisListType.X), reads=[t_lgp[i]], writes=[t_sm[i]])
            S.op("dve", I("tensor_scalar", out=sm[i][:, 1:2], in0=sm[i][:, 0:1], scalar1=-1.0, scalar2=None, op0=ALU.mult), reads=[t_sm[i]], writes=[t_sm[i]])
            S.op("act", I("activation", out=ex[i][:], in_=lgp[i][:, 0:NE], func=AF.Exp, bias=sm[i][:, 1:2], accum_out=sm[i][:, 2:3]),
                 reads=[t_lgp[i], t_sm[i]], writes=[t_ex[i], t_sm[i]])
            S.op("dve", I("reciprocal", out=sm[i][:, 3:4], in_=sm[i][:, 2:3]), reads=[t_sm[i]], writes=[t_sm[i]])
            S.op("dve", I("tensor_scalar", out=G.aff_all[:, ti, :], in0=ex[i][:], scalar1=sm[i][:, 3:4], scalar2=None, op0=ALU.mult),
                 reads=[t_ex[i], t_sm[i]], writes=[G.t_aff], merge=True)

        for k in range(len(tiles) + 1):
            if k < len(tiles):
                part1(k, tiles[k])
            if k >= 1:
                part2(k - 1, tiles[k - 1])
        if "affd" in G.debug:
            S.dma("sp", I("dma_start", out=G.affd.rearrange("(t p) e -> p t e", p=128), in_=G.aff_all[:]), reads=[G.t_aff], writes=[G.t["affd"]])
        S.flush(f"outproj{l}")


def phase_route(G, l, do_ctx):
    nc, S = G.nc, G.S
    NI = 24
    with ExitStack() as ps:
        sb, pp = _alloc(G, ps)
        U = sb("rt_U", [128, 128], BF16)
        iot = sb("rt_iota", [128, 512], F32)
        pidx = sb("rt_pidx", [128, 4], F32)
        tgrid = sb("rt_tgrid", [128, 34, NE], F32)
        t_c = Tok()
        S.dma("sp", I("dma_start", out=U[:], in_=G.triu), writes=[t_c], merge=True)
        S.dma("sp", I("dma_start", out=iot[:], in_=G.iota512), writes=[t_c], merge=True)
        S.dma("sp", I("dma_start", out=pidx[:], in_=G.pidx), writes=[t_c], merge=True)
        S.dma("sp", I("dma_start", out=tgrid[:].rearrange("p t e -> p (t e)"), in_=G.tgrid), writes=[t_c], merge=True)
        aff = G.aff_all
        sets = [(0, 32, CAP_L)] + ([(32, 34, CAP_C)] if do_ctx else [])
        R = sb("rt_R", [128, 34, NE, 6], BF16)
        t_R = Tok()
        r1 = sb("rt_r1", [128, 34, NE], F32)
        t_r1 = Tok()
        S.op("pool", I("memset", R[:], 1.0), writes=[t_R])
        S.op("dve", I("tensor_scalar", out=R[:, :, :, 0], in0=tgrid[:], scalar1=0.0, scalar2=pidx[:, 0:1], op0=ALU.mult, op1=ALU.add),
             reads=[t_c], writes=[t_R])
        S.op("dve", I("tensor_copy", out=R[:, :, :, 1], in_=tgrid[:]), reads=[t_c], writes=[t_R])
        S.op("dve", I("tensor_copy", out=R[:, :, :, 2], in_=aff[:]), reads=[G.t_aff], writes=[t_R])
        S.op("dve", I("tensor_tensor", out=r1[:], in0=aff[:], in1=R[:, :, :, 2], op=ALU.subtract), reads=[G.t_aff, t_R], writes=[t_r1])
        S.op("dve", I("tensor_copy", out=R[:, :, :, 3], in_=r1[:]), reads=[t_r1], writes=[t_R])
        S.op("dve", I("tensor_tensor", out=r1[:], in0=r1[:], in1=R[:, :, :, 3], op=ALU.subtract), reads=[t_r1, t_R], writes=[t_r1])
        S.op("dve", I("tensor_copy", out=R[:, :, :, 4], in_=r1[:]), reads=[t_r1], writes=[t_R])
        mids, t_mid, cmps, t_cmp, parts, t_part, cps, t_cps, tmps, t_tmp = [], [], [], [], [], [], [], [], [], []
        for si, (t0, t1, cap) in enumerate(sets):
            mids.append(sb(f"rt_mid{si}", [128, NE], F32)); t_mid.append(Tok())
            cmps.append(sb(f"rt_cmp{si}", [128, t1 - t0, NE], BF16)); t_cmp.append(Tok())
            parts.append(sb(f"rt_part{si}", [128, NE], F32)); t_part.append(Tok())
            cps.append(pp(f"rt_cps{si}", [128, 512])); t_cps.append(Tok())
            tmps.append(sb(f"rt_tmp{si}", [128, NE], F32)); t_tmp.append(Tok())
            S.op("pool", I("memset", mids[si][:], 0.5), writes=[t_mid[si]])
        for k in range(NI):
            wk = 0.5 ** (k + 1)
            wn = 0.5 ** (k + 2) if k < NI - 1 else 0.0
            for si, (t0, t1, cap) in enumerate(sets):
                nt = t1 - t0
                S.op("dve", I("tensor_tensor", out=cmps[si][:], in0=aff[:, t0:t1, :],
                              in1=mids[si][:].unsqueeze(1).to_broadcast([128, nt, NE]), op=ALU.is_gt),
                     reads=[G.t_aff, t_mid[si]], writes=[t_cmp[si]])
                S.op("dve", I("tensor_reduce", out=parts[si][:], in_=cmps[si][:].rearrange("p t e -> p e t"),
                              axis=mybir.AxisListType.X, op=ALU.add),
                     reads=[t_cmp[si]], writes=[t_part[si]])
                S.op("pe", I("matmul", cps[si][:, 0:NE], lhsT=G.ones_f[:], rhs=parts[si][:], start=True, stop=True),
                     reads=[t_part[si], G.t_const], writes=[t_cps[si]])
                S.op("dve", I("tensor_scalar", out=tmps[si][:], in0=cps[si][:, 0:NE], scalar1=cap + 0.5, scalar2=wk, op0=ALU.is_gt, op1=ALU.mult),
                     reads=[t_cps[si]], writes=[t_tmp[si]])
                S.op("dve", I("scalar_tensor_tensor", out=mids[si][:], in0=tmps[si][:], scalar=-wn, in1=mids[si][:], op0=ALU.add, op1=ALU.add),
                     reads=[t_tmp[si], t_mid[si]], writes=[t_mid[si]])
        Mb = sb("rt_Mb", [128, 34, NE], BF16)
        Mf = sb("rt_Mf", [128, 34, NE], F32)
        t_M = Tok()
        if not do_ctx:
            S.op("pool", I("memset", Mb[:, 32:34, :], 0.0), writes=[t_M])
            S.op("pool", I("memset", Mf[:, 32:34, :], 0.0), writes=[t_M], merge=True)
        for si, (t0, t1, cap) in enumerate(sets):
            nt = t1 - t0
            S.op("dve", I("tensor_tensor", out=Mf[:, t0:t1, :], in0=aff[:, t0:t1, :],
                          in1=mids[si][:].unsqueeze(1).to_broadcast([128, nt, NE]), op=ALU.is_gt),
                 reads=[G.t_aff, t_mid[si]], writes=[t_M], merge=True)
        S.op("dve", I("tensor_copy", out=Mb[:, 0:34 if do_ctx else 32, :], in_=Mf[:, 0:34 if do_ctx else 32, :]), reads=[t_M], writes=[t_M])
        posp = [pp(f"rt_pos{i}", [128, 512]) for i in range(2)]
        totp = [pp(f"rt_tot{i}", [128, 512]) for i in range(2)]
        t_pp = Tok()
        spans = [(0, 32)] + ([(32, 34)] if do_ctx else [])
        for i, (t0, t1) in enumerate(spans):
            n = (t1 - t0) * NE
            rhs = Mb[:, t0:t1, :].rearrange("p t e -> p (t e)")
            S.op("pe", I("matmul", posp[i][:, 0:n], lhsT=U[:], rhs=rhs, start=True, stop=True), reads=[t_M, t_c], writes=[t_pp], merge=(i > 0))
            S.op("pe", I("matmul", totp[i][:, 0:n], lhsT=G.ones_b[:], rhs=rhs, start=True, stop=True), reads=[t_M, G.t_const], writes=[t_pp], merge=True)
        incl = sb("rt_incl", [128, 34, NE], F32)
        t_incl = Tok()
        onesf = sb("rt_onesf", [128, 32], F32)
        S.op("pool", I("memset", onesf[:], 1.0), writes=[t_c], merge=True)
        for i, (t0, t1) in enumerate(spans):
            nt = t1 - t0
            tv = totp[i][:, 0:nt * NE].rearrange("p (t e) -> p t e", e=NE)
            for e in range(NE):
                S.op("dve", I("tensor_tensor_scan", out=incl[:, t0:t1, e], data0=onesf[:, 0:nt], data1=tv[:, :, e], initial=0.0,
                              op0=ALU.mult, op1=ALU.add),
                     reads=[t_pp, t_c], writes=[t_incl], merge=(e > 0 or i > 0))
        posf = sb("rt_posf", [128, 34, NE], F32)
        t_posf = Tok()
        for i, (t0, t1) in enumerate(spans):
            nt = t1 - t0
            tv = totp[i][:, 0:nt * NE].rearrange("p (t e) -> p t e", e=NE)
            pv = posp[i][:, 0:nt * NE].rearrange("p (t e) -> p t e", e=NE)
            S.op("dve", I("tensor_tensor", out=incl[:, t0:t1, :], in0=incl[:, t0:t1, :], in1=tv, op=ALU.subtract),
                 reads=[t_incl, t_pp], writes=[t_incl])
            S.op("dve", I("tensor_tensor", out=posf[:, t0:t1, :], in0=incl[:, t0:t1, :], in1=pv, op=ALU.add),
                 reads=[t_incl, t_pp], writes=[t_posf], merge=(i > 0))
        Oall = [sb(f"rt_O{i}", [128, 32, 512], BF16) for i in range(2)]
        t_O = [[Tok() for _ in range(32)] for _ in range(2)]
        Oc = [sb(f"rt_Oc{i}", [128, 2, 128], BF16) for i in range(2)]
        t_Oc = [Tok(), Tok()]
        ips = [pp(f"rt_ips{i}", [128, 512]) for i in range(2)]
        t_ips = [Tok(), Tok()]
        isb = [sb(f"rt_isb{i}", [128, 5, 8], F32) for i in range(2)]
        t_isb = [Tok(), Tok()]
        ia = [sb(f"rt_ia{i}", [128, 5], F32) for i in range(2)]
        t_ia = [Tok(), Tok()]
        nch = 5 if do_ctx else 4
        import os
        LVE = int(os.environ.get("RT_EXP", "16"))
        for e in range(LVE):
            b = e % 2
            for t in range(32):
                S.op("dve", I("tensor_scalar", out=Oall[b][:, t, :], in0=iot[:], scalar1=posf[:, t, e:e + 1], scalar2=Mf[:, t, e:e + 1],
                              op0=ALU.is_equal, op1=ALU.mult),
                     reads=[t_c, t_posf, t_M], writes=[t_O[b][t]])
            first = True
            for sc in range(4):
                for t in range(32):
                    S.op("pe", I("matmul", ips[b][:, sc * 8:sc * 8 + 6], lhsT=Oall[b][:, t, sc * 128:(sc + 1) * 128], rhs=R[:, t, e, :],
                                 start=(t == 0), stop=(t == 31)),
                         reads=[t_O[b][t], t_R], writes=[t_ips[b]], merge=not first)
                    first = False
            if do_ctx:
                for t in range(2):
                    S.op("dve", I("tensor_scalar", out=Oc[b][:, t, :], in0=iot[:, 0:128], scalar1=posf[:, 32 + t, e:e + 1],
                                  scalar2=Mf[:, 32 + t, e:e + 1], op0=ALU.is_equal, op1=ALU.mult),
                         reads=[t_c, t_posf, t_M], writes=[t_Oc[b]], merge=(t > 0))
                for t in range(2):
                    S.op("pe", I("matmul", ips[b][:, 32:38], lhsT=Oc[b][:, t, :], rhs=R[:, 32 + t, e, :], start=(t == 0), stop=(t == 1)),
                         reads=[t_Oc[b], t_R], writes=[t_ips[b]], merge=True)
            S.op("act", I("copy", out=isb[b][:, 0:nch, 0:6], in_=ips[b][:, 0:nch * 8].rearrange("p (c k) -> p c k", k=8)[:, :, 0:6]),
                 reads=[t_ips[b]], writes=[t_isb[b]])
            v = isb[b]
            S.op("dve", I("scalar_tensor_tensor", out=ia[b][:, 0:nch], in0=v[:, 0:nch, 1], scalar=128.0, in1=v[:, 0:nch, 0], op0=ALU.mult, op1=ALU.add),
                 reads=[t_isb[b]], writes=[t_ia[b]])
            S.op("dve", I("scalar_tensor_tensor", out=ia[b][:, 0:nch], in0=v[:, 0:nch, 5], scalar=pidx[:, 2:3], in1=ia[b][:, 0:nch], op0=ALU.mult, op1=ALU.add),
                 reads=[t_isb[b], t_ia[b], t_c], writes=[t_ia[b]])
            S.op("dve", I("tensor_scalar", out=ia[b][:, 0:nch], in0=ia[b][:, 0:nch], scalar1=pidx[:, 1:2], scalar2=None, op0=ALU.add),
                 reads=[t_ia[b], t_c], writes=[t_ia[b]])
            S.op("dve", I("tensor_copy", out=G.idx_all[:, e, 0:nch], in_=ia[b][:, 0:nch]), reads=[t_ia[b]], writes=[G.t_idx], merge=(e > 0))
            S.op("pool", I("tensor_tensor", out=G.g_all[:, e, 0:nch], in0=v[:, 0:nch, 2], in1=v[:, 0:nch, 3], op=ALU.add),
                 reads=[t_isb[b]], writes=[G.t_idx], merge=True)
            S.op("pool", I("tensor_tensor", out=G.g_all[:, e, 0:nch], in0=G.g_all[:, e, 0:nch], in1=v[:, 0:nch, 4], op=ALU.add),
                 reads=[t_isb[b], G.t_idx], writes=[G.t_idx], merge=True)
        if "idxd" in G.debug:
            S.dma("sp", I("dma_start", out=G.idxd, in_=G.idx_all[:].rearrange("p e c -> p (e c)")), reads=[G.t_idx], writes=[G.t["idxd"]])
            S.dma("sp", I("dma_start", out=G.gd, in_=G.g_all[:].rearrange("p e c -> p (e c)")), reads=[G.t_idx], writes=[G.t["gd"]])
        S.flush(f"route{l}")


def phase_moe(G, l, do_ctx):
    nc, S = G.nc, G.S
    with ExitStack() as ps:
        sb, pp = _alloc(G, ps)
        nch = 5 if do_ctx else 4
        NS = 544 if do_ctx else 512
        FB = 256
        ns = 2 if do_ctx else 1
        g2b = []
        t_bc = Tok()
        for s_ in range(ns):
            a = sb(f"mo_g2b{s_}", [128, D], F32)
            S.dma("sp", I("dma_start", out=a[:], in_=G.modrow[l, s_:s_ + 1, 5 * D:6 * D].to_broadcast([128, D])),
                  reads=[G.t["modrow"]], writes=[t_bc], merge=True)
            g2b.append(a)
        xs = [sb(f"mo_xs{i}", [128, D], BF16) for i in range(2)]
        t_xs = [Tok(), Tok()]
        xsT = sb("mo_xsT", [128, 16, NS], BF16)
        t_xsT = Tok()
        wg = [sb(f"mo_wg{i}", [128, 16, FB], BF16) for i in range(2)]
        wu = [sb(f"mo_wu{i}", [128, 16, FB], BF16) for i in range(2)]
        t_wgu = [Tok(), Tok()]
        wd = [sb(f"mo_wd{i}", [128, 16, 512], BF16) for i in range(2)]
        t_wd = [Tok(), Tok()]
        hm = sb("mo_hm", [128, 16, NS], BF16)
        t_hm = [Tok() for _ in range(16)]
        st = [sb(f"mo_st{i}", [128, 544], F32) for i in range(2)]
        t_st = [Tok(), Tok()]
        yst = [sb(f"mo_y{i}", [128, D], F32) for i in range(nch)]
        t_yst = [Tok() for _ in range(nch)]
        if do_ctx:
            S.op("pool", I("memset", yst[4][:], 0.0), writes=[t_yst[4]])
        aps = [pp(f"mo_a{i}", [128, 512]) for i in range(2)]
        ups = [pp(f"mo_u{i}", [128, 512]) for i in range(2)]
        t_aps = [Tok(), Tok()]
        t_ups = [Tok(), Tok()]
        cps = pp("mo_c", [128, 512])
        t_cps = Tok()
        yps = [pp(f"mo_yp{i}", [128, 512]) for i in range(2)]
        t_yps = [Tok(), Tok()]
        trp = pp("mo_tr", [128, 1024], BF16)
        t_trp = Tok()
        import os
        LVE = int(os.environ.get("MOE_EXP", "16"))
        jobs = []
        for e in range(LVE):
            for fb in range(D // FB):
                jobs.append(("gu", e, fb))
            for db in range(4):
                jobs.append(("d", e, db))
        cnt = {"gu": 0, "d": 0}
        slot_of = {}

        def issue(jb):
            kind, e, b = jb
            i = cnt[kind] % 2
            cnt[kind] += 1
            slot_of[jb] = i
            if kind == "gu":
                S.dma("pool", I("dma_start", out=wg[i][:].rearrange("p j n -> p (j n)").rearrange("p (a m) -> p a m", m=2048),
                                in_=G.w_gate[l, e, b].rearrange("p (a m) -> p a m", m=2048)),
                      writes=[t_wgu[i]])
                S.dma("pool", I("dma_start", out=wu[i][:].rearrange("p j n -> p (j n)").rearrange("p (a m) -> p a m", m=2048),
                                in_=G.w_up[l, e, b].rearrange("p (a m) -> p a m", m=2048)),
                      writes=[t_wgu[i]], merge=True)
            else:
                S.dma("pool", I("dma_start", out=wd[i][:].rearrange("p j n -> p (j n)").rearrange("p (a m) -> p a m", m=2048),
                                in_=G.w_down[l, e, b].rearrange("p (a m) -> p a m", m=2048)),
                      writes=[t_wd[i]])

        def gather(e):
            for sc in range(nch):
                i = sc % 2
                m = 128 if sc < 4 else 32
                S.dma("pool", I("indirect_dma_start", out=xs[i][:], out_offset=None, in_=G.fxd[:, :],
                                in_offset=bass.IndirectOffsetOnAxis(ap=G.idx_all[:, e, sc:sc + 1], axis=0)),
                      reads=[G.t_idx, G.t["fxd"]], writes=[t_xs[i]])
                for q in range(2):
                    for jj in range(8):
                        j = q * 8 + jj
                        S.op("pe", I("transpose", out=trp[:, jj * 128:jj * 128 + m], in_=xs[i][0:m, j * 128:(j + 1) * 128],
                                     identity=G.ident_b[0:m, 0:m]),
                             reads=[t_xs[i], G.t_const], writes=[t_trp], merge=(jj > 0))
                    src = trp[:].rearrange("p (a n) -> p a n", n=128)[:, :, 0:m]
                    dst = xsT[:, q * 8:(q + 1) * 8, sc * 128:sc * 128 + m]
                    if q == 0:
                        S.op("act", I("copy", out=dst, in_=src), reads=[t_trp], writes=[t_xsT], merge=not (sc == 0))
                    else:
                        S.op("dve", I("tensor_copy", out=dst, in_=src), reads=[t_trp], writes=[t_xsT], merge=True)

        ji = 0
        issue(jobs[0])
        if LVE > 0:
            gather(0)
        kk = 0
        for e in range(LVE):
            for fb in range(D // FB):
                jb = jobs[ji]
                ji += 1
                if ji < len(jobs):
                    issue(jobs[ji])
                wi = slot_of[jb]
                for fl in range(FB // 128):
                    fo = fb * (FB // 128) + fl
                    ab = kk % 2
                    kk += 1
                    for j in range(16):
                        S.op("pe", I("matmul", aps[ab][:], lhsT=wg[wi][:, j, fl * 128:(fl + 1) * 128], rhs=xsT[:, j, 0:512],
                                     start=(j == 0), stop=(j == 15)),
                             reads=[t_wgu[wi], t_xsT], writes=[t_aps[ab]], merge=(j > 0))
                    for j in range(16):
                        S.op("pe", I("matmul", ups[ab][:], lhsT=wu[wi][:, j, fl * 128:(fl + 1) * 128], rhs=xsT[:, j, 0:512],
                                     start=(j == 0), stop=(j == 15)),
                             reads=[t_wgu[wi], t_xsT], writes=[t_ups[ab]], merge=(j > 0))
                    if do_ctx:
                        for j in range(16):
                            S.op("pe", I("matmul", cps[:, 0:32], lhsT=wg[wi][:, j, fl * 128:(fl + 1) * 128], rhs=xsT[:, j, 512:544],
                                         start=(j == 0), stop=(j == 15)),
                                 reads=[t_wgu[wi], t_xsT], writes=[t_cps], merge=(j > 0))
                        for j in range(16):
                            S.op("pe", I("matmul", cps[:, 32:64], lhsT=wu[wi][:, j, fl * 128:(fl + 1) * 128], rhs=xsT[:, j, 512:544],
                                         start=(j == 0), stop=(j == 15)),
                                 reads=[t_wgu[wi], t_xsT], writes=[t_cps], merge=True)
                    S.op("act", I("activation", out=st[ab][:, 0:512], in_=aps[ab][:], func=AF.Silu), reads=[t_aps[ab]], writes=[t_st[ab]])
                    S.op("dve", I("tensor_tensor", out=hm[:, fo, 0:512], in0=ups[ab][:], in1=st[ab][:, 0:512], op=ALU.mult),
                         reads=[t_ups[ab], t_st[ab]], writes=[t_hm[fo]])
                    if do_ctx:
                        S.op("act", I("activation", out=st[ab][:, 512:544], in_=cps[:, 0:32], func=AF.Silu), reads=[t_cps], writes=[t_st[ab]], merge=True)
                        S.op("dve", I("tensor_tensor", out=hm[:, fo, 512:544], in0=cps[:, 32:64], in1=st[ab][:, 512:544], op=ALU.mult),
                             reads=[t_cps, t_st[ab]], writes=[t_hm[fo]], merge=True)
            if e + 1 < LVE:
                gather(e + 1)
            for db in range(4):
                jb = jobs[ji]
                ji += 1
                if ji < len(jobs):
                    issue(jobs[ji])
                wi = slot_of[jb]
                for sc in range(nch):
                    m = 128 if sc < 4 else 32
                    s_ = 0 if sc < 4 else 1
                    yi = kk % 2
                    kk += 1
                    for fo in range(16):
                        S.op("pe", I("matmul", yps[yi][0:m, :], lhsT=hm[:, fo, sc * 128:sc * 128 + m], rhs=wd[wi][:, fo, :],
                                     start=(fo == 0), stop=(fo == 15)),
                             reads=[t_hm[fo], t_wd[wi]], writes=[t_yps[yi]], merge=(fo > 0))
                    S.op("dve", I("scalar_tensor_tensor", out=yst[sc][0:m, db * 512:(db + 1) * 512], in0=yps[yi][0:m, :],
                                  scalar=G.g_all[0:m, e, sc:sc + 1], in1=g2b[s_][0:m, db * 512:(db + 1) * 512], op0=ALU.mult, op1=ALU.mult),
                         reads=[t_yps[yi], G.t_idx, t_bc], writes=[t_yst[sc]], merge=(db > 0))
            for sc in range(nch):
                S.dma("pool", I("indirect_dma_start", out=G.xres[:, :],
                                out_offset=bass.IndirectOffsetOnAxis(ap=G.idx_all[:, e, sc:sc + 1], axis=0),
                                in_=yst[sc][:], in_offset=None, bounds_check=NPAD - 1, oob_is_err=True, compute_op=ALU.add),
                      reads=[t_yst[sc], G.t_idx, G.t["xres"]], writes=[G.t["xres"]])
        S.flush(f"moe{l}")


def phase_final(G):
    nc, S = G.nc, G.S
    with ExitStack() as ps:
        sb, pp = _alloc(G, ps)
        fg = sb("fi_g", [128, D], F32)
        t_fg = Tok()
        S.dma("sp", I("dma_start", out=fg[:], in_=G.final_g.rearrange("(o d) -> o d", o=1).to_broadcast([128, D])), writes=[t_fg])
        xt = [sb(f"fi_x{i}", [128, D], F32) for i in range(2)]
        t_xt = [Tok(), Tok()]
        ot = [sb(f"fi_o{i}", [128, D], F32) for i in range(2)]
        t_ot = [Tok(), Tok()]
        junk = sb("fi_junk", [128, D], BF16)
        t_junk = Tok()
        ss = [sb(f"fi_ss{i}", [128, 1], F32) for i in range(2)]
        t_ss = [Tok(), Tok()]
        for ti in range(32):
            i = ti % 2
            S.dma("sp", I("dma_start", out=xt[i][:], in_=G.xres[ti * 128:(ti + 1) * 128, :]), reads=[G.t["xres"]], writes=[t_xt[i]])
            S.op("act", I("activation", out=junk[:], in_=xt[i][:], func=AF.Square, accum_out=ss[i][:, 0:1]), reads=[t_xt[i]], writes=[t_junk, t_ss[i]])
            rstd_from_ss(G, ss[i][:, 0:1], ss[i][:, 0:1], D, [t_ss[i]], [t_ss[i]])
            S.op("dve", I("scalar_tensor_tensor", out=ot[i][:], in0=xt[i][:], scalar=ss[i][:, 0:1], in1=fg[:], op0=ALU.mult, op1=ALU.mult),
                 reads=[t_xt[i], t_ss[i], t_fg], writes=[t_ot[i]])
            S.dma("sp", I("dma_start", out=G.out[ti * 128:(ti + 1) * 128, :], in_=ot[i][:]), reads=[t_ot[i]], writes=[G.t["out"]], merge=True)
        S.flush("final")


_CONSTS = None


def make_in_maps(inputs, batches, names=None):
    global _CONSTS
    if _CONSTS is None:
        _CONSTS = host_consts()
    w = host_weights(inputs)
    maps = []
    for b in batches:
        m = dict(w)
        m.update(_CONSTS)
        xin = np.zeros((NPAD, D), np.float32)
        xin[:NL] = inputs["x"][b]
        xin[NL:NT] = inputs["ctx"][b]
        m["xin"] = xin
        m["cvec"] = np.ascontiguousarray(np.stack([inputs["c"][b], inputs["c_ctx"]], axis=1)).astype(np.float32)
        if names is not None:
            m = {k: m[k] for k in names}
        maps.append(m)
    return maps


_PROG = None


def kernel(**inputs):
    global _PROG
    inputs = {k: np.asarray(v) for k, v in inputs.items()}
    if _PROG is None:
        _PROG = build_program()
    nc, G = _PROG
    B = inputs["x"].shape[0]
    maps = make_in_maps(inputs, list(range(B)), G.in_names)
    res = run_bass_kernel_spmd(nc, maps, core_ids=list(range(B)))
    out = np.stack([np.asarray(r["out"]) for r in res.results], axis=0)
    return out.astype(np.float32)
```

```python
import numpy as np
import ml_dtypes
from contextlib import ExitStack
import concourse.bass as bass
import concourse.mybir as mybir
from concourse.bass_utils import run_bass_kernel_spmd

F32 = mybir.dt.float32
BF16 = mybir.dt.bfloat16
I32 = mybir.dt.int32
AF = mybir.ActivationFunctionType
ALU = mybir.AluOpType
NPBF = ml_dtypes.bfloat16

D = 2048
NL = 4096
NCX = 256
NT = NL + NCX
NPAD = NT + 128
DEPTH = 2
EPS = 1e-6
NE = 16
CAP_L = 512
CAP_C = 32

WA_COLS = 3456
OFF_CB, OFF_CC, OFF_CH = 0, 512, 1024
OFF_SQ, OFF_SQW = 1536, 2048
OFF_SK, OFF_SKW = 2560, 2688
OFF_SV = 2816
OFF_FU = 2944
WB_COLS = 960
OFF_CQ, OFF_CKV, OFF_KPA, OFF_KPB = 0, 512, 768, 864

SEM_ROLL = 30000
NDMA_SEMS = 24


def I(name, *args, **kwargs):
    return (name, args, kwargs)


class Tok:
    __slots__ = ("w", "r", "base")

    def __init__(self):
        self.w = {}
        self.r = {}
        self.base = {}


class Sched:
    ENGS = ("pe", "act", "dve", "pool", "sp")

    def __init__(self, nc, stack):
        self.nc = nc
        self.stack = stack
        self.sems = []
        self.prog = {e: [] for e in self.ENGS}
        self.cur_sem = {}
        self.cnt = {}
        for e in ("pe", "act", "dve", "pool"):
            self.cur_sem[e] = self._new_sem(e)
            self.cnt[e] = 0
        self.dma_sems = {q: [self._new_sem("dma" + q) for _ in range(NDMA_SEMS)] for q in ("sp", "pool", "act")}
        self.dma_cnt = {q: [0] * NDMA_SEMS for q in ("sp", "pool", "act")}
        self.dma_rr = {"sp": 0, "pool": 0, "act": 0}
        self.waited = {e: {} for e in self.ENGS}
        self.n_inst = 0
        self.regcache = {}
        self.marks = []
        self.tot_pe = 0

    def _new_sem(self, name):
        h = self.stack.enter_context(self.nc.semaphore(f"s_{name}_{len(self.sems)}"))
        self.sems.append(h)
        return len(self.sems) - 1

    def _collect(self, eng, reads, writes, merge):
        need = {}
        for t in reads:
            for s, v in t.w.items():
                if need.get(s, 0) < v:
                    need[s] = v
        for t in writes:
            src = (t.base, t.r) if merge else (t.w, t.r)
            for dct in src:
                for s, v in dct.items():
                    if need.get(s, 0) < v:
                        need[s] = v
        out = []
        wd = self.waited[eng]
        for s, v in need.items():
            if eng == "pe" and s == self.cur_sem["pe"]:
                continue
            if wd.get(s, 0) >= v:
                continue
            wd[s] = v
            out.append((s, v))
        return out

    def _post(self, ev, reads, writes, merge):
        s, v = ev
        for t in reads:
            if t.r.get(s, 0) < v:
                t.r[s] = v
        for t in writes:
            if merge:
                if t.r:
                    nb = dict(t.base)
                    for rs, rv in t.r.items():
                        if nb.get(rs, 0) < rv:
                            nb[rs] = rv
                    t.base = nb
                if t.w.get(s, 0) < v:
                    t.w[s] = v
            else:
                nb = dict(t.w)
                for rs, rv in t.r.items():
                    if nb.get(rs, 0) < rv:
                        nb[rs] = rv
                t.base = nb
                t.w = {s: v}
            t.r = {}

    def op(self, eng, fn, reads=(), writes=(), merge=False):
        waits = self._collect(eng, reads, writes, merge)
        if self.cnt[eng] >= SEM_ROLL:
            self.cur_sem[eng] = self._new_sem(eng)
            self.cnt[eng] = 0
        self.cnt[eng] += 1
        if eng == "pe":
            self.tot_pe += 1
        s = self.cur_sem[eng]
        v = self.cnt[eng]
        sems = self.sems

        def emit(h, waits=waits, s=s, fn=fn):
            for ws, wv in waits:
                h.wait_ge(sems[ws], wv)
            getattr(h, fn[0])(*fn[1], **fn[2]).then_inc(sems[s], 1)

        self.prog[eng].append(emit)
        self._post((s, v), reads, writes, merge)
        self.n_inst += 1

    def dma(self, eng, fn, reads=(), writes=(), merge=False):
        i = self.dma_rr[eng]
        self.dma_rr[eng] = (i + 1) % NDMA_SEMS
        s = self.dma_sems[eng][i]
        waits = self._collect(eng, reads, writes, merge)
        prev = self.dma_cnt[eng][i]
        if prev > 0 and self.waited[eng].get(s, 0) < prev:
            self.waited[eng][s] = prev
            waits.append((s, prev))
        self.dma_cnt[eng][i] += 16
        v = self.dma_cnt[eng][i]
        sems = self.sems

        def emit(h, waits=waits, s=s, fn=fn):
            for ws, wv in waits:
                h.wait_ge(sems[ws], wv)
            try:
                kw = fn[2]
                if isinstance(kw.get("bounds_check"), int):
                    kw = dict(kw)
                    key = (id(h), kw["bounds_check"])
                    if key not in self.regcache:
                        self.regcache[key] = h.to_reg(kw["bounds_check"])
                    kw["bounds_check"] = self.regcache[key]
                ins = getattr(h, fn[0])(*fn[1], **kw)
            except Exception:
                print("DMA FAIL", fn[0], {k: (getattr(v, "shape", v), getattr(v, "ap", None)) for k, v in fn[2].items()})
                raise
            ins.then_inc(sems[s], 16)

        self.prog[eng].append(emit)
        self._post((s, v), reads, writes, merge)
        self.n_inst += 1

    def wait_all(self, eng, toks):
        waits = self._collect(eng, toks, (), False)
        sems = self.sems

        def emit(h, waits=waits):
            for ws, wv in waits:
                h.wait_ge(sems[ws], wv)

        self.prog[eng].append(emit)

    def barrier(self):
        cur = {}
        for e in ("pe", "act", "dve", "pool"):
            if self.cnt[e] > 0:
                cur[self.cur_sem[e]] = self.cnt[e]
        for q in self.dma_sems:
            for i, s in enumerate(self.dma_sems[q]):
                if self.dma_cnt[q][i] > 0:
                    cur[s] = self.dma_cnt[q][i]
        sems = self.sems
        for eng in self.ENGS:
            wd = self.waited[eng]
            waits = []
            for s, v in cur.items():
                if wd.get(s, 0) < v:
                    wd[s] = v
                    waits.append((s, v))

            def emit(h, waits=waits):
                for ws, wv in waits:
                    h.wait_ge(sems[ws], wv)

            self.prog[eng].append(emit)

    def flush(self, name=None):
        self.barrier()
        self.regcache = {}
        self.marks.append((name, self.tot_pe))
        nc = self.nc
        prog = self.prog
        with nc.Block(name) as block:
            if prog["sp"]:
                @block.sync
                def _(h):
                    for f in prog["sp"]:
                        f(h)
            if prog["pe"]:
                @block.tensor
                def _(h):
                    for f in prog["pe"]:
                        f(h)
            if prog["act"]:
                @block.scalar
                def _(h):
                    for f in prog["act"]:
                        f(h)
            if prog["dve"]:
                @block.vector
                def _(h):
                    for f in prog["dve"]:
                        f(h)
            if prog["pool"]:
                @block.gpsimd
                def _(h):
                    for f in prog["pool"]:
                        f(h)
        self.prog = {e: [] for e in self.ENGS}


def _rope_tables(rot_dim):
    rows = np.repeat(np.arange(64, dtype=np.float32), 64)
    cols = np.tile(np.arange(64, dtype=np.float32), 64)
    n_freq = rot_dim // 4
    inv = (np.float32(10000.0) ** (-np.arange(n_freq, dtype=np.float32) / np.float32(n_freq))).astype(np.float32)
    ang = np.concatenate([rows[:, None] * inv, cols[:, None] * inv], axis=-1).astype(np.float32)
    return np.cos(ang).astype(np.float32), np.sin(ang).astype(np.float32)


def host_consts():
    c = {}
    cs, sn = _rope_tables(64)
    cos2 = np.ones((128, NT), np.float32)
    sin2 = np.zeros((128, NT), np.float32)
    for hh in range(2):
        cos2[hh * 64:hh * 64 + 32, :NL] = cs.T
        cos2[hh * 64 + 32:hh * 64 + 64, :NL] = cs.T
        sin2[hh * 64:hh * 64 + 32, :NL] = -sn.T
        sin2[hh * 64 + 32:hh * 64 + 64, :NL] = sn.T
    c["ropeS"] = np.stack([cos2, sin2], 0)
    cm, sm = _rope_tables(32)
    cosm = np.ones((96, NT), np.float32)
    sinm = np.zeros((96, NT), np.float32)
    cosm[64:80, :NL] = cm.T
    cosm[80:96, :NL] = cm.T
    sinm[64:80, :NL] = -sm.T
    sinm[80:96, :NL] = sm.T
    c["ropeM"] = np.stack([cosm, sinm], 0)
    k = np.arange(64)
    a = 2 * np.pi * np.outer(k, k) / 64.0
    c64 = np.zeros((2, 128, 128), np.float64)
    for g in range(2):
        c64[0, g * 64:(g + 1) * 64, g * 64:(g + 1) * 64] = np.cos(a) / 8.0
        c64[1, g * 64:(g + 1) * 64, g * 64:(g + 1) * 64] = np.sin(a) / 8.0
    c["dft64"] = c64.astype(NPBF)
    def pos_tables(n):
        nt = n // 128
        idx = np.arange(n, dtype=np.int64)
        m = (np.outer(idx, idx) % n).astype(np.float64)
        ang = 2 * np.pi * m / n
        cn = np.cos(ang) / np.sqrt(n)
        sn_ = -np.sin(ang) / np.sqrt(n)
        out = np.zeros((nt, 128, 2, nt, 128), np.float32)
        for kt in range(nt):
            blkc = cn[:, kt * 128:(kt + 1) * 128].reshape(nt, 128, 128)
            blks = sn_[:, kt * 128:(kt + 1) * 128].reshape(nt, 128, 128)
            out[kt, :, 0] = blkc.transpose(1, 0, 2)
            out[kt, :, 1] = blks.transpose(1, 0, 2)
        return out.reshape(nt, 128, 2 * nt * 128).astype(NPBF)
    c["dftL"] = pos_tables(NL)
    c["dftC"] = pos_tables(NCX)
    ident = np.eye(128, dtype=np.float32)
    c["identf"] = ident
    kk = np.arange(128)[:, None]
    qq = np.arange(128)[None, :]
    mprev = (kk >= qq).astype(np.float32)
    mnext = (kk <= qq).astype(np.float32)
    c["masks"] = np.stack([np.tile(mprev, (1, 4)), np.tile(mnext, (1, 4))], 0).astype(NPBF)
    c["triu"] = (kk < qq).astype(NPBF)
    c["iota512"] = np.tile(np.arange(512, dtype=np.float32)[None, :], (128, 1))
    pp_ = np.arange(128, dtype=np.float32)
    c["pidx"] = np.stack([pp_, NT + pp_, -(NT + pp_), np.zeros(128, np.float32)], 1).astype(np.float32)
    c["tgrid"] = np.tile(np.repeat(np.arange(34, dtype=np.float32), NE)[None, :], (128, 1))
    return c


def host_weights(inp):
    w = {}
    w_in = inp["w_in"]
    L = w_in.shape[0]
    wa = np.zeros((L, D, WA_COLS), np.float32)
    wa[:, :, 0:1536] = w_in[:, :, 0:1536]
    sq = w_in[:, :, 1536:2048]
    wa[:, :, OFF_SQ:OFF_SQ + 512] = sq
    sq4 = sq.reshape(L, D, 8, 2, 32)
    wa[:, :, OFF_SQW:OFF_SQW + 512] = sq4[:, :, :, ::-1, :].reshape(L, D, 512)
    sk = w_in[:, :, 2048:2176]
    wa[:, :, OFF_SK:OFF_SK + 128] = sk
    wa[:, :, OFF_SKW:OFF_SKW + 128] = sk.reshape(L, D, 2, 2, 32)[:, :, :, ::-1, :].reshape(L, D, 128)
    wa[:, :, OFF_SV:OFF_SV + 128] = w_in[:, :, 2176:2304]
    wa[:, :, OFF_FU:OFF_FU + 512] = w_in[:, :, 2304:2816]
    w["w_a"] = wa
    wb = np.zeros((L, D, WB_COLS), np.float32)
    wb[:, :, 0:768] = w_in[:, :, 2816:3584]
    kpe = w_in[:, :, 3584:3616]
    wb[:, :, OFF_KPA + 64:OFF_KPA + 96] = kpe
    wb[:, :, OFF_KPB + 64:OFF_KPB + 96] = kpe.reshape(L, D, 2, 16)[:, :, ::-1, :].reshape(L, D, 32)
    w["w_b"] = wb
    uq = inp["mla_w_uq"]
    uq4 = uq.reshape(L, 512, 8, 96)
    uqb = uq4.copy()
    uqb[:, :, :, 64:80] = uq4[:, :, :, 80:96]
    uqb[:, :, :, 80:96] = uq4[:, :, :, 64:80]
    w["w_uq"] = np.stack([uq, uqb.reshape(L, 512, 768)], 1)
    ukv = inp["mla_w_ukv"].reshape(L, 256, 8, 2, 64)
    w["w_uk"] = np.ascontiguousarray(ukv[:, :, :, 0, :]).reshape(L, 256, 512)
    w["w_uv"] = np.ascontiguousarray(ukv[:, :, :, 1, :]).reshape(L, 256, 512)
    for k_ in ("ada_b", "norm1_g", "norm2_g", "conv_w", "swa_sink", "mla_q_norm_g",
               "mla_kv_norm_g", "out_norm_g", "w_out", "w_router", "final_norm_g"):
        w[k_] = inp[k_]
    def tile_cols(a, W):
        lead = a.shape[:-2]
        K, N = a.shape[-2:]
        a6 = a.reshape(lead + (K // 128, 128, N // W, W))
        nd = len(lead)
        perm = tuple(range(nd)) + (nd + 2, nd + 1, nd + 0, nd + 3)
        return np.ascontiguousarray(a6.transpose(perm)).reshape(lead + (N // W, 128, (K // 128) * W))
    w["ada_w"] = tile_cols(inp["ada_w"], 512)
    if "w_gate" in inp:
        w["w_gate"] = tile_cols(inp["w_gate"], 256)
        w["w_up"] = tile_cols(inp["w_up"], 256)
        w["w_down"] = tile_cols(inp["w_down"], 512)
    return w


class Ctx:
    pass


def build_program(debug=(), n_layers=DEPTH, stop_after=None):
    nc = bass.Bass("TRN2", target_bir_lowering=False)
    G = Ctx()
    G.nc = nc
    G.debug = set(debug)
    din = {}

    def inp(name, shape, dt=F32):
        din[name] = nc.dram_tensor(name, list(shape), dt, kind="ExternalInput").ap()
        return din[name]

    G.xin = inp("xin", [NPAD, D])
    G.cvec = inp("cvec", [D, 2])
    G.ada_w = inp("ada_w", [DEPTH, 24, 128, 16 * 512])
    G.ada_b = inp("ada_b", [DEPTH, 6 * D])
    G.norm1_g = inp("norm1_g", [DEPTH, D])
    G.norm2_g = inp("norm2_g", [DEPTH, D])
    G.w_a = inp("w_a", [DEPTH, D, WA_COLS])
    G.w_b = inp("w_b", [DEPTH, D, WB_COLS])
    G.conv_w = inp("conv_w", [DEPTH, 3, 512])
    G.swa_sink = inp("swa_sink", [DEPTH, 8])
    G.q_norm_g = inp("mla_q_norm_g", [DEPTH, 512])
    G.kv_norm_g = inp("mla_kv_norm_g", [DEPTH, 256])
    G.w_uq = inp("w_uq", [DEPTH, 2, 512, 768])
    G.w_uk = inp("w_uk", [DEPTH, 256, 512])
    G.w_uv = inp("w_uv", [DEPTH, 256, 512])
    G.out_norm_g = inp("out_norm_g", [DEPTH, D])
    G.w_out = inp("w_out", [DEPTH, D, D])
    G.w_router = inp("w_router", [DEPTH, D, NE])
    if stop_after is None or stop_after in ("moe", "moetest"):
        G.w_gate = inp("w_gate", [DEPTH, NE, 8, 128, 16 * 256])
        G.w_up = inp("w_up", [DEPTH, NE, 8, 128, 16 * 256])
        G.w_down = inp("w_down", [DEPTH, NE, 4, 128, 16 * 512])
    G.final_g = inp("final_norm_g", [D])
    G.ropeS = inp("ropeS", [2, 128, NT])
    G.ropeM = inp("ropeM", [2, 96, NT])
    G.dft64 = inp("dft64", [2, 128, 128], BF16)
    G.dftL = inp("dftL", [32, 128, 2 * 32 * 128], BF16)
    G.dftC = inp("dftC", [2, 128, 2 * 2 * 128], BF16)
    G.identf = inp("identf", [128, 128])
    G.masks = inp("masks", [2, 128, 512], BF16)
    G.triu = inp("triu", [128, 128], BF16)
    G.iota512 = inp("iota512", [128, 512])
    G.pidx = inp("pidx", [128, 4])
    G.tgrid = inp("tgrid", [128, 34 * NE])

    G.out = nc.dram_tensor("out", [NL, D], F32, kind="ExternalOutput").ap()

    def scratch(name, shape, dt):
        kind = "ExternalOutput" if name in G.debug else "Internal"
        return nc.dram_tensor(name, list(shape), dt, kind=kind).ap()

    G.xres = scratch("xres", [NPAD, D], F32)
    G.hT = scratch("hT", [D, NT], BF16)
    G.cbT = scratch("cbT", [512, NT], F32)
    G.uT = scratch("uT", [512, NT], F32)
    G.sqT = scratch("sqT", [512, NT], BF16)
    G.skT = scratch("skT", [128, NT], BF16)
    G.svd = scratch("svd", [NT, 128], BF16)
    G.ucd = scratch("ucd", [NT, 512], BF16)
    G.usd = scratch("usd", [NT, 512], BF16)
    G.mqT = scratch("mqT", [8, 96, NT], BF16)
    G.mkT = scratch("mkT", [8, 96, NT], BF16)
    G.mvd = scratch("mvd", [NT, 512], BF16)
    G.ynT = scratch("ynT", [D, NT], BF16)
    G.fxd = scratch("fxd", [NPAD, D], BF16)
    G.modrow = scratch("modrow", [DEPTH, 2, 6 * D], F32)
    G.affd = scratch("affd", [NT, NE], F32)
    if "idxd" in G.debug:
        G.idxd = nc.dram_tensor("idxd", [128, NE * 5], I32, kind="ExternalOutput").ap()
        G.gd = nc.dram_tensor("gd", [128, NE * 5], F32, kind="ExternalOutput").ap()
    if stop_after == "moetest":
        G.aff_in = inp("aff_in", [NT, NE])
        G.fx_in = inp("fx_in", [NPAD, D], BF16)

    with ExitStack() as st:
        S = Sched(nc, st)
        G.S = S
        G.st = st
        G.t = {k: Tok() for k in ("xres", "hT", "cbT", "uT", "sqT", "skT", "svd", "ucd", "usd", "mqT",
                                  "mkT", "mvd", "ynT", "fxd", "out", "modrow", "affd", "idxd", "gd")}
        G.ident_f = st.enter_context(nc.sbuf_tensor("ident_f", [128, 128], F32))
        G.ident_b = st.enter_context(nc.sbuf_tensor("ident_b", [128, 128], BF16))
        G.ones_b = st.enter_context(nc.sbuf_tensor("ones_b", [128, 128], BF16))
        G.ones_f = st.enter_context(nc.sbuf_tensor("ones_f", [128, 128], F32))
        G.t_const = Tok()
        G.eps_t = st.enter_context(nc.sbuf_tensor("eps_t", [128, 1], F32))
        S.op("pool", I("memset", G.eps_t[:], EPS), writes=[G.t_const], merge=True)
        S.dma("sp", I("dma_start", out=G.ident_f[:], in_=G.identf), writes=[G.t_const])
        S.op("dve", I("tensor_copy", out=G.ident_b[:], in_=G.ident_f[:]), reads=[G.t_const], writes=[G.t_const])
        S.op("pool", I("memset", G.ones_b[:], 1.0), writes=[G.t_const], merge=True)
        S.op("pool", I("memset", G.ones_f[:], 1.0), writes=[G.t_const], merge=True)
        G.aff_all = st.enter_context(nc.sbuf_tensor("aff_all", [128, 34, NE], F32))
        G.t_aff = Tok()
        S.op("pool", I("memset", G.aff_all[:], 0.0), writes=[G.t_aff])
        G.idx_all = st.enter_context(nc.sbuf_tensor("idx_all", [128, NE, 5], I32))
        G.g_all = st.enter_context(nc.sbuf_tensor("g_all", [128, NE, 5], F32))
        G.t_idx = Tok()
        S.op("pool", I("memset", G.idx_all[:], 0), writes=[G.t_idx])
        S.op("pool", I("memset", G.g_all[:], 0.0), writes=[G.t_idx], merge=True)
        S.dma("sp", I("dma_start", out=G.xres.rearrange("(a p) d -> p a d", p=128),
                                          in_=G.xin.rearrange("(a p) d -> p a d", p=128)),
              writes=[G.t["xres"]])
        S.dma("pool", I("dma_start", out=G.fxd[NT:NPAD, :], in_=G.xin[NT:NPAD, :]), writes=[G.t["fxd"]], merge=True)
        S.flush("init")
        if stop_after == "moetest":
            phase_adaln(G, 0)
            S.dma("sp", I("dma_start", out=G.aff_all[:], in_=G.aff_in.rearrange("(t p) e -> p t e", p=128)), writes=[G.t_aff])
            S.dma("sp", I("dma_start", out=G.fxd.rearrange("(a p) d -> p a d", p=128), in_=G.fx_in.rearrange("(a p) d -> p a d", p=128)),
                  writes=[G.t["fxd"]])
            phase_route(G, 0, True)
            if "moe" in G.debug:
                phase_moe(G, 0, True)
            n_layers = 0
        for l in range(n_layers):
            last = (l == DEPTH - 1)
            phase_adaln(G, l)
            if stop_after == "adaln":
                break
            phase_proj_a(G, l)
            if stop_after == "proj_a":
                break
            phase_proj_b(G, l)
            if stop_after == "proj_b":
                break
            phase_conv(G, l, not last)
            if stop_after == "conv":
                break
            phase_swa(G, l, not last)
            if stop_after == "swa":
                break
            phase_fnet(G, l, not last)
            if stop_after == "fnet":
                break
            phase_mla(G, l, not last)
            if stop_after == "mla":
                break
            phase_outproj(G, l, not last)
            if stop_after == "outproj":
                break
            phase_route(G, l, not last)
            if stop_after == "route":
                break
            phase_moe(G, l, not last)
            if stop_after == "moe":
                break
        if stop_after is None and n_layers == DEPTH:
            phase_final(G)
        S.wait_all("sp", list(G.t.values()))
        S.flush("fin")
        G.n_inst = S.n_inst
    G.in_names = list(din.keys())
    return nc, G


def _alloc(G, ps):
    nc = G.nc
    G.uid = getattr(G, "uid", 0) + 1
    u = G.uid
    sb = lambda name, shape, dt: ps.enter_context(nc.sbuf_tensor(f"{name}_{u}", list(shape), dt))
    pp = lambda name, shape, dt=F32: ps.enter_context(nc.psum_tensor(f"{name}_{u}", list(shape), dt))
    return sb, pp


BLOCKS = [(i * 512, 512, 0) for i in range(8)] + [(NL, NCX, 1)]


def phase_adaln(G, l):
    nc, S = G.nc, G.S
    with ExitStack() as ps:
        sb, pp = _alloc(G, ps)
        cT = sb("ad_cT", [128, 16, 2], F32)
        sl = sb("ad_sl", [128, 16, 64], BF16)
        sel = sb("ad_sel", [1, 64], BF16)
        brow = sb("ad_brow", [1, 6 * D], BF16)
        rows = sb("ad_rows", [64, 6 * D], F32)
        wt = [sb(f"ad_wt{i}", [128, 16, 512], BF16) for i in range(2)]
        pacc = [pp(f"ad_ps{i}", [64, 512]) for i in range(2)]
        t_c, t_sl, t_sel, t_b, t_rows = Tok(), Tok(), Tok(), Tok(), Tok()
        t_wt = [Tok(), Tok()]
        t_ps = [Tok(), Tok()]
        S.dma("sp", I("dma_start", out=cT[:], in_=G.cvec.rearrange("(j p) t -> p j t", p=128)), writes=[t_c])
        S.op("pool", I("memset", sl[:], 0.0), writes=[t_sl])
        S.op("pool", I("memset", sel[:], 0.0), writes=[t_sel])
        S.op("pool", I("memset", sel[0:1, 0:1], 1.0), writes=[t_sel])
        S.op("pool", I("memset", sel[0:1, 32:33], 1.0), writes=[t_sel])
        S.op("act", I("activation", out=sl[:, :, 0], in_=cT[:, :, 0], func=AF.Silu), reads=[t_c], writes=[t_sl])
        S.op("act", I("activation", out=sl[:, :, 32], in_=cT[:, :, 1], func=AF.Silu), reads=[t_c], writes=[t_sl])
        for q in range(6):
            S.dma("pool", I("dma_start", out=brow[0:1, q * D:(q + 1) * D], in_=G.ada_b[l:l + 1, q * D:(q + 1) * D]),
                  writes=[t_b], merge=True)
        for nb in range(24):
            b = nb % 2
            S.dma("pool", I("dma_start", out=wt[b][:].rearrange("p j n -> p (j n)").rearrange("p (a m) -> p a m", m=2048),
                            in_=G.ada_w[l, nb].rearrange("p (a m) -> p a m", m=2048)),
                  writes=[t_wt[b]])
            for j in range(16):
                S.op("pe", I("matmul", pacc[b][:], lhsT=sl[:, j, :], rhs=wt[b][:, j, :], start=(j == 0), stop=False),
                     reads=[t_sl, t_wt[b]], writes=[t_ps[b]], merge=(j > 0))
            S.op("pe", I("matmul", pacc[b][:], lhsT=sel[:], rhs=brow[0:1, nb * 512:(nb + 1) * 512], start=False, stop=True),
                 reads=[t_sel, t_b], writes=[t_ps[b]], merge=True)
            S.op("act", I("copy", out=rows[0:1, nb * 512:(nb + 1) * 512], in_=pacc[b][0:1, :]),
                 reads=[t_ps[b]], writes=[t_rows], merge=True)
            S.op("dve", I("tensor_copy", out=rows[32:33, nb * 512:(nb + 1) * 512], in_=pacc[b][32:33, :]),
                 reads=[t_ps[b]], writes=[t_rows], merge=True)
        S.dma("sp", I("dma_start", out=G.modrow[l, 0:1, :], in_=rows[0:1, :]), reads=[t_rows], writes=[G.t["modrow"]], merge=True)
        S.dma("sp", I("dma_start", out=G.modrow[l, 1:2, :], in_=rows[32:33, :]), reads=[t_rows], writes=[G.t["modrow"]], merge=True)
        S.flush(f"adaln{l}")


def load_T(G, row_aps, sb, pp, name):
    S = G.S
    nv = len(row_aps)
    t_stg0 = Tok()
    n = max(ap.shape[-1] for ap in row_aps) // 128
    stg = sb(name + "_stg", [16, nv, 128], F32)
    out = sb(name, [128, nv, n], F32)
    S.op("pool", I("memset", stg[:], 0.0), writes=[t_stg0])
    pst_full = pp(name + "_ps", [128, 512])
    pst = pst_full[:, 0:nv * 16].rearrange("p (v n) -> p v n", n=16)
    t_stg, t_ps, t_out = t_stg0, Tok(), Tok()
    for v, ap in enumerate(row_aps):
        S.dma("sp", I("dma_start", out=stg[0:ap.shape[-1] // 128, v, :], in_=ap.rearrange("(j p) -> j p", p=128)),
              reads=[G.t["modrow"]], writes=[t_stg], merge=(v > 0))
    for v in range(nv):
        S.op("pe", I("transpose", out=pst[:, v, 0:n], in_=stg[0:n, v, :], identity=G.ident_f[0:n, 0:n]),
             reads=[t_stg, G.t_const], writes=[t_ps], merge=(v > 0))
    S.op("dve", I("tensor_copy", out=out[:], in_=pst[:, :, 0:n]), reads=[t_ps], writes=[t_out])
    return out, t_out


def rstd_from_ss(G, out_ap, ss_ap, n, reads, writes, merge=False):
    S = G.S
    S.op("act", I("activation", out=out_ap, in_=ss_ap, func=AF.Ln, bias=G.eps_t[0:out_ap.shape[0], 0:1], scale=1.0 / n),
         reads=list(reads) + [G.t_const], writes=writes, merge=merge)
    S.op("act", I("activation", out=out_ap, in_=out_ap, func=AF.Exp, scale=-0.5),
         reads=writes, writes=writes)


def phase_proj_a(G, l):
    nc, S = G.nc, G.S
    with ExitStack() as ps:
        sb, pp = _alloc(G, ps)
        NCOL = 2560
        wA = sb("pa_w", [128, 16, NCOL], BF16)
        t_w = Tok()
        for c0 in range(0, NCOL, 512):
            S.dma("pool", I("dma_start",
                out=wA[:, :, c0:c0 + 512], in_=G.w_a[l][:, c0:c0 + 512].rearrange("(j p) n -> p j n", p=128)),
                writes=[t_w], merge=True)
        mv, t_mv = load_T(G, [G.modrow[l, 0, 0:D], G.modrow[l, 1, 0:D], G.modrow[l, 0, D:2 * D],
                              G.modrow[l, 1, D:2 * D], G.norm1_g[l]], sb, pp, "pa_mv")
        sh1T = mv
        t_sh = t_mv
        gs1T = sb("pa_gs", [128, 2, 16], F32)
        t_gs = Tok()
        for s in range(2):
            S.op("dve", I("scalar_tensor_tensor", out=gs1T[:, s, :], in0=mv[:, 2 + s, :], scalar=1.0, in1=mv[:, 4, :],
                                                           op0=ALU.add, op1=ALU.mult),
                 reads=[t_mv], writes=[t_gs], merge=True)
        xt = [sb(f"pa_xt{i}", [128, D], F32) for i in range(2)]
        t_xt = [Tok(), Tok()]
        junk = sb("pa_junk", [128, D], BF16)
        t_junk = Tok()
        ssq = sb("pa_ss", [128, 8], F32)
        t_ss = [Tok() for _ in range(8)]
        xh = sb("pa_xh", [128, 4, D], BF16)
        t_xh = [Tok() for _ in range(4)]
        hTbs = [sb(f"pa_hT{i}", [128, 16, 512], BF16) for i in range(2)]
        t_hTs = [Tok(), Tok()]
        rS = [sb(f"pa_rS{i}", [128, 2, 512], F32) for i in range(2)]
        t_rS = [Tok(), Tok()]
        cc_s = sb("pa_cc", [128, 4, 512], F32)
        t_cc = [Tok() for _ in range(4)]
        t1 = sb("pa_t1", [128, 4, 512], F32)
        t_t1 = [Tok() for _ in range(4)]
        NST = 4
        stf = [sb(f"pa_stf{i}", [128, 512], F32) for i in range(NST)]
        t_stf = [Tok() for _ in range(NST)]
        stb = [sb(f"pa_stb{i}", [128, 512], BF16) for i in range(NST)]
        t_stb = [Tok() for _ in range(NST)]
        pT = [pp(f"pa_pT{i}", [128, 1024], BF16) for i in range(2)]
        t_pT = [Tok(), Tok()]
        NACC = 4
        acc = [pp(f"pa_acc{i}", [128, 512]) for i in range(NACC)]
        t_acc = [Tok() for _ in range(NACC)]
        cnt = {"f": 0, "b": 0, "a": 0, "x": 0}

        import os
        LV = int(os.environ.get("PA_STOP", "9"))
        for bi, (tok0, nb, s) in enumerate(BLOCKS):
            if LV <= 1 or (LV <= 3 and bi > 0):
                break
            ntile = nb // 128
            rb = bi % 2
            hTb = hTbs[bi % 2]
            t_hT = t_hTs[bi % 2]
            S.dma("sp", I("dma_start",
                out=rS[rb][:, :, 0:nb], in_=G.ropeS[:, :, tok0:tok0 + nb].rearrange("t p n -> p t n")),
                writes=[t_rS[rb]])
            for t in range(ntile):
                xi = cnt["x"] % 2
                cnt["x"] += 1
                si = (bi * 4 + t) % 8
                r0 = tok0 + t * 128
                S.dma("sp", I("dma_start", out=xt[xi][:], in_=G.xres[r0:r0 + 128, :]),
                      reads=[G.t["xres"]], writes=[t_xt[xi]])
                S.op("act", I("activation", out=junk[:], in_=xt[xi][:], func=AF.Square,
                                                                 accum_out=ssq[:, si:si + 1]),
                     reads=[t_xt[xi]], writes=[t_junk, t_ss[si]])
                rstd_from_ss(G, ssq[:, si:si + 1], ssq[:, si:si + 1], D, [t_ss[si]], [t_ss[si]])
                S.op("act", I("activation", out=xh[:, t, :], in_=xt[xi][:], func=AF.Copy,
                                                                      scale=ssq[:, si:si + 1]),
                     reads=[t_xt[xi], t_ss[si]], writes=[t_xh[t]])
            for j in range(16):
                pb = j % 2
                for t in range(ntile):
                    S.op("pe", I("transpose", out=pT[pb][:, t * 128:(t + 1) * 128],
                                                                     in_=xh[:, t, j * 128:(j + 1) * 128], identity=G.ident_b[:]),
                         reads=[t_xh[t], G.t_const], writes=[t_pT[pb]], merge=(t > 0))
                S.op("act", I("activation",
                    out=hTb[:, j, 0:nb], in_=pT[pb][:, 0:nb], func=AF.Identity,
                    bias=sh1T[:, s, j:j + 1], scale=gs1T[:, s, j:j + 1]),
                    reads=[t_pT[pb], t_sh, t_gs], writes=[t_hT], merge=(j > 0))
            S.dma("sp", I("dma_start",
                out=G.hT[:, tok0:tok0 + nb].rearrange("(j p) n -> p j n", p=128), in_=hTb[:, :, 0:nb]),
                reads=[t_hT], writes=[G.t["hT"]], merge=True)

            if LV <= 2:
                break
            def proj(c0, width):
                ai = cnt["a"] % NACC
                cnt["a"] += 1
                for j in range(16):
                    S.op("pe", I("matmul",
                        acc[ai][0:width, 0:nb], lhsT=wA[:, j, c0:c0 + width], rhs=hTb[:, j, 0:nb],
                        start=(j == 0), stop=(j == 15)),
                        reads=[t_w, t_hT], writes=[t_acc[ai]], merge=(j > 0))
                return ai

            def stage_f():
                i = cnt["f"] % NST
                cnt["f"] += 1
                return i

            def stage_b():
                i = cnt["b"] % NST
                cnt["b"] += 1
                return i

            for c in range(4):
                ai = proj(OFF_CB + c * 128, 128)
                fi = stage_f()
                S.op("act", I("copy", out=stf[fi][:, 0:nb], in_=acc[ai][:, 0:nb]),
                     reads=[t_acc[ai]], writes=[t_stf[fi]])
                S.dma("sp", I("dma_start", out=G.cbT[c * 128:(c + 1) * 128, tok0:tok0 + nb], in_=stf[fi][:, 0:nb]),
                      reads=[t_stf[fi]], writes=[G.t["cbT"]], merge=True)
                ai = proj(OFF_CC + c * 128, 128)
                S.op("act", I("copy", out=cc_s[:, c, 0:nb], in_=acc[ai][:, 0:nb]),
                     reads=[t_acc[ai]], writes=[t_cc[c]])
                ai = proj(OFF_CH + c * 128, 128)
                fi = stage_f()
                S.op("dve", I("tensor_tensor", out=stf[fi][:, 0:nb], in0=acc[ai][:, 0:nb],
                                                                        in1=cc_s[:, c, 0:nb], op=ALU.mult),
                     reads=[t_acc[ai], t_cc[c]], writes=[t_stf[fi]])
                S.dma("sp", I("dma_start", out=G.uT[c * 128:(c + 1) * 128, tok0:tok0 + nb], in_=stf[fi][:, 0:nb]),
                      reads=[t_stf[fi]], writes=[G.t["uT"]], merge=True)
            for c in range(4):
                ai = proj(OFF_SQ + c * 128, 128)
                S.op("dve", I("tensor_tensor", out=t1[:, c, 0:nb], in0=acc[ai][:, 0:nb],
                                                                        in1=rS[rb][:, 0, 0:nb], op=ALU.mult),
                     reads=[t_acc[ai], t_rS[rb]], writes=[t_t1[c]])
                ai = proj(OFF_SQW + c * 128, 128)
                fi = stage_f()
                S.op("dve", I("tensor_tensor", out=stf[fi][:, 0:nb], in0=acc[ai][:, 0:nb],
                                                                          in1=rS[rb][:, 1, 0:nb], op=ALU.mult),
                     reads=[t_acc[ai], t_rS[rb]], writes=[t_stf[fi]])
                bi_ = stage_b()
                S.op("pool", I("tensor_tensor", out=stb[bi_][:, 0:nb], in0=t1[:, c, 0:nb],
                                                                           in1=stf[fi][:, 0:nb], op=ALU.add),
                     reads=[t_t1[c], t_stf[fi]], writes=[t_stb[bi_]])
                S.dma("sp", I("dma_start", out=G.sqT[c * 128:(c + 1) * 128, tok0:tok0 + nb], in_=stb[bi_][:, 0:nb]),
                      reads=[t_stb[bi_]], writes=[G.t["sqT"]], merge=True)
        S.flush(f"proja{l}")


def phase_proj_b(G, l):
    nc, S = G.nc, G.S
    with ExitStack() as ps:
        sb, pp = _alloc(G, ps)
        w1 = sb("pb_w1", [128, 16, 896], BF16)
        w2 = sb("pb_w2", [128, 16, 960], BF16)
        wq = sb("pb_wq", [128, 4, 2, 768], BF16)
        wk = sb("pb_wk", [128, 2, 512], BF16)
        wv = sb("pb_wv", [128, 2, 512], BF16)
        d64 = sb("pb_d64", [128, 2, 128], BF16)
        t_w = Tok()
        for c0 in range(0, 896, 448):
            S.dma("pool", I("dma_start", out=w1[:, :, c0:c0 + 448],
                            in_=G.w_a[l][:, 2560 + c0:2560 + c0 + 448].rearrange("(j p) n -> p j n", p=128)),
                  writes=[t_w], merge=True)
        for c0 in range(0, 960, 480):
            S.dma("pool", I("dma_start", out=w2[:, :, c0:c0 + 480],
                            in_=G.w_b[l][:, c0:c0 + 480].rearrange("(j p) n -> p j n", p=128)),
                  writes=[t_w], merge=True)
        for ab in range(2):
            S.dma("pool", I("dma_start", out=wq[:, :, ab, :], in_=G.w_uq[l, ab].rearrange("(c p) n -> p c n", p=128)),
                  writes=[t_w], merge=True)
        S.dma("pool", I("dma_start", out=wk[:], in_=G.w_uk[l].rearrange("(c p) n -> p c n", p=128)), writes=[t_w], merge=True)
        S.dma("pool", I("dma_start", out=wv[:], in_=G.w_uv[l].rearrange("(c p) n -> p c n", p=128)), writes=[t_w], merge=True)
        S.dma("sp", I("dma_start", out=d64[:], in_=G.dft64.rearrange("t p n -> p t n")), writes=[t_w], merge=True)
        gT, t_gT = load_T(G, [G.q_norm_g[l], G.kv_norm_g[l]], sb, pp, "pb_g")
        hTb = [sb(f"pb_hT{i}", [128, 16, 512], BF16) for i in range(2)]
        t_hT = [Tok(), Tok()]
        rS = sb("pb_rS", [128, 2, 512], F32)
        rM = sb("pb_rM", [96, 2, 512], F32)
        t_r = Tok()
        cq_f = sb("pb_cqf", [128, 4, 512], F32)
        t_cqf = [Tok() for _ in range(4)]
        sqb = [sb(f"pb_sqb{i}", [128, 512], BF16) for i in range(2)]
        t_sqb = [Tok(), Tok()]
        rstd = sb("pb_rstd", [128, 2, 512], F32)
        t_rstd = [Tok(), Tok()]
        cqn = sb("pb_cqn", [128, 4, 512], BF16)
        t_cqn = Tok()
        ckf = sb("pb_ckf", [128, 2, 512], F32)
        t_ckf = [Tok(), Tok()]
        ckn = sb("pb_ckn", [128, 2, 512], BF16)
        t_ckn = Tok()
        fuT = sb("pb_fuT", [128, 4, 512], BF16)
        t_fuT = Tok()
        mq_st = sb("pb_mq", [96, 8, 512], BF16)
        t_mq = Tok()
        mk_st = sb("pb_mk", [96, 8, 512], BF16)
        t_mk = Tok()
        kpe_r = sb("pb_kpe", [96, 512], BF16)
        t_kpe = Tok()
        tA = [sb(f"pb_tA{i}", [128, 512], F32) for i in range(2)]
        t_tA = [Tok(), Tok()]
        tB = [sb(f"pb_tB{i}", [128, 512], F32) for i in range(2)]
        t_tB = [Tok(), Tok()]
        NST = 3
        stb = [sb(f"pb_stb{i}", [128, 512], BF16) for i in range(NST)]
        t_stb = [Tok() for _ in range(NST)]
        NACC = 3
        acc = [pp(f"pb_acc{i}", [128, 512]) for i in range(NACC)]
        t_acc = [Tok() for _ in range(NACC)]
        ssp = pp("pb_ss", [128, 512])
        t_ssp = Tok()
        tkp = [pp(f"pb_tk{i}", [128, 512]) for i in range(2)]
        t_tkp = [Tok(), Tok()]
        cnt = {"a": 0, "b": 0, "t": 0, "k": 0}
        import os
        LV = int(os.environ.get("PB_STOP", "9"))

        for bi, (tok0, nb, s) in enumerate(BLOCKS):
            if LV <= 3 and bi > 0:
                break
            ntile = nb // 128
            hb = bi % 2
            hT_ = hTb[hb]
            S.dma("sp", I("dma_start", out=hT_[:, :, 0:nb], in_=G.hT[:, tok0:tok0 + nb].rearrange("(j p) n -> p j n", p=128)),
                  reads=[G.t["hT"]], writes=[t_hT[hb]])
            S.dma("sp", I("dma_start", out=rS[:, :, 0:nb], in_=G.ropeS[:, :, tok0:tok0 + nb].rearrange("t p n -> p t n")),
                  writes=[t_r])
            S.dma("sp", I("dma_start", out=rM[:, :, 0:nb], in_=G.ropeM[:, :, tok0:tok0 + nb].rearrange("t p n -> p t n")),
                  writes=[t_r], merge=True)

            def proj(wt, c0, width, rhs_tile=None):
                ai = cnt["a"] % NACC
                cnt["a"] += 1
                for j in range(16):
                    S.op("pe", I("matmul", acc[ai][0:width, 0:nb], lhsT=wt[:, j, c0:c0 + width], rhs=hT_[:, j, 0:nb],
                                 start=(j == 0), stop=(j == 15)),
                         reads=[t_w, t_hT[hb]], writes=[t_acc[ai]], merge=(j > 0))
                return ai

            def rope_pair(aiA, aiB, p0, p1, table, out_ap, t_out, merge_out=False):
                ti = cnt["t"] % 2
                cnt["t"] += 1
                S.op("dve", I("tensor_tensor", out=tA[ti][p0:p1, 0:nb], in0=acc[aiA][p0:p1, 0:nb], in1=table[p0:p1, 0, 0:nb], op=ALU.mult),
                     reads=[t_acc[aiA], t_r], writes=[t_tA[ti]])
                S.op("dve", I("tensor_tensor", out=tB[ti][p0:p1, 0:nb], in0=acc[aiB][p0:p1, 0:nb], in1=table[p0:p1, 1, 0:nb], op=ALU.mult),
                     reads=[t_acc[aiB], t_r], writes=[t_tB[ti]])
                S.op("pool", I("tensor_tensor", out=out_ap, in0=tA[ti][p0:p1, 0:nb], in1=tB[ti][p0:p1, 0:nb], op=ALU.add),
                     reads=[t_tA[ti], t_tB[ti]], writes=[t_out], merge=merge_out)

            def stage_b():
                i = cnt["b"] % NST
                cnt["b"] += 1
                return i

            aiA = proj(w1, 0, 128)
            aiB = proj(w1, 128, 128)
            bi_ = stage_b()
            rope_pair(aiA, aiB, 0, 128, rS, stb[bi_][:, 0:nb], t_stb[bi_])
            S.dma("sp", I("dma_start", out=G.skT[:, tok0:tok0 + nb], in_=stb[bi_][:, 0:nb]), reads=[t_stb[bi_]],
                  writes=[G.t["skT"]], merge=True)
            ki = cnt["k"] % 2
            cnt["k"] += 1
            for t in range(ntile):
                for j in range(16):
                    S.op("pe", I("matmul", tkp[ki][:, t * 128:(t + 1) * 128], lhsT=hT_[:, j, t * 128:(t + 1) * 128],
                                 rhs=w1[:, j, 256:384], start=(j == 0), stop=(j == 15)),
                         reads=[t_w, t_hT[hb]], writes=[t_tkp[ki]], merge=(j > 0 or t > 0))
            bi_ = stage_b()
            S.op("act", I("copy", out=stb[bi_][:, 0:nb], in_=tkp[ki][:, 0:nb]), reads=[t_tkp[ki]], writes=[t_stb[bi_]])
            S.dma("sp", I("dma_start", out=G.svd[tok0:tok0 + nb, :].rearrange("(t p) c -> p t c", p=128),
                          in_=stb[bi_][:, 0:nb].rearrange("p (t c) -> p t c", c=128)),
                  reads=[t_stb[bi_]], writes=[G.t["svd"]], merge=True)
            for c in range(4):
                ai = proj(w1, 384 + c * 128, 128)
                S.op("act", I("copy", out=fuT[:, c, 0:nb], in_=acc[ai][:, 0:nb]), reads=[t_acc[ai]], writes=[t_fuT], merge=(c > 0))
            for t in range(ntile):
                for cs_, dst, key in ((0, G.ucd, "ucd"), (1, G.usd, "usd")):
                    ki = cnt["k"] % 2
                    cnt["k"] += 1
                    for c in range(4):
                        S.op("pe", I("matmul", tkp[ki][:, c * 128:(c + 1) * 128], lhsT=fuT[:, c, t * 128:(t + 1) * 128],
                                     rhs=d64[:, cs_, :], start=True, stop=True),
                             reads=[t_w, t_fuT], writes=[t_tkp[ki]], merge=(c > 0))
                    bi_ = stage_b()
                    S.op("act" if cs_ == 0 else "dve",
                         I("copy", out=stb[bi_][:], in_=tkp[ki][:]) if cs_ == 0 else I("tensor_copy", out=stb[bi_][:], in_=tkp[ki][:]),
                         reads=[t_tkp[ki]], writes=[t_stb[bi_]])
                    S.dma("sp", I("dma_start", out=dst[tok0 + t * 128:tok0 + (t + 1) * 128, :], in_=stb[bi_][:]),
                          reads=[t_stb[bi_]], writes=[G.t[key]], merge=True)
            def latent_norm(c0, nch, f_tile, t_f, n_tile, t_n, gi, ri):
                for c in range(nch):
                    ai = proj(w2, c0 + c * 128, 128)
                    S.op("act", I("copy", out=f_tile[:, c, 0:nb], in_=acc[ai][:, 0:nb]), reads=[t_acc[ai]], writes=[t_f[c]])
                    qi = c % 2
                    S.op("act", I("activation", out=sqb[qi][:, 0:nb], in_=acc[ai][:, 0:nb], func=AF.Square),
                         reads=[t_acc[ai]], writes=[t_sqb[qi]])
                    S.op("pe", I("matmul", ssp[:, 0:nb], lhsT=G.ones_b[:], rhs=sqb[qi][:, 0:nb], start=(c == 0), stop=(c == nch - 1)),
                         reads=[t_sqb[qi], G.t_const], writes=[t_ssp], merge=(c > 0))
                rstd_from_ss(G, rstd[:, ri, 0:nb], ssp[:, 0:nb], nch * 128, [t_ssp], [t_rstd[ri]])
                for c in range(nch):
                    S.op("dve", I("scalar_tensor_tensor", out=n_tile[:, c, 0:nb], in0=f_tile[:, c, 0:nb], scalar=gT[:, gi, c:c + 1],
                                  in1=rstd[:, ri, 0:nb], op0=ALU.mult, op1=ALU.mult),
                         reads=[t_f[c], t_gT, t_rstd[ri]], writes=[t_n], merge=(c > 0))

            latent_norm(0, 4, cq_f, t_cqf, cqn, t_cqn, 0, 0)
            latent_norm(512, 2, ckf, t_ckf, ckn, t_ckn, 1, 1)
            for h in range(8):
                ais = []
                for ab in range(2):
                    ai = cnt["a"] % NACC
                    cnt["a"] += 1
                    for c in range(4):
                        S.op("pe", I("matmul", acc[ai][0:96, 0:nb], lhsT=wq[:, c, ab, h * 96:(h + 1) * 96], rhs=cqn[:, c, 0:nb],
                                     start=(c == 0), stop=(c == 3)),
                             reads=[t_w, t_cqn], writes=[t_acc[ai]], merge=(c > 0))
                    ais.append(ai)
                S.op("act", I("copy", out=mq_st[0:64, h, 0:nb], in_=acc[ais[0]][0:64, 0:nb]), reads=[t_acc[ais[0]]],
                     writes=[t_mq], merge=(h > 0))
                rope_pair(ais[0], ais[1], 64, 96, rM, mq_st[64:96, h, 0:nb], t_mq, merge_out=True)
            S.dma("sp", I("dma_start", out=G.mqT[:, :, tok0:tok0 + nb].rearrange("h p n -> p h n"), in_=mq_st[:, :, 0:nb]),
                  reads=[t_mq], writes=[G.t["mqT"]], merge=True)
            aiA = proj(w2, OFF_KPA, 96)
            aiB = proj(w2, OFF_KPB, 96)
            rope_pair(aiA, aiB, 64, 96, rM, kpe_r[64:96, 0:nb], t_kpe)
            for h in range(8):
                ai = cnt["a"] % NACC
                cnt["a"] += 1
                for c in range(2):
                    S.op("pe", I("matmul", acc[ai][0:64, 0:nb], lhsT=wk[:, c, h * 64:(h + 1) * 64], rhs=ckn[:, c, 0:nb],
                                 start=(c == 0), stop=(c == 1)),
                         reads=[t_w, t_ckn], writes=[t_acc[ai]], merge=(c > 0))
                S.op("act", I("copy", out=mk_st[0:64, h, 0:nb], in_=acc[ai][0:64, 0:nb]), reads=[t_acc[ai]],
                     writes=[t_mk], merge=(h > 0))
                S.op("pool" if h % 2 else "dve", I("tensor_copy", out=mk_st[64:96, h, 0:nb], in_=kpe_r[64:96, 0:nb]),
                     reads=[t_kpe], writes=[t_mk], merge=True)
            S.dma("sp", I("dma_start", out=G.mkT[:, :, tok0:tok0 + nb].rearrange("h p n -> p h n"), in_=mk_st[:, :, 0:nb]),
                  reads=[t_mk], writes=[G.t["mkT"]], merge=True)
            for t in range(ntile):
                ki = cnt["k"] % 2
                cnt["k"] += 1
                for c in range(2):
                    S.op("pe", I("matmul", tkp[ki][:], lhsT=ckn[:, c, t * 128:(t + 1) * 128], rhs=wv[:, c, :],
                                 start=(c == 0), stop=(c == 1)),
                         reads=[t_w, t_ckn], writes=[t_tkp[ki]], merge=(c > 0))
                bi_ = stage_b()
                S.op("act", I("copy", out=stb[bi_][:], in_=tkp[ki][:]), reads=[t_tkp[ki]], writes=[t_stb[bi_]])
                S.dma("sp", I("dma_start", out=G.mvd[tok0 + t * 128:tok0 + (t + 1) * 128, :], in_=stb[bi_][:]),
                      reads=[t_stb[bi_]], writes=[G.t["mvd"]], merge=True)
        S.flush(f"projb{l}")


class GroupTail:
    def __init__(self, G, l, gi, sb, pp, name):
        self.G, self.gi = G, gi
        self.gT, self.t_gT = load_T(G, [G.out_norm_g[l, gi * 512:(gi + 1) * 512]], sb, pp, name + "_g")
        self.junk = sb(name + "_junk", [128, 512], BF16)
        self.t_junk = Tok()
        self.ss = [sb(name + f"_ss{i}", [128, 1], F32) for i in range(2)]
        self.t_ss = [Tok(), Tok()]
        self.ynb = [sb(name + f"_ynb{i}", [128, 512], BF16) for i in range(2)]
        self.t_ynb = [Tok(), Tok()]
        self.tp = pp(name + "_tp", [128, 1024], BF16)
        self.t_tp = Tok()
        self.st = [sb(name + f"_st{i}", [128, 4, 128], BF16) for i in range(2)]
        self.t_st = [Tok(), Tok()]
        self.k = 0

    def emit(self, y_ap, t_y, tok0):
        G, S = self.G, self.G.S
        i = self.k % 2
        self.k += 1
        S.op("act", I("activation", out=self.junk[:], in_=y_ap, func=AF.Square, accum_out=self.ss[i][:, 0:1]),
             reads=[t_y], writes=[self.t_junk, self.t_ss[i]])
        rstd_from_ss(G, self.ss[i][:, 0:1], self.ss[i][:, 0:1], 512, [self.t_ss[i]], [self.t_ss[i]])
        S.op("act", I("activation", out=self.ynb[i][:], in_=y_ap, func=AF.Copy, scale=self.ss[i][:, 0:1]),
             reads=[t_y, self.t_ss[i]], writes=[self.t_ynb[i]])
        for c in range(4):
            S.op("pe", I("transpose", out=self.tp[:, c * 128:(c + 1) * 128], in_=self.ynb[i][:, c * 128:(c + 1) * 128],
                         identity=G.ident_b[:]),
                 reads=[self.t_ynb[i], G.t_const], writes=[self.t_tp], merge=(c > 0))
        for c in range(4):
            S.op("dve", I("tensor_scalar", out=self.st[i][:, c, :], in0=self.tp[:, c * 128:(c + 1) * 128],
                          scalar1=self.gT[:, 0, c:c + 1], scalar2=None, op0=ALU.mult),
                 reads=[self.t_tp, self.t_gT], writes=[self.t_st[i]], merge=(c > 0))
        r0 = self.gi * 512
        S.dma("sp", I("dma_start", out=G.ynT[r0:r0 + 512, tok0:tok0 + 128].rearrange("(c p) n -> p c n", p=128), in_=self.st[i][:]),
              reads=[self.t_st[i]], writes=[G.t["ynT"]], merge=True)


def phase_conv(G, l, do_ctx):
    nc, S = G.nc, G.S
    with ExitStack() as ps:
        sb, pp = _alloc(G, ps)
        cw, t_cw = load_T(G, [G.conv_w[l, 0], G.conv_w[l, 1], G.conv_w[l, 2], G.out_norm_g[l, 0:512]], sb, pp, "cv_w")
        ut = [sb(f"cv_u{i}", [128, 514], F32) for i in range(2)]
        t_ut = [Tok(), Tok()]
        cbt = [sb(f"cv_cb{i}", [128, 512], F32) for i in range(2)]
        t_cbt = [Tok(), Tok()]
        acc_t = [sb(f"cv_a{i}", [128, 512], F32) for i in range(2)]
        t_at = [Tok(), Tok()]
        yc = sb("cv_y", [128, 4, 512], F32)
        t_yc = [Tok() for _ in range(4)]
        sqb = [sb(f"cv_sq{i}", [128, 512], BF16) for i in range(2)]
        t_sqb = [Tok(), Tok()]
        rstd = sb("cv_rstd", [128, 512], F32)
        t_rstd = Tok()
        stb = [sb(f"cv_st{i}", [128, 512], BF16) for i in range(2)]
        t_stb = [Tok(), Tok()]
        ssp = pp("cv_ss", [128, 512])
        t_ssp = Tok()
        segs = [(0, NL)] + ([(NL, NT)] if do_ctx else [])
        k = 0
        for (s0, s1) in segs:
            for b0 in range(s0, s1, 512):
                nb = min(512, s1 - b0)
                for c in range(4):
                    i = k % 2
                    k += 1
                    lo = b0 - 1 if b0 > s0 else b0
                    hi = b0 + nb + 1 if b0 + nb < s1 else b0 + nb
                    first = True
                    if lo == b0:
                        S.op("pool", I("memset", ut[i][:, 0:1], 0.0), writes=[t_ut[i]])
                        first = False
                    if hi == b0 + nb:
                        S.op("pool", I("memset", ut[i][:, nb + 1:nb + 2], 0.0), writes=[t_ut[i]], merge=not first)
                        first = False
                    S.dma("sp", I("dma_start", out=ut[i][:, lo - (b0 - 1):hi - (b0 - 1)], in_=G.uT[c * 128:(c + 1) * 128, lo:hi]),
                          reads=[G.t["uT"]], writes=[t_ut[i]], merge=not first)
                    S.dma("sp", I("dma_start", out=cbt[i][:, 0:nb], in_=G.cbT[c * 128:(c + 1) * 128, b0:b0 + nb]),
                          reads=[G.t["cbT"]], writes=[t_cbt[i]])
                    S.op("dve", I("tensor_scalar", out=acc_t[i][:, 0:nb], in0=ut[i][:, 0:nb], scalar1=cw[:, 0, c:c + 1], scalar2=None,
                                  op0=ALU.mult), reads=[t_ut[i], t_cw], writes=[t_at[i]])
                    S.op("dve", I("scalar_tensor_tensor", out=acc_t[i][:, 0:nb], in0=ut[i][:, 1:nb + 1], scalar=cw[:, 1, c:c + 1],
                                  in1=acc_t[i][:, 0:nb], op0=ALU.mult, op1=ALU.add), reads=[t_ut[i], t_cw, t_at[i]], writes=[t_at[i]])
                    S.op("dve", I("scalar_tensor_tensor", out=acc_t[i][:, 0:nb], in0=ut[i][:, 2:nb + 2], scalar=cw[:, 2, c:c + 1],
                                  in1=acc_t[i][:, 0:nb], op0=ALU.mult, op1=ALU.add), reads=[t_ut[i], t_cw, t_at[i]], writes=[t_at[i]])
                    S.op("pool", I("tensor_tensor", out=yc[:, c, 0:nb], in0=acc_t[i][:, 0:nb], in1=cbt[i][:, 0:nb], op=ALU.mult),
                         reads=[t_at[i], t_cbt[i]], writes=[t_yc[c]])
                    S.op("act", I("activation", out=sqb[i][:, 0:nb], in_=yc[:, c, 0:nb], func=AF.Square), reads=[t_yc[c]], writes=[t_sqb[i]])
                    S.op("pe", I("matmul", ssp[:, 0:nb], lhsT=G.ones_b[:], rhs=sqb[i][:, 0:nb], start=(c == 0), stop=(c == 3)),
                         reads=[t_sqb[i], G.t_const], writes=[t_ssp], merge=(c > 0))
                rstd_from_ss(G, rstd[:, 0:nb], ssp[:, 0:nb], 512, [t_ssp], [t_rstd])
                for c in range(4):
                    i = k % 2
                    k += 1
                    S.op("dve", I("scalar_tensor_tensor", out=stb[i][:, 0:nb], in0=yc[:, c, 0:nb], scalar=cw[:, 3, c:c + 1],
                                  in1=rstd[:, 0:nb], op0=ALU.mult, op1=ALU.mult), reads=[t_yc[c], t_cw, t_rstd], writes=[t_stb[i]])
                    S.dma("sp", I("dma_start", out=G.ynT[c * 128:(c + 1) * 128, b0:b0 + nb], in_=stb[i][:, 0:nb]),
                          reads=[t_stb[i]], writes=[G.t["ynT"]], merge=True)
        S.flush(f"conv{l}")


def phase_swa(G, l, do_ctx):
    nc, S = G.nc, G.S
    SCALE = 64 ** -0.5
    with ExitStack() as ps:
        sb, pp = _alloc(G, ps)
        Qs = sb("sw_Q", [64, 8, NT], BF16)
        Ks = sb("sw_K", [64, 2, NT], BF16)
        Vs = sb("sw_V", [128, 34, 2, 65], BF16)
        mk = sb("sw_mask", [128, 2, 512], BF16)
        snk = sb("sw_sink", [128, 8], F32)
        t_in = Tok()
        t_V = Tok()
        for h in range(8):
            S.dma("sp", I("dma_start", out=Qs[:, h, :], in_=G.sqT[h * 64:(h + 1) * 64, :]), reads=[G.t["sqT"]], writes=[t_in], merge=True)
        for h in range(2):
            S.dma("sp", I("dma_start", out=Ks[:, h, :], in_=G.skT[h * 64:(h + 1) * 64, :]), reads=[G.t["skT"]], writes=[t_in], merge=True)
        S.op("pool", I("memset", Vs[:, :, :, 64:65], 1.0), writes=[t_V])
        for h in range(2):
            S.dma("sp", I("dma_start", out=Vs[:, :, h, 0:64], in_=G.svd[:, h * 64:(h + 1) * 64].rearrange("(t p) d -> p t d", p=128)),
                  reads=[G.t["svd"]], writes=[t_V], merge=True)
        S.dma("sp", I("dma_start", out=mk[:], in_=G.masks.rearrange("t p n -> p t n")), writes=[t_in], merge=True)
        S.dma("sp", I("dma_start", out=snk[:], in_=G.swa_sink[l:l + 1, :].to_broadcast([128, 8])), writes=[t_in], merge=True)
        S.op("act", I("activation", out=snk[:], in_=snk[:], func=AF.Exp), reads=[t_in], writes=[t_in])
        tail = GroupTail(G, l, 1, sb, pp, "sw_t")
        pT = [sb(f"sw_pT{i}", [128, 5, 512], BF16) for i in range(2)]
        t_pT = [[Tok() for _ in range(5)] for _ in range(2)]
        sps = [pp(f"sw_s{i}", [128, 512]) for i in range(2)]
        t_sps = [Tok(), Tok()]
        ops_ = [pp(f"sw_o{i}", [128, 512]) for i in range(2)]
        t_ops = [Tok(), Tok()]
        den = [sb(f"sw_den{i}", [128, 8], F32) for i in range(2)]
        t_den = [Tok(), Tok()]
        ysw = [sb(f"sw_y{i}", [128, 512], F32) for i in range(2)]
        t_ysw = [Tok(), Tok()]
        qblocks = [(i, "lat") for i in range(32)] + ([(32, "ctx"), (33, "ctx")] if do_ctx else [])
        import os
        LV = int(os.environ.get("SW_STOP", "99"))
        kq = 0
        ks = 0
        pend = None
        for bidx, (i, kind) in enumerate(qblocks[:LV]):
            if kind == "lat":
                kts = ([(i - 1, 0)] if i > 0 else []) + [(i, None)] + ([(i + 1, 1)] if i < 31 else []) + [(32, None), (33, None)]
            else:
                kts = [(32, None), (33, None)]
            yi = bidx % 2
            for kvh in range(2):
                pi = kq % 2
                oi = kq % 2
                kq += 1
                for n, (kt, msk) in enumerate(kts):
                    si = ks % 2
                    ks += 1
                    S.op("pe", I("matmul", sps[si][:], lhsT=Ks[:, kvh, kt * 128:(kt + 1) * 128],
                                 rhs=Qs[:, kvh * 4:(kvh + 1) * 4, i * 128:(i + 1) * 128], start=True, stop=True),
                         reads=[t_in], writes=[t_sps[si]])
                    S.op("act", I("activation", out=pT[pi][:, n, :], in_=sps[si][:], func=AF.Exp, scale=SCALE),
                         reads=[t_sps[si]], writes=[t_pT[pi][n]])
                    if msk is not None:
                        S.op("dve", I("tensor_tensor", out=pT[pi][:, n, :], in0=pT[pi][:, n, :], in1=mk[:, msk, :], op=ALU.mult),
                             reads=[t_pT[pi][n], t_in], writes=[t_pT[pi][n]])
                for g in range(4):
                    for n, (kt, msk) in enumerate(kts):
                        S.op("pe", I("matmul", ops_[oi][:, g * 65:(g + 1) * 65], lhsT=pT[pi][:, n, g * 128:(g + 1) * 128],
                                     rhs=Vs[:, kt, kvh, :], start=(n == 0), stop=(n == len(kts) - 1)),
                             reads=[t_pT[pi][n], t_V], writes=[t_ops[oi]], merge=(n > 0 or g > 0))
                ov = ops_[oi][:, 0:260].rearrange("p (g e) -> p g e", e=65)
                S.op("dve", I("tensor_tensor", out=den[yi][:, kvh * 4:(kvh + 1) * 4], in0=ov[:, :, 64], in1=snk[:, kvh * 4:(kvh + 1) * 4], op=ALU.add),
                     reads=[t_ops[oi], t_in], writes=[t_den[yi]], merge=(kvh > 0))
                S.op("dve", I("reciprocal", out=den[yi][:, kvh * 4:(kvh + 1) * 4], in_=den[yi][:, kvh * 4:(kvh + 1) * 4]),
                     reads=[t_den[yi]], writes=[t_den[yi]])
                for g in range(4):
                    h = kvh * 4 + g
                    S.op("dve", I("tensor_scalar", out=ysw[yi][:, h * 64:(h + 1) * 64], in0=ov[:, g, 0:64], scalar1=den[yi][:, h:h + 1],
                                  scalar2=None, op0=ALU.mult),
                         reads=[t_ops[oi], t_den[yi]], writes=[t_ysw[yi]], merge=(h > 0))
                if kvh == 0 and pend is not None:
                    tail.emit(*pend)
                    pend = None
            pend = (ysw[yi][:], t_ysw[yi], i * 128)
        if pend is not None:
            tail.emit(*pend)
        S.flush(f"swa{l}")


def phase_fnet(G, l, do_ctx):
    nc, S = G.nc, G.S
    with ExitStack() as ps:
        sb, pp = _alloc(G, ps)
        uc = sb("fn_uc", [128, 34, 512], BF16)
        us = sb("fn_us", [128, 34, 512], BF16)
        t_u = Tok()
        for (t0, t1) in ((0, 16), (16, 34)):
            S.dma("sp", I("dma_start", out=uc[:, t0:t1, :], in_=G.ucd[t0 * 128:t1 * 128, :].rearrange("(t p) c -> p t c", p=128)),
                  reads=[G.t["ucd"]], writes=[t_u], merge=True)
            S.dma("sp", I("dma_start", out=us[:, t0:t1, :], in_=G.usd[t0 * 128:t1 * 128, :].rearrange("(t p) c -> p t c", p=128)),
                  reads=[G.t["usd"]], writes=[t_u], merge=True)
        dt_ = [sb(f"fn_d{i}", [128, 2 * 32 * 128], BF16) for i in range(2)]
        t_dt = [Tok(), Tok()]
        acc = [pp(f"fn_acc{i}", [128, 512]) for i in range(2)]
        t_acc = [Tok(), Tok()]
        yf = [sb(f"fn_y{i}", [128, 512], F32) for i in range(2)]
        t_yf = [Tok(), Tok()]
        tail = GroupTail(G, l, 2, sb, pp, "fn_t")
        import os
        LV = int(os.environ.get("FN_STOP", "99"))
        jobs = [(kt, 32, 0, G.dftL) for kt in range(32)][:LV] + ([(kt, 2, 32, G.dftC) for kt in range(2)] if do_ctx else [])
        pend = None
        for k, (kt, nt, tb, tab) in enumerate(jobs):
            i = k % 2
            dv = dt_[i][:, 0:2 * nt * 128]
            S.dma("sp", I("dma_start", out=dv, in_=tab[kt]), writes=[t_dt[i]])
            d4 = dv.rearrange("p (a n k) -> p a n k", a=2, k=128)
            for n in range(nt):
                S.op("pe", I("matmul", acc[i][:], lhsT=d4[:, 0, n, :], rhs=uc[:, tb + n, :], start=(n == 0), stop=False),
                     reads=[t_dt[i], t_u], writes=[t_acc[i]], merge=(n > 0))
            for n in range(nt):
                S.op("pe", I("matmul", acc[i][:], lhsT=d4[:, 1, n, :], rhs=us[:, tb + n, :], start=False, stop=(n == nt - 1)),
                     reads=[t_dt[i], t_u], writes=[t_acc[i]], merge=True)
            if pend is not None:
                tail.emit(*pend)
            S.op("act", I("copy", out=yf[i][:], in_=acc[i][:]), reads=[t_acc[i]], writes=[t_yf[i]])
            pend = (yf[i][:], t_yf[i], (tb + kt) * 128)
        if pend is not None:
            tail.emit(*pend)
        S.flush(f"fnet{l}")


def phase_mla(G, l, do_ctx):
    nc, S = G.nc, G.S
    SCALE = 96 ** -0.5
    with ExitStack() as ps:
        sb, pp = _alloc(G, ps)
        Kh = [sb(f"ml_K{i}", [96, NT], BF16) for i in range(2)]
        Qh = [sb(f"ml_Q{i}", [96, NT], BF16) for i in range(2)]
        Vh = [sb(f"ml_V{i}", [128, 34, 65], BF16) for i in range(2)]
        t_K = [Tok(), Tok()]
        t_Q = [Tok(), Tok()]
        t_V = [Tok(), Tok()]
        PT = [sb(f"ml_PT{i}", [128, 34, 512], BF16) for i in range(2)]
        t_PT = [[Tok() for _ in range(34)] for _ in range(2)]
        yall = sb("ml_y", [128, 34, 512], F32)
        t_y = [Tok() for _ in range(34)]
        rc = [sb(f"ml_rc{i}", [128, 1], F32) for i in range(4)]
        t_rc = [Tok() for _ in range(4)]
        sps = [pp(f"ml_s{i}", [128, 512]) for i in range(2)]
        t_sps = [Tok(), Tok()]
        ops_ = [pp(f"ml_o{i}", [128, 512]) for i in range(4)]
        t_ops = [Tok() for _ in range(4)]
        import os
        LVH = int(os.environ.get("ML_HEADS", "8"))
        LVQ = int(os.environ.get("ML_QB", "99"))
        qblocks = [(i * 512, 512, list(range(34))) for i in range(8)][:LVQ] + ([(NL, NCX, [32, 33])] if do_ctx else [])
        ks = 0
        kp = 0
        ko = 0
        for h in range(LVH):
            hb = h % 2
            S.dma("sp", I("dma_start", out=Kh[hb][:], in_=G.mkT[h]), reads=[G.t["mkT"]], writes=[t_K[hb]])
            S.dma("sp", I("dma_start", out=Qh[hb][:], in_=G.mqT[h]), reads=[G.t["mqT"]], writes=[t_Q[hb]])
            S.op("pool", I("memset", Vh[hb][:, :, 64:65], 1.0), writes=[t_V[hb]])
            S.dma("sp", I("dma_start", out=Vh[hb][:, :, 0:64], in_=G.mvd[:, h * 64:(h + 1) * 64].rearrange("(t p) d -> p t d", p=128)),
                  reads=[G.t["mvd"]], writes=[t_V[hb]], merge=True)
            for (q0, nq, kts) in qblocks:
                pi = kp % 2
                kp += 1
                for kt in kts:
                    si = ks % 2
                    ks += 1
                    S.op("pe", I("matmul", sps[si][:, 0:nq], lhsT=Kh[hb][:, kt * 128:(kt + 1) * 128], rhs=Qh[hb][:, q0:q0 + nq],
                                 start=True, stop=True), reads=[t_K[hb], t_Q[hb]], writes=[t_sps[si]])
                    S.op("act", I("activation", out=PT[pi][:, kt, 0:nq], in_=sps[si][:, 0:nq], func=AF.Exp, scale=SCALE),
                         reads=[t_sps[si]], writes=[t_PT[pi][kt]])
                for j in range(nq // 128):
                    oi = ko % 4
                    ko += 1
                    for n, kt in enumerate(kts):
                        S.op("pe", I("matmul", ops_[oi][:, 0:65], lhsT=PT[pi][:, kt, j * 128:(j + 1) * 128], rhs=Vh[hb][:, kt, :],
                                     start=(n == 0), stop=(n == len(kts) - 1)),
                             reads=[t_PT[pi][kt], t_V[hb]], writes=[t_ops[oi]], merge=(n > 0))
                    S.op("dve", I("reciprocal", out=rc[oi][:], in_=ops_[oi][:, 64:65]), reads=[t_ops[oi]], writes=[t_rc[oi]])
                    tile_i = q0 // 128 + j
                    S.op("dve", I("tensor_scalar", out=yall[:, tile_i, h * 64:(h + 1) * 64], in0=ops_[oi][:, 0:64], scalar1=rc[oi][:, 0:1],
                                  scalar2=None, op0=ALU.mult),
                         reads=[t_ops[oi], t_rc[oi]], writes=[t_y[tile_i]], merge=(h > 0))
        if LVH == 8:
            tail = GroupTail(G, l, 3, sb, pp, "ml_t")
            ntile = (qblocks[-1][0] + qblocks[-1][1]) // 128 if LVQ >= 8 else LVQ * 4
            tiles = list(range(min(32, ntile))) + ([32, 33] if do_ctx else [])
            for ti in tiles:
                tail.emit(yall[:, ti, :], t_y[ti], ti * 128)
        else:
            G.dbg_yall = (yall, t_y)
        S.flush(f"mla{l}")


def phase_outproj(G, l, do_ctx):
    nc, S = G.nc, G.S
    with ExitStack() as ps:
        sb, pp = _alloc(G, ps)
        wo = sb("op_wo", [128, 16, D], BF16)
        t_w = Tok()
        for c0 in range(0, D, 512):
            S.dma("pool", I("dma_start", out=wo[:, :, c0:c0 + 512], in_=G.w_out[l][:, c0:c0 + 512].rearrange("(j p) n -> p j n", p=128)),
                  writes=[t_w], merge=True)
        wr = sb("op_wr", [128, 16, NE], F32)
        S.dma("sp", I("dma_start", out=wr[:], in_=G.w_router[l].rearrange("(j p) e -> p j e", p=128)), writes=[t_w], merge=True)
        g1b = sb("op_g1b", [128, D], F32)
        sh2b = sb("op_sh2b", [128, D], F32)
        gs2b = sb("op_gs2b", [128, D], F32)
        t_bc = Tok()
        NB = 2
        xt = [sb(f"op_x{i}", [128, D], F32) for i in range(NB)]
        t_xt = [Tok() for _ in range(NB)]
        xn = [sb(f"op_xn{i}", [128, D], F32) for i in range(NB)]
        t_xn = [Tok() for _ in range(NB)]
        fx = [sb(f"op_fx{i}", [128, D], F32) for i in range(NB)]
        t_fx = [Tok() for _ in range(NB)]
        fxb = [sb(f"op_fxb{i}", [128, D], BF16) for i in range(NB)]
        t_fxb = [Tok() for _ in range(NB)]
        junk = sb("op_junk", [128, D], BF16)
        t_junk = Tok()
        yn = [sb(f"op_yn{i}", [128, 16, 128], BF16) for i in range(NB)]
        t_yn = [Tok() for _ in range(NB)]
        fxT = [sb(f"op_fxT{i}", [128, 16, 128], F32) for i in range(NB)]
        t_fxT = [Tok() for _ in range(NB)]
        ss = [sb(f"op_ss{i}", [128, 1], F32) for i in range(NB)]
        t_ss = [Tok() for _ in range(NB)]
        sm = [sb(f"op_sm{i}", [128, 4], F32) for i in range(NB)]
        t_sm = [Tok() for _ in range(NB)]
        ex = [sb(f"op_ex{i}", [128, NE], F32) for i in range(NB)]
        t_ex = [Tok() for _ in range(NB)]
        acc = [pp(f"op_acc{i}", [128, 512]) for i in range(4)]
        t_acc = [Tok() for _ in range(4)]
        trp = [pp(f"op_tr{i}", [128, 512]) for i in range(2)]
        t_trp = [Tok(), Tok()]
        lgp = [pp(f"op_lg{i}", [128, 512]) for i in range(2)]
        t_lgp = [Tok(), Tok()]
        import os
        LV = int(os.environ.get("OP_STOP", "99"))
        tiles = (list(range(32)) + ([32, 33] if do_ctx else []))[:LV]
        cur_s = None
        kt = [0]
        def part1(k, ti):
            nonlocal cur_s
            s_ = 1 if ti >= 32 else 0
            if s_ != cur_s:
                cur_s = s_
                S.dma("sp", I("dma_start", out=g1b[:], in_=G.modrow[l, s_:s_ + 1, 2 * D:3 * D].to_broadcast([128, D])),
                      reads=[G.t["modrow"]], writes=[t_bc])
                S.dma("sp", I("dma_start", out=sh2b[:], in_=G.modrow[l, s_:s_ + 1, 3 * D:4 * D].to_broadcast([128, D])),
                      reads=[G.t["modrow"]], writes=[t_bc], merge=True)
                S.dma("sp", I("dma_start", out=gs2b[:], in_=G.modrow[l, s_:s_ + 1, 4 * D:5 * D].to_broadcast([128, D])),
                      reads=[G.t["modrow"]], writes=[t_bc], merge=True)
                S.dma("sp", I("dma_start", out=fx[0][:], in_=G.norm2_g[l:l + 1, :].to_broadcast([128, D])), writes=[t_fx[0]])
                S.op("dve", I("scalar_tensor_tensor", out=gs2b[:], in0=gs2b[:], scalar=1.0, in1=fx[0][:], op0=ALU.add, op1=ALU.mult),
                     reads=[t_bc, t_fx[0]], writes=[t_bc])
            tok0 = ti * 128
            i = k % NB
            S.dma("sp", I("dma_start", out=yn[i][:], in_=G.ynT[:, tok0:tok0 + 128].rearrange("(j p) n -> p j n", p=128)),
                  reads=[G.t["ynT"]], writes=[t_yn[i]])
            S.dma("sp", I("dma_start", out=xt[i][:], in_=G.xres[tok0:tok0 + 128, :]), reads=[G.t["xres"]], writes=[t_xt[i]])
            for nb in range(4):
                for j in range(16):
                    S.op("pe", I("matmul", acc[nb][:], lhsT=yn[i][:, j, :], rhs=wo[:, j, nb * 512:(nb + 1) * 512], start=(j == 0), stop=(j == 15)),
                         reads=[t_yn[i], t_w], writes=[t_acc[nb]], merge=(j > 0))
                S.op("dve", I("tensor_tensor", out=xn[i][:, nb * 512:(nb + 1) * 512], in0=acc[nb][:], in1=g1b[:, nb * 512:(nb + 1) * 512], op=ALU.mult),
                     reads=[t_acc[nb], t_bc], writes=[t_xn[i]], merge=(nb > 0))
        def part1c(k, ti):
            tok0 = ti * 128
            i = k % NB
            S.op("pool", I("tensor_tensor", out=xn[i][:], in0=xn[i][:], in1=xt[i][:], op=ALU.add), reads=[t_xn[i], t_xt[i]], writes=[t_xn[i]])
            S.dma("sp", I("dma_start", out=G.xres[tok0:tok0 + 128, :], in_=xn[i][:]), reads=[t_xn[i]], writes=[G.t["xres"]], merge=True)
            S.op("act", I("activation", out=junk[:], in_=xn[i][:], func=AF.Square, accum_out=ss[i][:, 0:1]), reads=[t_xn[i]], writes=[t_junk, t_ss[i]])
            rstd_from_ss(G, ss[i][:, 0:1], ss[i][:, 0:1], D, [t_ss[i]], [t_ss[i]])
            S.op("dve", I("scalar_tensor_tensor", out=fx[i][:], in0=xn[i][:], scalar=ss[i][:, 0:1], in1=gs2b[:], op0=ALU.mult, op1=ALU.mult),
                 reads=[t_xn[i], t_ss[i], t_bc], writes=[t_fx[i]])
            S.op("pool", I("tensor_tensor", out=fx[i][:], in0=fx[i][:], in1=sh2b[:], op=ALU.add), reads=[t_fx[i], t_bc], writes=[t_fx[i]])
            S.op("act", I("copy", out=fxb[i][:], in_=fx[i][:]), reads=[t_fx[i]], writes=[t_fxb[i]])
            S.dma("sp", I("dma_start", out=G.fxd[tok0:tok0 + 128, :], in_=fxb[i][:]), reads=[t_fxb[i]], writes=[G.t["fxd"]], merge=True)

        def part2(k, ti):
            i = k % NB
            for q in range(4):
                ti_ = kt[0] % 2
                kt[0] += 1
                for jj in range(4):
                    j = q * 4 + jj
                    S.op("pe", I("transpose", out=trp[ti_][:, jj * 128:(jj + 1) * 128], in_=fx[i][:, j * 128:(j + 1) * 128], identity=G.ident_f[:]),
                         reads=[t_fx[i], G.t_const], writes=[t_trp[ti_]], merge=(jj > 0))
                if q % 2 == 0:
                    S.op("act", I("copy", out=fxT[i][:, q * 4:(q + 1) * 4, :], in_=trp[ti_][:].rearrange("p (a n) -> p a n", n=128)),
                         reads=[t_trp[ti_]], writes=[t_fxT[i]], merge=(q > 0))
                else:
                    S.op("dve", I("tensor_copy", out=fxT[i][:, q * 4:(q + 1) * 4, :], in_=trp[ti_][:].rearrange("p (a n) -> p a n", n=128)),
                         reads=[t_trp[ti_]], writes=[t_fxT[i]], merge=True)
            for j in range(16):
                S.op("pe", I("matmul", lgp[i][:, 0:NE], lhsT=fxT[i][:, j, :], rhs=wr[:, j, :], start=(j == 0), stop=(j == 15)),
                     reads=[t_fxT[i], t_w], writes=[t_lgp[i]], merge=(j > 0))
            S.op("dve", I("reduce_max", out=sm[i][:, 0:1], in_=lgp[i][:, 0:NE], axis=mybir.AxisListType.X), reads=[t_lgp[i]], writes=[t_sm[i]])
            S.op("dve", I("tensor_scalar", out=sm[i][:, 1:2], in0=sm[i][:, 0:1], scalar1=-1.0, scalar2=None, op0=ALU.mult), reads=[t_sm[i]], writes=[t_sm[i]])
            S.op("act", I("activation", out=ex[i][:], in_=lgp[i][:, 0:NE], func=AF.Exp, bias=sm[i][:, 1:2], accum_out=sm[i][:, 2:3]),
                 reads=[t_lgp[i], t_sm[i]], writes=[t_ex[i], t_sm[i]])
            S.op("dve", I("reciprocal", out=sm[i][:, 3:4], in_=sm[i][:, 2:3]), reads=[t_sm[i]], writes=[t_sm[i]])
            S.op("dve", I("tensor_scalar", out=G.aff_all[:, ti, :], in0=ex[i][:], scalar1=sm[i][:, 3:4], scalar2=None, op0=ALU.mult),
                 reads=[t_ex[i], t_sm[i]], writes=[G.t_aff], merge=True)

        for k in range(len(tiles) + 1):
            if k < len(tiles):
                part1(k, tiles[k])
            if k >= 1:
                part2(k - 1, tiles[k - 1])
            if k < len(tiles):
                part1c(k, tiles[k])
        if "affd" in G.debug:
            S.dma("sp", I("dma_start", out=G.affd.rearrange("(t p) e -> p t e", p=128), in_=G.aff_all[:]), reads=[G.t_aff], writes=[G.t["affd"]])
        S.flush(f"outproj{l}")


def phase_route(G, l, do_ctx):
    nc, S = G.nc, G.S
    NI = 24
    with ExitStack() as ps:
        sb, pp = _alloc(G, ps)
        U = sb("rt_U", [128, 128], BF16)
        iot = sb("rt_iota", [128, 512], F32)
        pidx = sb("rt_pidx", [128, 4], F32)
        tgrid = sb("rt_tgrid", [128, 34, NE], F32)
        t_c = Tok()
        S.dma("sp", I("dma_start", out=U[:], in_=G.triu), writes=[t_c], merge=True)
        S.dma("sp", I("dma_start", out=iot[:], in_=G.iota512), writes=[t_c], merge=True)
        S.dma("sp", I("dma_start", out=pidx[:], in_=G.pidx), writes=[t_c], merge=True)
        S.dma("sp", I("dma_start", out=tgrid[:].rearrange("p t e -> p (t e)"), in_=G.tgrid), writes=[t_c], merge=True)
        aff = G.aff_all
        sets = [(0, 32, CAP_L)] + ([(32, 34, CAP_C)] if do_ctx else [])
        R = sb("rt_R", [128, 34, NE, 6], BF16)
        t_R = Tok()
        r1 = sb("rt_r1", [128, 34, NE], F32)
        t_r1 = Tok()
        S.op("pool", I("memset", R[:], 1.0), writes=[t_R])
        S.op("dve", I("tensor_scalar", out=R[:, :, :, 0], in0=tgrid[:], scalar1=0.0, scalar2=pidx[:, 0:1], op0=ALU.mult, op1=ALU.add),
             reads=[t_c], writes=[t_R])
        S.op("dve", I("tensor_copy", out=R[:, :, :, 1], in_=tgrid[:]), reads=[t_c], writes=[t_R])
        S.op("dve", I("tensor_copy", out=R[:, :, :, 2], in_=aff[:]), reads=[G.t_aff], writes=[t_R])
        S.op("dve", I("tensor_tensor", out=r1[:], in0=aff[:], in1=R[:, :, :, 2], op=ALU.subtract), reads=[G.t_aff, t_R], writes=[t_r1])
        S.op("dve", I("tensor_copy", out=R[:, :, :, 3], in_=r1[:]), reads=[t_r1], writes=[t_R])
        S.op("dve", I("tensor_tensor", out=r1[:], in0=r1[:], in1=R[:, :, :, 3], op=ALU.subtract), reads=[t_r1, t_R], writes=[t_r1])
        S.op("dve", I("tensor_copy", out=R[:, :, :, 4], in_=r1[:]), reads=[t_r1], writes=[t_R])
        mids, t_mid, cmps, t_cmp, parts, t_part, cps, t_cps, tmps, t_tmp = [], [], [], [], [], [], [], [], [], []
        for si, (t0, t1, cap) in enumerate(sets):
            mids.append(sb(f"rt_mid{si}", [128, NE], F32)); t_mid.append(Tok())
            cmps.append(sb(f"rt_cmp{si}", [128, t1 - t0, NE], BF16)); t_cmp.append(Tok())
            parts.append(sb(f"rt_part{si}", [128, NE], F32)); t_part.append(Tok())
            cps.append(pp(f"rt_cps{si}", [128, 512])); t_cps.append(Tok())
            tmps.append(sb(f"rt_tmp{si}", [128, NE], F32)); t_tmp.append(Tok())
            S.op("pool", I("memset", mids[si][:], 0.5), writes=[t_mid[si]])
        for k in range(NI):
            wk = 0.5 ** (k + 1)
            wn = 0.5 ** (k + 2) if k < NI - 1 else 0.0
            for si, (t0, t1, cap) in enumerate(sets):
                nt = t1 - t0
                S.op("dve", I("tensor_tensor", out=cmps[si][:], in0=aff[:, t0:t1, :],
                              in1=mids[si][:].unsqueeze(1).to_broadcast([128, nt, NE]), op=ALU.is_gt),
                     reads=[G.t_aff, t_mid[si]], writes=[t_cmp[si]])
                S.op("dve", I("tensor_reduce", out=parts[si][:], in_=cmps[si][:].rearrange("p t e -> p e t"),
                              axis=mybir.AxisListType.X, op=ALU.add),
                     reads=[t_cmp[si]], writes=[t_part[si]])
                S.op("pe", I("matmul", cps[si][:, 0:NE], lhsT=G.ones_f[:], rhs=parts[si][:], start=True, stop=True),
                     reads=[t_part[si], G.t_const], writes=[t_cps[si]])
                S.op("dve", I("tensor_scalar", out=tmps[si][:], in0=cps[si][:, 0:NE], scalar1=cap + 0.5, scalar2=wk, op0=ALU.is_gt, op1=ALU.mult),
                     reads=[t_cps[si]], writes=[t_tmp[si]])
                S.op("dve", I("scalar_tensor_tensor", out=mids[si][:], in0=tmps[si][:], scalar=-wn, in1=mids[si][:], op0=ALU.add, op1=ALU.add),
                     reads=[t_tmp[si], t_mid[si]], writes=[t_mid[si]])
        Mb = sb("rt_Mb", [128, 34, NE], BF16)
        Mf = sb("rt_Mf", [128, 34, NE], F32)
        t_M = Tok()
        if not do_ctx:
            S.op("pool", I("memset", Mb[:, 32:34, :], 0.0), writes=[t_M])
            S.op("pool", I("memset", Mf[:, 32:34, :], 0.0), writes=[t_M], merge=True)
        for si, (t0, t1, cap) in enumerate(sets):
            nt = t1 - t0
            S.op("dve", I("tensor_tensor", out=Mf[:, t0:t1, :], in0=aff[:, t0:t1, :],
                          in1=mids[si][:].unsqueeze(1).to_broadcast([128, nt, NE]), op=ALU.is_gt),
                 reads=[G.t_aff, t_mid[si]], writes=[t_M], merge=True)
        S.op("dve", I("tensor_copy", out=Mb[:, 0:34 if do_ctx else 32, :], in_=Mf[:, 0:34 if do_ctx else 32, :]), reads=[t_M], writes=[t_M])
        posp = [pp(f"rt_pos{i}", [128, 512]) for i in range(2)]
        totp = [pp(f"rt_tot{i}", [128, 512]) for i in range(2)]
        t_pp = Tok()
        spans = [(0, 32)] + ([(32, 34)] if do_ctx else [])
        for i, (t0, t1) in enumerate(spans):
            n = (t1 - t0) * NE
            rhs = Mb[:, t0:t1, :].rearrange("p t e -> p (t e)")
            S.op("pe", I("matmul", posp[i][:, 0:n], lhsT=U[:], rhs=rhs, start=True, stop=True), reads=[t_M, t_c], writes=[t_pp], merge=(i > 0))
            S.op("pe", I("matmul", totp[i][:, 0:n], lhsT=G.ones_b[:], rhs=rhs, start=True, stop=True), reads=[t_M, G.t_const], writes=[t_pp], merge=True)
        incl = sb("rt_incl", [128, 34, NE], F32)
        t_incl = Tok()
        onesf = sb("rt_onesf", [128, 32], F32)
        S.op("pool", I("memset", onesf[:], 1.0), writes=[t_c], merge=True)
        for i, (t0, t1) in enumerate(spans):
            nt = t1 - t0
            tv = totp[i][:, 0:nt * NE].rearrange("p (t e) -> p t e", e=NE)
            for e in range(NE):
                S.op("dve", I("tensor_tensor_scan", out=incl[:, t0:t1, e], data0=onesf[:, 0:nt], data1=tv[:, :, e], initial=0.0,
                              op0=ALU.mult, op1=ALU.add),
                     reads=[t_pp, t_c], writes=[t_incl], merge=(e > 0 or i > 0))
        posf = sb("rt_posf", [128, 34, NE], F32)
        t_posf = Tok()
        for i, (t0, t1) in enumerate(spans):
            nt = t1 - t0
            tv = totp[i][:, 0:nt * NE].rearrange("p (t e) -> p t e", e=NE)
            pv = posp[i][:, 0:nt * NE].rearrange("p (t e) -> p t e", e=NE)
            S.op("dve", I("tensor_tensor", out=incl[:, t0:t1, :], in0=incl[:, t0:t1, :], in1=tv, op=ALU.subtract),
                 reads=[t_incl, t_pp], writes=[t_incl])
            S.op("dve", I("tensor_tensor", out=posf[:, t0:t1, :], in0=incl[:, t0:t1, :], in1=pv, op=ALU.add),
                 reads=[t_incl, t_pp], writes=[t_posf], merge=(i > 0))
        Oall = [sb(f"rt_O{i}", [128, 32, 512], BF16) for i in range(2)]
        t_O = [[Tok() for _ in range(32)] for _ in range(2)]
        Oc = [sb(f"rt_Oc{i}", [128, 2, 128], BF16) for i in range(2)]
        t_Oc = [Tok(), Tok()]
        ips = [pp(f"rt_ips{i}", [128, 512]) for i in range(2)]
        t_ips = [Tok(), Tok()]
        isb = [sb(f"rt_isb{i}", [128, 5, 8], F32) for i in range(2)]
        t_isb = [Tok(), Tok()]
        ia = [sb(f"rt_ia{i}", [128, 5], F32) for i in range(2)]
        t_ia = [Tok(), Tok()]
        nch = 5 if do_ctx else 4
        import os
        LVE = int(os.environ.get("RT_EXP", "16"))
        for e in range(LVE):
            b = e % 2
            for t in range(32):
                S.op("dve", I("tensor_scalar", out=Oall[b][:, t, :], in0=iot[:], scalar1=posf[:, t, e:e + 1], scalar2=Mf[:, t, e:e + 1],
                              op0=ALU.is_equal, op1=ALU.mult),
                     reads=[t_c, t_posf, t_M], writes=[t_O[b][t]])
            first = True
            for sc in range(4):
                for t in range(32):
                    S.op("pe", I("matmul", ips[b][:, sc * 8:sc * 8 + 6], lhsT=Oall[b][:, t, sc * 128:(sc + 1) * 128], rhs=R[:, t, e, :],
                                 start=(t == 0), stop=(t == 31)),
                         reads=[t_O[b][t], t_R], writes=[t_ips[b]], merge=not first)
                    first = False
            if do_ctx:
                for t in range(2):
                    S.op("dve", I("tensor_scalar", out=Oc[b][:, t, :], in0=iot[:, 0:128], scalar1=posf[:, 32 + t, e:e + 1],
                                  scalar2=Mf[:, 32 + t, e:e + 1], op0=ALU.is_equal, op1=ALU.mult),
                         reads=[t_c, t_posf, t_M], writes=[t_Oc[b]], merge=(t > 0))
                for t in range(2):
                    S.op("pe", I("matmul", ips[b][:, 32:38], lhsT=Oc[b][:, t, :], rhs=R[:, 32 + t, e, :], start=(t == 0), stop=(t == 1)),
                         reads=[t_Oc[b], t_R], writes=[t_ips[b]], merge=True)
            S.op("act", I("copy", out=isb[b][:, 0:nch, 0:6], in_=ips[b][:, 0:nch * 8].rearrange("p (c k) -> p c k", k=8)[:, :, 0:6]),
                 reads=[t_ips[b]], writes=[t_isb[b]])
            v = isb[b]
            S.op("dve", I("scalar_tensor_tensor", out=ia[b][:, 0:nch], in0=v[:, 0:nch, 1], scalar=128.0, in1=v[:, 0:nch, 0], op0=ALU.mult, op1=ALU.add),
                 reads=[t_isb[b]], writes=[t_ia[b]])
            S.op("dve", I("scalar_tensor_tensor", out=ia[b][:, 0:nch], in0=v[:, 0:nch, 5], scalar=pidx[:, 2:3], in1=ia[b][:, 0:nch], op0=ALU.mult, op1=ALU.add),
                 reads=[t_isb[b], t_ia[b], t_c], writes=[t_ia[b]])
            S.op("dve", I("tensor_scalar", out=ia[b][:, 0:nch], in0=ia[b][:, 0:nch], scalar1=pidx[:, 1:2], scalar2=None, op0=ALU.add),
                 reads=[t_ia[b], t_c], writes=[t_ia[b]])
            S.op("dve", I("tensor_copy", out=G.idx_all[:, e, 0:nch], in_=ia[b][:, 0:nch]), reads=[t_ia[b]], writes=[G.t_idx], merge=(e > 0))
            S.op("pool", I("tensor_tensor", out=G.g_all[:, e, 0:nch], in0=v[:, 0:nch, 2], in1=v[:, 0:nch, 3], op=ALU.add),
                 reads=[t_isb[b]], writes=[G.t_idx], merge=True)
            S.op("pool", I("tensor_tensor", out=G.g_all[:, e, 0:nch], in0=G.g_all[:, e, 0:nch], in1=v[:, 0:nch, 4], op=ALU.add),
                 reads=[t_isb[b], G.t_idx], writes=[G.t_idx], merge=True)
        if "idxd" in G.debug:
            S.dma("sp", I("dma_start", out=G.idxd, in_=G.idx_all[:].rearrange("p e c -> p (e c)")), reads=[G.t_idx], writes=[G.t["idxd"]])
            S.dma("sp", I("dma_start", out=G.gd, in_=G.g_all[:].rearrange("p e c -> p (e c)")), reads=[G.t_idx], writes=[G.t["gd"]])
        S.flush(f"route{l}")


def phase_moe(G, l, do_ctx):
    nc, S = G.nc, G.S
    with ExitStack() as ps:
        sb, pp = _alloc(G, ps)
        nch = 5 if do_ctx else 4
        NS = 544 if do_ctx else 512
        FB = 256
        ns = 2 if do_ctx else 1
        g2b = []
        t_bc = Tok()
        for s_ in range(ns):
            a = sb(f"mo_g2b{s_}", [128, D], F32)
            S.dma("sp", I("dma_start", out=a[:], in_=G.modrow[l, s_:s_ + 1, 5 * D:6 * D].to_broadcast([128, D])),
                  reads=[G.t["modrow"]], writes=[t_bc], merge=True)
            g2b.append(a)
        xs = [sb(f"mo_xs{i}", [128, D], BF16) for i in range(2)]
        t_xs = [Tok(), Tok()]
        xsT = sb("mo_xsT", [128, 16, NS], BF16)
        t_xsT = Tok()
        wg = [sb(f"mo_wg{i}", [128, 16, FB], BF16) for i in range(2)]
        wu = [sb(f"mo_wu{i}", [128, 16, FB], BF16) for i in range(2)]
        t_wgu = [Tok(), Tok()]
        wd = [sb(f"mo_wd{i}", [128, 16, 512], BF16) for i in range(2)]
        t_wd = [Tok(), Tok()]
        hm = sb("mo_hm", [128, 16, NS], BF16)
        t_hm = [Tok() for _ in range(16)]
        st = [sb(f"mo_st{i}", [128, 544], F32) for i in range(2)]
        t_st = [Tok(), Tok()]
        yst = [sb(f"mo_y{i}", [128, D], F32) for i in range(nch)]
        t_yst = [Tok() for _ in range(nch)]
        if do_ctx:
            S.op("pool", I("memset", yst[4][:], 0.0), writes=[t_yst[4]])
        aps = [pp(f"mo_a{i}", [128, 512]) for i in range(2)]
        ups = [pp(f"mo_u{i}", [128, 512]) for i in range(2)]
        t_aps = [Tok(), Tok()]
        t_ups = [Tok(), Tok()]
        cps = pp("mo_c", [128, 512])
        t_cps = Tok()
        yps = [pp(f"mo_yp{i}", [128, 512]) for i in range(2)]
        t_yps = [Tok(), Tok()]
        trp = pp("mo_tr", [128, 1024], BF16)
        t_trp = Tok()
        import os
        LVE = int(os.environ.get("MOE_EXP", "16"))
        jobs = []
        for e in range(LVE):
            for fb in range(D // FB):
                jobs.append(("gu", e, fb))
            for db in range(4):
                jobs.append(("d", e, db))
        cnt = {"gu": 0, "d": 0}
        slot_of = {}

        def issue(jb):
            kind, e, b = jb
            i = cnt[kind] % 2
            cnt[kind] += 1
            slot_of[jb] = i
            if kind == "gu":
                S.dma("pool", I("dma_start", out=wg[i][:].rearrange("p j n -> p (j n)").rearrange("p (a m) -> p a m", m=2048),
                                in_=G.w_gate[l, e, b].rearrange("p (a m) -> p a m", m=2048)),
                      writes=[t_wgu[i]])
                S.dma("pool", I("dma_start", out=wu[i][:].rearrange("p j n -> p (j n)").rearrange("p (a m) -> p a m", m=2048),
                                in_=G.w_up[l, e, b].rearrange("p (a m) -> p a m", m=2048)),
                      writes=[t_wgu[i]], merge=True)
            else:
                S.dma("pool", I("dma_start", out=wd[i][:].rearrange("p j n -> p (j n)").rearrange("p (a m) -> p a m", m=2048),
                                in_=G.w_down[l, e, b].rearrange("p (a m) -> p a m", m=2048)),
                      writes=[t_wd[i]])

        def gather(e):
            for sc in range(nch):
                i = sc % 2
                m = 128 if sc < 4 else 32
                S.dma("pool", I("indirect_dma_start", out=xs[i][:], out_offset=None, in_=G.fxd[:, :],
                                in_offset=bass.IndirectOffsetOnAxis(ap=G.idx_all[:, e, sc:sc + 1], axis=0)),
                      reads=[G.t_idx, G.t["fxd"]], writes=[t_xs[i]])
                for q in range(2):
                    for jj in range(8):
                        j = q * 8 + jj
                        S.op("pe", I("transpose", out=trp[:, jj * 128:jj * 128 + m], in_=xs[i][0:m, j * 128:(j + 1) * 128],
                                     identity=G.ident_b[0:m, 0:m]),
                             reads=[t_xs[i], G.t_const], writes=[t_trp], merge=(jj > 0))
                    src = trp[:].rearrange("p (a n) -> p a n", n=128)[:, :, 0:m]
                    dst = xsT[:, q * 8:(q + 1) * 8, sc * 128:sc * 128 + m]
                    if q == 0:
                        S.op("act", I("copy", out=dst, in_=src), reads=[t_trp], writes=[t_xsT], merge=not (sc == 0))
                    else:
                        S.op("dve", I("tensor_copy", out=dst, in_=src), reads=[t_trp], writes=[t_xsT], merge=True)

        ji = 0
        issue(jobs[0])
        if LVE > 0:
            gather(0)
        kk = 0
        for e in range(LVE):
            for fb in range(D // FB):
                jb = jobs[ji]
                ji += 1
                if ji < len(jobs):
                    issue(jobs[ji])
                wi = slot_of[jb]
                for fl in range(FB // 128):
                    fo = fb * (FB // 128) + fl
                    ab = kk % 2
                    kk += 1
                    for j in range(16):
                        S.op("pe", I("matmul", aps[ab][:], lhsT=wg[wi][:, j, fl * 128:(fl + 1) * 128], rhs=xsT[:, j, 0:512],
                                     start=(j == 0), stop=(j == 15)),
                             reads=[t_wgu[wi], t_xsT], writes=[t_aps[ab]], merge=(j > 0))
                    for j in range(16):
                        S.op("pe", I("matmul", ups[ab][:], lhsT=wu[wi][:, j, fl * 128:(fl + 1) * 128], rhs=xsT[:, j, 0:512],
                                     start=(j == 0), stop=(j == 15)),
                             reads=[t_wgu[wi], t_xsT], writes=[t_ups[ab]], merge=(j > 0))
                    if do_ctx:
                        for j in range(16):
                            S.op("pe", I("matmul", cps[:, 0:32], lhsT=wg[wi][:, j, fl * 128:(fl + 1) * 128], rhs=xsT[:, j, 512:544],
                                         start=(j == 0), stop=(j == 15)),
                                 reads=[t_wgu[wi], t_xsT], writes=[t_cps], merge=(j > 0))
                        for j in range(16):
                            S.op("pe", I("matmul", cps[:, 32:64], lhsT=wu[wi][:, j, fl * 128:(fl + 1) * 128], rhs=xsT[:, j, 512:544],
                                         start=(j == 0), stop=(j == 15)),
                                 reads=[t_wgu[wi], t_xsT], writes=[t_cps], merge=True)
                    S.op("act", I("activation", out=st[ab][:, 0:512], in_=aps[ab][:], func=AF.Silu), reads=[t_aps[ab]], writes=[t_st[ab]])
                    S.op("dve", I("tensor_tensor", out=hm[:, fo, 0:512], in0=ups[ab][:], in1=st[ab][:, 0:512], op=ALU.mult),
                         reads=[t_ups[ab], t_st[ab]], writes=[t_hm[fo]])
                    if do_ctx:
                        S.op("act", I("activation", out=st[ab][:, 512:544], in_=cps[:, 0:32], func=AF.Silu), reads=[t_cps], writes=[t_st[ab]], merge=True)
                        S.op("dve", I("tensor_tensor", out=hm[:, fo, 512:544], in0=cps[:, 32:64], in1=st[ab][:, 512:544], op=ALU.mult),
                             reads=[t_cps, t_st[ab]], writes=[t_hm[fo]], merge=True)
            if e + 1 < LVE:
                gather(e + 1)
            for db in range(4):
                jb = jobs[ji]
                ji += 1
                if ji < len(jobs):
                    issue(jobs[ji])
                wi = slot_of[jb]
                for sc in range(nch):
                    m = 128 if sc < 4 else 32
                    s_ = 0 if sc < 4 else 1
                    yi = kk % 2
                    kk += 1
                    for fo in range(16):
                        S.op("pe", I("matmul", yps[yi][0:m, :], lhsT=hm[:, fo, sc * 128:sc * 128 + m], rhs=wd[wi][:, fo, :],
                                     start=(fo == 0), stop=(fo == 15)),
                             reads=[t_hm[fo], t_wd[wi]], writes=[t_yps[yi]], merge=(fo > 0))
                    S.op("dve", I("scalar_tensor_tensor", out=yst[sc][0:m, db * 512:(db + 1) * 512], in0=yps[yi][0:m, :],
                                  scalar=G.g_all[0:m, e, sc:sc + 1], in1=g2b[s_][0:m, db * 512:(db + 1) * 512], op0=ALU.mult, op1=ALU.mult),
                         reads=[t_yps[yi], G.t_idx, t_bc], writes=[t_yst[sc]], merge=(db > 0))
            for sc in range(nch):
                S.dma("pool", I("indirect_dma_start", out=G.xres[:, :],
                                out_offset=bass.IndirectOffsetOnAxis(ap=G.idx_all[:, e, sc:sc + 1], axis=0),
                                in_=yst[sc][:], in_offset=None, bounds_check=NPAD - 1, oob_is_err=True, compute_op=ALU.add),
                      reads=[t_yst[sc], G.t_idx, G.t["xres"]], writes=[G.t["xres"]])
        S.flush(f"moe{l}")


def phase_final(G):
    nc, S = G.nc, G.S
    with ExitStack() as ps:
        sb, pp = _alloc(G, ps)
        fg = sb("fi_g", [128, D], F32)
        t_fg = Tok()
        S.dma("sp", I("dma_start", out=fg[:], in_=G.final_g.rearrange("(o d) -> o d", o=1).to_broadcast([128, D])), writes=[t_fg])
        xt = [sb(f"fi_x{i}", [128, D], F32) for i in range(2)]
        t_xt = [Tok(), Tok()]
        ot = [sb(f"fi_o{i}", [128, D], F32) for i in range(2)]
        t_ot = [Tok(), Tok()]
        junk = sb("fi_junk", [128, D], BF16)
        t_junk = Tok()
        ss = [sb(f"fi_ss{i}", [128, 1], F32) for i in range(2)]
        t_ss = [Tok(), Tok()]
        for ti in range(32):
            i = ti % 2
            S.dma("sp", I("dma_start", out=xt[i][:], in_=G.xres[ti * 128:(ti + 1) * 128, :]), reads=[G.t["xres"]], writes=[t_xt[i]])
            S.op("act", I("activation", out=junk[:], in_=xt[i][:], func=AF.Square, accum_out=ss[i][:, 0:1]), reads=[t_xt[i]], writes=[t_junk, t_ss[i]])
            rstd_from_ss(G, ss[i][:, 0:1], ss[i][:, 0:1], D, [t_ss[i]], [t_ss[i]])
            S.op("dve", I("scalar_tensor_tensor", out=ot[i][:], in0=xt[i][:], scalar=ss[i][:, 0:1], in1=fg[:], op0=ALU.mult, op1=ALU.mult),
                 reads=[t_xt[i], t_ss[i], t_fg], writes=[t_ot[i]])
            S.dma("sp", I("dma_start", out=G.out[ti * 128:(ti + 1) * 128, :], in_=ot[i][:]), reads=[t_ot[i]], writes=[G.t["out"]], merge=True)
        S.flush("final")


_CONSTS = None


def make_in_maps(inputs, batches, names=None):
    global _CONSTS
    if _CONSTS is None:
        _CONSTS = host_consts()
    w = host_weights(inputs)
    maps = []
    for b in batches:
        m = dict(w)
        m.update(_CONSTS)
        xin = np.zeros((NPAD, D), np.float32)
        xin[:NL] = inputs["x"][b]
        xin[NL:NT] = inputs["ctx"][b]
        m["xin"] = xin
        m["cvec"] = np.ascontiguousarray(np.stack([inputs["c"][b], inputs["c_ctx"]], axis=1)).astype(np.float32)
        if names is not None:
            m = {k: m[k] for k in names}
        maps.append(m)
    return maps


_PROG = None


def kernel(**inputs):
    global _PROG
    inputs = {k: np.asarray(v) for k, v in inputs.items()}
    if _PROG is None:
        _PROG = build_program()
    nc, G = _PROG
    B = inputs["x"].shape[0]
    maps = make_in_maps(inputs, list(range(B)), G.in_names)
    res = run_bass_kernel_spmd(nc, maps, core_ids=list(range(B)))
    out = np.stack([np.asarray(r["out"]) for r in res.results], axis=0)
    return out.astype(np.float32)
```

```python
import numpy as np
import ml_dtypes
from contextlib import ExitStack
import concourse.bass as bass
import concourse.mybir as mybir
from concourse.bass_utils import run_bass_kernel_spmd

F32 = mybir.dt.float32
BF16 = mybir.dt.bfloat16
I32 = mybir.dt.int32
AF = mybir.ActivationFunctionType
ALU = mybir.AluOpType
NPBF = ml_dtypes.bfloat16

D = 2048
NL = 4096
NCX = 256
NT = NL + NCX
NPAD = NT + 128
DEPTH = 2
EPS = 1e-6
NE = 16
CAP_L = 512
CAP_C = 32

WA_COLS = 3456
OFF_CB, OFF_CC, OFF_CH = 0, 512, 1024
OFF_SQ, OFF_SQW = 1536, 2048
OFF_SK, OFF_SKW = 2560, 2688
OFF_SV = 2816
OFF_FU = 2944
WB_COLS = 960
OFF_CQ, OFF_CKV, OFF_KPA, OFF_KPB = 0, 512, 768, 864

SEM_ROLL = 30000
NDMA_SEMS = 24


def I(name, *args, **kwargs):
    return (name, args, kwargs)


class Tok:
    __slots__ = ("w", "r", "base")

    def __init__(self):
        self.w = {}
        self.r = {}
        self.base = {}


class Sched:
    ENGS = ("pe", "act", "dve", "pool", "sp")

    def __init__(self, nc, stack):
        self.nc = nc
        self.stack = stack
        self.sems = []
        self.prog = {e: [] for e in self.ENGS}
        self.cur_sem = {}
        self.cnt = {}
        for e in ("pe", "act", "dve", "pool"):
            self.cur_sem[e] = self._new_sem(e)
            self.cnt[e] = 0
        self.dma_sems = {q: [self._new_sem("dma" + q) for _ in range(NDMA_SEMS)] for q in ("sp", "pool", "act")}
        self.dma_cnt = {q: [0] * NDMA_SEMS for q in ("sp", "pool", "act")}
        self.dma_rr = {"sp": 0, "pool": 0, "act": 0}
        self.waited = {e: {} for e in self.ENGS}
        self.n_inst = 0
        self.regcache = {}
        self.marks = []
        self.tot_pe = 0

    def _new_sem(self, name):
        h = self.stack.enter_context(self.nc.semaphore(f"s_{name}_{len(self.sems)}"))
        self.sems.append(h)
        return len(self.sems) - 1

    def _collect(self, eng, reads, writes, merge):
        need = {}
        for t in reads:
            for s, v in t.w.items():
                if need.get(s, 0) < v:
                    need[s] = v
        for t in writes:
            src = (t.base, t.r) if merge else (t.w, t.r)
            for dct in src:
                for s, v in dct.items():
                    if need.get(s, 0) < v:
                        need[s] = v
        out = []
        wd = self.waited[eng]
        for s, v in need.items():
            if eng == "pe" and s == self.cur_sem["pe"]:
                continue
            if wd.get(s, 0) >= v:
                continue
            wd[s] = v
            out.append((s, v))
        return out

    def _post(self, ev, reads, writes, merge):
        s, v = ev
        for t in reads:
            if t.r.get(s, 0) < v:
                t.r[s] = v
        for t in writes:
            if merge:
                if t.r:
                    nb = dict(t.base)
                    for rs, rv in t.r.items():
                        if nb.get(rs, 0) < rv:
                            nb[rs] = rv
                    t.base = nb
                if t.w.get(s, 0) < v:
                    t.w[s] = v
            else:
                nb = dict(t.w)
                for rs, rv in t.r.items():
                    if nb.get(rs, 0) < rv:
                        nb[rs] = rv
                t.base = nb
                t.w = {s: v}
            t.r = {}

    def op(self, eng, fn, reads=(), writes=(), merge=False):
        waits = self._collect(eng, reads, writes, merge)
        if self.cnt[eng] >= SEM_ROLL:
            self.cur_sem[eng] = self._new_sem(eng)
            self.cnt[eng] = 0
        self.cnt[eng] += 1
        if eng == "pe":
            self.tot_pe += 1
        s = self.cur_sem[eng]
        v = self.cnt[eng]
        sems = self.sems

        def emit(h, waits=waits, s=s, fn=fn):
            for ws, wv in waits:
                h.wait_ge(sems[ws], wv)
            getattr(h, fn[0])(*fn[1], **fn[2]).then_inc(sems[s], 1)

        self.prog[eng].append(emit)
        self._post((s, v), reads, writes, merge)
        self.n_inst += 1

    def dma(self, eng, fn, reads=(), writes=(), merge=False):
        i = self.dma_rr[eng]
        self.dma_rr[eng] = (i + 1) % NDMA_SEMS
        s = self.dma_sems[eng][i]
        waits = self._collect(eng, reads, writes, merge)
        prev = self.dma_cnt[eng][i]
        if prev > 0 and self.waited[eng].get(s, 0) < prev:
            self.waited[eng][s] = prev
            waits.append((s, prev))
        self.dma_cnt[eng][i] += 16
        v = self.dma_cnt[eng][i]
        sems = self.sems

        def emit(h, waits=waits, s=s, fn=fn):
            for ws, wv in waits:
                h.wait_ge(sems[ws], wv)
            try:
                kw = fn[2]
                if isinstance(kw.get("bounds_check"), int):
                    kw = dict(kw)
                    key = (id(h), kw["bounds_check"])
                    if key not in self.regcache:
                        self.regcache[key] = h.to_reg(kw["bounds_check"])
                    kw["bounds_check"] = self.regcache[key]
                ins = getattr(h, fn[0])(*fn[1], **kw)
            except Exception:
                print("DMA FAIL", fn[0], {k: (getattr(v, "shape", v), getattr(v, "ap", None)) for k, v in fn[2].items()})
                raise
            ins.then_inc(sems[s], 16)

        self.prog[eng].append(emit)
        self._post((s, v), reads, writes, merge)
        self.n_inst += 1

    def wait_all(self, eng, toks):
        waits = self._collect(eng, toks, (), False)
        sems = self.sems

        def emit(h, waits=waits):
            for ws, wv in waits:
                h.wait_ge(sems[ws], wv)

        self.prog[eng].append(emit)

    def barrier(self):
        cur = {}
        for e in ("pe", "act", "dve", "pool"):
            if self.cnt[e] > 0:
                cur[self.cur_sem[e]] = self.cnt[e]
        for q in self.dma_sems:
            for i, s in enumerate(self.dma_sems[q]):
                if self.dma_cnt[q][i] > 0:
                    cur[s] = self.dma_cnt[q][i]
        sems = self.sems
        for eng in self.ENGS:
            wd = self.waited[eng]
            waits = []
            for s, v in cur.items():
                if wd.get(s, 0) < v:
                    wd[s] = v
                    waits.append((s, v))

            def emit(h, waits=waits):
                for ws, wv in waits:
                    h.wait_ge(sems[ws], wv)

            self.prog[eng].append(emit)

    def flush(self, name=None):
        self.barrier()
        self.regcache = {}
        self.marks.append((name, self.tot_pe))
        nc = self.nc
        prog = self.prog
        with nc.Block(name) as block:
            if prog["sp"]:
                @block.sync
                def _(h):
                    for f in prog["sp"]:
                        f(h)
            if prog["pe"]:
                @block.tensor
                def _(h):
                    for f in prog["pe"]:
                        f(h)
            if prog["act"]:
                @block.scalar
                def _(h):
                    for f in prog["act"]:
                        f(h)
            if prog["dve"]:
                @block.vector
                def _(h):
                    for f in prog["dve"]:
                        f(h)
            if prog["pool"]:
                @block.gpsimd
                def _(h):
                    for f in prog["pool"]:
                        f(h)
        self.prog = {e: [] for e in self.ENGS}


def _rope_tables(rot_dim):
    rows = np.repeat(np.arange(64, dtype=np.float32), 64)
    cols = np.tile(np.arange(64, dtype=np.float32), 64)
    n_freq = rot_dim // 4
    inv = (np.float32(10000.0) ** (-np.arange(n_freq, dtype=np.float32) / np.float32(n_freq))).astype(np.float32)
    ang = np.concatenate([rows[:, None] * inv, cols[:, None] * inv], axis=-1).astype(np.float32)
    return np.cos(ang).astype(np.float32), np.sin(ang).astype(np.float32)


def host_consts():
    c = {}
    cs, sn = _rope_tables(64)
    cos2 = np.ones((128, NT), np.float32)
    sin2 = np.zeros((128, NT), np.float32)
    for hh in range(2):
        cos2[hh * 64:hh * 64 + 32, :NL] = cs.T
        cos2[hh * 64 + 32:hh * 64 + 64, :NL] = cs.T
        sin2[hh * 64:hh * 64 + 32, :NL] = -sn.T
        sin2[hh * 64 + 32:hh * 64 + 64, :NL] = sn.T
    c["ropeS"] = np.stack([cos2, sin2], 0)
    cm, sm = _rope_tables(32)
    cosm = np.ones((96, NT), np.float32)
    sinm = np.zeros((96, NT), np.float32)
    cosm[64:80, :NL] = cm.T
    cosm[80:96, :NL] = cm.T
    sinm[64:80, :NL] = -sm.T
    sinm[80:96, :NL] = sm.T
    c["ropeM"] = np.stack([cosm, sinm], 0)
    k = np.arange(64)
    a = 2 * np.pi * np.outer(k, k) / 64.0
    c64 = np.zeros((2, 128, 128), np.float64)
    for g in range(2):
        c64[0, g * 64:(g + 1) * 64, g * 64:(g + 1) * 64] = np.cos(a) / 8.0
        c64[1, g * 64:(g + 1) * 64, g * 64:(g + 1) * 64] = np.sin(a) / 8.0
    c["dft64"] = c64.astype(NPBF)
    def pos_tables(n):
        nt = n // 128
        idx = np.arange(n, dtype=np.int64)
        m = (np.outer(idx, idx) % n).astype(np.float64)
        ang = 2 * np.pi * m / n
        cn = np.cos(ang) / np.sqrt(n)
        sn_ = -np.sin(ang) / np.sqrt(n)
        out = np.zeros((nt, 128, 2, nt, 128), np.float32)
        for kt in range(nt):
            blkc = cn[:, kt * 128:(kt + 1) * 128].reshape(nt, 128, 128)
            blks = sn_[:, kt * 128:(kt + 1) * 128].reshape(nt, 128, 128)
            out[kt, :, 0] = blkc.transpose(1, 0, 2)
            out[kt, :, 1] = blks.transpose(1, 0, 2)
        return out.reshape(nt, 128, 2 * nt * 128).astype(NPBF)
    c["dftL"] = pos_tables(NL)
    c["dftC"] = pos_tables(NCX)
    ident = np.eye(128, dtype=np.float32)
    c["identf"] = ident
    kk = np.arange(128)[:, None]
    qq = np.arange(128)[None, :]
    mprev = (kk >= qq).astype(np.float32)
    mnext = (kk <= qq).astype(np.float32)
    c["masks"] = np.stack([np.tile(mprev, (1, 4)), np.tile(mnext, (1, 4))], 0).astype(NPBF)
    c["triu"] = (kk < qq).astype(NPBF)
    c["iota512"] = np.tile(np.arange(512, dtype=np.float32)[None, :], (128, 1))
    pp_ = np.arange(128, dtype=np.float32)
    c["pidx"] = np.stack([pp_, NT + pp_, -(NT + pp_), np.zeros(128, np.float32)], 1).astype(np.float32)
    c["tgrid"] = np.tile(np.repeat(np.arange(34, dtype=np.float32), NE)[None, :], (128, 1))
    return c


def host_weights(inp):
    w = {}
    w_in = inp["w_in"]
    L = w_in.shape[0]
    wa = np.zeros((L, D, WA_COLS), np.float32)
    wa[:, :, 0:1536] = w_in[:, :, 0:1536]
    sq = w_in[:, :, 1536:2048]
    wa[:, :, OFF_SQ:OFF_SQ + 512] = sq
    sq4 = sq.reshape(L, D, 8, 2, 32)
    wa[:, :, OFF_SQW:OFF_SQW + 512] = sq4[:, :, :, ::-1, :].reshape(L, D, 512)
    sk = w_in[:, :, 2048:2176]
    wa[:, :, OFF_SK:OFF_SK + 128] = sk
    wa[:, :, OFF_SKW:OFF_SKW + 128] = sk.reshape(L, D, 2, 2, 32)[:, :, :, ::-1, :].reshape(L, D, 128)
    wa[:, :, OFF_SV:OFF_SV + 128] = w_in[:, :, 2176:2304]
    wa[:, :, OFF_FU:OFF_FU + 512] = w_in[:, :, 2304:2816]
    w["w_a"] = wa
    wb = np.zeros((L, D, WB_COLS), np.float32)
    wb[:, :, 0:768] = w_in[:, :, 2816:3584]
    kpe = w_in[:, :, 3584:3616]
    wb[:, :, OFF_KPA + 64:OFF_KPA + 96] = kpe
    wb[:, :, OFF_KPB + 64:OFF_KPB + 96] = kpe.reshape(L, D, 2, 16)[:, :, ::-1, :].reshape(L, D, 32)
    w["w_b"] = wb
    uq = inp["mla_w_uq"]
    uq4 = uq.reshape(L, 512, 8, 96)
    uqb = uq4.copy()
    uqb[:, :, :, 64:80] = uq4[:, :, :, 80:96]
    uqb[:, :, :, 80:96] = uq4[:, :, :, 64:80]
    w["w_uq"] = np.stack([uq, uqb.reshape(L, 512, 768)], 1)
    ukv = inp["mla_w_ukv"].reshape(L, 256, 8, 2, 64)
    w["w_uk"] = np.ascontiguousarray(ukv[:, :, :, 0, :]).reshape(L, 256, 512)
    w["w_uv"] = np.ascontiguousarray(ukv[:, :, :, 1, :]).reshape(L, 256, 512)
    for k_ in ("ada_b", "norm1_g", "norm2_g", "conv_w", "swa_sink", "mla_q_norm_g",
               "mla_kv_norm_g", "out_norm_g", "w_out", "w_router", "final_norm_g"):
        w[k_] = inp[k_]
    def tile_cols(a, W):
        lead = a.shape[:-2]
        K, N = a.shape[-2:]
        a6 = a.reshape(lead + (K // 128, 128, N // W, W))
        nd = len(lead)
        perm = tuple(range(nd)) + (nd + 2, nd + 1, nd + 0, nd + 3)
        return np.ascontiguousarray(a6.transpose(perm)).reshape(lead + (N // W, 128, (K // 128) * W))
    w["ada_w"] = tile_cols(inp["ada_w"], 512)
    if "w_gate" in inp:
        w["w_gate"] = tile_cols(inp["w_gate"], 256)
        w["w_up"] = tile_cols(inp["w_up"], 256)
        w["w_down"] = tile_cols(inp["w_down"], 512)
    return w


class Ctx:
    pass


def build_program(debug=(), n_layers=DEPTH, stop_after=None):
    nc = bass.Bass("TRN2", target_bir_lowering=False)
    G = Ctx()
    G.nc = nc
    G.debug = set(debug)
    din = {}

    def inp(name, shape, dt=F32):
        din[name] = nc.dram_tensor(name, list(shape), dt, kind="ExternalInput").ap()
        return din[name]

    G.xin = inp("xin", [NPAD, D])
    G.cvec = inp("cvec", [D, 2])
    G.ada_w = inp("ada_w", [DEPTH, 24, 128, 16 * 512])
    G.ada_b = inp("ada_b", [DEPTH, 6 * D])
    G.norm1_g = inp("norm1_g", [DEPTH, D])
    G.norm2_g = inp("norm2_g", [DEPTH, D])
    G.w_a = inp("w_a", [DEPTH, D, WA_COLS])
    G.w_b = inp("w_b", [DEPTH, D, WB_COLS])
    G.conv_w = inp("conv_w", [DEPTH, 3, 512])
    G.swa_sink = inp("swa_sink", [DEPTH, 8])
    G.q_norm_g = inp("mla_q_norm_g", [DEPTH, 512])
    G.kv_norm_g = inp("mla_kv_norm_g", [DEPTH, 256])
    G.w_uq = inp("w_uq", [DEPTH, 2, 512, 768])
    G.w_uk = inp("w_uk", [DEPTH, 256, 512])
    G.w_uv = inp("w_uv", [DEPTH, 256, 512])
    G.out_norm_g = inp("out_norm_g", [DEPTH, D])
    G.w_out = inp("w_out", [DEPTH, D, D])
    G.w_router = inp("w_router", [DEPTH, D, NE])
    if stop_after is None or stop_after in ("moe", "moetest"):
        G.w_gate = inp("w_gate", [DEPTH, NE, 8, 128, 16 * 256])
        G.w_up = inp("w_up", [DEPTH, NE, 8, 128, 16 * 256])
        G.w_down = inp("w_down", [DEPTH, NE, 4, 128, 16 * 512])
    G.final_g = inp("final_norm_g", [D])
    G.ropeS = inp("ropeS", [2, 128, NT])
    G.ropeM = inp("ropeM", [2, 96, NT])
    G.dft64 = inp("dft64", [2, 128, 128], BF16)
    G.dftL = inp("dftL", [32, 128, 2 * 32 * 128], BF16)
    G.dftC = inp("dftC", [2, 128, 2 * 2 * 128], BF16)
    G.identf = inp("identf", [128, 128])
    G.masks = inp("masks", [2, 128, 512], BF16)
    G.triu = inp("triu", [128, 128], BF16)
    G.iota512 = inp("iota512", [128, 512])
    G.pidx = inp("pidx", [128, 4])
    G.tgrid = inp("tgrid", [128, 34 * NE])

    G.out = nc.dram_tensor("out", [NL, D], F32, kind="ExternalOutput").ap()

    def scratch(name, shape, dt):
        kind = "ExternalOutput" if name in G.debug else "Internal"
        return nc.dram_tensor(name, list(shape), dt, kind=kind).ap()

    G.xres = scratch("xres", [NPAD, D], F32)
    G.hT = scratch("hT", [D, NT], BF16)
    G.cbT = scratch("cbT", [512, NT], F32)
    G.uT = scratch("uT", [512, NT], F32)
    G.sqT = scratch("sqT", [512, NT], BF16)
    G.skT = scratch("skT", [128, NT], BF16)
    G.svd = scratch("svd", [NT, 128], BF16)
    G.ucd = scratch("ucd", [NT, 512], BF16)
    G.usd = scratch("usd", [NT, 512], BF16)
    G.mqT = scratch("mqT", [8, 96, NT], BF16)
    G.mkT = scratch("mkT", [8, 96, NT], BF16)
    G.mvd = scratch("mvd", [NT, 512], BF16)
    G.ynT = scratch("ynT", [D, NT], BF16)
    G.fxd = scratch("fxd", [NPAD, D], BF16)
    G.modrow = scratch("modrow", [DEPTH, 2, 6 * D], F32)
    G.affd = scratch("affd", [NT, NE], F32)
    if "idxd" in G.debug:
        G.idxd = nc.dram_tensor("idxd", [128, NE * 5], I32, kind="ExternalOutput").ap()
        G.gd = nc.dram_tensor("gd", [128, NE * 5], F32, kind="ExternalOutput").ap()
    if stop_after == "moetest":
        G.aff_in = inp("aff_in", [NT, NE])
        G.fx_in = inp("fx_in", [NPAD, D], BF16)

    with ExitStack() as st:
        S = Sched(nc, st)
        G.S = S
        G.st = st
        G.t = {k: Tok() for k in ("xres", "hT", "cbT", "uT", "sqT", "skT", "svd", "ucd", "usd", "mqT",
                                  "mkT", "mvd", "ynT", "fxd", "out", "modrow", "affd", "idxd", "gd")}
        G.ident_f = st.enter_context(nc.sbuf_tensor("ident_f", [128, 128], F32))
        G.ident_b = st.enter_context(nc.sbuf_tensor("ident_b", [128, 128], BF16))
        G.ones_b = st.enter_context(nc.sbuf_tensor("ones_b", [128, 128], BF16))
        G.ones_f = st.enter_context(nc.sbuf_tensor("ones_f", [128, 128], F32))
        G.t_const = Tok()
        G.eps_t = st.enter_context(nc.sbuf_tensor("eps_t", [128, 1], F32))
        S.op("pool", I("memset", G.eps_t[:], EPS), writes=[G.t_const], merge=True)
        S.dma("sp", I("dma_start", out=G.ident_f[:], in_=G.identf), writes=[G.t_const])
        S.op("dve", I("tensor_copy", out=G.ident_b[:], in_=G.ident_f[:]), reads=[G.t_const], writes=[G.t_const])
        S.op("pool", I("memset", G.ones_b[:], 1.0), writes=[G.t_const], merge=True)
        S.op("pool", I("memset", G.ones_f[:], 1.0), writes=[G.t_const], merge=True)
        G.aff_all = st.enter_context(nc.sbuf_tensor("aff_all", [128, 34, NE], F32))
        G.t_aff = Tok()
        S.op("pool", I("memset", G.aff_all[:], 0.0), writes=[G.t_aff])
        G.idx_all = st.enter_context(nc.sbuf_tensor("idx_all", [128, NE, 5], I32))
        G.g_all = st.enter_context(nc.sbuf_tensor("g_all", [128, NE, 5], F32))
        G.t_idx = Tok()
        S.op("pool", I("memset", G.idx_all[:], 0), writes=[G.t_idx])
        S.op("pool", I("memset", G.g_all[:], 0.0), writes=[G.t_idx], merge=True)
        S.dma("sp", I("dma_start", out=G.xres.rearrange("(a p) d -> p a d", p=128),
                                          in_=G.xin.rearrange("(a p) d -> p a d", p=128)),
              writes=[G.t["xres"]])
        S.dma("pool", I("dma_start", out=G.fxd[NT:NPAD, :], in_=G.xin[NT:NPAD, :]), writes=[G.t["fxd"]], merge=True)
        S.flush("init")
        if stop_after == "moetest":
            phase_adaln(G, 0)
            S.dma("sp", I("dma_start", out=G.aff_all[:], in_=G.aff_in.rearrange("(t p) e -> p t e", p=128)), writes=[G.t_aff])
            S.dma("sp", I("dma_start", out=G.fxd.rearrange("(a p) d -> p a d", p=128), in_=G.fx_in.rearrange("(a p) d -> p a d", p=128)),
                  writes=[G.t["fxd"]])
            phase_route(G, 0, True)
            if "moe" in G.debug:
                phase_moe(G, 0, True)
            n_layers = 0
        for l in range(n_layers):
            last = (l == DEPTH - 1)
            phase_adaln(G, l)
            if stop_after == "adaln":
                break
            phase_proj_a(G, l)
            if stop_after == "proj_a":
                break
            phase_proj_b(G, l)
            if stop_after == "proj_b":
                break
            phase_conv(G, l, not last)
            if stop_after == "conv":
                break
            phase_swa(G, l, not last)
            if stop_after == "swa":
                break
            phase_fnet(G, l, not last)
            if stop_after == "fnet":
                break
            phase_mla(G, l, not last)
            if stop_after == "mla":
                break
            phase_outproj(G, l, not last)
            if stop_after == "outproj":
                break
            phase_route(G, l, not last)
            if stop_after == "route":
                break
            phase_moe(G, l, not last)
            if stop_after == "moe":
                break
        if stop_after is None and n_layers == DEPTH:
            phase_final(G)
        S.wait_all("sp", list(G.t.values()))
        S.flush("fin")
        G.n_inst = S.n_inst
    G.in_names = list(din.keys())
    return nc, G


def _alloc(G, ps):
    nc = G.nc
    G.uid = getattr(G, "uid", 0) + 1
    u = G.uid
    sb = lambda name, shape, dt: ps.enter_context(nc.sbuf_tensor(f"{name}_{u}", list(shape), dt))
    pp = lambda name, shape, dt=F32: ps.enter_context(nc.psum_tensor(f"{name}_{u}", list(shape), dt))
    return sb, pp


BLOCKS = [(i * 512, 512, 0) for i in range(8)] + [(NL, NCX, 1)]


def phase_adaln(G, l):
    nc, S = G.nc, G.S
    with ExitStack() as ps:
        sb, pp = _alloc(G, ps)
        cT = sb("ad_cT", [128, 16, 2], F32)
        sl = sb("ad_sl", [128, 16, 64], BF16)
        sel = sb("ad_sel", [1, 64], BF16)
        brow = sb("ad_brow", [1, 6 * D], BF16)
        rows = sb("ad_rows", [64, 6 * D], F32)
        wt = [sb(f"ad_wt{i}", [128, 16, 512], BF16) for i in range(2)]
        pacc = [pp(f"ad_ps{i}", [64, 512]) for i in range(2)]
        t_c, t_sl, t_sel, t_b, t_rows = Tok(), Tok(), Tok(), Tok(), Tok()
        t_wt = [Tok(), Tok()]
        t_ps = [Tok(), Tok()]
        S.dma("sp", I("dma_start", out=cT[:], in_=G.cvec.rearrange("(j p) t -> p j t", p=128)), writes=[t_c])
        S.op("pool", I("memset", sl[:], 0.0), writes=[t_sl])
        S.op("pool", I("memset", sel[:], 0.0), writes=[t_sel])
        S.op("pool", I("memset", sel[0:1, 0:1], 1.0), writes=[t_sel])
        S.op("pool", I("memset", sel[0:1, 32:33], 1.0), writes=[t_sel])
        S.op("act", I("activation", out=sl[:, :, 0], in_=cT[:, :, 0], func=AF.Silu), reads=[t_c], writes=[t_sl])
        S.op("act", I("activation", out=sl[:, :, 32], in_=cT[:, :, 1], func=AF.Silu), reads=[t_c], writes=[t_sl])
        for q in range(6):
            S.dma("pool", I("dma_start", out=brow[0:1, q * D:(q + 1) * D], in_=G.ada_b[l:l + 1, q * D:(q + 1) * D]),
                  writes=[t_b], merge=True)
        for nb in range(24):
            b = nb % 2
            S.dma("pool", I("dma_start", out=wt[b][:].rearrange("p j n -> p (j n)").rearrange("p (a m) -> p a m", m=2048),
                            in_=G.ada_w[l, nb].rearrange("p (a m) -> p a m", m=2048)),
                  writes=[t_wt[b]])
            for j in range(16):
                S.op("pe", I("matmul", pacc[b][:], lhsT=sl[:, j, :], rhs=wt[b][:, j, :], start=(j == 0), stop=False),
                     reads=[t_sl, t_wt[b]], writes=[t_ps[b]], merge=(j > 0))
            S.op("pe", I("matmul", pacc[b][:], lhsT=sel[:], rhs=brow[0:1, nb * 512:(nb + 1) * 512], start=False, stop=True),
                 reads=[t_sel, t_b], writes=[t_ps[b]], merge=True)
            S.op("act", I("copy", out=rows[0:1, nb * 512:(nb + 1) * 512], in_=pacc[b][0:1, :]),
                 reads=[t_ps[b]], writes=[t_rows], merge=True)
            S.op("dve", I("tensor_copy", out=rows[32:33, nb * 512:(nb + 1) * 512], in_=pacc[b][32:33, :]),
                 reads=[t_ps[b]], writes=[t_rows], merge=True)
        S.dma("sp", I("dma_start", out=G.modrow[l, 0:1, :], in_=rows[0:1, :]), reads=[t_rows], writes=[G.t["modrow"]], merge=True)
        S.dma("sp", I("dma_start", out=G.modrow[l, 1:2, :], in_=rows[32:33, :]), reads=[t_rows], writes=[G.t["modrow"]], merge=True)
        S.flush(f"adaln{l}")


def load_T(G, row_aps, sb, pp, name):
    S = G.S
    nv = len(row_aps)
    t_stg0 = Tok()
    n = max(ap.shape[-1] for ap in row_aps) // 128
    stg = sb(name + "_stg", [16, nv, 128], F32)
    out = sb(name, [128, nv, n], F32)
    S.op("pool", I("memset", stg[:], 0.0), writes=[t_stg0])
    pst_full = pp(name + "_ps", [128, 512])
    pst = pst_full[:, 0:nv * 16].rearrange("p (v n) -> p v n", n=16)
    t_stg, t_ps, t_out = t_stg0, Tok(), Tok()
    for v, ap in enumerate(row_aps):
        S.dma("sp", I("dma_start", out=stg[0:ap.shape[-1] // 128, v, :], in_=ap.rearrange("(j p) -> j p", p=128)),
              reads=[G.t["modrow"]], writes=[t_stg], merge=(v > 0))
    for v in range(nv):
        S.op("pe", I("transpose", out=pst[:, v, 0:n], in_=stg[0:n, v, :], identity=G.ident_f[0:n, 0:n]),
             reads=[t_stg, G.t_const], writes=[t_ps], merge=(v > 0))
    S.op("dve", I("tensor_copy", out=out[:], in_=pst[:, :, 0:n]), reads=[t_ps], writes=[t_out])
    return out, t_out


def rstd_from_ss(G, out_ap, ss_ap, n, reads, writes, merge=False):
    S = G.S
    S.op("act", I("activation", out=out_ap, in_=ss_ap, func=AF.Ln, bias=G.eps_t[0:out_ap.shape[0], 0:1], scale=1.0 / n),
         reads=list(reads) + [G.t_const], writes=writes, merge=merge)
    S.op("act", I("activation", out=out_ap, in_=out_ap, func=AF.Exp, scale=-0.5),
         reads=writes, writes=writes)


def phase_proj_a(G, l):
    nc, S = G.nc, G.S
    with ExitStack() as ps:
        sb, pp = _alloc(G, ps)
        NCOL = 2560
        wA = sb("pa_w", [128, 16, NCOL], BF16)
        t_w = Tok()
        for c0 in range(0, NCOL, 512):
            S.dma("pool", I("dma_start",
                out=wA[:, :, c0:c0 + 512], in_=G.w_a[l][:, c0:c0 + 512].rearrange("(j p) n -> p j n", p=128)),
                writes=[t_w], merge=True)
        mv, t_mv = load_T(G, [G.modrow[l, 0, 0:D], G.modrow[l, 1, 0:D], G.modrow[l, 0, D:2 * D],
                              G.modrow[l, 1, D:2 * D], G.norm1_g[l]], sb, pp, "pa_mv")
        sh1T = mv
        t_sh = t_mv
        gs1T = sb("pa_gs", [128, 2, 16], F32)
        t_gs = Tok()
        for s in range(2):
            S.op("dve", I("scalar_tensor_tensor", out=gs1T[:, s, :], in0=mv[:, 2 + s, :], scalar=1.0, in1=mv[:, 4, :],
                                                           op0=ALU.add, op1=ALU.mult),
                 reads=[t_mv], writes=[t_gs], merge=True)
        xt = [sb(f"pa_xt{i}", [128, D], F32) for i in range(2)]
        t_xt = [Tok(), Tok()]
        junk = sb("pa_junk", [128, D], BF16)
        t_junk = Tok()
        ssq = sb("pa_ss", [128, 8], F32)
        t_ss = [Tok() for _ in range(8)]
        xh = sb("pa_xh", [128, 4, D], BF16)
        t_xh = [Tok() for _ in range(4)]
        hTbs = [sb(f"pa_hT{i}", [128, 16, 512], BF16) for i in range(2)]
        t_hTs = [Tok(), Tok()]
        rS = [sb(f"pa_rS{i}", [128, 2, 512], F32) for i in range(2)]
        t_rS = [Tok(), Tok()]
        cc_s = sb("pa_cc", [128, 4, 512], F32)
        t_cc = [Tok() for _ in range(4)]
        t1 = sb("pa_t1", [128, 4, 512], F32)
        t_t1 = [Tok() for _ in range(4)]
        NST = 4
        stf = [sb(f"pa_stf{i}", [128, 512], F32) for i in range(NST)]
        t_stf = [Tok() for _ in range(NST)]
        stb = [sb(f"pa_stb{i}", [128, 512], BF16) for i in range(NST)]
        t_stb = [Tok() for _ in range(NST)]
        pT = [pp(f"pa_pT{i}", [128, 1024], BF16) for i in range(2)]
        t_pT = [Tok(), Tok()]
        NACC = 4
        acc = [pp(f"pa_acc{i}", [128, 512]) for i in range(NACC)]
        t_acc = [Tok() for _ in range(NACC)]
        cnt = {"f": 0, "b": 0, "a": 0, "x": 0}

        import os
        LV = int(os.environ.get("PA_STOP", "9"))
        for bi, (tok0, nb, s) in enumerate(BLOCKS):
            if LV <= 1 or (LV <= 3 and bi > 0):
                break
            ntile = nb // 128
            rb = bi % 2
            hTb = hTbs[bi % 2]
            t_hT = t_hTs[bi % 2]
            S.dma("sp", I("dma_start",
                out=rS[rb][:, :, 0:nb], in_=G.ropeS[:, :, tok0:tok0 + nb].rearrange("t p n -> p t n")),
                writes=[t_rS[rb]])
            for t in range(ntile):
                xi = cnt["x"] % 2
                cnt["x"] += 1
                si = (bi * 4 + t) % 8
                r0 = tok0 + t * 128
                S.dma("sp", I("dma_start", out=xt[xi][:], in_=G.xres[r0:r0 + 128, :]),
                      reads=[G.t["xres"]], writes=[t_xt[xi]])
                S.op("act", I("activation", out=junk[:], in_=xt[xi][:], func=AF.Square,
                                                                 accum_out=ssq[:, si:si + 1]),
                     reads=[t_xt[xi]], writes=[t_junk, t_ss[si]])
                rstd_from_ss(G, ssq[:, si:si + 1], ssq[:, si:si + 1], D, [t_ss[si]], [t_ss[si]])
                S.op("act", I("activation", out=xh[:, t, :], in_=xt[xi][:], func=AF.Copy,
                                                                      scale=ssq[:, si:si + 1]),
                     reads=[t_xt[xi], t_ss[si]], writes=[t_xh[t]])
            for j in range(16):
                pb = j % 2
                for t in range(ntile):
                    S.op("pe", I("transpose", out=pT[pb][:, t * 128:(t + 1) * 128],
                                                                     in_=xh[:, t, j * 128:(j + 1) * 128], identity=G.ident_b[:]),
                         reads=[t_xh[t], G.t_const], writes=[t_pT[pb]], merge=(t > 0))
                S.op("act", I("activation",
                    out=hTb[:, j, 0:nb], in_=pT[pb][:, 0:nb], func=AF.Identity,
                    bias=sh1T[:, s, j:j + 1], scale=gs1T[:, s, j:j + 1]),
                    reads=[t_pT[pb], t_sh, t_gs], writes=[t_hT], merge=(j > 0))
            S.dma("sp", I("dma_start",
                out=G.hT[:, tok0:tok0 + nb].rearrange("(j p) n -> p j n", p=128), in_=hTb[:, :, 0:nb]),
                reads=[t_hT], writes=[G.t["hT"]], merge=True)

            if LV <= 2:
                break
            def proj(c0, width):
                ai = cnt["a"] % NACC
                cnt["a"] += 1
                for j in range(16):
                    S.op("pe", I("matmul",
                        acc[ai][0:width, 0:nb], lhsT=wA[:, j, c0:c0 + width], rhs=hTb[:, j, 0:nb],
                        start=(j == 0), stop=(j == 15)),
                        reads=[t_w, t_hT], writes=[t_acc[ai]], merge=(j > 0))
                return ai

            def stage_f():
                i = cnt["f"] % NST
                cnt["f"] += 1
                return i

            def stage_b():
                i = cnt["b"] % NST
                cnt["b"] += 1
                return i

            for c in range(4):
                ai = proj(OFF_CB + c * 128, 128)
                fi = stage_f()
                S.op("act", I("copy", out=stf[fi][:, 0:nb], in_=acc[ai][:, 0:nb]),
                     reads=[t_acc[ai]], writes=[t_stf[fi]])
                S.dma("sp", I("dma_start", out=G.cbT[c * 128:(c + 1) * 128, tok0:tok0 + nb], in_=stf[fi][:, 0:nb]),
                      reads=[t_stf[fi]], writes=[G.t["cbT"]], merge=True)
                ai = proj(OFF_CC + c * 128, 128)
                S.op("act", I("copy", out=cc_s[:, c, 0:nb], in_=acc[ai][:, 0:nb]),
                     reads=[t_acc[ai]], writes=[t_cc[c]])
                ai = proj(OFF_CH + c * 128, 128)
                fi = stage_f()
                S.op("dve", I("tensor_tensor", out=stf[fi][:, 0:nb], in0=acc[ai][:, 0:nb],
                                                                        in1=cc_s[:, c, 0:nb], op=ALU.mult),
                     reads=[t_acc[ai], t_cc[c]], writes=[t_stf[fi]])
                S.dma("sp", I("dma_start", out=G.uT[c * 128:(c + 1) * 128, tok0:tok0 + nb], in_=stf[fi][:, 0:nb]),
                      reads=[t_stf[fi]], writes=[G.t["uT"]], merge=True)
            for c in range(4):
                ai = proj(OFF_SQ + c * 128, 128)
                S.op("dve", I("tensor_tensor", out=t1[:, c, 0:nb], in0=acc[ai][:, 0:nb],
                                                                        in1=rS[rb][:, 0, 0:nb], op=ALU.mult),
                     reads=[t_acc[ai], t_rS[rb]], writes=[t_t1[c]])
                ai = proj(OFF_SQW + c * 128, 128)
                fi = stage_f()
                S.op("dve", I("tensor_tensor", out=stf[fi][:, 0:nb], in0=acc[ai][:, 0:nb],
                                                                          in1=rS[rb][:, 1, 0:nb], op=ALU.mult),
                     reads=[t_acc[ai], t_rS[rb]], writes=[t_stf[fi]])
                bi_ = stage_b()
                S.op("pool", I("tensor_tensor", out=stb[bi_][:, 0:nb], in0=t1[:, c, 0:nb],
                                                                           in1=stf[fi][:, 0:nb], op=ALU.add),
                     reads=[t_t1[c], t_stf[fi]], writes=[t_stb[bi_]])
                S.dma("sp", I("dma_start", out=G.sqT[c * 128:(c + 1) * 128, tok0:tok0 + nb], in_=stb[bi_][:, 0:nb]),
                      reads=[t_stb[bi_]], writes=[G.t["sqT"]], merge=True)
        S.flush(f"proja{l}")


def phase_proj_b(G, l):
    nc, S = G.nc, G.S
    with ExitStack() as ps:
        sb, pp = _alloc(G, ps)
        w1 = sb("pb_w1", [128, 16, 896], BF16)
        w2 = sb("pb_w2", [128, 16, 960], BF16)
        wq = sb("pb_wq", [128, 4, 2, 768], BF16)
        wk = sb("pb_wk", [128, 2, 512], BF16)
        wv = sb("pb_wv", [128, 2, 512], BF16)
        d64 = sb("pb_d64", [128, 2, 128], BF16)
        t_w = Tok()
        for c0 in range(0, 896, 448):
            S.dma("pool", I("dma_start", out=w1[:, :, c0:c0 + 448],
                            in_=G.w_a[l][:, 2560 + c0:2560 + c0 + 448].rearrange("(j p) n -> p j n", p=128)),
                  writes=[t_w], merge=True)
        for c0 in range(0, 960, 480):
            S.dma("pool", I("dma_start", out=w2[:, :, c0:c0 + 480],
                            in_=G.w_b[l][:, c0:c0 + 480].rearrange("(j p) n -> p j n", p=128)),
                  writes=[t_w], merge=True)
        for ab in range(2):
            S.dma("pool", I("dma_start", out=wq[:, :, ab, :], in_=G.w_uq[l, ab].rearrange("(c p) n -> p c n", p=128)),
                  writes=[t_w], merge=True)
        S.dma("pool", I("dma_start", out=wk[:], in_=G.w_uk[l].rearrange("(c p) n -> p c n", p=128)), writes=[t_w], merge=True)
        S.dma("pool", I("dma_start", out=wv[:], in_=G.w_uv[l].rearrange("(c p) n -> p c n", p=128)), writes=[t_w], merge=True)
        S.dma("sp", I("dma_start", out=d64[:], in_=G.dft64.rearrange("t p n -> p t n")), writes=[t_w], merge=True)
        gT, t_gT = load_T(G, [G.q_norm_g[l], G.kv_norm_g[l]], sb, pp, "pb_g")
        hTb = [sb(f"pb_hT{i}", [128, 16, 512], BF16) for i in range(2)]
        t_hT = [Tok(), Tok()]
        rS = sb("pb_rS", [128, 2, 512], F32)
        rM = sb("pb_rM", [96, 2, 512], F32)
        t_r = Tok()
        cq_f = sb("pb_cqf", [128, 4, 512], F32)
        t_cqf = [Tok() for _ in range(4)]
        sqb = [sb(f"pb_sqb{i}", [128, 512], BF16) for i in range(2)]
        t_sqb = [Tok(), Tok()]
        rstd = sb("pb_rstd", [128, 2, 512], F32)
        t_rstd = [Tok(), Tok()]
        cqn = sb("pb_cqn", [128, 4, 512], BF16)
        t_cqn = Tok()
        ckf = sb("pb_ckf", [128, 2, 512], F32)
        t_ckf = [Tok(), Tok()]
        ckn = sb("pb_ckn", [128, 2, 512], BF16)
        t_ckn = Tok()
        fuT = sb("pb_fuT", [128, 4, 512], BF16)
        t_fuT = Tok()
        mq_st = sb("pb_mq", [96, 8, 512], BF16)
        t_mq = Tok()
        mk_st = sb("pb_mk", [96, 8, 512], BF16)
        t_mk = Tok()
        kpe_r = sb("pb_kpe", [96, 512], BF16)
        t_kpe = Tok()
        tA = [sb(f"pb_tA{i}", [128, 512], F32) for i in range(2)]
        t_tA = [Tok(), Tok()]
        tB = [sb(f"pb_tB{i}", [128, 512], F32) for i in range(2)]
        t_tB = [Tok(), Tok()]
        NST = 3
        stb = [sb(f"pb_stb{i}", [128, 512], BF16) for i in range(NST)]
        t_stb = [Tok() for _ in range(NST)]
        NACC = 3
        acc = [pp(f"pb_acc{i}", [128, 512]) for i in range(NACC)]
        t_acc = [Tok() for _ in range(NACC)]
        ssp = pp("pb_ss", [128, 512])
        t_ssp = Tok()
        tkp = [pp(f"pb_tk{i}", [128, 512]) for i in range(2)]
        t_tkp = [Tok(), Tok()]
        cnt = {"a": 0, "b": 0, "t": 0, "k": 0}
        import os
        LV = int(os.environ.get("PB_STOP", "9"))

        for bi, (tok0, nb, s) in enumerate(BLOCKS):
            if LV <= 3 and bi > 0:
                break
            ntile = nb // 128
            hb = bi % 2
            hT_ = hTb[hb]
            S.dma("sp", I("dma_start", out=hT_[:, :, 0:nb], in_=G.hT[:, tok0:tok0 + nb].rearrange("(j p) n -> p j n", p=128)),
                  reads=[G.t["hT"]], writes=[t_hT[hb]])
            S.dma("sp", I("dma_start", out=rS[:, :, 0:nb], in_=G.ropeS[:, :, tok0:tok0 + nb].rearrange("t p n -> p t n")),
                  writes=[t_r])
            S.dma("sp", I("dma_start", out=rM[:, :, 0:nb], in_=G.ropeM[:, :, tok0:tok0 + nb].rearrange("t p n -> p t n")),
                  writes=[t_r], merge=True)

            def proj(wt, c0, width, rhs_tile=None):
                ai = cnt["a"] % NACC
                cnt["a"] += 1
                for j in range(16):
                    S.op("pe", I("matmul", acc[ai][0:width, 0:nb], lhsT=wt[:, j, c0:c0 + width], rhs=hT_[:, j, 0:nb],
                                 start=(j == 0), stop=(j == 15)),
                         reads=[t_w, t_hT[hb]], writes=[t_acc[ai]], merge=(j > 0))
                return ai

            def rope_pair(aiA, aiB, p0, p1, table, out_ap, t_out, merge_out=False):
                ti = cnt["t"] % 2
                cnt["t"] += 1
                S.op("dve", I("tensor_tensor", out=tA[ti][p0:p1, 0:nb], in0=acc[aiA][p0:p1, 0:nb], in1=table[p0:p1, 0, 0:nb], op=ALU.mult),
                     reads=[t_acc[aiA], t_r], writes=[t_tA[ti]])
                S.op("dve", I("tensor_tensor", out=tB[ti][p0:p1, 0:nb], in0=acc[aiB][p0:p1, 0:nb], in1=table[p0:p1, 1, 0:nb], op=ALU.mult),
                     reads=[t_acc[aiB], t_r], writes=[t_tB[ti]])
                S.op("pool", I("tensor_tensor", out=out_ap, in0=tA[ti][p0:p1, 0:nb], in1=tB[ti][p0:p1, 0:nb], op=ALU.add),
                     reads=[t_tA[ti], t_tB[ti]], writes=[t_out], merge=merge_out)

            def stage_b():
                i = cnt["b"] % NST
                cnt["b"] += 1
                return i

            aiA = proj(w1, 0, 128)
            aiB = proj(w1, 128, 128)
            bi_ = stage_b()
            rope_pair(aiA, aiB, 0, 128, rS, stb[bi_][:, 0:nb], t_stb[bi_])
            S.dma("sp", I("dma_start", out=G.skT[:, tok0:tok0 + nb], in_=stb[bi_][:, 0:nb]), reads=[t_stb[bi_]],
                  writes=[G.t["skT"]], merge=True)
            ki = cnt["k"] % 2
            cnt["k"] += 1
            for t in range(ntile):
                for j in range(16):
                    S.op("pe", I("matmul", tkp[ki][:, t * 128:(t + 1) * 128], lhsT=hT_[:, j, t * 128:(t + 1) * 128],
                                 rhs=w1[:, j, 256:384], start=(j == 0), stop=(j == 15)),
                         reads=[t_w, t_hT[hb]], writes=[t_tkp[ki]], merge=(j > 0 or t > 0))
            bi_ = stage_b()
            S.op("act", I("copy", out=stb[bi_][:, 0:nb], in_=tkp[ki][:, 0:nb]), reads=[t_tkp[ki]], writes=[t_stb[bi_]])
            S.dma("sp", I("dma_start", out=G.svd[tok0:tok0 + nb, :].rearrange("(t p) c -> p t c", p=128),
                          in_=stb[bi_][:, 0:nb].rearrange("p (t c) -> p t c", c=128)),
                  reads=[t_stb[bi_]], writes=[G.t["svd"]], merge=True)
            for c in range(4):
                ai = proj(w1, 384 + c * 128, 128)
                S.op("act", I("copy", out=fuT[:, c, 0:nb], in_=acc[ai][:, 0:nb]), reads=[t_acc[ai]], writes=[t_fuT], merge=(c > 0))
            for t in range(ntile):
                for cs_, dst, key in ((0, G.ucd, "ucd"), (1, G.usd, "usd")):
                    ki = cnt["k"] % 2
                    cnt["k"] += 1
                    for c in range(4):
                        S.op("pe", I("matmul", tkp[ki][:, c * 128:(c + 1) * 128], lhsT=fuT[:, c, t * 128:(t + 1) * 128],
                                     rhs=d64[:, cs_, :], start=True, stop=True),
                             reads=[t_w, t_fuT], writes=[t_tkp[ki]], merge=(c > 0))
                    bi_ = stage_b()
                    S.op("act" if cs_ == 0 else "dve",
                         I("copy", out=stb[bi_][:], in_=tkp[ki][:]) if cs_ == 0 else I("tensor_copy", out=stb[bi_][:], in_=tkp[ki][:]),
                         reads=[t_tkp[ki]], writes=[t_stb[bi_]])
                    S.dma("sp", I("dma_start", out=dst[tok0 + t * 128:tok0 + (t + 1) * 128, :], in_=stb[bi_][:]),
                          reads=[t_stb[bi_]], writes=[G.t[key]], merge=True)
            def latent_norm(c0, nch, f_tile, t_f, n_tile, t_n, gi, ri):
                for c in range(nch):
                    ai = proj(w2, c0 + c * 128, 128)
                    S.op("act", I("copy", out=f_tile[:, c, 0:nb], in_=acc[ai][:, 0:nb]), reads=[t_acc[ai]], writes=[t_f[c]])
                    qi = c % 2
                    S.op("act", I("activation", out=sqb[qi][:, 0:nb], in_=acc[ai][:, 0:nb], func=AF.Square),
                         reads=[t_acc[ai]], writes=[t_sqb[qi]])
                    S.op("pe", I("matmul", ssp[:, 0:nb], lhsT=G.ones_b[:], rhs=sqb[qi][:, 0:nb], start=(c == 0), stop=(c == nch - 1)),
                         reads=[t_sqb[qi], G.t_const], writes=[t_ssp], merge=(c > 0))
                rstd_from_ss(G, rstd[:, ri, 0:nb], ssp[:, 0:nb], nch * 128, [t_ssp], [t_rstd[ri]])
                for c in range(nch):
                    S.op("dve", I("scalar_tensor_tensor", out=n_tile[:, c, 0:nb], in0=f_tile[:, c, 0:nb], scalar=gT[:, gi, c:c + 1],
                                  in1=rstd[:, ri, 0:nb], op0=ALU.mult, op1=ALU.mult),
                         reads=[t_f[c], t_gT, t_rstd[ri]], writes=[t_n], merge=(c > 0))

            latent_norm(0, 4, cq_f, t_cqf, cqn, t_cqn, 0, 0)
            latent_norm(512, 2, ckf, t_ckf, ckn, t_ckn, 1, 1)
            for h in range(8):
                ais = []
                for ab in range(2):
                    ai = cnt["a"] % NACC
                    cnt["a"] += 1
                    for c in range(4):
                        S.op("pe", I("matmul", acc[ai][0:96, 0:nb], lhsT=wq[:, c, ab, h * 96:(h + 1) * 96], rhs=cqn[:, c, 0:nb],
                                     start=(c == 0), stop=(c == 3)),
                             reads=[t_w, t_cqn], writes=[t_acc[ai]], merge=(c > 0))
                    ais.append(ai)
                S.op("act", I("copy", out=mq_st[0:64, h, 0:nb], in_=acc[ais[0]][0:64, 0:nb]), reads=[t_acc[ais[0]]],
                     writes=[t_mq], merge=(h > 0))
                rope_pair(ais[0], ais[1], 64, 96, rM, mq_st[64:96, h, 0:nb], t_mq, merge_out=True)
            S.dma("sp", I("dma_start", out=G.mqT[:, :, tok0:tok0 + nb].rearrange("h p n -> p h n"), in_=mq_st[:, :, 0:nb]),
                  reads=[t_mq], writes=[G.t["mqT"]], merge=True)
            aiA = proj(w2, OFF_KPA, 96)
            aiB = proj(w2, OFF_KPB, 96)
            rope_pair(aiA, aiB, 64, 96, rM, kpe_r[64:96, 0:nb], t_kpe)
            for h in range(8):
                ai = cnt["a"] % NACC
                cnt["a"] += 1
                for c in range(2):
                    S.op("pe", I("matmul", acc[ai][0:64, 0:nb], lhsT=wk[:, c, h * 64:(h + 1) * 64], rhs=ckn[:, c, 0:nb],
                                 start=(c == 0), stop=(c == 1)),
                         reads=[t_w, t_ckn], writes=[t_acc[ai]], merge=(c > 0))
                S.op("act", I("copy", out=mk_st[0:64, h, 0:nb], in_=acc[ai][0:64, 0:nb]), reads=[t_acc[ai]],
                     writes=[t_mk], merge=(h > 0))
                S.op("pool" if h % 2 else "dve", I("tensor_copy", out=mk_st[64:96, h, 0:nb], in_=kpe_r[64:96, 0:nb]),
                     reads=[t_kpe], writes=[t_mk], merge=True)
            S.dma("sp", I("dma_start", out=G.mkT[:, :, tok0:tok0 + nb].rearrange("h p n -> p h n"), in_=mk_st[:, :, 0:nb]),
                  reads=[t_mk], writes=[G.t["mkT"]], merge=True)
            for t in range(ntile):
                ki = cnt["k"] % 2
                cnt["k"] += 1
                for c in range(2):
                    S.op("pe", I("matmul", tkp[ki][:], lhsT=ckn[:, c, t * 128:(t + 1) * 128], rhs=wv[:, c, :],
                                 start=(c == 0), stop=(c == 1)),
                         reads=[t_w, t_ckn], writes=[t_tkp[ki]], merge=(c > 0))
                bi_ = stage_b()
                S.op("act", I("copy", out=stb[bi_][:], in_=tkp[ki][:]), reads=[t_tkp[ki]], writes=[t_stb[bi_]])
                S.dma("sp", I("dma_start", out=G.mvd[tok0 + t * 128:tok0 + (t + 1) * 128, :], in_=stb[bi_][:]),
                      reads=[t_stb[bi_]], writes=[G.t["mvd"]], merge=True)
        S.flush(f"projb{l}")


class GroupTail:
    def __init__(self, G, l, gi, sb, pp, name):
        self.G, self.gi = G, gi
        self.gT, self.t_gT = load_T(G, [G.out_norm_g[l, gi * 512:(gi + 1) * 512]], sb, pp, name + "_g")
        self.junk = sb(name + "_junk", [128, 512], BF16)
        self.t_junk = Tok()
        self.ss = [sb(name + f"_ss{i}", [128, 1], F32) for i in range(2)]
        self.t_ss = [Tok(), Tok()]
        self.ynb = [sb(name + f"_ynb{i}", [128, 512], BF16) for i in range(2)]
        self.t_ynb = [Tok(), Tok()]
        self.tp = pp(name + "_tp", [128, 1024], BF16)
        self.t_tp = Tok()
        self.st = [sb(name + f"_st{i}", [128, 4, 128], BF16) for i in range(2)]
        self.t_st = [Tok(), Tok()]
        self.k = 0

    def emit(self, y_ap, t_y, tok0):
        G, S = self.G, self.G.S
        i = self.k % 2
        self.k += 1
        S.op("act", I("activation", out=self.junk[:], in_=y_ap, func=AF.Square, accum_out=self.ss[i][:, 0:1]),
             reads=[t_y], writes=[self.t_junk, self.t_ss[i]])
        rstd_from_ss(G, self.ss[i][:, 0:1], self.ss[i][:, 0:1], 512, [self.t_ss[i]], [self.t_ss[i]])
        S.op("act", I("activation", out=self.ynb[i][:], in_=y_ap, func=AF.Copy, scale=self.ss[i][:, 0:1]),
             reads=[t_y, self.t_ss[i]], writes=[self.t_ynb[i]])
        for c in range(4):
            S.op("pe", I("transpose", out=self.tp[:, c * 128:(c + 1) * 128], in_=self.ynb[i][:, c * 128:(c + 1) * 128],
                         identity=G.ident_b[:]),
                 reads=[self.t_ynb[i], G.t_const], writes=[self.t_tp], merge=(c > 0))
        for c in range(4):
            S.op("dve", I("tensor_scalar", out=self.st[i][:, c, :], in0=self.tp[:, c * 128:(c + 1) * 128],
                          scalar1=self.gT[:, 0, c:c + 1], scalar2=None, op0=ALU.mult),
                 reads=[self.t_tp, self.t_gT], writes=[self.t_st[i]], merge=(c > 0))
        r0 = self.gi * 512
        S.dma("sp", I("dma_start", out=G.ynT[r0:r0 + 512, tok0:tok0 + 128].rearrange("(c p) n -> p c n", p=128), in_=self.st[i][:]),
              reads=[self.t_st[i]], writes=[G.t["ynT"]], merge=True)


def phase_conv(G, l, do_ctx):
    nc, S = G.nc, G.S
    with ExitStack() as ps:
        sb, pp = _alloc(G, ps)
        cw, t_cw = load_T(G, [G.conv_w[l, 0], G.conv_w[l, 1], G.conv_w[l, 2], G.out_norm_g[l, 0:512]], sb, pp, "cv_w")
        ut = [sb(f"cv_u{i}", [128, 514], F32) for i in range(2)]
        t_ut = [Tok(), Tok()]
        cbt = [sb(f"cv_cb{i}", [128, 512], F32) for i in range(2)]
        t_cbt = [Tok(), Tok()]
        acc_t = [sb(f"cv_a{i}", [128, 512], F32) for i in range(2)]
        t_at = [Tok(), Tok()]
        yc = sb("cv_y", [128, 4, 512], F32)
        t_yc = [Tok() for _ in range(4)]
        sqb = [sb(f"cv_sq{i}", [128, 512], BF16) for i in range(2)]
        t_sqb = [Tok(), Tok()]
        rstd = sb("cv_rstd", [128, 512], F32)
        t_rstd = Tok()
        stb = [sb(f"cv_st{i}", [128, 512], BF16) for i in range(2)]
        t_stb = [Tok(), Tok()]
        ssp = pp("cv_ss", [128, 512])
        t_ssp = Tok()
        segs = [(0, NL)] + ([(NL, NT)] if do_ctx else [])
        k = 0
        for (s0, s1) in segs:
            for b0 in range(s0, s1, 512):
                nb = min(512, s1 - b0)
                for c in range(4):
                    i = k % 2
                    k += 1
                    lo = b0 - 1 if b0 > s0 else b0
                    hi = b0 + nb + 1 if b0 + nb < s1 else b0 + nb
                    first = True
                    if lo == b0:
                        S.op("pool", I("memset", ut[i][:, 0:1], 0.0), writes=[t_ut[i]])
                        first = False
                    if hi == b0 + nb:
                        S.op("pool", I("memset", ut[i][:, nb + 1:nb + 2], 0.0), writes=[t_ut[i]], merge=not first)
                        first = False
                    S.dma("sp", I("dma_start", out=ut[i][:, lo - (b0 - 1):hi - (b0 - 1)], in_=G.uT[c * 128:(c + 1) * 128, lo:hi]),
                          reads=[G.t["uT"]], writes=[t_ut[i]], merge=not first)
                    S.dma("sp", I("dma_start", out=cbt[i][:, 0:nb], in_=G.cbT[c * 128:(c + 1) * 128, b0:b0 + nb]),
                          reads=[G.t["cbT"]], writes=[t_cbt[i]])
                    S.op("dve", I("tensor_scalar", out=acc_t[i][:, 0:nb], in0=ut[i][:, 0:nb], scalar1=cw[:, 0, c:c + 1], scalar2=None,
                                  op0=ALU.mult), reads=[t_ut[i], t_cw], writes=[t_at[i]])
                    S.op("dve", I("scalar_tensor_tensor", out=acc_t[i][:, 0:nb], in0=ut[i][:, 1:nb + 1], scalar=cw[:, 1, c:c + 1],
                                  in1=acc_t[i][:, 0:nb], op0=ALU.mult, op1=ALU.add), reads=[t_ut[i], t_cw, t_at[i]], writes=[t_at[i]])
                    S.op("dve", I("scalar_tensor_tensor", out=acc_t[i][:, 0:nb], in0=ut[i][:, 2:nb + 2], scalar=cw[:, 2, c:c + 1],
                                  in1=acc_t[i][:, 0:nb], op0=ALU.mult, op1=ALU.add), reads=[t_ut[i], t_cw, t_at[i]], writes=[t_at[i]])
                    S.op("pool", I("tensor_tensor", out=yc[:, c, 0:nb], in0=acc_t[i][:, 0:nb], in1=cbt[i][:, 0:nb], op=ALU.mult),
                         reads=[t_at[i], t_cbt[i]], writes=[t_yc[c]])
                    S.op("act", I("activation", out=sqb[i][:, 0:nb], in_=yc[:, c, 0:nb], func=AF.Square), reads=[t_yc[c]], writes=[t_sqb[i]])
                    S.op("pe", I("matmul", ssp[:, 0:nb], lhsT=G.ones_b[:], rhs=sqb[i][:, 0:nb], start=(c == 0), stop=(c == 3)),
                         reads=[t_sqb[i], G.t_const], writes=[t_ssp], merge=(c > 0))
                rstd_from_ss(G, rstd[:, 0:nb], ssp[:, 0:nb], 512, [t_ssp], [t_rstd])
                for c in range(4):
                    i = k % 2
                    k += 1
                    S.op("dve", I("scalar_tensor_tensor", out=stb[i][:, 0:nb], in0=yc[:, c, 0:nb], scalar=cw[:, 3, c:c + 1],
                                  in1=rstd[:, 0:nb], op0=ALU.mult, op1=ALU.mult), reads=[t_yc[c], t_cw, t_rstd], writes=[t_stb[i]])
                    S.dma("sp", I("dma_start", out=G.ynT[c * 128:(c + 1) * 128, b0:b0 + nb], in_=stb[i][:, 0:nb]),
                          reads=[t_stb[i]], writes=[G.t["ynT"]], merge=True)
        S.flush(f"conv{l}")


def phase_swa(G, l, do_ctx):
    nc, S = G.nc, G.S
    SCALE = 64 ** -0.5
    with ExitStack() as ps:
        sb, pp = _alloc(G, ps)
        Qs = sb("sw_Q", [64, 8, NT], BF16)
        Ks = sb("sw_K", [64, 2, NT], BF16)
        Vs = sb("sw_V", [128, 34, 2, 65], BF16)
        mk = sb("sw_mask", [128, 2, 512], BF16)
        snk = sb("sw_sink", [128, 8], F32)
        t_in = Tok()
        t_V = Tok()
        for h in range(8):
            S.dma("sp", I("dma_start", out=Qs[:, h, :], in_=G.sqT[h * 64:(h + 1) * 64, :]), reads=[G.t["sqT"]], writes=[t_in], merge=True)
        for h in range(2):
            S.dma("sp", I("dma_start", out=Ks[:, h, :], in_=G.skT[h * 64:(h + 1) * 64, :]), reads=[G.t["skT"]], writes=[t_in], merge=True)
        S.op("pool", I("memset", Vs[:, :, :, 64:65], 1.0), writes=[t_V])
        for h in range(2):
            S.dma("sp", I("dma_start", out=Vs[:, :, h, 0:64], in_=G.svd[:, h * 64:(h + 1) * 64].rearrange("(t p) d -> p t d", p=128)),
                  reads=[G.t["svd"]], writes=[t_V], merge=True)
        S.dma("sp", I("dma_start", out=mk[:], in_=G.masks.rearrange("t p n -> p t n")), writes=[t_in], merge=True)
        S.dma("sp", I("dma_start", out=snk[:], in_=G.swa_sink[l:l + 1, :].to_broadcast([128, 8])), writes=[t_in], merge=True)
        S.op("act", I("activation", out=snk[:], in_=snk[:], func=AF.Exp), reads=[t_in], writes=[t_in])
        tail = GroupTail(G, l, 1, sb, pp, "sw_t")
        pT = [sb(f"sw_pT{i}", [128, 5, 512], BF16) for i in range(2)]
        t_pT = [[Tok() for _ in range(5)] for _ in range(2)]
        sps = [pp(f"sw_s{i}", [128, 512]) for i in range(2)]
        t_sps = [Tok(), Tok()]
        ops_ = [pp(f"sw_o{i}", [128, 512]) for i in range(2)]
        t_ops = [Tok(), Tok()]
        den = [sb(f"sw_den{i}", [128, 8], F32) for i in range(2)]
        t_den = [Tok(), Tok()]
        ysw = [sb(f"sw_y{i}", [128, 512], F32) for i in range(2)]
        t_ysw = [Tok(), Tok()]
        qblocks = [(i, "lat") for i in range(32)] + ([(32, "ctx"), (33, "ctx")] if do_ctx else [])
        import os
        LV = int(os.environ.get("SW_STOP", "99"))
        kq = 0
        ks = 0
        pend = None
        for bidx, (i, kind) in enumerate(qblocks[:LV]):
            if kind == "lat":
                kts = ([(i - 1, 0)] if i > 0 else []) + [(i, None)] + ([(i + 1, 1)] if i < 31 else []) + [(32, None), (33, None)]
            else:
                kts = [(32, None), (33, None)]
            yi = bidx % 2
            for kvh in range(2):
                pi = kq % 2
                oi = kq % 2
                kq += 1
                for n, (kt, msk) in enumerate(kts):
                    si = ks % 2
                    ks += 1
                    S.op("pe", I("matmul", sps[si][:], lhsT=Ks[:, kvh, kt * 128:(kt + 1) * 128],
                                 rhs=Qs[:, kvh * 4:(kvh + 1) * 4, i * 128:(i + 1) * 128], start=True, stop=True),
                         reads=[t_in], writes=[t_sps[si]])
                    S.op("act", I("activation", out=pT[pi][:, n, :], in_=sps[si][:], func=AF.Exp, scale=SCALE),
                         reads=[t_sps[si]], writes=[t_pT[pi][n]])
                    if msk is not None:
                        S.op("dve", I("tensor_tensor", out=pT[pi][:, n, :], in0=pT[pi][:, n, :], in1=mk[:, msk, :], op=ALU.mult),
                             reads=[t_pT[pi][n], t_in], writes=[t_pT[pi][n]])
                for g in range(4):
                    for n, (kt, msk) in enumerate(kts):
                        S.op("pe", I("matmul", ops_[oi][:, g * 65:(g + 1) * 65], lhsT=pT[pi][:, n, g * 128:(g + 1) * 128],
                                     rhs=Vs[:, kt, kvh, :], start=(n == 0), stop=(n == len(kts) - 1)),
                             reads=[t_pT[pi][n], t_V], writes=[t_ops[oi]], merge=(n > 0 or g > 0))
                ov = ops_[oi][:, 0:260].rearrange("p (g e) -> p g e", e=65)
                S.op("dve", I("tensor_tensor", out=den[yi][:, kvh * 4:(kvh + 1) * 4], in0=ov[:, :, 64], in1=snk[:, kvh * 4:(kvh + 1) * 4], op=ALU.add),
                     reads=[t_ops[oi], t_in], writes=[t_den[yi]], merge=(kvh > 0))
                S.op("dve", I("reciprocal", out=den[yi][:, kvh * 4:(kvh + 1) * 4], in_=den[yi][:, kvh * 4:(kvh + 1) * 4]),
                     reads=[t_den[yi]], writes=[t_den[yi]])
                for g in range(4):
                    h = kvh * 4 + g
                    S.op("dve", I("tensor_scalar", out=ysw[yi][:, h * 64:(h + 1) * 64], in0=ov[:, g, 0:64], scalar1=den[yi][:, h:h + 1],
                                  scalar2=None, op0=ALU.mult),
                         reads=[t_ops[oi], t_den[yi]], writes=[t_ysw[yi]], merge=(h > 0))
                if kvh == 0 and pend is not None:
                    tail.emit(*pend)
                    pend = None
            pend = (ysw[yi][:], t_ysw[yi], i * 128)
        if pend is not None:
            tail.emit(*pend)
        S.flush(f"swa{l}")


def phase_fnet(G, l, do_ctx):
    nc, S = G.nc, G.S
    with ExitStack() as ps:
        sb, pp = _alloc(G, ps)
        uc = sb("fn_uc", [128, 34, 512], BF16)
        us = sb("fn_us", [128, 34, 512], BF16)
        t_u = Tok()
        for (t0, t1) in ((0, 16), (16, 34)):
            S.dma("sp", I("dma_start", out=uc[:, t0:t1, :], in_=G.ucd[t0 * 128:t1 * 128, :].rearrange("(t p) c -> p t c", p=128)),
                  reads=[G.t["ucd"]], writes=[t_u], merge=True)
            S.dma("sp", I("dma_start", out=us[:, t0:t1, :], in_=G.usd[t0 * 128:t1 * 128, :].rearrange("(t p) c -> p t c", p=128)),
                  reads=[G.t["usd"]], writes=[t_u], merge=True)
        dt_ = [sb(f"fn_d{i}", [128, 2 * 32 * 128], BF16) for i in range(2)]
        t_dt = [Tok(), Tok()]
        acc = [pp(f"fn_acc{i}", [128, 512]) for i in range(2)]
        t_acc = [Tok(), Tok()]
        yf = [sb(f"fn_y{i}", [128, 512], F32) for i in range(2)]
        t_yf = [Tok(), Tok()]
        tail = GroupTail(G, l, 2, sb, pp, "fn_t")
        import os
        LV = int(os.environ.get("FN_STOP", "99"))
        jobs = [(kt, 32, 0, G.dftL) for kt in range(32)][:LV] + ([(kt, 2, 32, G.dftC) for kt in range(2)] if do_ctx else [])
        pend = None
        for k, (kt, nt, tb, tab) in enumerate(jobs):
            i = k % 2
            dv = dt_[i][:, 0:2 * nt * 128]
            S.dma("sp", I("dma_start", out=dv, in_=tab[kt]), writes=[t_dt[i]])
            d4 = dv.rearrange("p (a n k) -> p a n k", a=2, k=128)
            for n in range(nt):
                S.op("pe", I("matmul", acc[i][:], lhsT=d4[:, 0, n, :], rhs=uc[:, tb + n, :], start=(n == 0), stop=False),
                     reads=[t_dt[i], t_u], writes=[t_acc[i]], merge=(n > 0))
            for n in range(nt):
                S.op("pe", I("matmul", acc[i][:], lhsT=d4[:, 1, n, :], rhs=us[:, tb + n, :], start=False, stop=(n == nt - 1)),
                     reads=[t_dt[i], t_u], writes=[t_acc[i]], merge=True)
            if pend is not None:
                tail.emit(*pend)
            S.op("act", I("copy", out=yf[i][:], in_=acc[i][:]), reads=[t_acc[i]], writes=[t_yf[i]])
            pend = (yf[i][:], t_yf[i], (tb + kt) * 128)
        if pend is not None:
            tail.emit(*pend)
        S.flush(f"fnet{l}")


def phase_mla(G, l, do_ctx):
    nc, S = G.nc, G.S
    SCALE = 96 ** -0.5
    with ExitStack() as ps:
        sb, pp = _alloc(G, ps)
        Kh = [sb(f"ml_K{i}", [96, NT], BF16) for i in range(2)]
        Qh = [sb(f"ml_Q{i}", [96, NT], BF16) for i in range(2)]
        Vh = [sb(f"ml_V{i}", [128, 34, 65], BF16) for i in range(2)]
        t_K = [Tok(), Tok()]
        t_Q = [Tok(), Tok()]
        t_V = [Tok(), Tok()]
        PT = [sb(f"ml_PT{i}", [128, 34, 512], BF16) for i in range(2)]
        t_PT = [[Tok() for _ in range(34)] for _ in range(2)]
        yall = sb("ml_y", [128, 34, 512], F32)
        t_y = [Tok() for _ in range(34)]
        rc = [sb(f"ml_rc{i}", [128, 1], F32) for i in range(4)]
        t_rc = [Tok() for _ in range(4)]
        sps = [pp(f"ml_s{i}", [128, 512]) for i in range(2)]
        t_sps = [Tok(), Tok()]
        ops_ = [pp(f"ml_o{i}", [128, 512]) for i in range(4)]
        t_ops = [Tok() for _ in range(4)]
        import os
        LVH = int(os.environ.get("ML_HEADS", "8"))
        LVQ = int(os.environ.get("ML_QB", "99"))
        qblocks = [(i * 512, 512, list(range(34))) for i in range(8)][:LVQ] + ([(NL, NCX, [32, 33])] if do_ctx else [])
        ks = 0
        kp = 0
        ko = 0
        for h in range(LVH):
            hb = h % 2
            S.dma("sp", I("dma_start", out=Kh[hb][:], in_=G.mkT[h]), reads=[G.t["mkT"]], writes=[t_K[hb]])
            S.dma("sp", I("dma_start", out=Qh[hb][:], in_=G.mqT[h]), reads=[G.t["mqT"]], writes=[t_Q[hb]])
            S.op("pool", I("memset", Vh[hb][:, :, 64:65], 1.0), writes=[t_V[hb]])
            S.dma("sp", I("dma_start", out=Vh[hb][:, :, 0:64], in_=G.mvd[:, h * 64:(h + 1) * 64].rearrange("(t p) d -> p t d", p=128)),
                  reads=[G.t["mvd"]], writes=[t_V[hb]], merge=True)
            for (q0, nq, kts) in qblocks:
                pi = kp % 2
                kp += 1
                for kt in kts:
                    si = ks % 2
                    ks += 1
                    S.op("pe", I("matmul", sps[si][:, 0:nq], lhsT=Kh[hb][:, kt * 128:(kt + 1) * 128], rhs=Qh[hb][:, q0:q0 + nq],
                                 start=True, stop=True), reads=[t_K[hb], t_Q[hb]], writes=[t_sps[si]])
                    S.op("act", I("activation", out=PT[pi][:, kt, 0:nq], in_=sps[si][:, 0:nq], func=AF.Exp, scale=SCALE),
                         reads=[t_sps[si]], writes=[t_PT[pi][kt]])
                for j in range(nq // 128):
                    oi = ko % 4
                    ko += 1
                    for n, kt in enumerate(kts):
                        S.op("pe", I("matmul", ops_[oi][:, 0:65], lhsT=PT[pi][:, kt, j * 128:(j + 1) * 128], rhs=Vh[hb][:, kt, :],
                                     start=(n == 0), stop=(n == len(kts) - 1)),
                             reads=[t_PT[pi][kt], t_V[hb]], writes=[t_ops[oi]], merge=(n > 0))
                    S.op("dve", I("reciprocal", out=rc[oi][:], in_=ops_[oi][:, 64:65]), reads=[t_ops[oi]], writes=[t_rc[oi]])
                    tile_i = q0 // 128 + j
                    S.op("dve", I("tensor_scalar", out=yall[:, tile_i, h * 64:(h + 1) * 64], in0=ops_[oi][:, 0:64], scalar1=rc[oi][:, 0:1],
                                  scalar2=None, op0=ALU.mult),
                         reads=[t_ops[oi], t_rc[oi]], writes=[t_y[tile_i]], merge=(h > 0))
        if LVH == 8:
            tail = GroupTail(G, l, 3, sb, pp, "ml_t")
            ntile = (qblocks[-1][0] + qblocks[-1][1]) // 128 if LVQ >= 8 else LVQ * 4
            tiles = list(range(min(32, ntile))) + ([32, 33] if do_ctx else [])
            for ti in tiles:
                tail.emit(yall[:, ti, :], t_y[ti], ti * 128)
        else:
            G.dbg_yall = (yall, t_y)
        S.flush(f"mla{l}")


def phase_outproj(G, l, do_ctx):
    nc, S = G.nc, G.S
    with ExitStack() as ps:
        sb, pp = _alloc(G, ps)
        wo = sb("op_wo", [128, 16, D], BF16)
        t_w = Tok()
        for c0 in range(0, D, 512):
            S.dma("pool", I("dma_start", out=wo[:, :, c0:c0 + 512], in_=G.w_out[l][:, c0:c0 + 512].rearrange("(j p) n -> p j n", p=128)),
                  writes=[t_w], merge=True)
        wr = sb("op_wr", [128, 16, NE], F32)
        S.dma("sp", I("dma_start", out=wr[:], in_=G.w_router[l].rearrange("(j p) e -> p j e", p=128)), writes=[t_w], merge=True)
        g1b = sb("op_g1b", [128, D], F32)
        sh2b = sb("op_sh2b", [128, D], F32)
        gs2b = sb("op_gs2b", [128, D], F32)
        t_bc = Tok()
        NB = 2
        xt = [sb(f"op_x{i}", [128, D], F32) for i in range(NB)]
        t_xt = [Tok() for _ in range(NB)]
        xn = [sb(f"op_xn{i}", [128, D], F32) for i in range(NB)]
        t_xn = [Tok() for _ in range(NB)]
        fx = [sb(f"op_fx{i}", [128, D], F32) for i in range(NB)]
        t_fx = [Tok() for _ in range(NB)]
        fxb = [sb(f"op_fxb{i}", [128, D], BF16) for i in range(NB)]
        t_fxb = [Tok() for _ in range(NB)]
        junk = sb("op_junk", [128, D], BF16)
        t_junk = Tok()
        yn = [sb(f"op_yn{i}", [128, 16, 128], BF16) for i in range(NB)]
        t_yn = [Tok() for _ in range(NB)]
        fxT = [sb(f"op_fxT{i}", [128, 16, 128], F32) for i in range(NB)]
        t_fxT = [Tok() for _ in range(NB)]
        ss = [sb(f"op_ss{i}", [128, 1], F32) for i in range(NB)]
        t_ss = [Tok() for _ in range(NB)]
        sm = [sb(f"op_sm{i}", [128, 4], F32) for i in range(NB)]
        t_sm = [Tok() for _ in range(NB)]
        ex = [sb(f"op_ex{i}", [128, NE], F32) for i in range(NB)]
        t_ex = [Tok() for _ in range(NB)]
        acc = [pp(f"op_acc{i}", [128, 512]) for i in range(4)]
        t_acc = [Tok() for _ in range(4)]
        trp = [pp(f"op_tr{i}", [128, 512]) for i in range(2)]
        t_trp = [Tok(), Tok()]
        lgp = [pp(f"op_lg{i}", [128, 512]) for i in range(2)]
        t_lgp = [Tok(), Tok()]
        import os
        LV = int(os.environ.get("OP_STOP", "99"))
        tiles = (list(range(32)) + ([32, 33] if do_ctx else []))[:LV]
        cur_s = None
        kt = [0]
        def loads(k, ti):
            tok0 = ti * 128
            i = k % NB
            S.dma("sp", I("dma_start", out=yn[i][:], in_=G.ynT[:, tok0:tok0 + 128].rearrange("(j p) n -> p j n", p=128)),
                  reads=[G.t["ynT"]], writes=[t_yn[i]])
            S.dma("sp", I("dma_start", out=xt[i][:], in_=G.xres[tok0:tok0 + 128, :]), reads=[G.t["xres"]], writes=[t_xt[i]])

        def part1(k, ti):
            nonlocal cur_s
            s_ = 1 if ti >= 32 else 0
            if s_ != cur_s:
                cur_s = s_
                S.dma("sp", I("dma_start", out=g1b[:], in_=G.modrow[l, s_:s_ + 1, 2 * D:3 * D].to_broadcast([128, D])),
                      reads=[G.t["modrow"]], writes=[t_bc])
                S.dma("sp", I("dma_start", out=sh2b[:], in_=G.modrow[l, s_:s_ + 1, 3 * D:4 * D].to_broadcast([128, D])),
                      reads=[G.t["modrow"]], writes=[t_bc], merge=True)
                S.dma("sp", I("dma_start", out=gs2b[:], in_=G.modrow[l, s_:s_ + 1, 4 * D:5 * D].to_broadcast([128, D])),
                      reads=[G.t["modrow"]], writes=[t_bc], merge=True)
                S.dma("sp", I("dma_start", out=fx[0][:], in_=G.norm2_g[l:l + 1, :].to_broadcast([128, D])), writes=[t_fx[0]])
                S.op("dve", I("scalar_tensor_tensor", out=gs2b[:], in0=gs2b[:], scalar=1.0, in1=fx[0][:], op0=ALU.add, op1=ALU.mult),
                     reads=[t_bc, t_fx[0]], writes=[t_bc])
            tok0 = ti * 128
            i = k % NB
            if k == 0:
                loads(k, ti)
            if k + 1 < len(tiles):
                loads(k + 1, tiles[k + 1])
            for nb in range(4):
                for j in range(16):
                    S.op("pe", I("matmul", acc[nb][:], lhsT=yn[i][:, j, :], rhs=wo[:, j, nb * 512:(nb + 1) * 512], start=(j == 0), stop=(j == 15)),
                         reads=[t_yn[i], t_w], writes=[t_acc[nb]], merge=(j > 0))
                S.op("dve", I("tensor_tensor", out=xn[i][:, nb * 512:(nb + 1) * 512], in0=acc[nb][:], in1=g1b[:, nb * 512:(nb + 1) * 512], op=ALU.mult),
                     reads=[t_acc[nb], t_bc], writes=[t_xn[i]], merge=(nb > 0))
        def part1c(k, ti):
            tok0 = ti * 128
            i = k % NB
            S.op("pool", I("tensor_tensor", out=xn[i][:], in0=xn[i][:], in1=xt[i][:], op=ALU.add), reads=[t_xn[i], t_xt[i]], writes=[t_xn[i]])
            S.dma("sp", I("dma_start", out=G.xres[tok0:tok0 + 128, :], in_=xn[i][:]), reads=[t_xn[i]], writes=[G.t["xres"]], merge=True)
            S.op("act", I("activation", out=junk[:], in_=xn[i][:], func=AF.Square, accum_out=ss[i][:, 0:1]), reads=[t_xn[i]], writes=[t_junk, t_ss[i]])
            rstd_from_ss(G, ss[i][:, 0:1], ss[i][:, 0:1], D, [t_ss[i]], [t_ss[i]])
            S.op("dve", I("scalar_tensor_tensor", out=fx[i][:], in0=xn[i][:], scalar=ss[i][:, 0:1], in1=gs2b[:], op0=ALU.mult, op1=ALU.mult),
                 reads=[t_xn[i], t_ss[i], t_bc], writes=[t_fx[i]])
            S.op("pool", I("tensor_tensor", out=fx[i][:], in0=fx[i][:], in1=sh2b[:], op=ALU.add), reads=[t_fx[i], t_bc], writes=[t_fx[i]])
            S.op("act", I("copy", out=fxb[i][:], in_=fx[i][:]), reads=[t_fx[i]], writes=[t_fxb[i]])
            S.dma("sp", I("dma_start", out=G.fxd[tok0:tok0 + 128, :], in_=fxb[i][:]), reads=[t_fxb[i]], writes=[G.t["fxd"]], merge=True)

        def part2(k, ti):
            i = k % NB
            for q in range(4):
                ti_ = kt[0] % 2
                kt[0] += 1
                for jj in range(4):
                    j = q * 4 + jj
                    S.op("pe", I("transpose", out=trp[ti_][:, jj * 128:(jj + 1) * 128], in_=fx[i][:, j * 128:(j + 1) * 128], identity=G.ident_f[:]),
                         reads=[t_fx[i], G.t_const], writes=[t_trp[ti_]], merge=(jj > 0))
                if q % 2 == 0:
                    S.op("act", I("copy", out=fxT[i][:, q * 4:(q + 1) * 4, :], in_=trp[ti_][:].rearrange("p (a n) -> p a n", n=128)),
                         reads=[t_trp[ti_]], writes=[t_fxT[i]], merge=(q > 0))
                else:
                    S.op("dve", I("tensor_copy", out=fxT[i][:, q * 4:(q + 1) * 4, :], in_=trp[ti_][:].rearrange("p (a n) -> p a n", n=128)),
                         reads=[t_trp[ti_]], writes=[t_fxT[i]], merge=True)
            for j in range(16):
                S.op("pe", I("matmul", lgp[i][:, 0:NE], lhsT=fxT[i][:, j, :], rhs=wr[:, j, :], start=(j == 0), stop=(j == 15)),
                     reads=[t_fxT[i], t_w], writes=[t_lgp[i]], merge=(j > 0))
            S.op("dve", I("reduce_max", out=sm[i][:, 0:1], in_=lgp[i][:, 0:NE], axis=mybir.AxisListType.X), reads=[t_lgp[i]], writes=[t_sm[i]])
            S.op("dve", I("tensor_scalar", out=sm[i][:, 1:2], in0=sm[i][:, 0:1], scalar1=-1.0, scalar2=None, op0=ALU.mult), reads=[t_sm[i]], writes=[t_sm[i]])
            S.op("act", I("activation", out=ex[i][:], in_=lgp[i][:, 0:NE], func=AF.Exp, bias=sm[i][:, 1:2], accum_out=sm[i][:, 2:3]),
                 reads=[t_lgp[i], t_sm[i]], writes=[t_ex[i], t_sm[i]])
            S.op("dve", I("reciprocal", out=sm[i][:, 3:4], in_=sm[i][:, 2:3]), reads=[t_sm[i]], writes=[t_sm[i]])
            S.op("dve", I("tensor_scalar", out=G.aff_all[:, ti, :], in0=ex[i][:], scalar1=sm[i][:, 3:4], scalar2=None, op0=ALU.mult),
                 reads=[t_ex[i], t_sm[i]], writes=[G.t_aff], merge=True)

        for k in range(len(tiles) + 1):
            if k < len(tiles):
                part1(k, tiles[k])
            if k >= 1:
                part2(k - 1, tiles[k - 1])
            if k < len(tiles):
                part1c(k, tiles[k])
        if "affd" in G.debug:
            S.dma("sp", I("dma_start", out=G.affd.rearrange("(t p) e -> p t e", p=128), in_=G.aff_all[:]), reads=[G.t_aff], writes=[G.t["affd"]])
        S.flush(f"outproj{l}")


def phase_route(G, l, do_ctx):
    nc, S = G.nc, G.S
    NI = 24
    with ExitStack() as ps:
        sb, pp = _alloc(G, ps)
        U = sb("rt_U", [128, 128], BF16)
        iot = sb("rt_iota", [128, 512], F32)
        pidx = sb("rt_pidx", [128, 4], F32)
        tgrid = sb("rt_tgrid", [128, 34, NE], F32)
        t_c = Tok()
        S.dma("sp", I("dma_start", out=U[:], in_=G.triu), writes=[t_c], merge=True)
        S.dma("sp", I("dma_start", out=iot[:], in_=G.iota512), writes=[t_c], merge=True)
        S.dma("sp", I("dma_start", out=pidx[:], in_=G.pidx), writes=[t_c], merge=True)
        S.dma("sp", I("dma_start", out=tgrid[:].rearrange("p t e -> p (t e)"), in_=G.tgrid), writes=[t_c], merge=True)
        aff = G.aff_all
        sets = [(0, 32, CAP_L)] + ([(32, 34, CAP_C)] if do_ctx else [])
        R = sb("rt_R", [128, 34, NE, 6], BF16)
        t_R = Tok()
        r1 = sb("rt_r1", [128, 34, NE], F32)
        t_r1 = Tok()
        S.op("pool", I("memset", R[:], 1.0), writes=[t_R])
        S.op("dve", I("tensor_scalar", out=R[:, :, :, 0], in0=tgrid[:], scalar1=0.0, scalar2=pidx[:, 0:1], op0=ALU.mult, op1=ALU.add),
             reads=[t_c], writes=[t_R])
        S.op("dve", I("tensor_copy", out=R[:, :, :, 1], in_=tgrid[:]), reads=[t_c], writes=[t_R])
        S.op("dve", I("tensor_copy", out=R[:, :, :, 2], in_=aff[:]), reads=[G.t_aff], writes=[t_R])
        S.op("dve", I("tensor_tensor", out=r1[:], in0=aff[:], in1=R[:, :, :, 2], op=ALU.subtract), reads=[G.t_aff, t_R], writes=[t_r1])
        S.op("dve", I("tensor_copy", out=R[:, :, :, 3], in_=r1[:]), reads=[t_r1], writes=[t_R])
        S.op("dve", I("tensor_tensor", out=r1[:], in0=r1[:], in1=R[:, :, :, 3], op=ALU.subtract), reads=[t_r1, t_R], writes=[t_r1])
        S.op("dve", I("tensor_copy", out=R[:, :, :, 4], in_=r1[:]), reads=[t_r1], writes=[t_R])
        mids, t_mid, cmps, t_cmp, parts, t_part, cps, t_cps, tmps, t_tmp = [], [], [], [], [], [], [], [], [], []
        for si, (t0, t1, cap) in enumerate(sets):
            mids.append(sb(f"rt_mid{si}", [128, NE], F32)); t_mid.append(Tok())
            cmps.append(sb(f"rt_cmp{si}", [128, t1 - t0, NE], BF16)); t_cmp.append(Tok())
            parts.append(sb(f"rt_part{si}", [128, NE], F32)); t_part.append(Tok())
            cps.append(pp(f"rt_cps{si}", [128, 512])); t_cps.append(Tok())
            tmps.append(sb(f"rt_tmp{si}", [128, NE], F32)); t_tmp.append(Tok())
            S.op("pool", I("memset", mids[si][:], 0.5), writes=[t_mid[si]])
        for k in range(NI):
            wk = 0.5 ** (k + 1)
            wn = 0.5 ** (k + 2) if k < NI - 1 else 0.0
            for si, (t0, t1, cap) in enumerate(sets):
                nt = t1 - t0
                S.op("dve", I("tensor_tensor", out=cmps[si][:], in0=aff[:, t0:t1, :],
                              in1=mids[si][:].unsqueeze(1).to_broadcast([128, nt, NE]), op=ALU.is_gt),
                     reads=[G.t_aff, t_mid[si]], writes=[t_cmp[si]])
                S.op("dve", I("tensor_reduce", out=parts[si][:], in_=cmps[si][:].rearrange("p t e -> p e t"),
                              axis=mybir.AxisListType.X, op=ALU.add),
                     reads=[t_cmp[si]], writes=[t_part[si]])
                S.op("pe", I("matmul", cps[si][:, 0:NE], lhsT=G.ones_f[:], rhs=parts[si][:], start=True, stop=True),
                     reads=[t_part[si], G.t_const], writes=[t_cps[si]])
                S.op("dve", I("tensor_scalar", out=tmps[si][:], in0=cps[si][:, 0:NE], scalar1=cap + 0.5, scalar2=wk, op0=ALU.is_gt, op1=ALU.mult),
                     reads=[t_cps[si]], writes=[t_tmp[si]])
                S.op("dve", I("scalar_tensor_tensor", out=mids[si][:], in0=tmps[si][:], scalar=-wn, in1=mids[si][:], op0=ALU.add, op1=ALU.add),
                     reads=[t_tmp[si], t_mid[si]], writes=[t_mid[si]])
        Mb = sb("rt_Mb", [128, 34, NE], BF16)
        Mf = sb("rt_Mf", [128, 34, NE], F32)
        t_M = Tok()
        if not do_ctx:
            S.op("pool", I("memset", Mb[:, 32:34, :], 0.0), writes=[t_M])
            S.op("pool", I("memset", Mf[:, 32:34, :], 0.0), writes=[t_M], merge=True)
        for si, (t0, t1, cap) in enumerate(sets):
            nt = t1 - t0
            S.op("dve", I("tensor_tensor", out=Mf[:, t0:t1, :], in0=aff[:, t0:t1, :],
                          in1=mids[si][:].unsqueeze(1).to_broadcast([128, nt, NE]), op=ALU.is_gt),
                 reads=[G.t_aff, t_mid[si]], writes=[t_M], merge=True)
        S.op("dve", I("tensor_copy", out=Mb[:, 0:34 if do_ctx else 32, :], in_=Mf[:, 0:34 if do_ctx else 32, :]), reads=[t_M], writes=[t_M])
        posp = [pp(f"rt_pos{i}", [128, 512]) for i in range(2)]
        totp = [pp(f"rt_tot{i}", [128, 512]) for i in range(2)]
        t_pp = Tok()
        spans = [(0, 32)] + ([(32, 34)] if do_ctx else [])
        for i, (t0, t1) in enumerate(spans):
            n = (t1 - t0) * NE
            rhs = Mb[:, t0:t1, :].rearrange("p t e -> p (t e)")
            S.op("pe", I("matmul", posp[i][:, 0:n], lhsT=U[:], rhs=rhs, start=True, stop=True), reads=[t_M, t_c], writes=[t_pp], merge=(i > 0))
            S.op("pe", I("matmul", totp[i][:, 0:n], lhsT=G.ones_b[:], rhs=rhs, start=True, stop=True), reads=[t_M, G.t_const], writes=[t_pp], merge=True)
        incl = sb("rt_incl", [128, 34, NE], F32)
        t_incl = Tok()
        onesf = sb("rt_onesf", [128, 32], F32)
        S.op("pool", I("memset", onesf[:], 1.0), writes=[t_c], merge=True)
        for i, (t0, t1) in enumerate(spans):
            nt = t1 - t0
            tv = totp[i][:, 0:nt * NE].rearrange("p (t e) -> p t e", e=NE)
            for e in range(NE):
                S.op("dve", I("tensor_tensor_scan", out=incl[:, t0:t1, e], data0=onesf[:, 0:nt], data1=tv[:, :, e], initial=0.0,
                              op0=ALU.mult, op1=ALU.add),
                     reads=[t_pp, t_c], writes=[t_incl], merge=(e > 0 or i > 0))
        posf = sb("rt_posf", [128, 34, NE], F32)
        t_posf = Tok()
        for i, (t0, t1) in enumerate(spans):
            nt = t1 - t0
            tv = totp[i][:, 0:nt * NE].rearrange("p (t e) -> p t e", e=NE)
            pv = posp[i][:, 0:nt * NE].rearrange("p (t e) -> p t e", e=NE)
            S.op("dve", I("tensor_tensor", out=incl[:, t0:t1, :], in0=incl[:, t0:t1, :], in1=tv, op=ALU.subtract),
                 reads=[t_incl, t_pp], writes=[t_incl])
            S.op("dve", I("tensor_tensor", out=posf[:, t0:t1, :], in0=incl[:, t0:t1, :], in1=pv, op=ALU.add),
                 reads=[t_incl, t_pp], writes=[t_posf], merge=(i > 0))
        Oall = [sb(f"rt_O{i}", [128, 32, 512], BF16) for i in range(2)]
        t_O = [[Tok() for _ in range(32)] for _ in range(2)]
        Oc = [sb(f"rt_Oc{i}", [128, 2, 128], BF16) for i in range(2)]
        t_Oc = [Tok(), Tok()]
        ips = [pp(f"rt_ips{i}", [128, 512]) for i in range(2)]
        t_ips = [Tok(), Tok()]
        isb = [sb(f"rt_isb{i}", [128, 5, 8], F32) for i in range(2)]
        t_isb = [Tok(), Tok()]
        ia = [sb(f"rt_ia{i}", [128, 5], F32) for i in range(2)]
        t_ia = [Tok(), Tok()]
        nch = 5 if do_ctx else 4
        import os
        LVE = int(os.environ.get("RT_EXP", "16"))
        for e in range(LVE):
            b = e % 2
            for t in range(32):
                S.op("dve", I("tensor_scalar", out=Oall[b][:, t, :], in0=iot[:], scalar1=posf[:, t, e:e + 1], scalar2=Mf[:, t, e:e + 1],
                              op0=ALU.is_equal, op1=ALU.mult),
                     reads=[t_c, t_posf, t_M], writes=[t_O[b][t]])
            first = True
            for sc in range(4):
                for t in range(32):
                    S.op("pe", I("matmul", ips[b][:, sc * 8:sc * 8 + 6], lhsT=Oall[b][:, t, sc * 128:(sc + 1) * 128], rhs=R[:, t, e, :],
                                 start=(t == 0), stop=(t == 31)),
                         reads=[t_O[b][t], t_R], writes=[t_ips[b]], merge=not first)
                    first = False
            if do_ctx:
                for t in range(2):
                    S.op("dve", I("tensor_scalar", out=Oc[b][:, t, :], in0=iot[:, 0:128], scalar1=posf[:, 32 + t, e:e + 1],
                                  scalar2=Mf[:, 32 + t, e:e + 1], op0=ALU.is_equal, op1=ALU.mult),
                         reads=[t_c, t_posf, t_M], writes=[t_Oc[b]], merge=(t > 0))
                for t in range(2):
                    S.op("pe", I("matmul", ips[b][:, 32:38], lhsT=Oc[b][:, t, :], rhs=R[:, 32 + t, e, :], start=(t == 0), stop=(t == 1)),
                         reads=[t_Oc[b], t_R], writes=[t_ips[b]], merge=True)
            S.op("act", I("copy", out=isb[b][:, 0:nch, 0:6], in_=ips[b][:, 0:nch * 8].rearrange("p (c k) -> p c k", k=8)[:, :, 0:6]),
                 reads=[t_ips[b]], writes=[t_isb[b]])
            v = isb[b]
            S.op("dve", I("scalar_tensor_tensor", out=ia[b][:, 0:nch], in0=v[:, 0:nch, 1], scalar=128.0, in1=v[:, 0:nch, 0], op0=ALU.mult, op1=ALU.add),
                 reads=[t_isb[b]], writes=[t_ia[b]])
            S.op("dve", I("scalar_tensor_tensor", out=ia[b][:, 0:nch], in0=v[:, 0:nch, 5], scalar=pidx[:, 2:3], in1=ia[b][:, 0:nch], op0=ALU.mult, op1=ALU.add),
                 reads=[t_isb[b], t_ia[b], t_c], writes=[t_ia[b]])
            S.op("dve", I("tensor_scalar", out=ia[b][:, 0:nch], in0=ia[b][:, 0:nch], scalar1=pidx[:, 1:2], scalar2=None, op0=ALU.add),
                 reads=[t_ia[b], t_c], writes=[t_ia[b]])
            S.op("dve", I("tensor_copy", out=G.idx_all[:, e, 0:nch], in_=ia[b][:, 0:nch]), reads=[t_ia[b]], writes=[G.t_idx], merge=(e > 0))
            S.op("pool", I("tensor_tensor", out=G.g_all[:, e, 0:nch], in0=v[:, 0:nch, 2], in1=v[:, 0:nch, 3], op=ALU.add),
                 reads=[t_isb[b]], writes=[G.t_idx], merge=True)
            S.op("pool", I("tensor_tensor", out=G.g_all[:, e, 0:nch], in0=G.g_all[:, e, 0:nch], in1=v[:, 0:nch, 4], op=ALU.add),
                 reads=[t_isb[b], G.t_idx], writes=[G.t_idx], merge=True)
        if "idxd" in G.debug:
            S.dma("sp", I("dma_start", out=G.idxd, in_=G.idx_all[:].rearrange("p e c -> p (e c)")), reads=[G.t_idx], writes=[G.t["idxd"]])
            S.dma("sp", I("dma_start", out=G.gd, in_=G.g_all[:].rearrange("p e c -> p (e c)")), reads=[G.t_idx], writes=[G.t["gd"]])
        S.flush(f"route{l}")


def phase_moe(G, l, do_ctx):
    nc, S = G.nc, G.S
    with ExitStack() as ps:
        sb, pp = _alloc(G, ps)
        nch = 5 if do_ctx else 4
        NS = 544 if do_ctx else 512
        FB = 256
        ns = 2 if do_ctx else 1
        g2b = []
        t_bc = Tok()
        for s_ in range(ns):
            a = sb(f"mo_g2b{s_}", [128, D], F32)
            S.dma("sp", I("dma_start", out=a[:], in_=G.modrow[l, s_:s_ + 1, 5 * D:6 * D].to_broadcast([128, D])),
                  reads=[G.t["modrow"]], writes=[t_bc], merge=True)
            g2b.append(a)
        xs = [sb(f"mo_xs{i}", [128, D], BF16) for i in range(2)]
        t_xs = [Tok(), Tok()]
        xsT = sb("mo_xsT", [128, 16, NS], BF16)
        t_xsT = Tok()
        wg = [sb(f"mo_wg{i}", [128, 16, FB], BF16) for i in range(2)]
        wu = [sb(f"mo_wu{i}", [128, 16, FB], BF16) for i in range(2)]
        t_wgu = [Tok(), Tok()]
        wd = [sb(f"mo_wd{i}", [128, 16, 512], BF16) for i in range(2)]
        t_wd = [Tok(), Tok()]
        hm = sb("mo_hm", [128, 16, NS], BF16)
        t_hm = [Tok() for _ in range(16)]
        st = [sb(f"mo_st{i}", [128, 544], F32) for i in range(2)]
        t_st = [Tok(), Tok()]
        yst = [sb(f"mo_y{i}", [128, D], F32) for i in range(nch)]
        t_yst = [Tok() for _ in range(nch)]
        if do_ctx:
            S.op("pool", I("memset", yst[4][:], 0.0), writes=[t_yst[4]])
        aps = [pp(f"mo_a{i}", [128, 512]) for i in range(2)]
        ups = [pp(f"mo_u{i}", [128, 512]) for i in range(2)]
        t_aps = [Tok(), Tok()]
        t_ups = [Tok(), Tok()]
        cps = pp("mo_c", [128, 512])
        t_cps = Tok()
        yps = [pp(f"mo_yp{i}", [128, 512]) for i in range(2)]
        t_yps = [Tok(), Tok()]
        trp = pp("mo_tr", [128, 1024], BF16)
        t_trp = Tok()
        import os
        LVE = int(os.environ.get("MOE_EXP", "16"))
        jobs = []
        for e in range(LVE):
            for fb in range(D // FB):
                jobs.append(("gu", e, fb))
            for db in range(4):
                jobs.append(("d", e, db))
        cnt = {"gu": 0, "d": 0}
        slot_of = {}

        def issue(jb):
            kind, e, b = jb
            i = cnt[kind] % 2
            cnt[kind] += 1
            slot_of[jb] = i
            if kind == "gu":
                S.dma("pool", I("dma_start", out=wg[i][:].rearrange("p j n -> p (j n)").rearrange("p (a m) -> p a m", m=2048),
                                in_=G.w_gate[l, e, b].rearrange("p (a m) -> p a m", m=2048)),
                      writes=[t_wgu[i]])
                S.dma("pool", I("dma_start", out=wu[i][:].rearrange("p j n -> p (j n)").rearrange("p (a m) -> p a m", m=2048),
                                in_=G.w_up[l, e, b].rearrange("p (a m) -> p a m", m=2048)),
                      writes=[t_wgu[i]], merge=True)
            else:
                S.dma("pool", I("dma_start", out=wd[i][:].rearrange("p j n -> p (j n)").rearrange("p (a m) -> p a m", m=2048),
                                in_=G.w_down[l, e, b].rearrange("p (a m) -> p a m", m=2048)),
                      writes=[t_wd[i]])

        def gather(e):
            for sc in range(nch):
                i = sc % 2
                m = 128 if sc < 4 else 32
                S.dma("pool", I("indirect_dma_start", out=xs[i][:], out_offset=None, in_=G.fxd[:, :],
                                in_offset=bass.IndirectOffsetOnAxis(ap=G.idx_all[:, e, sc:sc + 1], axis=0)),
                      reads=[G.t_idx, G.t["fxd"]], writes=[t_xs[i]])
                for q in range(2):
                    for jj in range(8):
                        j = q * 8 + jj
                        S.op("pe", I("transpose", out=trp[:, jj * 128:jj * 128 + m], in_=xs[i][0:m, j * 128:(j + 1) * 128],
                                     identity=G.ident_b[0:m, 0:m]),
                             reads=[t_xs[i], G.t_const], writes=[t_trp], merge=(jj > 0))
                    src = trp[:].rearrange("p (a n) -> p a n", n=128)[:, :, 0:m]
                    dst = xsT[:, q * 8:(q + 1) * 8, sc * 128:sc * 128 + m]
                    if q == 0:
                        S.op("act", I("copy", out=dst, in_=src), reads=[t_trp], writes=[t_xsT], merge=not (sc == 0))
                    else:
                        S.op("dve", I("tensor_copy", out=dst, in_=src), reads=[t_trp], writes=[t_xsT], merge=True)

        ji = 0
        issue(jobs[0])
        if LVE > 0:
            gather(0)
        kk = 0
        for e in range(LVE):
            for fb in range(D // FB):
                jb = jobs[ji]
                ji += 1
                if ji < len(jobs):
                    issue(jobs[ji])
                wi = slot_of[jb]
                for fl in range(FB // 128):
                    fo = fb * (FB // 128) + fl
                    ab = kk % 2
                    kk += 1
                    for j in range(16):
                        S.op("pe", I("matmul", aps[ab][:], lhsT=wg[wi][:, j, fl * 128:(fl + 1) * 128], rhs=xsT[:, j, 0:512],
                                     start=(j == 0), stop=(j == 15)),
                             reads=[t_wgu[wi], t_xsT], writes=[t_aps[ab]], merge=(j > 0))
                    for j in range(16):
                        S.op("pe", I("matmul", ups[ab][:], lhsT=wu[wi][:, j, fl * 128:(fl + 1) * 128], rhs=xsT[:, j, 0:512],
                                     start=(j == 0), stop=(j == 15)),
                             reads=[t_wgu[wi], t_xsT], writes=[t_ups[ab]], merge=(j > 0))
                    if do_ctx:
                        for j in range(16):
                            S.op("pe", I("matmul", cps[:, 0:32], lhsT=wg[wi][:, j, fl * 128:(fl + 1) * 128], rhs=xsT[:, j, 512:544],
                                         start=(j == 0), stop=(j == 15)),
                                 reads=[t_wgu[wi], t_xsT], writes=[t_cps], merge=(j > 0))
                        for j in range(16):
                            S.op("pe", I("matmul", cps[:, 32:64], lhsT=wu[wi][:, j, fl * 128:(fl + 1) * 128], rhs=xsT[:, j, 512:544],
                                         start=(j == 0), stop=(j == 15)),
                                 reads=[t_wgu[wi], t_xsT], writes=[t_cps], merge=True)
                    S.op("act", I("activation", out=st[ab][:, 0:512], in_=aps[ab][:], func=AF.Silu), reads=[t_aps[ab]], writes=[t_st[ab]])
                    S.op("dve", I("tensor_tensor", out=hm[:, fo, 0:512], in0=ups[ab][:], in1=st[ab][:, 0:512], op=ALU.mult),
                         reads=[t_ups[ab], t_st[ab]], writes=[t_hm[fo]])
                    if do_ctx:
                        S.op("act", I("activation", out=st[ab][:, 512:544], in_=cps[:, 0:32], func=AF.Silu), reads=[t_cps], writes=[t_st[ab]], merge=True)
                        S.op("dve", I("tensor_tensor", out=hm[:, fo, 512:544], in0=cps[:, 32:64], in1=st[ab][:, 512:544], op=ALU.mult),
                             reads=[t_cps, t_st[ab]], writes=[t_hm[fo]], merge=True)
            if e + 1 < LVE:
                gather(e + 1)
            for db in range(4):
                jb = jobs[ji]
                ji += 1
                if ji < len(jobs):
                    issue(jobs[ji])
                wi = slot_of[jb]
                for sc in range(nch):
                    m = 128 if sc < 4 else 32
                    s_ = 0 if sc < 4 else 1
                    yi = kk % 2
                    kk += 1
                    for fo in range(16):
                        S.op("pe", I("matmul", yps[yi][0:m, :], lhsT=hm[:, fo, sc * 128:sc * 128 + m], rhs=wd[wi][:, fo, :],
                                     start=(fo == 0), stop=(fo == 15)),
                             reads=[t_hm[fo], t_wd[wi]], writes=[t_yps[yi]], merge=(fo > 0))
                    S.op("dve", I("scalar_tensor_tensor", out=yst[sc][0:m, db * 512:(db + 1) * 512], in0=yps[yi][0:m, :],
                                  scalar=G.g_all[0:m, e, sc:sc + 1], in1=g2b[s_][0:m, db * 512:(db + 1) * 512], op0=ALU.mult, op1=ALU.mult),
                         reads=[t_yps[yi], G.t_idx, t_bc], writes=[t_yst[sc]], merge=(db > 0))
            for sc in range(nch):
                S.dma("pool", I("indirect_dma_start", out=G.xres[:, :],
                                out_offset=bass.IndirectOffsetOnAxis(ap=G.idx_all[:, e, sc:sc + 1], axis=0),
                                in_=yst[sc][:], in_offset=None, bounds_check=NPAD - 1, oob_is_err=True, compute_op=ALU.add),
                      reads=[t_yst[sc], G.t_idx, G.t["xres"]], writes=[G.t["xres"]])
        S.flush(f"moe{l}")


def phase_final(G):
    nc, S = G.nc, G.S
    with ExitStack() as ps:
        sb, pp = _alloc(G, ps)
        fg = sb("fi_g", [128, D], F32)
        t_fg = Tok()
        S.dma("sp", I("dma_start", out=fg[:], in_=G.final_g.rearrange("(o d) -> o d", o=1).to_broadcast([128, D])), writes=[t_fg])
        xt = [sb(f"fi_x{i}", [128, D], F32) for i in range(2)]
        t_xt = [Tok(), Tok()]
        ot = [sb(f"fi_o{i}", [128, D], F32) for i in range(2)]
        t_ot = [Tok(), Tok()]
        junk = sb("fi_junk", [128, D], BF16)
        t_junk = Tok()
        ss = [sb(f"fi_ss{i}", [128, 1], F32) for i in range(2)]
        t_ss = [Tok(), Tok()]
        for ti in range(32):
            i = ti % 2
            S.dma("sp", I("dma_start", out=xt[i][:], in_=G.xres[ti * 128:(ti + 1) * 128, :]), reads=[G.t["xres"]], writes=[t_xt[i]])
            S.op("act", I("activation", out=junk[:], in_=xt[i][:], func=AF.Square, accum_out=ss[i][:, 0:1]), reads=[t_xt[i]], writes=[t_junk, t_ss[i]])
            rstd_from_ss(G, ss[i][:, 0:1], ss[i][:, 0:1], D, [t_ss[i]], [t_ss[i]])
            S.op("dve", I("scalar_tensor_tensor", out=ot[i][:], in0=xt[i][:], scalar=ss[i][:, 0:1], in1=fg[:], op0=ALU.mult, op1=ALU.mult),
                 reads=[t_xt[i], t_ss[i], t_fg], writes=[t_ot[i]])
            S.dma("sp", I("dma_start", out=G.out[ti * 128:(ti + 1) * 128, :], in_=ot[i][:]), reads=[t_ot[i]], writes=[G.t["out"]], merge=True)
        S.flush("final")


_CONSTS = None


def make_in_maps(inputs, batches, names=None):
    global _CONSTS
    if _CONSTS is None:
        _CONSTS = host_consts()
    w = host_weights(inputs)
    maps = []
    for b in batches:
        m = dict(w)
        m.update(_CONSTS)
        xin = np.zeros((NPAD, D), np.float32)
        xin[:NL] = inputs["x"][b]
        xin[NL:NT] = inputs["ctx"][b]
        m["xin"] = xin
        m["cvec"] = np.ascontiguousarray(np.stack([inputs["c"][b], inputs["c_ctx"]], axis=1)).astype(np.float32)
        if names is not None:
            m = {k: m[k] for k in names}
        maps.append(m)
    return maps


_PROG = None


def kernel(**inputs):
    global _PROG
    inputs = {k: np.asarray(v) for k, v in inputs.items()}
    if _PROG is None:
        _PROG = build_program()
    nc, G = _PROG
    B = inputs["x"].shape[0]
    maps = make_in_maps(inputs, list(range(B)), G.in_names)
    res = run_bass_kernel_spmd(nc, maps, core_ids=list(range(B)))
    out = np.stack([np.asarray(r["out"]) for r in res.results], axis=0)
    return out.astype(np.float32)
```

```python
import numpy as np
import ml_dtypes
from contextlib import ExitStack
import concourse.bass as bass
import concourse.mybir as mybir
from concourse.bass_utils import run_bass_kernel_spmd

F32 = mybir.dt.float32
BF16 = mybir.dt.bfloat16
I32 = mybir.dt.int32
AF = mybir.ActivationFunctionType
ALU = mybir.AluOpType
NPBF = ml_dtypes.bfloat16

D = 2048
NL = 4096
NCX = 256
NT = NL + NCX
NPAD = NT + 128
DEPTH = 2
EPS = 1e-6
NE = 16
CAP_L = 512
CAP_C = 32

WA_COLS = 3456
OFF_CB, OFF_CC, OFF_CH = 0, 512, 1024
OFF_SQ, OFF_SQW = 1536, 2048
OFF_SK, OFF_SKW = 2560, 2688
OFF_SV = 2816
OFF_FU = 2944
WB_COLS = 960
OFF_CQ, OFF_CKV, OFF_KPA, OFF_KPB = 0, 512, 768, 864

SEM_ROLL = 30000
NDMA_SEMS = 24


def I(name, *args, **kwargs):
    return (name, args, kwargs)


class Tok:
    __slots__ = ("w", "r", "base")

    def __init__(self):
        self.w = {}
        self.r = {}
        self.base = {}


class Sched:
    ENGS = ("pe", "act", "dve", "pool", "sp")

    def __init__(self, nc, stack):
        self.nc = nc
        self.stack = stack
        self.sems = []
        self.prog = {e: [] for e in self.ENGS}
        self.cur_sem = {}
        self.cnt = {}
        for e in ("pe", "act", "dve", "pool"):
            self.cur_sem[e] = self._new_sem(e)
            self.cnt[e] = 0
        self.dma_sems = {q: [self._new_sem("dma" + q) for _ in range(NDMA_SEMS)] for q in ("sp", "pool", "act")}
        self.dma_cnt = {q: [0] * NDMA_SEMS for q in ("sp", "pool", "act")}
        self.dma_rr = {"sp": 0, "pool": 0, "act": 0}
        self.waited = {e: {} for e in self.ENGS}
        self.n_inst = 0
        self.regcache = {}
        self.marks = []
        self.tot_pe = 0

    def _new_sem(self, name):
        h = self.stack.enter_context(self.nc.semaphore(f"s_{name}_{len(self.sems)}"))
        self.sems.append(h)
        return len(self.sems) - 1

    def _collect(self, eng, reads, writes, merge):
        need = {}
        for t in reads:
            for s, v in t.w.items():
                if need.get(s, 0) < v:
                    need[s] = v
        for t in writes:
            src = (t.base, t.r) if merge else (t.w, t.r)
            for dct in src:
                for s, v in dct.items():
                    if need.get(s, 0) < v:
                        need[s] = v
        out = []
        wd = self.waited[eng]
        for s, v in need.items():
            if eng == "pe" and s == self.cur_sem["pe"]:
                continue
            if wd.get(s, 0) >= v:
                continue
            wd[s] = v
            out.append((s, v))
        return out

    def _post(self, ev, reads, writes, merge):
        s, v = ev
        for t in reads:
            if t.r.get(s, 0) < v:
                t.r[s] = v
        for t in writes:
            if merge:
                if t.r:
                    nb = dict(t.base)
                    for rs, rv in t.r.items():
                        if nb.get(rs, 0) < rv:
                            nb[rs] = rv
                    t.base = nb
                if t.w.get(s, 0) < v:
                    t.w[s] = v
            else:
                nb = dict(t.w)
                for rs, rv in t.r.items():
                    if nb.get(rs, 0) < rv:
                        nb[rs] = rv
                t.base = nb
                t.w = {s: v}
            t.r = {}

    def op(self, eng, fn, reads=(), writes=(), merge=False):
        waits = self._collect(eng, reads, writes, merge)
        if self.cnt[eng] >= SEM_ROLL:
            self.cur_sem[eng] = self._new_sem(eng)
            self.cnt[eng] = 0
        self.cnt[eng] += 1
        if eng == "pe":
            self.tot_pe += 1
        s = self.cur_sem[eng]
        v = self.cnt[eng]
        sems = self.sems

        def emit(h, waits=waits, s=s, fn=fn):
            for ws, wv in waits:
                h.wait_ge(sems[ws], wv)
            getattr(h, fn[0])(*fn[1], **fn[2]).then_inc(sems[s], 1)

        self.prog[eng].append(emit)
        self._post((s, v), reads, writes, merge)
        self.n_inst += 1

    def dma(self, eng, fn, reads=(), writes=(), merge=False):
        i = self.dma_rr[eng]
        self.dma_rr[eng] = (i + 1) % NDMA_SEMS
        s = self.dma_sems[eng][i]
        waits = self._collect(eng, reads, writes, merge)
        prev = self.dma_cnt[eng][i]
        if prev > 0 and self.waited[eng].get(s, 0) < prev:
            self.waited[eng][s] = prev
            waits.append((s, prev))
        self.dma_cnt[eng][i] += 16
        v = self.dma_cnt[eng][i]
        sems = self.sems

        def emit(h, waits=waits, s=s, fn=fn):
            for ws, wv in waits:
                h.wait_ge(sems[ws], wv)
            try:
                kw = fn[2]
                if isinstance(kw.get("bounds_check"), int):
                    kw = dict(kw)
                    key = (id(h), kw["bounds_check"])
                    if key not in self.regcache:
                        self.regcache[key] = h.to_reg(kw["bounds_check"])
                    kw["bounds_check"] = self.regcache[key]
                ins = getattr(h, fn[0])(*fn[1], **kw)
            except Exception:
                print("DMA FAIL", fn[0], {k: (getattr(v, "shape", v), getattr(v, "ap", None)) for k, v in fn[2].items()})
                raise
            ins.then_inc(sems[s], 16)

        self.prog[eng].append(emit)
        self._post((s, v), reads, writes, merge)
        self.n_inst += 1

    def wait_all(self, eng, toks):
        waits = self._collect(eng, toks, (), False)
        sems = self.sems

        def emit(h, waits=waits):
            for ws, wv in waits:
                h.wait_ge(sems[ws], wv)

        self.prog[eng].append(emit)

    def barrier(self):
        cur = {}
        for e in ("pe", "act", "dve", "pool"):
            if self.cnt[e] > 0:
                cur[self.cur_sem[e]] = self.cnt[e]
        for q in self.dma_sems:
            for i, s in enumerate(self.dma_sems[q]):
                if self.dma_cnt[q][i] > 0:
                    cur[s] = self.dma_cnt[q][i]
        sems = self.sems
        for eng in self.ENGS:
            wd = self.waited[eng]
            waits = []
            for s, v in cur.items():
                if wd.get(s, 0) < v:
                    wd[s] = v
                    waits.append((s, v))

            def emit(h, waits=waits):
                for ws, wv in waits:
                    h.wait_ge(sems[ws], wv)

            self.prog[eng].append(emit)

    def flush(self, name=None):
        self.barrier()
        self.regcache = {}
        self.marks.append((name, self.tot_pe))
        nc = self.nc
        prog = self.prog
        with nc.Block(name) as block:
            if prog["sp"]:
                @block.sync
                def _(h):
                    for f in prog["sp"]:
                        f(h)
            if prog["pe"]:
                @block.tensor
                def _(h):
                    for f in prog["pe"]:
                        f(h)
            if prog["act"]:
                @block.scalar
                def _(h):
                    for f in prog["act"]:
                        f(h)
            if prog["dve"]:
                @block.vector
                def _(h):
                    for f in prog["dve"]:
                        f(h)
            if prog["pool"]:
                @block.gpsimd
                def _(h):
                    for f in prog["pool"]:
                        f(h)
        self.prog = {e: [] for e in self.ENGS}


def _rope_tables(rot_dim):
    rows = np.repeat(np.arange(64, dtype=np.float32), 64)
    cols = np.tile(np.arange(64, dtype=np.float32), 64)
    n_freq = rot_dim // 4
    inv = (np.float32(10000.0) ** (-np.arange(n_freq, dtype=np.float32) / np.float32(n_freq))).astype(np.float32)
    ang = np.concatenate([rows[:, None] * inv, cols[:, None] * inv], axis=-1).astype(np.float32)
    return np.cos(ang).astype(np.float32), np.sin(ang).astype(np.float32)


def host_consts():
    c = {}
    cs, sn = _rope_tables(64)
    cos2 = np.ones((128, NT), np.float32)
    sin2 = np.zeros((128, NT), np.float32)
    for hh in range(2):
        cos2[hh * 64:hh * 64 + 32, :NL] = cs.T
        cos2[hh * 64 + 32:hh * 64 + 64, :NL] = cs.T
        sin2[hh * 64:hh * 64 + 32, :NL] = -sn.T
        sin2[hh * 64 + 32:hh * 64 + 64, :NL] = sn.T
    c["ropeS"] = np.stack([cos2, sin2], 0)
    cm, sm = _rope_tables(32)
    cosm = np.ones((96, NT), np.float32)
    sinm = np.zeros((96, NT), np.float32)
    cosm[64:80, :NL] = cm.T
    cosm[80:96, :NL] = cm.T
    sinm[64:80, :NL] = -sm.T
    sinm[80:96, :NL] = sm.T
    c["ropeM"] = np.stack([cosm, sinm], 0)
    k = np.arange(64)
    a = 2 * np.pi * np.outer(k, k) / 64.0
    c64 = np.zeros((2, 128, 128), np.float64)
    for g in range(2):
        c64[0, g * 64:(g + 1) * 64, g * 64:(g + 1) * 64] = np.cos(a) / 8.0
        c64[1, g * 64:(g + 1) * 64, g * 64:(g + 1) * 64] = np.sin(a) / 8.0
    c["dft64"] = c64.astype(NPBF)
    def pos_tables(n):
        nt = n // 128
        idx = np.arange(n, dtype=np.int64)
        m = (np.outer(idx, idx) % n).astype(np.float64)
        ang = 2 * np.pi * m / n
        cn = np.cos(ang) / np.sqrt(n)
        sn_ = -np.sin(ang) / np.sqrt(n)
        out = np.zeros((nt, 128, 2, nt, 128), np.float32)
        for kt in range(nt):
            blkc = cn[:, kt * 128:(kt + 1) * 128].reshape(nt, 128, 128)
            blks = sn_[:, kt * 128:(kt + 1) * 128].reshape(nt, 128, 128)
            out[kt, :, 0] = blkc.transpose(1, 0, 2)
            out[kt, :, 1] = blks.transpose(1, 0, 2)
        return out.reshape(nt, 128, 2 * nt * 128).astype(NPBF)
    c["dftL"] = pos_tables(NL)
    c["dftC"] = pos_tables(NCX)
    ident = np.eye(128, dtype=np.float32)
    c["identf"] = ident
    kk = np.arange(128)[:, None]
    qq = np.arange(128)[None, :]
    mprev = (kk >= qq).astype(np.float32)
    mnext = (kk <= qq).astype(np.float32)
    c["masks"] = np.stack([np.tile(mprev, (1, 4)), np.tile(mnext, (1, 4))], 0).astype(NPBF)
    c["triu"] = (kk < qq).astype(NPBF)
    c["iota512"] = np.tile(np.arange(512, dtype=np.float32)[None, :], (128, 1))
    pp_ = np.arange(128, dtype=np.float32)
    c["pidx"] = np.stack([pp_, NT + pp_, -(NT + pp_), np.zeros(128, np.float32)], 1).astype(np.float32)
    c["tgrid"] = np.tile(np.repeat(np.arange(34, dtype=np.float32), NE)[None, :], (128, 1))
    return c


def host_weights(inp):
    w = {}
    w_in = inp["w_in"]
    L = w_in.shape[0]
    wa = np.zeros((L, D, WA_COLS), np.float32)
    wa[:, :, 0:1536] = w_in[:, :, 0:1536]
    sq = w_in[:, :, 1536:2048]
    wa[:, :, OFF_SQ:OFF_SQ + 512] = sq
    sq4 = sq.reshape(L, D, 8, 2, 32)
    wa[:, :, OFF_SQW:OFF_SQW + 512] = sq4[:, :, :, ::-1, :].reshape(L, D, 512)
    sk = w_in[:, :, 2048:2176]
    wa[:, :, OFF_SK:OFF_SK + 128] = sk
    wa[:, :, OFF_SKW:OFF_SKW + 128] = sk.reshape(L, D, 2, 2, 32)[:, :, :, ::-1, :].reshape(L, D, 128)
    wa[:, :, OFF_SV:OFF_SV + 128] = w_in[:, :, 2176:2304]
    wa[:, :, OFF_FU:OFF_FU + 512] = w_in[:, :, 2304:2816]
    w["w_a"] = wa
    wb = np.zeros((L, D, WB_COLS), np.float32)
    wb[:, :, 0:768] = w_in[:, :, 2816:3584]
    kpe = w_in[:, :, 3584:3616]
    wb[:, :, OFF_KPA + 64:OFF_KPA + 96] = kpe
    wb[:, :, OFF_KPB + 64:OFF_KPB + 96] = kpe.reshape(L, D, 2, 16)[:, :, ::-1, :].reshape(L, D, 32)
    w["w_b"] = wb
    uq = inp["mla_w_uq"]
    uq4 = uq.reshape(L, 512, 8, 96)
    uqb = uq4.copy()
    uqb[:, :, :, 64:80] = uq4[:, :, :, 80:96]
    uqb[:, :, :, 80:96] = uq4[:, :, :, 64:80]
    w["w_uq"] = np.stack([uq, uqb.reshape(L, 512, 768)], 1)
    ukv = inp["mla_w_ukv"].reshape(L, 256, 8, 2, 64)
    w["w_uk"] = np.ascontiguousarray(ukv[:, :, :, 0, :]).reshape(L, 256, 512)
    w["w_uv"] = np.ascontiguousarray(ukv[:, :, :, 1, :]).reshape(L, 256, 512)
    for k_ in ("ada_b", "norm1_g", "norm2_g", "conv_w", "swa_sink", "mla_q_norm_g",
               "mla_kv_norm_g", "out_norm_g", "w_out", "w_router", "final_norm_g"):
        w[k_] = inp[k_]
    def tile_cols(a, W):
        lead = a.shape[:-2]
        K, N = a.shape[-2:]
        a6 = a.reshape(lead + (K // 128, 128, N // W, W))
        nd = len(lead)
        perm = tuple(range(nd)) + (nd + 2, nd + 1, nd + 0, nd + 3)
        return np.ascontiguousarray(a6.transpose(perm)).reshape(lead + (N // W, 128, (K // 128) * W))
    w["ada_w"] = tile_cols(inp["ada_w"], 512)
    if "w_gate" in inp:
        w["w_gate"] = tile_cols(inp["w_gate"], 256)
        w["w_up"] = tile_cols(inp["w_up"], 256)
        w["w_down"] = tile_cols(inp["w_down"], 512)
    return w


class Ctx:
    pass


def build_program(debug=(), n_layers=DEPTH, stop_after=None):
    nc = bass.Bass("TRN2", target_bir_lowering=False)
    G = Ctx()
    G.nc = nc
    G.debug = set(debug)
    din = {}

    def inp(name, shape, dt=F32):
        din[name] = nc.dram_tensor(name, list(shape), dt, kind="ExternalInput").ap()
        return din[name]

    G.xin = inp("xin", [NPAD, D])
    G.cvec = inp("cvec", [D, 2])
    G.ada_w = inp("ada_w", [DEPTH, 24, 128, 16 * 512])
    G.ada_b = inp("ada_b", [DEPTH, 6 * D])
    G.norm1_g = inp("norm1_g", [DEPTH, D])
    G.norm2_g = inp("norm2_g", [DEPTH, D])
    G.w_a = inp("w_a", [DEPTH, D, WA_COLS])
    G.w_b = inp("w_b", [DEPTH, D, WB_COLS])
    G.conv_w = inp("conv_w", [DEPTH, 3, 512])
    G.swa_sink = inp("swa_sink", [DEPTH, 8])
    G.q_norm_g = inp("mla_q_norm_g", [DEPTH, 512])
    G.kv_norm_g = inp("mla_kv_norm_g", [DEPTH, 256])
    G.w_uq = inp("w_uq", [DEPTH, 2, 512, 768])
    G.w_uk = inp("w_uk", [DEPTH, 256, 512])
    G.w_uv = inp("w_uv", [DEPTH, 256, 512])
    G.out_norm_g = inp("out_norm_g", [DEPTH, D])
    G.w_out = inp("w_out", [DEPTH, D, D])
    G.w_router = inp("w_router", [DEPTH, D, NE])
    if stop_after is None or stop_after in ("moe", "moetest"):
        G.w_gate = inp("w_gate", [DEPTH, NE, 8, 128, 16 * 256])
        G.w_up = inp("w_up", [DEPTH, NE, 8, 128, 16 * 256])
        G.w_down = inp("w_down", [DEPTH, NE, 4, 128, 16 * 512])
    G.final_g = inp("final_norm_g", [D])
    G.ropeS = inp("ropeS", [2, 128, NT])
    G.ropeM = inp("ropeM", [2, 96, NT])
    G.dft64 = inp("dft64", [2, 128, 128], BF16)
    G.dftL = inp("dftL", [32, 128, 2 * 32 * 128], BF16)
    G.dftC = inp("dftC", [2, 128, 2 * 2 * 128], BF16)
    G.identf = inp("identf", [128, 128])
    G.masks = inp("masks", [2, 128, 512], BF16)
    G.triu = inp("triu", [128, 128], BF16)
    G.iota512 = inp("iota512", [128, 512])
    G.pidx = inp("pidx", [128, 4])
    G.tgrid = inp("tgrid", [128, 34 * NE])

    G.out = nc.dram_tensor("out", [NL, D], F32, kind="ExternalOutput").ap()

    def scratch(name, shape, dt):
        kind = "ExternalOutput" if name in G.debug else "Internal"
        return nc.dram_tensor(name, list(shape), dt, kind=kind).ap()

    G.xres = scratch("xres", [NPAD, D], F32)
    G.hT = scratch("hT", [D, NT], BF16)
    G.cbT = scratch("cbT", [512, NT], F32)
    G.uT = scratch("uT", [512, NT], F32)
    G.sqT = scratch("sqT", [512, NT], BF16)
    G.skT = scratch("skT", [128, NT], BF16)
    G.svd = scratch("svd", [NT, 128], BF16)
    G.ucd = scratch("ucd", [NT, 512], BF16)
    G.usd = scratch("usd", [NT, 512], BF16)
    G.mqT = scratch("mqT", [8, 96, NT], BF16)
    G.mkT = scratch("mkT", [8, 96, NT], BF16)
    G.mvd = scratch("mvd", [NT, 512], BF16)
    G.ynT = scratch("ynT", [D, NT], BF16)
    G.fxd = scratch("fxd", [NPAD, D], BF16)
    G.modrow = scratch("modrow", [DEPTH, 2, 6 * D], F32)
    G.affd = scratch("affd", [NT, NE], F32)
    if "idxd" in G.debug:
        G.idxd = nc.dram_tensor("idxd", [128, NE * 5], I32, kind="ExternalOutput").ap()
        G.gd = nc.dram_tensor("gd", [128, NE * 5], F32, kind="ExternalOutput").ap()
    if stop_after == "moetest":
        G.aff_in = inp("aff_in", [NT, NE])
        G.fx_in = inp("fx_in", [NPAD, D], BF16)

    with ExitStack() as st:
        S = Sched(nc, st)
        G.S = S
        G.st = st
        G.t = {k: Tok() for k in ("xres", "hT", "cbT", "uT", "sqT", "skT", "svd", "ucd", "usd", "mqT",
                                  "mkT", "mvd", "ynT", "fxd", "out", "modrow", "affd", "idxd", "gd")}
        G.ident_f = st.enter_context(nc.sbuf_tensor("ident_f", [128, 128], F32))
        G.ident_b = st.enter_context(nc.sbuf_tensor("ident_b", [128, 128], BF16))
        G.ones_b = st.enter_context(nc.sbuf_tensor("ones_b", [128, 128], BF16))
        G.ones_f = st.enter_context(nc.sbuf_tensor("ones_f", [128, 128], F32))
        G.t_const = Tok()
        G.eps_t = st.enter_context(nc.sbuf_tensor("eps_t", [128, 1], F32))
        S.op("pool", I("memset", G.eps_t[:], EPS), writes=[G.t_const], merge=True)
        S.dma("sp", I("dma_start", out=G.ident_f[:], in_=G.identf), writes=[G.t_const])
        S.op("dve", I("tensor_copy", out=G.ident_b[:], in_=G.ident_f[:]), reads=[G.t_const], writes=[G.t_const])
        S.op("pool", I("memset", G.ones_b[:], 1.0), writes=[G.t_const], merge=True)
        S.op("pool", I("memset", G.ones_f[:], 1.0), writes=[G.t_const], merge=True)
        G.aff_all = st.enter_context(nc.sbuf_tensor("aff_all", [128, 34, NE], F32))
        G.t_aff = Tok()
        S.op("pool", I("memset", G.aff_all[:], 0.0), writes=[G.t_aff])
        G.idx_all = st.enter_context(nc.sbuf_tensor("idx_all", [128, NE, 5], I32))
        G.g_all = st.enter_context(nc.sbuf_tensor("g_all", [128, NE, 5], F32))
        G.t_idx = Tok()
        S.op("pool", I("memset", G.idx_all[:], 0), writes=[G.t_idx])
        S.op("pool", I("memset", G.g_all[:], 0.0), writes=[G.t_idx], merge=True)
        S.dma("sp", I("dma_start", out=G.xres.rearrange("(a p) d -> p a d", p=128),
                                          in_=G.xin.rearrange("(a p) d -> p a d", p=128)),
              writes=[G.t["xres"]])
        S.dma("pool", I("dma_start", out=G.fxd[NT:NPAD, :], in_=G.xin[NT:NPAD, :]), writes=[G.t["fxd"]], merge=True)
        S.flush("init")
        if stop_after == "moetest":
            phase_adaln(G, 0)
            S.dma("sp", I("dma_start", out=G.aff_all[:], in_=G.aff_in.rearrange("(t p) e -> p t e", p=128)), writes=[G.t_aff])
            S.dma("sp", I("dma_start", out=G.fxd.rearrange("(a p) d -> p a d", p=128), in_=G.fx_in.rearrange("(a p) d -> p a d", p=128)),
                  writes=[G.t["fxd"]])
            phase_route(G, 0, True)
            if "moe" in G.debug:
                phase_moe(G, 0, True)
            n_layers = 0
        for l in range(n_layers):
            last = (l == DEPTH - 1)
            phase_adaln(G, l)
            if stop_after == "adaln":
                break
            phase_proj_a(G, l)
            if stop_after == "proj_a":
                break
            phase_proj_b(G, l)
            if stop_after == "proj_b":
                break
            phase_conv(G, l, not last)
            if stop_after == "conv":
                break
            phase_swa(G, l, not last)
            if stop_after == "swa":
                break
            phase_fnet(G, l, not last)
            if stop_after == "fnet":
                break
            phase_mla(G, l, not last)
            if stop_after == "mla":
                break
            phase_outproj(G, l, not last)
            if stop_after == "outproj":
                break
            phase_route(G, l, not last)
            if stop_after == "route":
                break
            phase_moe(G, l, not last)
            if stop_after == "moe":
                break
        if stop_after is None and n_layers == DEPTH:
            phase_final(G)
        S.wait_all("sp", list(G.t.values()))
        S.flush("fin")
        G.n_inst = S.n_inst
    G.in_names = list(din.keys())
    return nc, G


def _alloc(G, ps):
    nc = G.nc
    G.uid = getattr(G, "uid", 0) + 1
    u = G.uid
    sb = lambda name, shape, dt: ps.enter_context(nc.sbuf_tensor(f"{name}_{u}", list(shape), dt))
    pp = lambda name, shape, dt=F32: ps.enter_context(nc.psum_tensor(f"{name}_{u}", list(shape), dt))
    return sb, pp


BLOCKS = [(i * 512, 512, 0) for i in range(8)] + [(NL, NCX, 1)]


def phase_adaln(G, l):
    nc, S = G.nc, G.S
    with ExitStack() as ps:
        sb, pp = _alloc(G, ps)
        cT = sb("ad_cT", [128, 16, 2], F32)
        sl = sb("ad_sl", [128, 16, 64], BF16)
        sel = sb("ad_sel", [1, 64], BF16)
        brow = sb("ad_brow", [1, 6 * D], BF16)
        rows = sb("ad_rows", [64, 6 * D], F32)
        wt = [sb(f"ad_wt{i}", [128, 16, 512], BF16) for i in range(2)]
        pacc = [pp(f"ad_ps{i}", [64, 512]) for i in range(2)]
        t_c, t_sl, t_sel, t_b, t_rows = Tok(), Tok(), Tok(), Tok(), Tok()
        t_wt = [Tok(), Tok()]
        t_ps = [Tok(), Tok()]
        S.dma("sp", I("dma_start", out=cT[:], in_=G.cvec.rearrange("(j p) t -> p j t", p=128)), writes=[t_c])
        S.op("pool", I("memset", sl[:], 0.0), writes=[t_sl])
        S.op("pool", I("memset", sel[:], 0.0), writes=[t_sel])
        S.op("pool", I("memset", sel[0:1, 0:1], 1.0), writes=[t_sel])
        S.op("pool", I("memset", sel[0:1, 32:33], 1.0), writes=[t_sel])
        S.op("act", I("activation", out=sl[:, :, 0], in_=cT[:, :, 0], func=AF.Silu), reads=[t_c], writes=[t_sl])
        S.op("act", I("activation", out=sl[:, :, 32], in_=cT[:, :, 1], func=AF.Silu), reads=[t_c], writes=[t_sl])
        for q in range(6):
            S.dma("pool", I("dma_start", out=brow[0:1, q * D:(q + 1) * D], in_=G.ada_b[l:l + 1, q * D:(q + 1) * D]),
                  writes=[t_b], merge=True)
        for nb in range(24):
            b = nb % 2
            S.dma("pool", I("dma_start", out=wt[b][:].rearrange("p j n -> p (j n)").rearrange("p (a m) -> p a m", m=2048),
                            in_=G.ada_w[l, nb].rearrange("p (a m) -> p a m", m=2048)),
                  writes=[t_wt[b]])
            for j in range(16):
                S.op("pe", I("matmul", pacc[b][:], lhsT=sl[:, j, :], rhs=wt[b][:, j, :], start=(j == 0), stop=False),
                     reads=[t_sl, t_wt[b]], writes=[t_ps[b]], merge=(j > 0))
            S.op("pe", I("matmul", pacc[b][:], lhsT=sel[:], rhs=brow[0:1, nb * 512:(nb + 1) * 512], start=False, stop=True),
                 reads=[t_sel, t_b], writes=[t_ps[b]], merge=True)
            S.op("act", I("copy", out=rows[0:1, nb * 512:(nb + 1) * 512], in_=pacc[b][0:1, :]),
                 reads=[t_ps[b]], writes=[t_rows], merge=True)
            S.op("dve", I("tensor_copy", out=rows[32:33, nb * 512:(nb + 1) * 512], in_=pacc[b][32:33, :]),
                 reads=[t_ps[b]], writes=[t_rows], merge=True)
        S.dma("sp", I("dma_start", out=G.modrow[l, 0:1, :], in_=rows[0:1, :]), reads=[t_rows], writes=[G.t["modrow"]], merge=True)
        S.dma("sp", I("dma_start", out=G.modrow[l, 1:2, :], in_=rows[32:33, :]), reads=[t_rows], writes=[G.t["modrow"]], merge=True)
        S.flush(f"adaln{l}")


def load_T(G, row_aps, sb, pp, name):
    S = G.S
    nv = len(row_aps)
    t_stg0 = Tok()
    n = max(ap.shape[-1] for ap in row_aps) // 128
    stg = sb(name + "_stg", [16, nv, 128], F32)
    out = sb(name, [128, nv, n], F32)
    S.op("pool", I("memset", stg[:], 0.0), writes=[t_stg0])
    pst_full = pp(name + "_ps", [128, 512])
    pst = pst_full[:, 0:nv * 16].rearrange("p (v n) -> p v n", n=16)
    t_stg, t_ps, t_out = t_stg0, Tok(), Tok()
    for v, ap in enumerate(row_aps):
        S.dma("sp", I("dma_start", out=stg[0:ap.shape[-1] // 128, v, :], in_=ap.rearrange("(j p) -> j p", p=128)),
              reads=[G.t["modrow"]], writes=[t_stg], merge=(v > 0))
    for v in range(nv):
        S.op("pe", I("transpose", out=pst[:, v, 0:n], in_=stg[0:n, v, :], identity=G.ident_f[0:n, 0:n]),
             reads=[t_stg, G.t_const], writes=[t_ps], merge=(v > 0))
    S.op("dve", I("tensor_copy", out=out[:], in_=pst[:, :, 0:n]), reads=[t_ps], writes=[t_out])
    return out, t_out


def rstd_from_ss(G, out_ap, ss_ap, n, reads, writes, merge=False):
    S = G.S
    S.op("act", I("activation", out=out_ap, in_=ss_ap, func=AF.Ln, bias=G.eps_t[0:out_ap.shape[0], 0:1], scale=1.0 / n),
         reads=list(reads) + [G.t_const], writes=writes, merge=merge)
    S.op("act", I("activation", out=out_ap, in_=out_ap, func=AF.Exp, scale=-0.5),
         reads=writes, writes=writes)


def phase_proj_a(G, l):
    nc, S = G.nc, G.S
    with ExitStack() as ps:
        sb, pp = _alloc(G, ps)
        NCOL = 2560
        wA = sb("pa_w", [128, 16, NCOL], BF16)
        t_w = Tok()
        for c0 in range(0, NCOL, 512):
            S.dma("pool", I("dma_start",
                out=wA[:, :, c0:c0 + 512], in_=G.w_a[l][:, c0:c0 + 512].rearrange("(j p) n -> p j n", p=128)),
                writes=[t_w], merge=True)
        mv, t_mv = load_T(G, [G.modrow[l, 0, 0:D], G.modrow[l, 1, 0:D], G.modrow[l, 0, D:2 * D],
                              G.modrow[l, 1, D:2 * D], G.norm1_g[l]], sb, pp, "pa_mv")
        sh1T = mv
        t_sh = t_mv
        gs1T = sb("pa_gs", [128, 2, 16], F32)
        t_gs = Tok()
        for s in range(2):
            S.op("dve", I("scalar_tensor_tensor", out=gs1T[:, s, :], in0=mv[:, 2 + s, :], scalar=1.0, in1=mv[:, 4, :],
                                                           op0=ALU.add, op1=ALU.mult),
                 reads=[t_mv], writes=[t_gs], merge=True)
        xt = [sb(f"pa_xt{i}", [128, D], F32) for i in range(2)]
        t_xt = [Tok(), Tok()]
        junk = sb("pa_junk", [128, D], BF16)
        t_junk = Tok()
        ssq = sb("pa_ss", [128, 8], F32)
        t_ss = [Tok() for _ in range(8)]
        xh = sb("pa_xh", [128, 4, D], BF16)
        t_xh = [Tok() for _ in range(4)]
        hTbs = [sb(f"pa_hT{i}", [128, 16, 512], BF16) for i in range(2)]
        t_hTs = [Tok(), Tok()]
        rS = [sb(f"pa_rS{i}", [128, 2, 512], F32) for i in range(2)]
        t_rS = [Tok(), Tok()]
        cc_s = sb("pa_cc", [128, 4, 512], F32)
        t_cc = [Tok() for _ in range(4)]
        t1 = sb("pa_t1", [128, 4, 512], F32)
        t_t1 = [Tok() for _ in range(4)]
        NST = 4
        stf = [sb(f"pa_stf{i}", [128, 512], F32) for i in range(NST)]
        t_stf = [Tok() for _ in range(NST)]
        stb = [sb(f"pa_stb{i}", [128, 512], BF16) for i in range(NST)]
        t_stb = [Tok() for _ in range(NST)]
        pT = [pp(f"pa_pT{i}", [128, 1024], BF16) for i in range(2)]
        t_pT = [Tok(), Tok()]
        NACC = 4
        acc = [pp(f"pa_acc{i}", [128, 512]) for i in range(NACC)]
        t_acc = [Tok() for _ in range(NACC)]
        cnt = {"f": 0, "b": 0, "a": 0, "x": 0}

        import os
        LV = int(os.environ.get("PA_STOP", "9"))
        for bi, (tok0, nb, s) in enumerate(BLOCKS):
            if LV <= 1 or (LV <= 3 and bi > 0):
                break
            ntile = nb // 128
            rb = bi % 2
            hTb = hTbs[bi % 2]
            t_hT = t_hTs[bi % 2]
            S.dma("sp", I("dma_start",
                out=rS[rb][:, :, 0:nb], in_=G.ropeS[:, :, tok0:tok0 + nb].rearrange("t p n -> p t n")),
                writes=[t_rS[rb]])
            for t in range(ntile):
                xi = cnt["x"] % 2
                cnt["x"] += 1
                si = (bi * 4 + t) % 8
                r0 = tok0 + t * 128
                S.dma("sp", I("dma_start", out=xt[xi][:], in_=G.xres[r0:r0 + 128, :]),
                      reads=[G.t["xres"]], writes=[t_xt[xi]])
                S.op("act", I("activation", out=junk[:], in_=xt[xi][:], func=AF.Square,
                                                                 accum_out=ssq[:, si:si + 1]),
                     reads=[t_xt[xi]], writes=[t_junk, t_ss[si]])
                rstd_from_ss(G, ssq[:, si:si + 1], ssq[:, si:si + 1], D, [t_ss[si]], [t_ss[si]])
                S.op("act", I("activation", out=xh[:, t, :], in_=xt[xi][:], func=AF.Copy,
                                                                      scale=ssq[:, si:si + 1]),
                     reads=[t_xt[xi], t_ss[si]], writes=[t_xh[t]])
            for j in range(16):
                pb = j % 2
                for t in range(ntile):
                    S.op("pe", I("transpose", out=pT[pb][:, t * 128:(t + 1) * 128],
                                                                     in_=xh[:, t, j * 128:(j + 1) * 128], identity=G.ident_b[:]),
                         reads=[t_xh[t], G.t_const], writes=[t_pT[pb]], merge=(t > 0))
                S.op("act", I("activation",
                    out=hTb[:, j, 0:nb], in_=pT[pb][:, 0:nb], func=AF.Identity,
                    bias=sh1T[:, s, j:j + 1], scale=gs1T[:, s, j:j + 1]),
                    reads=[t_pT[pb], t_sh, t_gs], writes=[t_hT], merge=(j > 0))
            S.dma("sp", I("dma_start",
                out=G.hT[:, tok0:tok0 + nb].rearrange("(j p) n -> p j n", p=128), in_=hTb[:, :, 0:nb]),
                reads=[t_hT], writes=[G.t["hT"]], merge=True)

            if LV <= 2:
                break
            def proj(c0, width):
                ai = cnt["a"] % NACC
                cnt["a"] += 1
                for j in range(16):
                    S.op("pe", I("matmul",
                        acc[ai][0:width, 0:nb], lhsT=wA[:, j, c0:c0 + width], rhs=hTb[:, j, 0:nb],
                        start=(j == 0), stop=(j == 15)),
                        reads=[t_w, t_hT], writes=[t_acc[ai]], merge=(j > 0))
                return ai

            def stage_f():
                i = cnt["f"] % NST
                cnt["f"] += 1
                return i

            def stage_b():
                i = cnt["b"] % NST
                cnt["b"] += 1
                return i

            for c in range(4):
                ai = proj(OFF_CB + c * 128, 128)
                fi = stage_f()
                S.op("act", I("copy", out=stf[fi][:, 0:nb], in_=acc[ai][:, 0:nb]),
                     reads=[t_acc[ai]], writes=[t_stf[fi]])
                S.dma("sp", I("dma_start", out=G.cbT[c * 128:(c + 1) * 128, tok0:tok0 + nb], in_=stf[fi][:, 0:nb]),
                      reads=[t_stf[fi]], writes=[G.t["cbT"]], merge=True)
                ai = proj(OFF_CC + c * 128, 128)
                S.op("act", I("copy", out=cc_s[:, c, 0:nb], in_=acc[ai][:, 0:nb]),
                     reads=[t_acc[ai]], writes=[t_cc[c]])
                ai = proj(OFF_CH + c * 128, 128)
                fi = stage_f()
                S.op("dve", I("tensor_tensor", out=stf[fi][:, 0:nb], in0=acc[ai][:, 0:nb],
                                                                        in1=cc_s[:, c, 0:nb], op=ALU.mult),
                     reads=[t_acc[ai], t_cc[c]], writes=[t_stf[fi]])
                S.dma("sp", I("dma_start", out=G.uT[c * 128:(c + 1) * 128, tok0:tok0 + nb], in_=stf[fi][:, 0:nb]),
                      reads=[t_stf[fi]], writes=[G.t["uT"]], merge=True)
            for c in range(4):
                ai = proj(OFF_SQ + c * 128, 128)
                S.op("dve", I("tensor_tensor", out=t1[:, c, 0:nb], in0=acc[ai][:, 0:nb],
                                                                        in1=rS[rb][:, 0, 0:nb], op=ALU.mult),
                     reads=[t_acc[ai], t_rS[rb]], writes=[t_t1[c]])
                ai = proj(OFF_SQW + c * 128, 128)
                fi = stage_f()
                S.op("dve", I("tensor_tensor", out=stf[fi][:, 0:nb], in0=acc[ai][:, 0:nb],
                                                                          in1=rS[rb][:, 1, 0:nb], op=ALU.mult),
                     reads=[t_acc[ai], t_rS[rb]], writes=[t_stf[fi]])
                bi_ = stage_b()
                S.op("pool", I("tensor_tensor", out=stb[bi_][:, 0:nb], in0=t1[:, c, 0:nb],
                                                                           in1=stf[fi][:, 0:nb], op=ALU.add),
                     reads=[t_t1[c], t_stf[fi]], writes=[t_stb[bi_]])
                S.dma("sp", I("dma_start", out=G.sqT[c * 128:(c + 1) * 128, tok0:tok0 + nb], in_=stb[bi_][:, 0:nb]),
                      reads=[t_stb[bi_]], writes=[G.t["sqT"]], merge=True)
        S.flush(f"proja{l}")


def phase_proj_b(G, l):
    nc, S = G.nc, G.S
    with ExitStack() as ps:
        sb, pp = _alloc(G, ps)
        w1 = sb("pb_w1", [128, 16, 896], BF16)
        w2 = sb("pb_w2", [128, 16, 960], BF16)
        wq = sb("pb_wq", [128, 4, 2, 768], BF16)
        wk = sb("pb_wk", [128, 2, 512], BF16)
        wv = sb("pb_wv", [128, 2, 512], BF16)
        d64 = sb("pb_d64", [128, 2, 128], BF16)
        t_w = Tok()
        for c0 in range(0, 896, 448):
            S.dma("pool", I("dma_start", out=w1[:, :, c0:c0 + 448],
                            in_=G.w_a[l][:, 2560 + c0:2560 + c0 + 448].rearrange("(j p) n -> p j n", p=128)),
                  writes=[t_w], merge=True)
        for c0 in range(0, 960, 480):
            S.dma("pool", I("dma_start", out=w2[:, :, c0:c0 + 480],
                            in_=G.w_b[l][:, c0:c0 + 480].rearrange("(j p) n -> p j n", p=128)),
                  writes=[t_w], merge=True)
        for ab in range(2):
            S.dma("pool", I("dma_start", out=wq[:, :, ab, :], in_=G.w_uq[l, ab].rearrange("(c p) n -> p c n", p=128)),
                  writes=[t_w], merge=True)
        S.dma("pool", I("dma_start", out=wk[:], in_=G.w_uk[l].rearrange("(c p) n -> p c n", p=128)), writes=[t_w], merge=True)
        S.dma("pool", I("dma_start", out=wv[:], in_=G.w_uv[l].rearrange("(c p) n -> p c n", p=128)), writes=[t_w], merge=True)
        S.dma("sp", I("dma_start", out=d64[:], in_=G.dft64.rearrange("t p n -> p t n")), writes=[t_w], merge=True)
        gT, t_gT = load_T(G, [G.q_norm_g[l], G.kv_norm_g[l]], sb, pp, "pb_g")
        hTb = [sb(f"pb_hT{i}", [128, 16, 512], BF16) for i in range(2)]
        t_hT = [Tok(), Tok()]
        rS = sb("pb_rS", [128, 2, 512], F32)
        rM = sb("pb_rM", [96, 2, 512], F32)
        t_r = Tok()
        cq_f = sb("pb_cqf", [128, 4, 512], F32)
        t_cqf = [Tok() for _ in range(4)]
        sqb = [sb(f"pb_sqb{i}", [128, 512], BF16) for i in range(2)]
        t_sqb = [Tok(), Tok()]
        rstd = sb("pb_rstd", [128, 2, 512], F32)
        t_rstd = [Tok(), Tok()]
        cqn = sb("pb_cqn", [128, 4, 512], BF16)
        t_cqn = Tok()
        ckf = sb("pb_ckf", [128, 2, 512], F32)
        t_ckf = [Tok(), Tok()]
        ckn = sb("pb_ckn", [128, 2, 512], BF16)
        t_ckn = Tok()
        fuT = sb("pb_fuT", [128, 4, 512], BF16)
        t_fuT = Tok()
        mq_st = sb("pb_mq", [96, 8, 512], BF16)
        t_mq = Tok()
        mk_st = sb("pb_mk", [96, 8, 512], BF16)
        t_mk = Tok()
        kpe_r = sb("pb_kpe", [96, 512], BF16)
        t_kpe = Tok()
        tA = [sb(f"pb_tA{i}", [128, 512], F32) for i in range(2)]
        t_tA = [Tok(), Tok()]
        tB = [sb(f"pb_tB{i}", [128, 512], F32) for i in range(2)]
        t_tB = [Tok(), Tok()]
        NST = 3
        stb = [sb(f"pb_stb{i}", [128, 512], BF16) for i in range(NST)]
        t_stb = [Tok() for _ in range(NST)]
        NACC = 3
        acc = [pp(f"pb_acc{i}", [128, 512]) for i in range(NACC)]
        t_acc = [Tok() for _ in range(NACC)]
        ssp = pp("pb_ss", [128, 512])
        t_ssp = Tok()
        tkp = [pp(f"pb_tk{i}", [128, 512]) for i in range(2)]
        t_tkp = [Tok(), Tok()]
        cnt = {"a": 0, "b": 0, "t": 0, "k": 0}
        import os
        LV = int(os.environ.get("PB_STOP", "9"))

        for bi, (tok0, nb, s) in enumerate(BLOCKS):
            if LV <= 3 and bi > 0:
                break
            ntile = nb // 128
            hb = bi % 2
            hT_ = hTb[hb]
            S.dma("sp", I("dma_start", out=hT_[:, :, 0:nb], in_=G.hT[:, tok0:tok0 + nb].rearrange("(j p) n -> p j n", p=128)),
                  reads=[G.t["hT"]], writes=[t_hT[hb]])
            S.dma("sp", I("dma_start", out=rS[:, :, 0:nb], in_=G.ropeS[:, :, tok0:tok0 + nb].rearrange("t p n -> p t n")),
                  writes=[t_r])
            S.dma("sp", I("dma_start", out=rM[:, :, 0:nb], in_=G.ropeM[:, :, tok0:tok0 + nb].rearrange("t p n -> p t n")),
                  writes=[t_r], merge=True)

            def proj(wt, c0, width, rhs_tile=None):
                ai = cnt["a"] % NACC
                cnt["a"] += 1
                for j in range(16):
                    S.op("pe", I("matmul", acc[ai][0:width, 0:nb], lhsT=wt[:, j, c0:c0 + width], rhs=hT_[:, j, 0:nb],
                                 start=(j == 0), stop=(j == 15)),
                         reads=[t_w, t_hT[hb]], writes=[t_acc[ai]], merge=(j > 0))
                return ai

            def rope_pair(aiA, aiB, p0, p1, table, out_ap, t_out, merge_out=False):
                ti = cnt["t"] % 2
                cnt["t"] += 1
                S.op("dve", I("tensor_tensor", out=tA[ti][p0:p1, 0:nb], in0=acc[aiA][p0:p1, 0:nb], in1=table[p0:p1, 0, 0:nb], op=ALU.mult),
                     reads=[t_acc[aiA], t_r], writes=[t_tA[ti]])
                S.op("dve", I("tensor_tensor", out=tB[ti][p0:p1, 0:nb], in0=acc[aiB][p0:p1, 0:nb], in1=table[p0:p1, 1, 0:nb], op=ALU.mult),
                     reads=[t_acc[aiB], t_r], writes=[t_tB[ti]])
                S.op("pool", I("tensor_tensor", out=out_ap, in0=tA[ti][p0:p1, 0:nb], in1=tB[ti][p0:p1, 0:nb], op=ALU.add),
                     reads=[t_tA[ti], t_tB[ti]], writes=[t_out], merge=merge_out)

            def stage_b():
                i = cnt["b"] % NST
                cnt["b"] += 1
                return i

            aiA = proj(w1, 0, 128)
            aiB = proj(w1, 128, 128)
            bi_ = stage_b()
            rope_pair(aiA, aiB, 0, 128, rS, stb[bi_][:, 0:nb], t_stb[bi_])
            S.dma("sp", I("dma_start", out=G.skT[:, tok0:tok0 + nb], in_=stb[bi_][:, 0:nb]), reads=[t_stb[bi_]],
                  writes=[G.t["skT"]], merge=True)
            ki = cnt["k"] % 2
            cnt["k"] += 1
            for t in range(ntile):
                for j in range(16):
                    S.op("pe", I("matmul", tkp[ki][:, t * 128:(t + 1) * 128], lhsT=hT_[:, j, t * 128:(t + 1) * 128],
                                 rhs=w1[:, j, 256:384], start=(j == 0), stop=(j == 15)),
                         reads=[t_w, t_hT[hb]], writes=[t_tkp[ki]], merge=(j > 0 or t > 0))
            bi_ = stage_b()
            S.op("act", I("copy", out=stb[bi_][:, 0:nb], in_=tkp[ki][:, 0:nb]), reads=[t_tkp[ki]], writes=[t_stb[bi_]])
            S.dma("sp", I("dma_start", out=G.svd[tok0:tok0 + nb, :].rearrange("(t p) c -> p t c", p=128),
                          in_=stb[bi_][:, 0:nb].rearrange("p (t c) -> p t c", c=128)),
                  reads=[t_stb[bi_]], writes=[G.t["svd"]], merge=True)
            for c in range(4):
                ai = proj(w1, 384 + c * 128, 128)
                S.op("act", I("copy", out=fuT[:, c, 0:nb], in_=acc[ai][:, 0:nb]), reads=[t_acc[ai]], writes=[t_fuT], merge=(c > 0))
            for t in range(ntile):
                for cs_, dst, key in ((0, G.ucd, "ucd"), (1, G.usd, "usd")):
                    ki = cnt["k"] % 2
                    cnt["k"] += 1
                    for c in range(4):
                        S.op("pe", I("matmul", tkp[ki][:, c * 128:(c + 1) * 128], lhsT=fuT[:, c, t * 128:(t + 1) * 128],
                                     rhs=d64[:, cs_, :], start=True, stop=True),
                             reads=[t_w, t_fuT], writes=[t_tkp[ki]], merge=(c > 0))
                    bi_ = stage_b()
                    S.op("act" if cs_ == 0 else "dve",
                         I("copy", out=stb[bi_][:], in_=tkp[ki][:]) if cs_ == 0 else I("tensor_copy", out=stb[bi_][:], in_=tkp[ki][:]),
                         reads=[t_tkp[ki]], writes=[t_stb[bi_]])
                    S.dma("sp", I("dma_start", out=dst[tok0 + t * 128:tok0 + (t + 1) * 128, :], in_=stb[bi_][:]),
                          reads=[t_stb[bi_]], writes=[G.t[key]], merge=True)
            def latent_norm(c0, nch, f_tile, t_f, n_tile, t_n, gi, ri):
                for c in range(nch):
                    ai = proj(w2, c0 + c * 128, 128)
                    S.op("act", I("copy", out=f_tile[:, c, 0:nb], in_=acc[ai][:, 0:nb]), reads=[t_acc[ai]], writes=[t_f[c]])
                    qi = c % 2
                    S.op("act", I("activation", out=sqb[qi][:, 0:nb], in_=acc[ai][:, 0:nb], func=AF.Square),
                         reads=[t_acc[ai]], writes=[t_sqb[qi]])
                    S.op("pe", I("matmul", ssp[:, 0:nb], lhsT=G.ones_b[:], rhs=sqb[qi][:, 0:nb], start=(c == 0), stop=(c == nch - 1)),
                         reads=[t_sqb[qi], G.t_const], writes=[t_ssp], merge=(c > 0))
                rstd_from_ss(G, rstd[:, ri, 0:nb], ssp[:, 0:nb], nch * 128, [t_ssp], [t_rstd[ri]])
                for c in range(nch):
                    S.op("dve", I("scalar_tensor_tensor", out=n_tile[:, c, 0:nb], in0=f_tile[:, c, 0:nb], scalar=gT[:, gi, c:c + 1],
                                  in1=rstd[:, ri, 0:nb], op0=ALU.mult, op1=ALU.mult),
                         reads=[t_f[c], t_gT, t_rstd[ri]], writes=[t_n], merge=(c > 0))

            latent_norm(0, 4, cq_f, t_cqf, cqn, t_cqn, 0, 0)
            latent_norm(512, 2, ckf, t_ckf, ckn, t_ckn, 1, 1)
            for h in range(8):
                ais = []
                for ab in range(2):
                    ai = cnt["a"] % NACC
                    cnt["a"] += 1
                    for c in range(4):
                        S.op("pe", I("matmul", acc[ai][0:96, 0:nb], lhsT=wq[:, c, ab, h * 96:(h + 1) * 96], rhs=cqn[:, c, 0:nb],
                                     start=(c == 0), stop=(c == 3)),
                             reads=[t_w, t_cqn], writes=[t_acc[ai]], merge=(c > 0))
                    ais.append(ai)
                S.op("act", I("copy", out=mq_st[0:64, h, 0:nb], in_=acc[ais[0]][0:64, 0:nb]), reads=[t_acc[ais[0]]],
                     writes=[t_mq], merge=(h > 0))
                rope_pair(ais[0], ais[1], 64, 96, rM, mq_st[64:96, h, 0:nb], t_mq, merge_out=True)
            S.dma("sp", I("dma_start", out=G.mqT[:, :, tok0:tok0 + nb].rearrange("h p n -> p h n"), in_=mq_st[:, :, 0:nb]),
                  reads=[t_mq], writes=[G.t["mqT"]], merge=True)
            aiA = proj(w2, OFF_KPA, 96)
            aiB = proj(w2, OFF_KPB, 96)
            rope_pair(aiA, aiB, 64, 96, rM, kpe_r[64:96, 0:nb], t_kpe)
            for h in range(8):
                ai = cnt["a"] % NACC
                cnt["a"] += 1
                for c in range(2):
                    S.op("pe", I("matmul", acc[ai][0:64, 0:nb], lhsT=wk[:, c, h * 64:(h + 1) * 64], rhs=ckn[:, c, 0:nb],
                                 start=(c == 0), stop=(c == 1)),
                         reads=[t_w, t_ckn], writes=[t_acc[ai]], merge=(c > 0))
                S.op("act", I("copy", out=mk_st[0:64, h, 0:nb], in_=acc[ai][0:64, 0:nb]), reads=[t_acc[ai]],
                     writes=[t_mk], merge=(h > 0))
                S.op("pool" if h % 2 else "dve", I("tensor_copy", out=mk_st[64:96, h, 0:nb], in_=kpe_r[64:96, 0:nb]),
                     reads=[t_kpe], writes=[t_mk], merge=True)
            S.dma("sp", I("dma_start", out=G.mkT[:, :, tok0:tok0 + nb].rearrange("h p n -> p h n"), in_=mk_st[:, :, 0:nb]),
                  reads=[t_mk], writes=[G.t["mkT"]], merge=True)
            for t in range(ntile):
                ki = cnt["k"] % 2
                cnt["k"] += 1
                for c in range(2):
                    S.op("pe", I("matmul", tkp[ki][:], lhsT=ckn[:, c, t * 128:(t + 1) * 128], rhs=wv[:, c, :],
                                 start=(c == 0), stop=(c == 1)),
                         reads=[t_w, t_ckn], writes=[t_tkp[ki]], merge=(c > 0))
                bi_ = stage_b()
                S.op("act", I("copy", out=stb[bi_][:], in_=tkp[ki][:]), reads=[t_tkp[ki]], writes=[t_stb[bi_]])
                S.dma("sp", I("dma_start", out=G.mvd[tok0 + t * 128:tok0 + (t + 1) * 128, :], in_=stb[bi_][:]),
                      reads=[t_stb[bi_]], writes=[G.t["mvd"]], merge=True)
        S.flush(f"projb{l}")


class GroupTail:
    def __init__(self, G, l, gi, sb, pp, name):
        self.G, self.gi = G, gi
        self.gT, self.t_gT = load_T(G, [G.out_norm_g[l, gi * 512:(gi + 1) * 512]], sb, pp, name + "_g")
        self.junk = sb(name + "_junk", [128, 512], BF16)
        self.t_junk = Tok()
        self.ss = [sb(name + f"_ss{i}", [128, 1], F32) for i in range(2)]
        self.t_ss = [Tok(), Tok()]
        self.ynb = [sb(name + f"_ynb{i}", [128, 512], BF16) for i in range(2)]
        self.t_ynb = [Tok(), Tok()]
        self.tp = pp(name + "_tp", [128, 1024], BF16)
        self.t_tp = Tok()
        self.st = [sb(name + f"_st{i}", [128, 4, 128], BF16) for i in range(2)]
        self.t_st = [Tok(), Tok()]
        self.k = 0

    def emit(self, y_ap, t_y, tok0):
        G, S = self.G, self.G.S
        i = self.k % 2
        self.k += 1
        S.op("act", I("activation", out=self.junk[:], in_=y_ap, func=AF.Square, accum_out=self.ss[i][:, 0:1]),
             reads=[t_y], writes=[self.t_junk, self.t_ss[i]])
        rstd_from_ss(G, self.ss[i][:, 0:1], self.ss[i][:, 0:1], 512, [self.t_ss[i]], [self.t_ss[i]])
        S.op("act", I("activation", out=self.ynb[i][:], in_=y_ap, func=AF.Copy, scale=self.ss[i][:, 0:1]),
             reads=[t_y, self.t_ss[i]], writes=[self.t_ynb[i]])
        for c in range(4):
            S.op("pe", I("transpose", out=self.tp[:, c * 128:(c + 1) * 128], in_=self.ynb[i][:, c * 128:(c + 1) * 128],
                         identity=G.ident_b[:]),
                 reads=[self.t_ynb[i], G.t_const], writes=[self.t_tp], merge=(c > 0))
        for c in range(4):
            S.op("dve", I("tensor_scalar", out=self.st[i][:, c, :], in0=self.tp[:, c * 128:(c + 1) * 128],
                          scalar1=self.gT[:, 0, c:c + 1], scalar2=None, op0=ALU.mult),
                 reads=[self.t_tp, self.t_gT], writes=[self.t_st[i]], merge=(c > 0))
        r0 = self.gi * 512
        S.dma("pool", I("dma_start", out=G.ynT[r0:r0 + 512, tok0:tok0 + 128].rearrange("(c p) n -> p c n", p=128), in_=self.st[i][:]),
              reads=[self.t_st[i]], writes=[G.t["ynT"]], merge=True)


def phase_conv(G, l, do_ctx):
    nc, S = G.nc, G.S
    with ExitStack() as ps:
        sb, pp = _alloc(G, ps)
        cw, t_cw = load_T(G, [G.conv_w[l, 0], G.conv_w[l, 1], G.conv_w[l, 2], G.out_norm_g[l, 0:512]], sb, pp, "cv_w")
        ut = [sb(f"cv_u{i}", [128, 514], F32) for i in range(2)]
        t_ut = [Tok(), Tok()]
        cbt = [sb(f"cv_cb{i}", [128, 512], F32) for i in range(2)]
        t_cbt = [Tok(), Tok()]
        acc_t = [sb(f"cv_a{i}", [128, 512], F32) for i in range(2)]
        t_at = [Tok(), Tok()]
        yc = sb("cv_y", [128, 4, 512], F32)
        t_yc = [Tok() for _ in range(4)]
        sqb = [sb(f"cv_sq{i}", [128, 512], BF16) for i in range(2)]
        t_sqb = [Tok(), Tok()]
        rstd = sb("cv_rstd", [128, 512], F32)
        t_rstd = Tok()
        stb = [sb(f"cv_st{i}", [128, 512], BF16) for i in range(2)]
        t_stb = [Tok(), Tok()]
        ssp = pp("cv_ss", [128, 512])
        t_ssp = Tok()
        segs = [(0, NL)] + ([(NL, NT)] if do_ctx else [])
        k = 0
        for (s0, s1) in segs:
            for b0 in range(s0, s1, 512):
                nb = min(512, s1 - b0)
                for c in range(4):
                    i = k % 2
                    k += 1
                    lo = b0 - 1 if b0 > s0 else b0
                    hi = b0 + nb + 1 if b0 + nb < s1 else b0 + nb
                    first = True
                    if lo == b0:
                        S.op("pool", I("memset", ut[i][:, 0:1], 0.0), writes=[t_ut[i]])
                        first = False
                    if hi == b0 + nb:
                        S.op("pool", I("memset", ut[i][:, nb + 1:nb + 2], 0.0), writes=[t_ut[i]], merge=not first)
                        first = False
                    S.dma("sp", I("dma_start", out=ut[i][:, lo - (b0 - 1):hi - (b0 - 1)], in_=G.uT[c * 128:(c + 1) * 128, lo:hi]),
                          reads=[G.t["uT"]], writes=[t_ut[i]], merge=not first)
                    S.dma("sp", I("dma_start", out=cbt[i][:, 0:nb], in_=G.cbT[c * 128:(c + 1) * 128, b0:b0 + nb]),
                          reads=[G.t["cbT"]], writes=[t_cbt[i]])
                    S.op("dve", I("tensor_scalar", out=acc_t[i][:, 0:nb], in0=ut[i][:, 0:nb], scalar1=cw[:, 0, c:c + 1], scalar2=None,
                                  op0=ALU.mult), reads=[t_ut[i], t_cw], writes=[t_at[i]])
                    S.op("dve", I("scalar_tensor_tensor", out=acc_t[i][:, 0:nb], in0=ut[i][:, 1:nb + 1], scalar=cw[:, 1, c:c + 1],
                                  in1=acc_t[i][:, 0:nb], op0=ALU.mult, op1=ALU.add), reads=[t_ut[i], t_cw, t_at[i]], writes=[t_at[i]])
                    S.op("dve", I("scalar_tensor_tensor", out=acc_t[i][:, 0:nb], in0=ut[i][:, 2:nb + 2], scalar=cw[:, 2, c:c + 1],
                                  in1=acc_t[i][:, 0:nb], op0=ALU.mult, op1=ALU.add), reads=[t_ut[i], t_cw, t_at[i]], writes=[t_at[i]])
                    S.op("pool", I("tensor_tensor", out=yc[:, c, 0:nb], in0=acc_t[i][:, 0:nb], in1=cbt[i][:, 0:nb], op=ALU.mult),
                         reads=[t_at[i], t_cbt[i]], writes=[t_yc[c]])
                    S.op("act", I("activation", out=sqb[i][:, 0:nb], in_=yc[:, c, 0:nb], func=AF.Square), reads=[t_yc[c]], writes=[t_sqb[i]])
                    S.op("pe", I("matmul", ssp[:, 0:nb], lhsT=G.ones_b[:], rhs=sqb[i][:, 0:nb], start=(c == 0), stop=(c == 3)),
                         reads=[t_sqb[i], G.t_const], writes=[t_ssp], merge=(c > 0))
                rstd_from_ss(G, rstd[:, 0:nb], ssp[:, 0:nb], 512, [t_ssp], [t_rstd])
                for c in range(4):
                    i = k % 2
                    k += 1
                    S.op("dve", I("scalar_tensor_tensor", out=stb[i][:, 0:nb], in0=yc[:, c, 0:nb], scalar=cw[:, 3, c:c + 1],
                                  in1=rstd[:, 0:nb], op0=ALU.mult, op1=ALU.mult), reads=[t_yc[c], t_cw, t_rstd], writes=[t_stb[i]])
                    S.dma("sp", I("dma_start", out=G.ynT[c * 128:(c + 1) * 128, b0:b0 + nb], in_=stb[i][:, 0:nb]),
                          reads=[t_stb[i]], writes=[G.t["ynT"]], merge=True)
        S.flush(f"conv{l}")


def phase_swa(G, l, do_ctx):
    nc, S = G.nc, G.S
    SCALE = 64 ** -0.5
    with ExitStack() as ps:
        sb, pp = _alloc(G, ps)
        Qs = sb("sw_Q", [64, 8, NT], BF16)
        Ks = sb("sw_K", [64, 2, NT], BF16)
        Vs = sb("sw_V", [128, 34, 2, 65], BF16)
        mk = sb("sw_mask", [128, 2, 512], BF16)
        snk = sb("sw_sink", [128, 8], F32)
        t_in = Tok()
        t_V = Tok()
        for h in range(8):
            S.dma("sp", I("dma_start", out=Qs[:, h, :], in_=G.sqT[h * 64:(h + 1) * 64, :]), reads=[G.t["sqT"]], writes=[t_in], merge=True)
        for h in range(2):
            S.dma("sp", I("dma_start", out=Ks[:, h, :], in_=G.skT[h * 64:(h + 1) * 64, :]), reads=[G.t["skT"]], writes=[t_in], merge=True)
        S.op("pool", I("memset", Vs[:, :, :, 64:65], 1.0), writes=[t_V])
        for h in range(2):
            S.dma("sp", I("dma_start", out=Vs[:, :, h, 0:64], in_=G.svd[:, h * 64:(h + 1) * 64].rearrange("(t p) d -> p t d", p=128)),
                  reads=[G.t["svd"]], writes=[t_V], merge=True)
        S.dma("sp", I("dma_start", out=mk[:], in_=G.masks.rearrange("t p n -> p t n")), writes=[t_in], merge=True)
        S.dma("sp", I("dma_start", out=snk[:], in_=G.swa_sink[l:l + 1, :].to_broadcast([128, 8])), writes=[t_in], merge=True)
        S.op("act", I("activation", out=snk[:], in_=snk[:], func=AF.Exp), reads=[t_in], writes=[t_in])
        tail = GroupTail(G, l, 1, sb, pp, "sw_t")
        pT = [sb(f"sw_pT{i}", [128, 5, 512], BF16) for i in range(2)]
        t_pT = [[Tok() for _ in range(5)] for _ in range(2)]
        sps = [pp(f"sw_s{i}", [128, 512]) for i in range(2)]
        t_sps = [Tok(), Tok()]
        ops_ = [pp(f"sw_o{i}", [128, 512]) for i in range(2)]
        t_ops = [Tok(), Tok()]
        den = [sb(f"sw_den{i}", [128, 8], F32) for i in range(2)]
        t_den = [Tok(), Tok()]
        ysw = [sb(f"sw_y{i}", [128, 512], F32) for i in range(2)]
        t_ysw = [Tok(), Tok()]
        qblocks = [(i, "lat") for i in range(32)] + ([(32, "ctx"), (33, "ctx")] if do_ctx else [])
        import os
        LV = int(os.environ.get("SW_STOP", "99"))
        kq = 0
        ks = 0
        pend = None
        for bidx, (i, kind) in enumerate(qblocks[:LV]):
            if kind == "lat":
                kts = ([(i - 1, 0)] if i > 0 else []) + [(i, None)] + ([(i + 1, 1)] if i < 31 else []) + [(32, None), (33, None)]
            else:
                kts = [(32, None), (33, None)]
            yi = bidx % 2
            for kvh in range(2):
                pi = kq % 2
                oi = kq % 2
                kq += 1
                for n, (kt, msk) in enumerate(kts):
                    si = ks % 2
                    ks += 1
                    S.op("pe", I("matmul", sps[si][:], lhsT=Ks[:, kvh, kt * 128:(kt + 1) * 128],
                                 rhs=Qs[:, kvh * 4:(kvh + 1) * 4, i * 128:(i + 1) * 128], start=True, stop=True),
                         reads=[t_in], writes=[t_sps[si]])
                    S.op("act", I("activation", out=pT[pi][:, n, :], in_=sps[si][:], func=AF.Exp, scale=SCALE),
                         reads=[t_sps[si]], writes=[t_pT[pi][n]])
                    if msk is not None:
                        S.op("dve", I("tensor_tensor", out=pT[pi][:, n, :], in0=pT[pi][:, n, :], in1=mk[:, msk, :], op=ALU.mult),
                             reads=[t_pT[pi][n], t_in], writes=[t_pT[pi][n]])
                for g in range(4):
                    for n, (kt, msk) in enumerate(kts):
                        S.op("pe", I("matmul", ops_[oi][:, g * 65:(g + 1) * 65], lhsT=pT[pi][:, n, g * 128:(g + 1) * 128],
                                     rhs=Vs[:, kt, kvh, :], start=(n == 0), stop=(n == len(kts) - 1)),
                             reads=[t_pT[pi][n], t_V], writes=[t_ops[oi]], merge=(n > 0 or g > 0))
                ov = ops_[oi][:, 0:260].rearrange("p (g e) -> p g e", e=65)
                S.op("dve", I("tensor_tensor", out=den[yi][:, kvh * 4:(kvh + 1) * 4], in0=ov[:, :, 64], in1=snk[:, kvh * 4:(kvh + 1) * 4], op=ALU.add),
                     reads=[t_ops[oi], t_in], writes=[t_den[yi]], merge=(kvh > 0))
                S.op("dve", I("reciprocal", out=den[yi][:, kvh * 4:(kvh + 1) * 4], in_=den[yi][:, kvh * 4:(kvh + 1) * 4]),
                     reads=[t_den[yi]], writes=[t_den[yi]])
                for g in range(4):
                    h = kvh * 4 + g
                    S.op("dve", I("tensor_scalar", out=ysw[yi][:, h * 64:(h + 1) * 64], in0=ov[:, g, 0:64], scalar1=den[yi][:, h:h + 1],
                                  scalar2=None, op0=ALU.mult),
                         reads=[t_ops[oi], t_den[yi]], writes=[t_ysw[yi]], merge=(h > 0))
                if kvh == 0 and pend is not None:
                    tail.emit(*pend)
                    pend = None
            pend = (ysw[yi][:], t_ysw[yi], i * 128)
        if pend is not None:
            tail.emit(*pend)
        S.flush(f"swa{l}")


def phase_fnet(G, l, do_ctx):
    nc, S = G.nc, G.S
    with ExitStack() as ps:
        sb, pp = _alloc(G, ps)
        uc = sb("fn_uc", [128, 34, 512], BF16)
        us = sb("fn_us", [128, 34, 512], BF16)
        t_u = Tok()
        for (t0, t1) in ((0, 16), (16, 34)):
            S.dma("sp", I("dma_start", out=uc[:, t0:t1, :], in_=G.ucd[t0 * 128:t1 * 128, :].rearrange("(t p) c -> p t c", p=128)),
                  reads=[G.t["ucd"]], writes=[t_u], merge=True)
            S.dma("sp", I("dma_start", out=us[:, t0:t1, :], in_=G.usd[t0 * 128:t1 * 128, :].rearrange("(t p) c -> p t c", p=128)),
                  reads=[G.t["usd"]], writes=[t_u], merge=True)
        dt_ = [sb(f"fn_d{i}", [128, 2 * 32 * 128], BF16) for i in range(2)]
        t_dt = [Tok(), Tok()]
        acc = [pp(f"fn_acc{i}", [128, 512]) for i in range(2)]
        t_acc = [Tok(), Tok()]
        yf = [sb(f"fn_y{i}", [128, 512], F32) for i in range(2)]
        t_yf = [Tok(), Tok()]
        tail = GroupTail(G, l, 2, sb, pp, "fn_t")
        import os
        LV = int(os.environ.get("FN_STOP", "99"))
        jobs = [(kt, 32, 0, G.dftL) for kt in range(32)][:LV] + ([(kt, 2, 32, G.dftC) for kt in range(2)] if do_ctx else [])
        pend = None
        for k, (kt, nt, tb, tab) in enumerate(jobs):
            i = k % 2
            dv = dt_[i][:, 0:2 * nt * 128]
            S.dma("sp", I("dma_start", out=dv, in_=tab[kt]), writes=[t_dt[i]])
            d4 = dv.rearrange("p (a n k) -> p a n k", a=2, k=128)
            for n in range(nt):
                S.op("pe", I("matmul", acc[i][:], lhsT=d4[:, 0, n, :], rhs=uc[:, tb + n, :], start=(n == 0), stop=False),
                     reads=[t_dt[i], t_u], writes=[t_acc[i]], merge=(n > 0))
            for n in range(nt):
                S.op("pe", I("matmul", acc[i][:], lhsT=d4[:, 1, n, :], rhs=us[:, tb + n, :], start=False, stop=(n == nt - 1)),
                     reads=[t_dt[i], t_u], writes=[t_acc[i]], merge=True)
            if pend is not None:
                tail.emit(*pend)
            S.op("act", I("copy", out=yf[i][:], in_=acc[i][:]), reads=[t_acc[i]], writes=[t_yf[i]])
            pend = (yf[i][:], t_yf[i], (tb + kt) * 128)
        if pend is not None:
            tail.emit(*pend)
        S.flush(f"fnet{l}")


def phase_mla(G, l, do_ctx):
    nc, S = G.nc, G.S
    SCALE = 96 ** -0.5
    with ExitStack() as ps:
        sb, pp = _alloc(G, ps)
        Kh = [sb(f"ml_K{i}", [96, NT], BF16) for i in range(2)]
        Qh = [sb(f"ml_Q{i}", [96, NT], BF16) for i in range(2)]
        Vh = [sb(f"ml_V{i}", [128, 34, 65], BF16) for i in range(2)]
        t_K = [Tok(), Tok()]
        t_Q = [Tok(), Tok()]
        t_V = [Tok(), Tok()]
        PT = [sb(f"ml_PT{i}", [128, 34, 512], BF16) for i in range(2)]
        t_PT = [[Tok() for _ in range(34)] for _ in range(2)]
        yall = sb("ml_y", [128, 34, 512], F32)
        t_y = [Tok() for _ in range(34)]
        rc = [sb(f"ml_rc{i}", [128, 1], F32) for i in range(4)]
        t_rc = [Tok() for _ in range(4)]
        sps = [pp(f"ml_s{i}", [128, 512]) for i in range(2)]
        t_sps = [Tok(), Tok()]
        ops_ = [pp(f"ml_o{i}", [128, 512]) for i in range(4)]
        t_ops = [Tok() for _ in range(4)]
        import os
        LVH = int(os.environ.get("ML_HEADS", "8"))
        LVQ = int(os.environ.get("ML_QB", "99"))
        qblocks = [(i * 512, 512, list(range(34))) for i in range(8)][:LVQ] + ([(NL, NCX, [32, 33])] if do_ctx else [])
        ks = 0
        kp = 0
        ko = 0
        for h in range(LVH):
            hb = h % 2
            S.dma("sp", I("dma_start", out=Kh[hb][:], in_=G.mkT[h]), reads=[G.t["mkT"]], writes=[t_K[hb]])
            S.dma("sp", I("dma_start", out=Qh[hb][:], in_=G.mqT[h]), reads=[G.t["mqT"]], writes=[t_Q[hb]])
            S.op("pool", I("memset", Vh[hb][:, :, 64:65], 1.0), writes=[t_V[hb]])
            S.dma("sp", I("dma_start", out=Vh[hb][:, :, 0:64], in_=G.mvd[:, h * 64:(h + 1) * 64].rearrange("(t p) d -> p t d", p=128)),
                  reads=[G.t["mvd"]], writes=[t_V[hb]], merge=True)
            for (q0, nq, kts) in qblocks:
                pi = kp % 2
                kp += 1
                for kt in kts:
                    si = ks % 2
                    ks += 1
                    S.op("pe", I("matmul", sps[si][:, 0:nq], lhsT=Kh[hb][:, kt * 128:(kt + 1) * 128], rhs=Qh[hb][:, q0:q0 + nq],
                                 start=True, stop=True), reads=[t_K[hb], t_Q[hb]], writes=[t_sps[si]])
                    S.op("act", I("activation", out=PT[pi][:, kt, 0:nq], in_=sps[si][:, 0:nq], func=AF.Exp, scale=SCALE),
                         reads=[t_sps[si]], writes=[t_PT[pi][kt]])
                for j in range(nq // 128):
                    oi = ko % 4
                    ko += 1
                    for n, kt in enumerate(kts):
                        S.op("pe", I("matmul", ops_[oi][:, 0:65], lhsT=PT[pi][:, kt, j * 128:(j + 1) * 128], rhs=Vh[hb][:, kt, :],
                                     start=(n == 0), stop=(n == len(kts) - 1)),
                             reads=[t_PT[pi][kt], t_V[hb]], writes=[t_ops[oi]], merge=(n > 0))
                    S.op("dve", I("reciprocal", out=rc[oi][:], in_=ops_[oi][:, 64:65]), reads=[t_ops[oi]], writes=[t_rc[oi]])
                    tile_i = q0 // 128 + j
                    S.op("dve", I("tensor_scalar", out=yall[:, tile_i, h * 64:(h + 1) * 64], in0=ops_[oi][:, 0:64], scalar1=rc[oi][:, 0:1],
                                  scalar2=None, op0=ALU.mult),
                         reads=[t_ops[oi], t_rc[oi]], writes=[t_y[tile_i]], merge=(h > 0))
        if LVH == 8:
            tail = GroupTail(G, l, 3, sb, pp, "ml_t")
            ntile = (qblocks[-1][0] + qblocks[-1][1]) // 128 if LVQ >= 8 else LVQ * 4
            tiles = list(range(min(32, ntile))) + ([32, 33] if do_ctx else [])
            for ti in tiles:
                tail.emit(yall[:, ti, :], t_y[ti], ti * 128)
        else:
            G.dbg_yall = (yall, t_y)
        S.flush(f"mla{l}")


def phase_outproj(G, l, do_ctx):
    nc, S = G.nc, G.S
    with ExitStack() as ps:
        sb, pp = _alloc(G, ps)
        wo = sb("op_wo", [128, 16, D], BF16)
        t_w = Tok()
        for c0 in range(0, D, 512):
            S.dma("pool", I("dma_start", out=wo[:, :, c0:c0 + 512], in_=G.w_out[l][:, c0:c0 + 512].rearrange("(j p) n -> p j n", p=128)),
                  writes=[t_w], merge=True)
        wr = sb("op_wr", [128, 16, NE], F32)
        S.dma("sp", I("dma_start", out=wr[:], in_=G.w_router[l].rearrange("(j p) e -> p j e", p=128)), writes=[t_w], merge=True)
        g1b = sb("op_g1b", [128, D], F32)
        sh2b = sb("op_sh2b", [128, D], F32)
        gs2b = sb("op_gs2b", [128, D], F32)
        t_bc = Tok()
        NB = 2
        xt = [sb(f"op_x{i}", [128, D], F32) for i in range(NB)]
        t_xt = [Tok() for _ in range(NB)]
        xn = [sb(f"op_xn{i}", [128, D], F32) for i in range(NB)]
        t_xn = [Tok() for _ in range(NB)]
        fx = [sb(f"op_fx{i}", [128, D], F32) for i in range(NB)]
        t_fx = [Tok() for _ in range(NB)]
        fxb = [sb(f"op_fxb{i}", [128, D], BF16) for i in range(NB)]
        t_fxb = [Tok() for _ in range(NB)]
        junk = sb("op_junk", [128, D], BF16)
        t_junk = Tok()
        yn = [sb(f"op_yn{i}", [128, 16, 128], BF16) for i in range(NB)]
        t_yn = [Tok() for _ in range(NB)]
        fxT = [sb(f"op_fxT{i}", [128, 16, 128], F32) for i in range(NB)]
        t_fxT = [Tok() for _ in range(NB)]
        ss = [sb(f"op_ss{i}", [128, 1], F32) for i in range(NB)]
        t_ss = [Tok() for _ in range(NB)]
        sm = [sb(f"op_sm{i}", [128, 4], F32) for i in range(NB)]
        t_sm = [Tok() for _ in range(NB)]
        ex = [sb(f"op_ex{i}", [128, NE], F32) for i in range(NB)]
        t_ex = [Tok() for _ in range(NB)]
        acc = [pp(f"op_acc{i}", [128, 512]) for i in range(4)]
        t_acc = [Tok() for _ in range(4)]
        trp = [pp(f"op_tr{i}", [128, 512]) for i in range(2)]
        t_trp = [Tok(), Tok()]
        lgp = [pp(f"op_lg{i}", [128, 512]) for i in range(2)]
        t_lgp = [Tok(), Tok()]
        import os
        LV = int(os.environ.get("OP_STOP", "99"))
        tiles = (list(range(32)) + ([32, 33] if do_ctx else []))[:LV]
        cur_s = None
        kt = [0]
        def loads(k, ti):
            tok0 = ti * 128
            i = k % NB
            S.dma("sp", I("dma_start", out=yn[i][:], in_=G.ynT[:, tok0:tok0 + 128].rearrange("(j p) n -> p j n", p=128)),
                  reads=[G.t["ynT"]], writes=[t_yn[i]])
            S.dma("sp", I("dma_start", out=xt[i][:], in_=G.xres[tok0:tok0 + 128, :]), reads=[G.t["xres"]], writes=[t_xt[i]])

        def part1(k, ti):
            nonlocal cur_s
            s_ = 1 if ti >= 32 else 0
            if s_ != cur_s:
                cur_s = s_
                S.dma("sp", I("dma_start", out=g1b[:], in_=G.modrow[l, s_:s_ + 1, 2 * D:3 * D].to_broadcast([128, D])),
                      reads=[G.t["modrow"]], writes=[t_bc])
                S.dma("sp", I("dma_start", out=sh2b[:], in_=G.modrow[l, s_:s_ + 1, 3 * D:4 * D].to_broadcast([128, D])),
                      reads=[G.t["modrow"]], writes=[t_bc], merge=True)
                S.dma("sp", I("dma_start", out=gs2b[:], in_=G.modrow[l, s_:s_ + 1, 4 * D:5 * D].to_broadcast([128, D])),
                      reads=[G.t["modrow"]], writes=[t_bc], merge=True)
                S.dma("sp", I("dma_start", out=fx[0][:], in_=G.norm2_g[l:l + 1, :].to_broadcast([128, D])), writes=[t_fx[0]])
                S.op("dve", I("scalar_tensor_tensor", out=gs2b[:], in0=gs2b[:], scalar=1.0, in1=fx[0][:], op0=ALU.add, op1=ALU.mult),
                     reads=[t_bc, t_fx[0]], writes=[t_bc])
            tok0 = ti * 128
            i = k % NB
            if k == 0:
                loads(k, ti)
            if k + 1 < len(tiles):
                loads(k + 1, tiles[k + 1])
            for nb in range(4):
                for j in range(16):
                    S.op("pe", I("matmul", acc[nb][:], lhsT=yn[i][:, j, :], rhs=wo[:, j, nb * 512:(nb + 1) * 512], start=(j == 0), stop=(j == 15)),
                         reads=[t_yn[i], t_w], writes=[t_acc[nb]], merge=(j > 0))
                S.op("dve", I("tensor_tensor", out=xn[i][:, nb * 512:(nb + 1) * 512], in0=acc[nb][:], in1=g1b[:, nb * 512:(nb + 1) * 512], op=ALU.mult),
                     reads=[t_acc[nb], t_bc], writes=[t_xn[i]], merge=(nb > 0))
        def part1c(k, ti):
            tok0 = ti * 128
            i = k % NB
            S.op("pool", I("tensor_tensor", out=xn[i][:], in0=xn[i][:], in1=xt[i][:], op=ALU.add), reads=[t_xn[i], t_xt[i]], writes=[t_xn[i]])
            S.dma("sp", I("dma_start", out=G.xres[tok0:tok0 + 128, :], in_=xn[i][:]), reads=[t_xn[i]], writes=[G.t["xres"]], merge=True)
            S.op("act", I("activation", out=junk[:], in_=xn[i][:], func=AF.Square, accum_out=ss[i][:, 0:1]), reads=[t_xn[i]], writes=[t_junk, t_ss[i]])
            rstd_from_ss(G, ss[i][:, 0:1], ss[i][:, 0:1], D, [t_ss[i]], [t_ss[i]])
            S.op("dve", I("scalar_tensor_tensor", out=fx[i][:], in0=xn[i][:], scalar=ss[i][:, 0:1], in1=gs2b[:], op0=ALU.mult, op1=ALU.mult),
                 reads=[t_xn[i], t_ss[i], t_bc], writes=[t_fx[i]])
            S.op("pool", I("tensor_tensor", out=fx[i][:], in0=fx[i][:], in1=sh2b[:], op=ALU.add), reads=[t_fx[i], t_bc], writes=[t_fx[i]])
            S.op("act", I("copy", out=fxb[i][:], in_=fx[i][:]), reads=[t_fx[i]], writes=[t_fxb[i]])
            S.dma("sp", I("dma_start", out=G.fxd[tok0:tok0 + 128, :], in_=fxb[i][:]), reads=[t_fxb[i]], writes=[G.t["fxd"]], merge=True)

        def part2(k, ti):
            i = k % NB
            for q in range(4):
                ti_ = kt[0] % 2
                kt[0] += 1
                for jj in range(4):
                    j = q * 4 + jj
                    S.op("pe", I("transpose", out=trp[ti_][:, jj * 128:(jj + 1) * 128], in_=fx[i][:, j * 128:(j + 1) * 128], identity=G.ident_f[:]),
                         reads=[t_fx[i], G.t_const], writes=[t_trp[ti_]], merge=(jj > 0))
                if q % 2 == 0:
                    S.op("act", I("copy", out=fxT[i][:, q * 4:(q + 1) * 4, :], in_=trp[ti_][:].rearrange("p (a n) -> p a n", n=128)),
                         reads=[t_trp[ti_]], writes=[t_fxT[i]], merge=(q > 0))
                else:
                    S.op("dve", I("tensor_copy", out=fxT[i][:, q * 4:(q + 1) * 4, :], in_=trp[ti_][:].rearrange("p (a n) -> p a n", n=128)),
                         reads=[t_trp[ti_]], writes=[t_fxT[i]], merge=True)
            for j in range(16):
                S.op("pe", I("matmul", lgp[i][:, 0:NE], lhsT=fxT[i][:, j, :], rhs=wr[:, j, :], start=(j == 0), stop=(j == 15)),
                     reads=[t_fxT[i], t_w], writes=[t_lgp[i]], merge=(j > 0))
            S.op("dve", I("reduce_max", out=sm[i][:, 0:1], in_=lgp[i][:, 0:NE], axis=mybir.AxisListType.X), reads=[t_lgp[i]], writes=[t_sm[i]])
            S.op("dve", I("tensor_scalar", out=sm[i][:, 1:2], in0=sm[i][:, 0:1], scalar1=-1.0, scalar2=None, op0=ALU.mult), reads=[t_sm[i]], writes=[t_sm[i]])
            S.op("act", I("activation", out=ex[i][:], in_=lgp[i][:, 0:NE], func=AF.Exp, bias=sm[i][:, 1:2], accum_out=sm[i][:, 2:3]),
                 reads=[t_lgp[i], t_sm[i]], writes=[t_ex[i], t_sm[i]])
            S.op("dve", I("reciprocal", out=sm[i][:, 3:4], in_=sm[i][:, 2:3]), reads=[t_sm[i]], writes=[t_sm[i]])
            S.op("dve", I("tensor_scalar", out=G.aff_all[:, ti, :], in0=ex[i][:], scalar1=sm[i][:, 3:4], scalar2=None, op0=ALU.mult),
                 reads=[t_ex[i], t_sm[i]], writes=[G.t_aff], merge=True)

        for k in range(len(tiles) + 1):
            if k < len(tiles):
                part1(k, tiles[k])
            if k >= 1:
                part2(k - 1, tiles[k - 1])
            if k < len(tiles):
                part1c(k, tiles[k])
        if "affd" in G.debug:
            S.dma("sp", I("dma_start", out=G.affd.rearrange("(t p) e -> p t e", p=128), in_=G.aff_all[:]), reads=[G.t_aff], writes=[G.t["affd"]])
        S.flush(f"outproj{l}")


def phase_route(G, l, do_ctx):
    nc, S = G.nc, G.S
    NI = 24
    with ExitStack() as ps:
        sb, pp = _alloc(G, ps)
        U = sb("rt_U", [128, 128], BF16)
        iot = sb("rt_iota", [128, 512], F32)
        pidx = sb("rt_pidx", [128, 4], F32)
        tgrid = sb("rt_tgrid", [128, 34, NE], F32)
        t_c = Tok()
        S.dma("sp", I("dma_start", out=U[:], in_=G.triu), writes=[t_c], merge=True)
        S.dma("sp", I("dma_start", out=iot[:], in_=G.iota512), writes=[t_c], merge=True)
        S.dma("sp", I("dma_start", out=pidx[:], in_=G.pidx), writes=[t_c], merge=True)
        S.dma("sp", I("dma_start", out=tgrid[:].rearrange("p t e -> p (t e)"), in_=G.tgrid), writes=[t_c], merge=True)
        aff = G.aff_all
        sets = [(0, 32, CAP_L)] + ([(32, 34, CAP_C)] if do_ctx else [])
        R = sb("rt_R", [128, 34, NE, 6], BF16)
        t_R = Tok()
        r1 = sb("rt_r1", [128, 34, NE], F32)
        t_r1 = Tok()
        S.op("pool", I("memset", R[:], 1.0), writes=[t_R])
        S.op("dve", I("tensor_scalar", out=R[:, :, :, 0], in0=tgrid[:], scalar1=0.0, scalar2=pidx[:, 0:1], op0=ALU.mult, op1=ALU.add),
             reads=[t_c], writes=[t_R])
        S.op("dve", I("tensor_copy", out=R[:, :, :, 1], in_=tgrid[:]), reads=[t_c], writes=[t_R])
        S.op("dve", I("tensor_copy", out=R[:, :, :, 2], in_=aff[:]), reads=[G.t_aff], writes=[t_R])
        S.op("dve", I("tensor_tensor", out=r1[:], in0=aff[:], in1=R[:, :, :, 2], op=ALU.subtract), reads=[G.t_aff, t_R], writes=[t_r1])
        S.op("dve", I("tensor_copy", out=R[:, :, :, 3], in_=r1[:]), reads=[t_r1], writes=[t_R])
        S.op("dve", I("tensor_tensor", out=r1[:], in0=r1[:], in1=R[:, :, :, 3], op=ALU.subtract), reads=[t_r1, t_R], writes=[t_r1])
        S.op("dve", I("tensor_copy", out=R[:, :, :, 4], in_=r1[:]), reads=[t_r1], writes=[t_R])
        mids, t_mid, cmps, t_cmp, parts, t_part, cps, t_cps, tmps, t_tmp = [], [], [], [], [], [], [], [], [], []
        for si, (t0, t1, cap) in enumerate(sets):
            mids.append(sb(f"rt_mid{si}", [128, NE], F32)); t_mid.append(Tok())
            cmps.append(sb(f"rt_cmp{si}", [128, t1 - t0, NE], BF16)); t_cmp.append(Tok())
            parts.append(sb(f"rt_part{si}", [128, NE], F32)); t_part.append(Tok())
            cps.append(pp(f"rt_cps{si}", [128, 512])); t_cps.append(Tok())
            tmps.append(sb(f"rt_tmp{si}", [128, NE], F32)); t_tmp.append(Tok())
            S.op("pool", I("memset", mids[si][:], 0.5), writes=[t_mid[si]])
        for k in range(NI):
            wk = 0.5 ** (k + 1)
            wn = 0.5 ** (k + 2) if k < NI - 1 else 0.0
            for si, (t0, t1, cap) in enumerate(sets):
                nt = t1 - t0
                S.op("dve", I("tensor_tensor", out=cmps[si][:], in0=aff[:, t0:t1, :],
                              in1=mids[si][:].unsqueeze(1).to_broadcast([128, nt, NE]), op=ALU.is_gt),
                     reads=[G.t_aff, t_mid[si]], writes=[t_cmp[si]])
                S.op("dve", I("tensor_reduce", out=parts[si][:], in_=cmps[si][:].rearrange("p t e -> p e t"),
                              axis=mybir.AxisListType.X, op=ALU.add),
                     reads=[t_cmp[si]], writes=[t_part[si]])
                S.op("pe", I("matmul", cps[si][:, 0:NE], lhsT=G.ones_f[:], rhs=parts[si][:], start=True, stop=True),
                     reads=[t_part[si], G.t_const], writes=[t_cps[si]])
                S.op("dve", I("tensor_scalar", out=tmps[si][:], in0=cps[si][:, 0:NE], scalar1=cap + 0.5, scalar2=wk, op0=ALU.is_gt, op1=ALU.mult),
                     reads=[t_cps[si]], writes=[t_tmp[si]])
                S.op("dve", I("scalar_tensor_tensor", out=mids[si][:], in0=tmps[si][:], scalar=-wn, in1=mids[si][:], op0=ALU.add, op1=ALU.add),
                     reads=[t_tmp[si], t_mid[si]], writes=[t_mid[si]])
        Mb = sb("rt_Mb", [128, 34, NE], BF16)
        Mf = sb("rt_Mf", [128, 34, NE], F32)
        t_M = Tok()
        if not do_ctx:
            S.op("pool", I("memset", Mb[:, 32:34, :], 0.0), writes=[t_M])
            S.op("pool", I("memset", Mf[:, 32:34, :], 0.0), writes=[t_M], merge=True)
        for si, (t0, t1, cap) in enumerate(sets):
            nt = t1 - t0
            S.op("dve", I("tensor_tensor", out=Mf[:, t0:t1, :], in0=aff[:, t0:t1, :],
                          in1=mids[si][:].unsqueeze(1).to_broadcast([128, nt, NE]), op=ALU.is_gt),
                 reads=[G.t_aff, t_mid[si]], writes=[t_M], merge=True)
        S.op("dve", I("tensor_copy", out=Mb[:, 0:34 if do_ctx else 32, :], in_=Mf[:, 0:34 if do_ctx else 32, :]), reads=[t_M], writes=[t_M])
        posp = [pp(f"rt_pos{i}", [128, 512]) for i in range(2)]
        totp = [pp(f"rt_tot{i}", [128, 512]) for i in range(2)]
        t_pp = Tok()
        spans = [(0, 32)] + ([(32, 34)] if do_ctx else [])
        for i, (t0, t1) in enumerate(spans):
            n = (t1 - t0) * NE
            rhs = Mb[:, t0:t1, :].rearrange("p t e -> p (t e)")
            S.op("pe", I("matmul", posp[i][:, 0:n], lhsT=U[:], rhs=rhs, start=True, stop=True), reads=[t_M, t_c], writes=[t_pp], merge=(i > 0))
            S.op("pe", I("matmul", totp[i][:, 0:n], lhsT=G.ones_b[:], rhs=rhs, start=True, stop=True), reads=[t_M, G.t_const], writes=[t_pp], merge=True)
        incl = sb("rt_incl", [128, 34, NE], F32)
        t_incl = Tok()
        onesf = sb("rt_onesf", [128, 32], F32)
        S.op("pool", I("memset", onesf[:], 1.0), writes=[t_c], merge=True)
        for i, (t0, t1) in enumerate(spans):
            nt = t1 - t0
            tv = totp[i][:, 0:nt * NE].rearrange("p (t e) -> p t e", e=NE)
            for e in range(NE):
                S.op("dve", I("tensor_tensor_scan", out=incl[:, t0:t1, e], data0=onesf[:, 0:nt], data1=tv[:, :, e], initial=0.0,
                              op0=ALU.mult, op1=ALU.add),
                     reads=[t_pp, t_c], writes=[t_incl], merge=(e > 0 or i > 0))
        posf = sb("rt_posf", [128, 34, NE], F32)
        t_posf = Tok()
        for i, (t0, t1) in enumerate(spans):
            nt = t1 - t0
            tv = totp[i][:, 0:nt * NE].rearrange("p (t e) -> p t e", e=NE)
            pv = posp[i][:, 0:nt * NE].rearrange("p (t e) -> p t e", e=NE)
            S.op("dve", I("tensor_tensor", out=incl[:, t0:t1, :], in0=incl[:, t0:t1, :], in1=tv, op=ALU.subtract),
                 reads=[t_incl, t_pp], writes=[t_incl])
            S.op("dve", I("tensor_tensor", out=posf[:, t0:t1, :], in0=incl[:, t0:t1, :], in1=pv, op=ALU.add),
                 reads=[t_incl, t_pp], writes=[t_posf], merge=(i > 0))
        Oall = [sb(f"rt_O{i}", [128, 32, 512], BF16) for i in range(2)]
        t_O = [[Tok() for _ in range(32)] for _ in range(2)]
        Oc = [sb(f"rt_Oc{i}", [128, 2, 128], BF16) for i in range(2)]
        t_Oc = [Tok(), Tok()]
        ips = [pp(f"rt_ips{i}", [128, 512]) for i in range(2)]
        t_ips = [Tok(), Tok()]
        isb = [sb(f"rt_isb{i}", [128, 5, 8], F32) for i in range(2)]
        t_isb = [Tok(), Tok()]
        ia = [sb(f"rt_ia{i}", [128, 5], F32) for i in range(2)]
        t_ia = [Tok(), Tok()]
        nch = 5 if do_ctx else 4
        import os
        LVE = int(os.environ.get("RT_EXP", "16"))
        for e in range(LVE):
            b = e % 2
            for t in range(32):
                S.op("dve", I("tensor_scalar", out=Oall[b][:, t, :], in0=iot[:], scalar1=posf[:, t, e:e + 1], scalar2=Mf[:, t, e:e + 1],
                              op0=ALU.is_equal, op1=ALU.mult),
                     reads=[t_c, t_posf, t_M], writes=[t_O[b][t]])
            first = True
            for sc in range(4):
                for t in range(32):
                    S.op("pe", I("matmul", ips[b][:, sc * 8:sc * 8 + 6], lhsT=Oall[b][:, t, sc * 128:(sc + 1) * 128], rhs=R[:, t, e, :],
                                 start=(t == 0), stop=(t == 31)),
                         reads=[t_O[b][t], t_R], writes=[t_ips[b]], merge=not first)
                    first = False
            if do_ctx:
                for t in range(2):
                    S.op("dve", I("tensor_scalar", out=Oc[b][:, t, :], in0=iot[:, 0:128], scalar1=posf[:, 32 + t, e:e + 1],
                                  scalar2=Mf[:, 32 + t, e:e + 1], op0=ALU.is_equal, op1=ALU.mult),
                         reads=[t_c, t_posf, t_M], writes=[t_Oc[b]], merge=(t > 0))
                for t in range(2):
                    S.op("pe", I("matmul", ips[b][:, 32:38], lhsT=Oc[b][:, t, :], rhs=R[:, 32 + t, e, :], start=(t == 0), stop=(t == 1)),
                         reads=[t_Oc[b], t_R], writes=[t_ips[b]], merge=True)
            S.op("act", I("copy", out=isb[b][:, 0:nch, 0:6], in_=ips[b][:, 0:nch * 8].rearrange("p (c k) -> p c k", k=8)[:, :, 0:6]),
                 reads=[t_ips[b]], writes=[t_isb[b]])
            v = isb[b]
            S.op("dve", I("scalar_tensor_tensor", out=ia[b][:, 0:nch], in0=v[:, 0:nch, 1], scalar=128.0, in1=v[:, 0:nch, 0], op0=ALU.mult, op1=ALU.add),
                 reads=[t_isb[b]], writes=[t_ia[b]])
            S.op("dve", I("scalar_tensor_tensor", out=ia[b][:, 0:nch], in0=v[:, 0:nch, 5], scalar=pidx[:, 2:3], in1=ia[b][:, 0:nch], op0=ALU.mult, op1=ALU.add),
                 reads=[t_isb[b], t_ia[b], t_c], writes=[t_ia[b]])
            S.op("dve", I("tensor_scalar", out=ia[b][:, 0:nch], in0=ia[b][:, 0:nch], scalar1=pidx[:, 1:2], scalar2=None, op0=ALU.add),
                 reads=[t_ia[b], t_c], writes=[t_ia[b]])
            S.op("dve", I("tensor_copy", out=G.idx_all[:, e, 0:nch], in_=ia[b][:, 0:nch]), reads=[t_ia[b]], writes=[G.t_idx], merge=(e > 0))
            S.op("pool", I("tensor_tensor", out=G.g_all[:, e, 0:nch], in0=v[:, 0:nch, 2], in1=v[:, 0:nch, 3], op=ALU.add),
                 reads=[t_isb[b]], writes=[G.t_idx], merge=True)
            S.op("pool", I("tensor_tensor", out=G.g_all[:, e, 0:nch], in0=G.g_all[:, e, 0:nch], in1=v[:, 0:nch, 4], op=ALU.add),
                 reads=[t_isb[b], G.t_idx], writes=[G.t_idx], merge=True)
        if "idxd" in G.debug:
            S.dma("sp", I("dma_start", out=G.idxd, in_=G.idx_all[:].rearrange("p e c -> p (e c)")), reads=[G.t_idx], writes=[G.t["idxd"]])
            S.dma("sp", I("dma_start", out=G.gd, in_=G.g_all[:].rearrange("p e c -> p (e c)")), reads=[G.t_idx], writes=[G.t["gd"]])
        S.flush(f"route{l}")


def phase_moe(G, l, do_ctx):
    nc, S = G.nc, G.S
    with ExitStack() as ps:
        sb, pp = _alloc(G, ps)
        nch = 5 if do_ctx else 4
        NS = 544 if do_ctx else 512
        FB = 256
        ns = 2 if do_ctx else 1
        g2b = []
        t_bc = Tok()
        for s_ in range(ns):
            a = sb(f"mo_g2b{s_}", [128, D], F32)
            S.dma("sp", I("dma_start", out=a[:], in_=G.modrow[l, s_:s_ + 1, 5 * D:6 * D].to_broadcast([128, D])),
                  reads=[G.t["modrow"]], writes=[t_bc], merge=True)
            g2b.append(a)
        xs = [sb(f"mo_xs{i}", [128, D], BF16) for i in range(2)]
        t_xs = [Tok(), Tok()]
        xsT = sb("mo_xsT", [128, 16, NS], BF16)
        t_xsT = Tok()
        wg = [sb(f"mo_wg{i}", [128, 16, FB], BF16) for i in range(2)]
        wu = [sb(f"mo_wu{i}", [128, 16, FB], BF16) for i in range(2)]
        t_wgu = [Tok(), Tok()]
        wd = [sb(f"mo_wd{i}", [128, 16, 512], BF16) for i in range(2)]
        t_wd = [Tok(), Tok()]
        hm = sb("mo_hm", [128, 16, NS], BF16)
        t_hm = [Tok() for _ in range(16)]
        st = [sb(f"mo_st{i}", [128, 544], F32) for i in range(2)]
        t_st = [Tok(), Tok()]
        yst = [sb(f"mo_y{i}", [128, D], F32) for i in range(nch)]
        t_yst = [Tok() for _ in range(nch)]
        if do_ctx:
            S.op("pool", I("memset", yst[4][:], 0.0), writes=[t_yst[4]])
        aps = [pp(f"mo_a{i}", [128, 512]) for i in range(2)]
        ups = [pp(f"mo_u{i}", [128, 512]) for i in range(2)]
        t_aps = [Tok(), Tok()]
        t_ups = [Tok(), Tok()]
        cps = pp("mo_c", [128, 512])
        t_cps = Tok()
        yps = [pp(f"mo_yp{i}", [128, 512]) for i in range(2)]
        t_yps = [Tok(), Tok()]
        trp = pp("mo_tr", [128, 1024], BF16)
        t_trp = Tok()
        import os
        LVE = int(os.environ.get("MOE_EXP", "16"))
        jobs = []
        for e in range(LVE):
            for fb in range(D // FB):
                jobs.append(("gu", e, fb))
            for db in range(4):
                jobs.append(("d", e, db))
        cnt = {"gu": 0, "d": 0}
        slot_of = {}

        def issue(jb):
            kind, e, b = jb
            i = cnt[kind] % 2
            cnt[kind] += 1
            slot_of[jb] = i
            if kind == "gu":
                S.dma("pool", I("dma_start", out=wg[i][:].rearrange("p j n -> p (j n)").rearrange("p (a m) -> p a m", m=2048),
                                in_=G.w_gate[l, e, b].rearrange("p (a m) -> p a m", m=2048)),
                      writes=[t_wgu[i]])
                S.dma("pool", I("dma_start", out=wu[i][:].rearrange("p j n -> p (j n)").rearrange("p (a m) -> p a m", m=2048),
                                in_=G.w_up[l, e, b].rearrange("p (a m) -> p a m", m=2048)),
                      writes=[t_wgu[i]], merge=True)
            else:
                S.dma("pool", I("dma_start", out=wd[i][:].rearrange("p j n -> p (j n)").rearrange("p (a m) -> p a m", m=2048),
                                in_=G.w_down[l, e, b].rearrange("p (a m) -> p a m", m=2048)),
                      writes=[t_wd[i]])

        def gather(e):
            for sc in range(nch):
                i = sc % 2
                m = 128 if sc < 4 else 32
                S.dma("pool", I("indirect_dma_start", out=xs[i][:], out_offset=None, in_=G.fxd[:, :],
                                in_offset=bass.IndirectOffsetOnAxis(ap=G.idx_all[:, e, sc:sc + 1], axis=0)),
                      reads=[G.t_idx, G.t["fxd"]], writes=[t_xs[i]])
                for q in range(2):
                    for jj in range(8):
                        j = q * 8 + jj
                        S.op("pe", I("transpose", out=trp[:, jj * 128:jj * 128 + m], in_=xs[i][0:m, j * 128:(j + 1) * 128],
                                     identity=G.ident_b[0:m, 0:m]),
                             reads=[t_xs[i], G.t_const], writes=[t_trp], merge=(jj > 0))
                    src = trp[:].rearrange("p (a n) -> p a n", n=128)[:, :, 0:m]
                    dst = xsT[:, q * 8:(q + 1) * 8, sc * 128:sc * 128 + m]
                    if q == 0:
                        S.op("act", I("copy", out=dst, in_=src), reads=[t_trp], writes=[t_xsT], merge=not (sc == 0))
                    else:
                        S.op("dve", I("tensor_copy", out=dst, in_=src), reads=[t_trp], writes=[t_xsT], merge=True)

        pend_sc = []

        def flush_scatter():
            while pend_sc:
                e_ = pend_sc.pop(0)
                for sc in range(nch):
                    S.dma("pool", I("indirect_dma_start", out=G.xres[:, :],
                                    out_offset=bass.IndirectOffsetOnAxis(ap=G.idx_all[:, e_, sc:sc + 1], axis=0),
                                    in_=yst[sc][:], in_offset=None, bounds_check=NPAD - 1, oob_is_err=True, compute_op=ALU.add),
                          reads=[t_yst[sc], G.t_idx, G.t["xres"]], writes=[G.t["xres"]])

        ji = 0
        issue(jobs[0])
        if LVE > 0:
            gather(0)
        kk = 0
        for e in range(LVE):
            for fb in range(D // FB):
                jb = jobs[ji]
                ji += 1
                if ji < len(jobs):
                    issue(jobs[ji])
                if fb == 2:
                    flush_scatter()
                wi = slot_of[jb]
                for fl in range(FB // 128):
                    fo = fb * (FB // 128) + fl
                    ab = kk % 2
                    kk += 1
                    for j in range(16):
                        S.op("pe", I("matmul", aps[ab][:], lhsT=wg[wi][:, j, fl * 128:(fl + 1) * 128], rhs=xsT[:, j, 0:512],
                                     start=(j == 0), stop=(j == 15)),
                             reads=[t_wgu[wi], t_xsT], writes=[t_aps[ab]], merge=(j > 0))
                    for j in range(16):
                        S.op("pe", I("matmul", ups[ab][:], lhsT=wu[wi][:, j, fl * 128:(fl + 1) * 128], rhs=xsT[:, j, 0:512],
                                     start=(j == 0), stop=(j == 15)),
                             reads=[t_wgu[wi], t_xsT], writes=[t_ups[ab]], merge=(j > 0))
                    if do_ctx:
                        for j in range(16):
                            S.op("pe", I("matmul", cps[:, 0:32], lhsT=wg[wi][:, j, fl * 128:(fl + 1) * 128], rhs=xsT[:, j, 512:544],
                                         start=(j == 0), stop=(j == 15)),
                                 reads=[t_wgu[wi], t_xsT], writes=[t_cps], merge=(j > 0))
                        for j in range(16):
                            S.op("pe", I("matmul", cps[:, 32:64], lhsT=wu[wi][:, j, fl * 128:(fl + 1) * 128], rhs=xsT[:, j, 512:544],
                                         start=(j == 0), stop=(j == 15)),
                                 reads=[t_wgu[wi], t_xsT], writes=[t_cps], merge=True)
                    S.op("act", I("activation", out=st[ab][:, 0:512], in_=aps[ab][:], func=AF.Silu), reads=[t_aps[ab]], writes=[t_st[ab]])
                    S.op("dve", I("tensor_tensor", out=hm[:, fo, 0:512], in0=ups[ab][:], in1=st[ab][:, 0:512], op=ALU.mult),
                         reads=[t_ups[ab], t_st[ab]], writes=[t_hm[fo]])
                    if do_ctx:
                        S.op("act", I("activation", out=st[ab][:, 512:544], in_=cps[:, 0:32], func=AF.Silu), reads=[t_cps], writes=[t_st[ab]], merge=True)
                        S.op("dve", I("tensor_tensor", out=hm[:, fo, 512:544], in0=cps[:, 32:64], in1=st[ab][:, 512:544], op=ALU.mult),
                             reads=[t_cps, t_st[ab]], writes=[t_hm[fo]], merge=True)
            if e + 1 < LVE:
                gather(e + 1)
            for db in range(4):
                jb = jobs[ji]
                ji += 1
                if ji < len(jobs):
                    issue(jobs[ji])
                wi = slot_of[jb]
                for sc in range(nch):
                    m = 128 if sc < 4 else 32
                    s_ = 0 if sc < 4 else 1
                    yi = kk % 2
                    kk += 1
                    for fo in range(16):
                        S.op("pe", I("matmul", yps[yi][0:m, :], lhsT=hm[:, fo, sc * 128:sc * 128 + m], rhs=wd[wi][:, fo, :],
                                     start=(fo == 0), stop=(fo == 15)),
                             reads=[t_hm[fo], t_wd[wi]], writes=[t_yps[yi]], merge=(fo > 0))
                    S.op("dve", I("scalar_tensor_tensor", out=yst[sc][0:m, db * 512:(db + 1) * 512], in0=yps[yi][0:m, :],
                                  scalar=G.g_all[0:m, e, sc:sc + 1], in1=g2b[s_][0:m, db * 512:(db + 1) * 512], op0=ALU.mult, op1=ALU.mult),
                         reads=[t_yps[yi], G.t_idx, t_bc], writes=[t_yst[sc]], merge=(db > 0))
            pend_sc.append(e)
        flush_scatter()
        S.flush(f"moe{l}")


def phase_final(G):
    nc, S = G.nc, G.S
    with ExitStack() as ps:
        sb, pp = _alloc(G, ps)
        fg = sb("fi_g", [128, D], F32)
        t_fg = Tok()
        S.dma("sp", I("dma_start", out=fg[:], in_=G.final_g.rearrange("(o d) -> o d", o=1).to_broadcast([128, D])), writes=[t_fg])
        xt = [sb(f"fi_x{i}", [128, D], F32) for i in range(2)]
        t_xt = [Tok(), Tok()]
        ot = [sb(f"fi_o{i}", [128, D], F32) for i in range(2)]
        t_ot = [Tok(), Tok()]
        junk = sb("fi_junk", [128, D], BF16)
        t_junk = Tok()
        ss = [sb(f"fi_ss{i}", [128, 1], F32) for i in range(2)]
        t_ss = [Tok(), Tok()]
        for ti in range(32):
            i = ti % 2
            S.dma("sp", I("dma_start", out=xt[i][:], in_=G.xres[ti * 128:(ti + 1) * 128, :]), reads=[G.t["xres"]], writes=[t_xt[i]])
            S.op("act", I("activation", out=junk[:], in_=xt[i][:], func=AF.Square, accum_out=ss[i][:, 0:1]), reads=[t_xt[i]], writes=[t_junk, t_ss[i]])
            rstd_from_ss(G, ss[i][:, 0:1], ss[i][:, 0:1], D, [t_ss[i]], [t_ss[i]])
            S.op("dve", I("scalar_tensor_tensor", out=ot[i][:], in0=xt[i][:], scalar=ss[i][:, 0:1], in1=fg[:], op0=ALU.mult, op1=ALU.mult),
                 reads=[t_xt[i], t_ss[i], t_fg], writes=[t_ot[i]])
            S.dma("sp", I("dma_start", out=G.out[ti * 128:(ti + 1) * 128, :], in_=ot[i][:]), reads=[t_ot[i]], writes=[G.t["out"]], merge=True)
        S.flush("final")


_CONSTS = None


def make_in_maps(inputs, batches, names=None):
    global _CONSTS
    if _CONSTS is None:
        _CONSTS = host_consts()
    w = host_weights(inputs)
    maps = []
    for b in batches:
        m = dict(w)
        m.update(_CONSTS)
        xin = np.zeros((NPAD, D), np.float32)
        xin[:NL] = inputs["x"][b]
        xin[NL:NT] = inputs["ctx"][b]
        m["xin"] = xin
        m["cvec"] = np.ascontiguousarray(np.stack([inputs["c"][b], inputs["c_ctx"]], axis=1)).astype(np.float32)
        if names is not None:
            m = {k: m[k] for k in names}
        maps.append(m)
    return maps


_PROG = None


def kernel(**inputs):
    global _PROG
    inputs = {k: np.asarray(v) for k, v in inputs.items()}
    if _PROG is None:
        _PROG = build_program()
    nc, G = _PROG
    B = inputs["x"].shape[0]
    maps = make_in_maps(inputs, list(range(B)), G.in_names)
    res = run_bass_kernel_spmd(nc, maps, core_ids=list(range(B)))
    out = np.stack([np.asarray(r["out"]) for r in res.results], axis=0)
    return out.astype(np.float32)
```
